# Optimizing a Trainium2 kernel written in Bass

```python
import math
import jax, jax.numpy as jnp
from jax import lax
import numpy as np

D_MODEL = 1024
BATCH = 4
SEQ = 4096
DEPTH = 4

N_EVEN = (DEPTH + 1) // 2
N_ODD = DEPTH // 2

POOL_WINDOWS = (2, 4, 8, 16)
POOL_GROUPS = 4
POOL_WIDTH = D_MODEL // 2
POOL_GDIM = POOL_WIDTH // POOL_GROUPS
SGU_GROUPS = 4
SGU_WIDTH = D_MODEL // 2
SGU_GDIM = SGU_WIDTH // SGU_GROUPS
SGU_CHUNK = 128
EVEN_IN = POOL_WIDTH + 2 * SGU_WIDTH
EVEN_MIX = POOL_WIDTH + SGU_WIDTH
NSA_HEADS = 8
NSA_KV_GROUPS = 2
NSA_HPG = NSA_HEADS // NSA_KV_GROUPS
NSA_HEAD_DIM = 64
NSA_WIDTH = NSA_HEADS * NSA_HEAD_DIM
NSA_KV_WIDTH = NSA_KV_GROUPS * NSA_HEAD_DIM
CMP_BLOCK = 32
CMP_STRIDE = 16
CMP_HIDDEN = 128
SEL_BLOCK = 64
SEL_TOPK = 16
WINDOW = 512
Q_BLOCK = 128
FORCE_SCORE = 1.0e4
ROPE_THETA = 500000.0
ROPE_DIM = NSA_HEAD_DIM // 4
RET_HEADS = 4
RET_HEAD_DIM = 128
RET_WIDTH = RET_HEADS * RET_HEAD_DIM
RET_CHUNK = 128
RET_THETA = 10000.0
ODD_IN = NSA_WIDTH + 6 * NSA_KV_WIDTH + 3 * NSA_HEADS + 4 * RET_WIDTH
ODD_MIX = NSA_WIDTH + RET_WIDTH
FFN_HIDDEN = -(-8 * D_MODEL // (3 * 256)) * 256
NEG_INF = -1.0e30

kernel_name = "hybrid_pool_sgu_nsa_retention_trunk"


def rms_norm(x, g, eps=1e-6):
    xf = x.astype(jnp.float32)
    y = xf * lax.rsqrt(jnp.mean(xf * xf, axis=-1, keepdims=True) + eps)
    return (y * g.astype(jnp.float32)).astype(x.dtype)


def layer_norm(x, g, b, eps=1e-5):
    xf = x.astype(jnp.float32)
    mu = jnp.mean(xf, axis=-1, keepdims=True)
    var = jnp.mean(jnp.square(xf - mu), axis=-1, keepdims=True)
    return ((xf - mu) * lax.rsqrt(var + eps) * g.astype(jnp.float32) + b.astype(jnp.float32)).astype(x.dtype)


def masked_softmax(s, mask):
    return jax.nn.softmax(jnp.where(mask, s.astype(jnp.float32), NEG_INF), axis=-1)


def rope_tables(positions, rot_dim, theta):
    inv_freq = 1.0 / (theta ** (jnp.arange(0, rot_dim, 2, dtype=jnp.float32) / rot_dim))
    ang = positions.astype(jnp.float32)[..., None] * inv_freq
    return jnp.cos(ang)[:, None], jnp.sin(ang)[:, None]


def apply_rope(x, cos, sin):
    half = cos.shape[-1]
    x1, x2, rest = x[..., :half], x[..., half:2 * half], x[..., 2 * half:]
    c, s = cos.astype(x.dtype), sin.astype(x.dtype)
    return jnp.concatenate([x1 * c - x2 * s, x2 * c + x1 * s, rest], axis=-1)


def to_heads(x, n):
    b, t, _ = x.shape
    return x.reshape(b, t, n, -1).transpose(0, 2, 1, 3)


def swiglu(h, w_gate, w_up, w_down):
    return (jax.nn.silu(h @ w_gate) * (h @ w_up)) @ w_down


def pool_mixer(a, pool_w, pool_scale):
    b, t_len, _ = a.shape
    af = a.astype(jnp.float32).reshape(b, t_len, POOL_GROUPS, POOL_GDIM)
    cs = jnp.concatenate([jnp.zeros_like(af[:, :1]), jnp.cumsum(af, axis=1)], axis=1)
    t = jnp.arange(t_len)
    pooled = []
    for gi, w in enumerate(POOL_WINDOWS):
        lo = jnp.maximum(t + 1 - w, 0)
        win_sum = cs[:, 1:, gi] - cs[:, lo, gi]
        pooled.append(win_sum / (t + 1 - lo).astype(jnp.float32)[None, :, None])
    pooled = jnp.stack(pooled, axis=2)
    diff = (pooled - af).astype(a.dtype)
    y = jnp.einsum('btgc,gcd->btgd', diff, pool_w)
    return y.reshape(b, t_len, POOL_WIDTH) * pool_scale


def sgu_mixer(u, v, ln_g, ln_b, w_s, b_s):
    b, t_len, _ = u.shape
    u = jax.nn.gelu(u)
    v = layer_norm(jax.nn.gelu(v), ln_g, ln_b)
    n_chunk = t_len // SGU_CHUNK
    vc = v.reshape(b, n_chunk, SGU_CHUNK, SGU_GROUPS, SGU_GDIM)
    causal = jnp.tril(jnp.ones((SGU_CHUNK, SGU_CHUNK), dtype=bool))
    w_masked = jnp.where(causal[None], w_s, 0.0).astype(v.dtype)
    mixed = jnp.einsum('gts,bcsgd->bctgd', w_masked, vc) + b_s.T[None, None, :, :, None]
    return u * mixed.reshape(b, t_len, SGU_WIDTH)


def even_mixer(h, w_in, pool_w, pool_scale, sgu_ln_g, sgu_ln_b, sgu_w, sgu_b, w_out):
    z = h @ w_in
    a = z[..., :POOL_WIDTH]
    u = z[..., POOL_WIDTH:POOL_WIDTH + SGU_WIDTH]
    v = z[..., POOL_WIDTH + SGU_WIDTH:]
    y_a = pool_mixer(a, pool_w, pool_scale)
    y_b = sgu_mixer(u, v, sgu_ln_g, sgu_ln_b, sgu_w, sgu_b)
    return jnp.concatenate([y_a, y_b], axis=-1) @ w_out


def compress_blocks(k, pos_emb, w1, w2):
    t_len = k.shape[2]
    n_cmp = (t_len - CMP_BLOCK) // CMP_STRIDE + 1
    idx = np.arange(n_cmp)[:, None] * CMP_STRIDE + np.arange(CMP_BLOCK)[None, :]
    blocks = k[:, :, idx] + pos_emb
    flat = blocks.reshape(blocks.shape[:3] + (CMP_BLOCK * NSA_HEAD_DIM,))
    return jax.nn.gelu(flat @ w1) @ w2


def nsa_attention(q, k_cmp, v_cmp, k_sel, v_sel, k_win, v_win, gates, positions,
                  cmp_k_pos, cmp_k_w1, cmp_k_w2, cmp_v_pos, cmp_v_w1, cmp_v_w2):
    b, t_len, _ = q.shape
    G, M, d = NSA_KV_GROUPS, NSA_HPG, NSA_HEAD_DIM
    cos, sin = rope_tables(positions, ROPE_DIM, ROPE_THETA)
    q = apply_rope(to_heads(q, NSA_HEADS), cos, sin) * (d ** -0.5)
    k_cmp = apply_rope(to_heads(k_cmp, G), cos, sin)
    k_sel = apply_rope(to_heads(k_sel, G), cos, sin)
    k_win = apply_rope(to_heads(k_win, G), cos, sin)
    v_cmp, v_sel, v_win = to_heads(v_cmp, G), to_heads(v_sel, G), to_heads(v_win, G)

    kc = compress_blocks(k_cmp, cmp_k_pos, cmp_k_w1, cmp_k_w2)
    vc = compress_blocks(v_cmp, cmp_v_pos, cmp_v_w1, cmp_v_w2)
    n_cmp = kc.shape[2]
    cmp_end = jnp.arange(n_cmp) * CMP_STRIDE + CMP_BLOCK - 1

    n_sel = t_len // SEL_BLOCK
    top_k = min(SEL_TOPK, n_sel)
    c_start = np.arange(n_cmp) * CMP_STRIDE
    s_start = np.arange(n_sel) * SEL_BLOCK
    cmp_to_sel = ((c_start[:, None] < s_start[None, :] + SEL_BLOCK)
                  & (c_start[:, None] + CMP_BLOCK > s_start[None, :])).astype(np.float32)
    ks_blk = k_sel.reshape(b, G, n_sel, SEL_BLOCK, d)
    vs_blk = v_sel.reshape(b, G, n_sel, SEL_BLOCK, d)
    kw_pad = jnp.pad(k_win, ((0, 0), (0, 0), (WINDOW, 0), (0, 0)))
    vw_pad = jnp.pad(v_win, ((0, 0), (0, 0), (WINDOW, 0), (0, 0)))
    g = jax.nn.sigmoid(gates.astype(jnp.float32)).reshape(b, t_len, NSA_HEADS, 3)
    g = g.transpose(0, 2, 1, 3).astype(q.dtype)
    b_idx = jnp.arange(b)[:, None, None, None]
    g_idx = jnp.arange(G)[None, :, None, None]
    j_sel = jnp.arange(n_sel)

    def block(i):
        t0 = i * Q_BLOCK
        tq = t0 + jnp.arange(Q_BLOCK)
        qb = lax.dynamic_slice_in_dim(q, t0, Q_BLOCK, axis=2).reshape(b, G, M, Q_BLOCK, d)
        gb = lax.dynamic_slice_in_dim(g, t0, Q_BLOCK, axis=2)
        s = jnp.einsum('bgmqd,bgnd->bgmqn', qb, kc)
        p_cmp = masked_softmax(s, cmp_end[None, :] <= tq[:, None])
        p_cmp = p_cmp * (tq >= CMP_BLOCK - 1).astype(jnp.float32)[:, None]
        o_cmp = jnp.einsum('bgmqn,bgnd->bgmqd', p_cmp.astype(vc.dtype), vc)
        p_slc = jnp.einsum('bgmqn,nj->bgqj', p_cmp, cmp_to_sel)
        cur = tq // SEL_BLOCK
        sel_ok = j_sel[None, :] <= cur[:, None]
        forced = (j_sel[None, :] == 0) | (j_sel[None, :] == cur[:, None]) | (j_sel[None, :] == cur[:, None] - 1)
        score = jnp.where(sel_ok, jnp.where(forced, FORCE_SCORE, p_slc), -1.0)
        top_val, top_idx = lax.top_k(score, top_k)
        ks = ks_blk[b_idx, g_idx, top_idx]
        vs = vs_blk[b_idx, g_idx, top_idx]
        tok = top_idx[..., None] * SEL_BLOCK + jnp.arange(SEL_BLOCK)
        tok_ok = (top_val >= 0.0)[..., None] & (tok <= tq[:, None, None])
        s = jnp.einsum('bgmqd,bgqkld->bgmqkl', qb, ks).reshape(b, G, M, Q_BLOCK, top_k * SEL_BLOCK)
        p = masked_softmax(s, tok_ok.reshape(b, G, 1, Q_BLOCK, top_k * SEL_BLOCK))
        p = p.reshape(b, G, M, Q_BLOCK, top_k, SEL_BLOCK).astype(vs.dtype)
        o_sel = jnp.einsum('bgmqkl,bgqkld->bgmqd', p, vs)
        kwb = lax.dynamic_slice_in_dim(kw_pad, t0, WINDOW + Q_BLOCK, axis=2)
        vwb = lax.dynamic_slice_in_dim(vw_pad, t0, WINDOW + Q_BLOCK, axis=2)
        pos = t0 - WINDOW + jnp.arange(WINDOW + Q_BLOCK)
        win_ok = (pos[None, :] >= 0) & (pos[None, :] <= tq[:, None]) & (pos[None, :] > tq[:, None] - WINDOW)
        s = jnp.einsum('bgmqd,bgsd->bgmqs', qb, kwb)
        o_win = jnp.einsum('bgmqs,bgsd->bgmqd', masked_softmax(s, win_ok).astype(vwb.dtype), vwb)
        o = jnp.stack([o_cmp, o_sel, o_win], axis=-1).reshape(b, NSA_HEADS, Q_BLOCK, d, 3)
        return jnp.einsum('bhqdc,bhqc->bqhd', o, gb)

    out = lax.map(block, jnp.arange(t_len // Q_BLOCK))
    return out.transpose(1, 0, 2, 3, 4).reshape(b, t_len, NSA_WIDTH)


def retention(q, k, v, gate, positions, gn_g):
    b, t_len, _ = q.shape
    H, d, C = RET_HEADS, RET_HEAD_DIM, RET_CHUNK
    n_chunk = t_len // C
    cos, sin = rope_tables(positions, d, RET_THETA)
    q = apply_rope(to_heads(q, H), cos, sin)
    k = apply_rope(to_heads(k, H), cos, sin) * (d ** -0.5)
    v = to_heads(v, H)
    log_gamma = jnp.log1p(-jnp.exp2(-5.0 - jnp.arange(H, dtype=jnp.float32)))
    idx = jnp.arange(C, dtype=jnp.float32)
    rel = idx[:, None] - idx[None, :]
    decay = jnp.where(rel >= 0, jnp.exp(jnp.maximum(rel, 0.0)[None] * log_gamma[:, None, None]), 0.0)
    qc = q.reshape(b, H, n_chunk, C, d)
    kc = k.reshape(b, H, n_chunk, C, d)
    vc = v.reshape(b, H, n_chunk, C, d)
    s = jnp.einsum('bhcid,bhcjd->bhcij', qc, kc) * decay[None, :, None].astype(q.dtype)
    o_intra = jnp.einsum('bhcij,bhcje->bhcie', s, vc)
    zeta = jnp.exp((C - 1 - idx)[None, :] * log_gamma[:, None])
    chunk_kv = jnp.einsum('bhcjd,bhcje->bhcde', kc * zeta[None, :, None, :, None].astype(k.dtype), vc)
    g_chunk = jnp.exp(C * log_gamma)[None, :, None, None]

    def step(state, kv):
        return state * g_chunk + kv, state

    _, prev = lax.scan(step, jnp.zeros((b, H, d, d), jnp.float32),
                       jnp.moveaxis(chunk_kv, 2, 0).astype(jnp.float32))
    prev = jnp.moveaxis(prev, 0, 2)
    xi = jnp.exp((idx + 1.0)[None, :] * log_gamma[:, None])
    o_cross = jnp.einsum('bhcid,bhcde->bhcie', qc.astype(jnp.float32) * xi[None, :, None, :, None], prev)
    o = (o_intra.astype(jnp.float32) + o_cross).reshape(b, H, t_len, d).transpose(0, 2, 1, 3)
    mu = jnp.mean(o, axis=-1, keepdims=True)
    var = jnp.mean(jnp.square(o - mu), axis=-1, keepdims=True)
    o = ((o - mu) * lax.rsqrt(var + 1e-5)).reshape(b, t_len, RET_WIDTH) * gn_g.astype(jnp.float32)
    return (jax.nn.silu(gate.astype(jnp.float32)) * o).astype(gate.dtype)


def odd_mixer(h, positions, w_in, cmp_k_pos, cmp_k_w1, cmp_k_w2, cmp_v_pos, cmp_v_w1, cmp_v_w2,
              ret_gn_g, w_out):
    z = h @ w_in
    sizes = [NSA_WIDTH] + [NSA_KV_WIDTH] * 6 + [3 * NSA_HEADS] + [RET_WIDTH] * 4
    splits = np.cumsum(sizes)[:-1].tolist()
    q, kc, vc, ks, vs, kw, vw, gt, rq, rk, rv, rg = jnp.split(z, splits, axis=-1)
    y_c = nsa_attention(q, kc, vc, ks, vs, kw, vw, gt, positions,
                        cmp_k_pos, cmp_k_w1, cmp_k_w2, cmp_v_pos, cmp_v_w1, cmp_v_w2)
    y_d = retention(rq, rk, rv, rg, positions, ret_gn_g)
    return jnp.concatenate([y_c, y_d], axis=-1) @ w_out


def setup_inputs(seed: int = 0) -> dict:
    key = jax.random.key(seed)
    ks = jax.random.split(key, 32)

    def nrm(k, shape, scale):
        return jax.random.normal(k, shape, jnp.float32) * scale

    def gain(k, shape):
        return 1.0 + 0.05 * jax.random.normal(k, shape, jnp.float32)

    NE, NO, D, F = N_EVEN, N_ODD, D_MODEL, FFN_HIDDEN
    x = nrm(ks[0], (BATCH, SEQ, D), 1.0)
    start = jax.random.randint(ks[1], (BATCH, 1), 0, 2048, dtype=jnp.int32)
    positions = start + jnp.arange(SEQ, dtype=jnp.int32)[None, :]
    cmp_in = CMP_BLOCK * NSA_HEAD_DIM
    return {
        "x": x,
        "positions": positions,
        "ln_mix_pre": gain(ks[2], (DEPTH, D)),
        "ln_mix_post": gain(ks[3], (DEPTH, D)),
        "ln_ffn_pre": gain(ks[4], (DEPTH, D)),
        "ln_ffn_post": gain(ks[5], (DEPTH, D)),
        "ffn_w_gate": nrm(ks[6], (DEPTH, D, F), D ** -0.5),
        "ffn_w_up": nrm(ks[7], (DEPTH, D, F), D ** -0.5),
        "ffn_w_down": nrm(ks[8], (DEPTH, F, D), F ** -0.5),
        "ev_w_in": nrm(ks[9], (NE, D, EVEN_IN), D ** -0.5),
        "ev_pool_w": nrm(ks[10], (NE, POOL_GROUPS, POOL_GDIM, POOL_GDIM), POOL_GDIM ** -0.5),
        "ev_pool_scale": 1.0 + 0.1 * jax.random.normal(ks[11], (NE, POOL_WIDTH), jnp.float32),
        "ev_sgu_ln_g": gain(ks[12], (NE, SGU_WIDTH)),
        "ev_sgu_ln_b": nrm(ks[13], (NE, SGU_WIDTH), 0.02),
        "ev_sgu_w": nrm(ks[14], (NE, SGU_GROUPS, SGU_CHUNK, SGU_CHUNK), SGU_CHUNK ** -0.5),
        "ev_sgu_b": 1.0 + 0.1 * jax.random.normal(ks[15], (NE, SGU_GROUPS, SGU_CHUNK), jnp.float32),
        "ev_w_out": nrm(ks[16], (NE, EVEN_MIX, D), EVEN_MIX ** -0.5),
        "od_w_in": nrm(ks[17], (NO, D, ODD_IN), D ** -0.5),
        "od_cmp_k_pos": nrm(ks[18], (NO, CMP_BLOCK, NSA_HEAD_DIM), 0.1),
        "od_cmp_k_w1": nrm(ks[19], (NO, cmp_in, CMP_HIDDEN), cmp_in ** -0.5),
        "od_cmp_k_w2": nrm(ks[20], (NO, CMP_HIDDEN, NSA_HEAD_DIM), CMP_HIDDEN ** -0.5),
        "od_cmp_v_pos": nrm(ks[21], (NO, CMP_BLOCK, NSA_HEAD_DIM), 0.1),
        "od_cmp_v_w1": nrm(ks[22], (NO, cmp_in, CMP_HIDDEN), cmp_in ** -0.5),
        "od_cmp_v_w2": nrm(ks[23], (NO, CMP_HIDDEN, NSA_HEAD_DIM), CMP_HIDDEN ** -0.5),
        "od_ret_gn_g": gain(ks[24], (NO, RET_WIDTH)),
        "od_w_out": nrm(ks[25], (NO, ODD_MIX, D), ODD_MIX ** -0.5),
    }


def reference(x, positions, ln_mix_pre, ln_mix_post, ln_ffn_pre, ln_ffn_post,
              ffn_w_gate, ffn_w_up, ffn_w_down,
              ev_w_in, ev_pool_w, ev_pool_scale, ev_sgu_ln_g, ev_sgu_ln_b, ev_sgu_w, ev_sgu_b, ev_w_out,
              od_w_in, od_cmp_k_pos, od_cmp_k_w1, od_cmp_k_w2, od_cmp_v_pos, od_cmp_v_w1, od_cmp_v_w2,
              od_ret_gn_g, od_w_out):
    for layer in range(DEPTH):
        h = rms_norm(x, ln_mix_pre[layer])
        if layer % 2 == 0:
            e = layer // 2
            m = even_mixer(h, ev_w_in[e], ev_pool_w[e], ev_pool_scale[e], ev_sgu_ln_g[e],
                           ev_sgu_ln_b[e], ev_sgu_w[e], ev_sgu_b[e], ev_w_out[e])
        else:
            o = layer // 2
            m = odd_mixer(h, positions, od_w_in[o], od_cmp_k_pos[o], od_cmp_k_w1[o], od_cmp_k_w2[o],
                          od_cmp_v_pos[o], od_cmp_v_w1[o], od_cmp_v_w2[o], od_ret_gn_g[o], od_w_out[o])
        x = x + rms_norm(m, ln_mix_post[layer])
        h = rms_norm(x, ln_ffn_pre[layer])
        x = x + rms_norm(swiglu(h, ffn_w_gate[layer], ffn_w_up[layer], ffn_w_down[layer]), ln_ffn_post[layer])
    return x
```

```python
import numpy as np
import ml_dtypes
import concourse.bass as bass
import concourse.mybir as mybir
from concourse.bass_utils import run_bass_kernel_spmd

F32 = mybir.dt.float32
BF16 = mybir.dt.bfloat16
I32 = mybir.dt.int32
AF = mybir.ActivationFunctionType
ALU = mybir.AluOpType
AX = mybir.AxisListType

SAME_ENG_SYNC = True


class Prog:
    def __init__(self, nc):
        self.nc = nc
        self.E = {'pe': nc.tensor, 'dve': nc.vector, 'act': nc.scalar, 'pool': nc.gpsimd, 'sp': nc.sync}
        self.semh = {k: nc.alloc_semaphore('s_' + k) for k in ['pe', 'dve', 'act', 'pool']}
        self.cnt = {k: 0 for k in self.semh}
        self.seen = {e: {} for e in self.E}
        self.lastw = {}
        self.readers = {}
        self.nwait = 0
        self.dslot = 0
        self.NSLOT = 32
        self.nins = 0

    def _deps(self, reads, writes):
        deps = {}
        for r in reads:
            w = self.lastw.get(r)
            if w and deps.get(w[0], 0) < w[1]:
                deps[w[0]] = w[1]
        for w_ in writes:
            w = self.lastw.get(w_)
            if w and deps.get(w[0], 0) < w[1]:
                deps[w[0]] = w[1]
            for k, v in self.readers.get(w_, {}).items():
                if deps.get(k, 0) < v:
                    deps[k] = v
        return deps

    def _wait(self, eng, deps):
        e = self.E[eng]
        seen = self.seen[eng]
        for k, v in deps.items():
            if k == eng and (eng == 'pe' or not SAME_ENG_SYNC):
                continue
            if seen.get(k, 0) >= v:
                continue
            e.wait_ge(self.semh[k], v)
            seen[k] = v
            self.nwait += 1

    def _record(self, tag, reads, writes):
        for w in writes:
            self.lastw[w] = tag
            self.readers[w] = {}
        for r in reads:
            d = self.readers.setdefault(r, {})
            if d.get(tag[0], 0) < tag[1]:
                d[tag[0]] = tag[1]

    def op(self, eng, fn, reads=(), writes=()):
        self._wait(eng, self._deps(reads, writes))
        ins = fn(self.E[eng])
        self.cnt[eng] += 1
        ins.then_inc(self.semh[eng], 1)
        self.nins += 1
        self._record((eng, self.cnt[eng]), reads, writes)

    def dma(self, q, out, in_, reads=(), writes=(), stream='d0', **kw):
        slot = self.dslot % self.NSLOT
        self.dslot += 1
        key = 'd:%d' % slot
        if key not in self.semh:
            self.semh[key] = self.nc.alloc_semaphore('sd_%d' % slot)
            self.cnt[key] = 0
        e = self.E[q]
        if self.cnt[key] > 0 and self.seen[q].get(key, 0) < self.cnt[key]:
            e.wait_ge(self.semh[key], self.cnt[key])
            self.seen[q][key] = self.cnt[key]
        self._wait(q, self._deps(reads, writes))
        e.dma_start(out=out, in_=in_, **kw).then_inc(self.semh[key], 16)
        self.cnt[key] += 16
        self.nins += 1
        self._record((key, self.cnt[key]), reads, writes)

    def barrier(self):
        for eng in self.E:
            deps = {k: v for k, v in self.cnt.items() if v > 0}
            e = self.E[eng]
            for k, v in deps.items():
                if k == eng and eng == 'pe':
                    continue
                if self.seen[eng].get(k, 0) >= v:
                    continue
                e.wait_ge(self.semh[k], v)
                self.seen[eng][k] = v
        self.lastw = {}
        self.readers = {}

    def finish(self, eng='sp'):
        deps = {k: v for k, v in self.cnt.items() if v > 0}
        e = self.E[eng]
        for k, v in deps.items():
            e.wait_ge(self.semh[k], v)


T = 4096
D = 1024
NT = T // 128
FH = 2816
NFC = FH // 128
DEPTH = 4
POOL_WINDOWS = (2, 4, 8, 16)
EPS = 1e-6


def _flat2(ap, c=1024):
    n = len(ap.shape)
    names = ' '.join('d%d' % i for i in range(n))
    f = ap.rearrange('%s -> (%s)' % (names, names)) if n > 1 else ap
    return f.rearrange('(r c) -> r c', c=c)


class K:
    def __init__(self, nc, layers=(0, 1, 2, 3), Tn=T):
        self.nc = nc
        self.P = Prog(nc)
        self.layers = layers
        self.uid = 0

    def sb(self, name, shape, dt):
        return self.nc.alloc_sbuf_tensor(name, list(shape), dt).ap()

    def dram(self, name, shape, dt, kind="Internal"):
        return self.nc.dram_tensor(name, list(shape), dt, kind=kind).ap()


def cast_copy(k, dst, src, res):
    d2 = _flat2(dst)
    s2 = _flat2(src)
    rows = d2.shape[0]
    r0 = 0
    while r0 < rows:
        r1 = min(rows, r0 + 2048)
        k.P.dma('pool', d2[r0:r1, :], s2[r0:r1, :], writes=[res], stream='cast')
        r0 = r1


def rstd_from_ssq(k, ssq, rstd, tmp, n, eps, tag):
    P = k.P
    P.op('dve', lambda e: e.tensor_scalar(out=tmp, in0=ssq, scalar1=1.0 / n, scalar2=eps, op0=ALU.mult, op1=ALU.add),
         reads=[tag + 'ssq'], writes=[tag + 'tmp'])
    P.op('act', lambda e: e.activation(out=tmp, in_=tmp, func=AF.Sqrt), reads=[tag + 'tmp'], writes=[tag + 'tmp'])
    P.op('dve', lambda e: e.reciprocal(out=rstd, in_=tmp), reads=[tag + 'tmp'], writes=[tag + 'rstd'])


def setup_common(k, ins):
    nc, P = k.nc, k.P
    k.ins = ins
    k.bank = [nc.alloc_psum_tensor('bank%d' % i, [128, 512], F32).ap() for i in range(8)]
    k.bankb = [b.bitcast(BF16) for b in k.bank]
    k.io_f = k.sb('io_f', [128, 128], F32)
    k.iop = k.sb('iop', [128, 1], F32)
    k.ident = k.sb('ident', [128, 128], BF16)
    k.ones_row = k.sb('ones_row', [1, 128], BF16)
    P.op('pool', lambda e: e.iota(k.io_f, pattern=[[1, 128]], base=0, channel_multiplier=0,
                                  allow_small_or_imprecise_dtypes=True), writes=['io_f'])
    P.op('pool', lambda e: e.iota(k.iop, pattern=[[1, 1]], base=0, channel_multiplier=1,
                                  allow_small_or_imprecise_dtypes=True), writes=['iop'])
    P.op('dve', lambda e: e.tensor_scalar(out=k.ident, in0=k.io_f, scalar1=k.iop[:, 0:1], scalar2=None,
                                          op0=ALU.is_equal), reads=['io_f', 'iop'], writes=['ident'])
    P.op('dve', lambda e: e.memset(k.ones_row, 1.0), writes=['ones_row'])
    k.wb = {}
    order = []
    for l in k.layers:
        if l % 2 == 0:
            order += [('ev_w_in', l // 2), ('ev_pool_w', l // 2), ('ev_w_out', l // 2)]
        else:
            order += [('od_w_in', l // 2), ('od_cmp_k_w1', l // 2), ('od_cmp_k_w2', l // 2), ('od_cmp_v_w1', l // 2),
                      ('od_cmp_v_w2', l // 2), ('od_w_out', l // 2)]
        order += [('ffn_w_gate', l), ('ffn_w_up', l), ('ffn_w_down', l)]
    for name, idx in order:
        src = ins[name][idx]
        dst = k.dram('wb_%s_%d' % (name, idx), src.shape, BF16)
        k.wb[(name, idx)] = dst
        cast_copy(k, dst, src, ('wb', name, idx))
    k.xt = [k.sb('xt%d' % s, [128, D], F32) for s in range(2)]
    k.hb = k.sb('hb', [128, D], BF16)
    k.junk = k.sb('junk', [128, D], BF16)
    k.tmpf = k.sb('tmpf', [128, D], F32)
    k.G = {n: k.sb('G_' + n, [128, D], F32) for n in ['mix_pre', 'mix_post', 'ffn_pre', 'ffn_post']}
    k.st = {n: k.sb('st_' + n, [128, 1], F32) for n in ['ssq', 'tmp', 'rstd', 'ssq2', 'tmp2', 'rstd2']}


def load_gains(k, l):
    for n in ['mix_pre', 'mix_post', 'ffn_pre', 'ffn_post']:
        k.P.dma('sp', k.G[n], k.ins['ln_' + n][l:l + 1, :].broadcast_to([128, D]), writes=['G_' + n], stream='small')


def norm_to_hT(k, xt_ap, xres, gname, hT_dst, hTres):
    P = k.P
    st = k.st
    P.op('act', lambda e: e.activation(out=k.junk, in_=xt_ap, func=AF.Square, accum_out=st['ssq']),
         reads=[xres], writes=['junk', 'ssq'])
    rstd_from_ssq(k, st['ssq'], st['rstd'], st['tmp'], D, EPS, '')
    P.op('dve', lambda e: e.scalar_tensor_tensor(out=k.hb, in0=xt_ap, scalar=st['rstd'][:, 0:1], in1=k.G[gname],
                                                 op0=ALU.mult, op1=ALU.mult),
         reads=[xres, 'rstd', 'G_' + gname], writes=['hb'])
    tp = k.bankb[0]
    for c in range(8):
        P.op('pe', lambda e, c=c: e.transpose(out=tp[:, c * 128:(c + 1) * 128], in_=k.hb[:, c * 128:(c + 1) * 128],
                                              identity=k.ident), reads=['hb', 'ident'], writes=['bank0'])
    P.op('act', lambda e: e.copy(out=hT_dst, in_=tp.rearrange('p (c t) -> p c t', c=8)), reads=['bank0'],
         writes=[hTres])


def post_norm_residual(k, pm, pmres, gname, xt_ap, xres):
    P = k.P
    st = k.st
    ssa = k.sb2
    for cb in range(2):
        P.op('act', lambda e, cb=cb: e.activation(out=k.junk[:, cb * 512:(cb + 1) * 512], in_=pm[cb], func=AF.Square,
                                                  accum_out=ssa[:, cb:cb + 1]),
             reads=[pmres[cb]], writes=['junk', 'ssa'])
    P.op('dve', lambda e: e.tensor_tensor(out=st['ssq2'], in0=ssa[:, 0:1], in1=ssa[:, 1:2], op=ALU.add),
         reads=['ssa'], writes=['2ssq'])
    rstd_from_ssq(k, st['ssq2'], st['rstd2'], st['tmp2'], D, EPS, '2')
    for cb in range(2):
        sl = slice(cb * 512, (cb + 1) * 512)
        P.op('dve', lambda e, cb=cb, sl=sl: e.scalar_tensor_tensor(out=k.tmpf[:, sl], in0=pm[cb], scalar=st['rstd2'][:, 0:1],
                                                                   in1=k.G[gname][:, sl], op0=ALU.mult, op1=ALU.mult),
             reads=[pmres[cb], '2rstd', 'G_' + gname], writes=['tmpf'])
    P.op('pool', lambda e: e.tensor_tensor(out=xt_ap, in0=xt_ap, in1=k.tmpf, op=ALU.add), reads=['tmpf', xres],
         writes=[xres])


def ffn_phase(k, l, xsrc, xdst):
    nc, P = k.nc, k.P
    wg, wu, wd = k.wb[('ffn_w_gate', l)], k.wb[('ffn_w_up', l)], k.wb[('ffn_w_down', l)]
    P.dma('sp', k.wd_sb, wd.rearrange('(c p) n -> p c n', p=128), reads=[('wb', 'ffn_w_down', l)], writes=['wd_sb'],
          stream='w')
    groups = [(0, 4), (4, 4), (8, 4), (12, 4), (16, 4), (20, 2)]
    wgv = wg.rearrange('(c p) n -> p c n', p=128)
    wuv = wu.rearrange('(c p) n -> p c n', p=128)
    gi = 0
    for tb in range(T // 512):
        for s in range(4):
            t0 = tb * 512 + s * 128
            P.dma('sp', k.xt[s], xsrc[t0:t0 + 128, :], reads=[('x', t0 // 128)], writes=['xt%d' % s], stream='x')
            norm_to_hT(k, k.xt[s], 'xt%d' % s, 'ffn_pre', k.hT[:, :, s * 128:(s + 1) * 128], 'hT')
        for (j0, nj) in groups:
            buf = gi % 2
            gi += 1
            wt = k.wgu[buf]
            P.dma('sp', wt[:, 0, :, 0:nj * 128], wgv[:, :, j0 * 128:(j0 + nj) * 128], reads=[('wb', 'ffn_w_gate', l)],
                  writes=['wgu%d' % buf], stream='w')
            P.dma('sp', wt[:, 1, :, 0:nj * 128], wuv[:, :, j0 * 128:(j0 + nj) * 128], reads=[('wb', 'ffn_w_up', l)],
                  writes=['wgu%d' % buf], stream='w')
            for jj in range(nj):
                j = j0 + jj
                pb = 1 + 2 * (j % 2)
                pg, pu = k.bank[pb], k.bank[pb + 1]
                for kc in range(8):
                    P.op('pe', lambda e, kc=kc, jj=jj: e.matmul(pg, lhsT=wt[:, 0, kc, jj * 128:(jj + 1) * 128], rhs=k.hT[:, kc, :],
                                                               start=(kc == 0), stop=(kc == 7)),
                         reads=['wgu%d' % buf, 'hT'], writes=['bank%d' % pb])
                for kc in range(8):
                    P.op('pe', lambda e, kc=kc, jj=jj: e.matmul(pu, lhsT=wt[:, 1, kc, jj * 128:(jj + 1) * 128], rhs=k.hT[:, kc, :],
                                                               start=(kc == 0), stop=(kc == 7)),
                         reads=['wgu%d' % buf, 'hT'], writes=['bank%d' % (pb + 1)])
                sg = k.sg[j % 2]
                P.op('act', lambda e: e.activation(out=sg, in_=pg, func=AF.Silu), reads=['bank%d' % pb],
                     writes=['sg%d' % (j % 2)])
                P.op('dve', lambda e, j=j: e.tensor_tensor(out=k.actT[:, j, :], in0=pu, in1=sg, op=ALU.mult),
                     reads=['bank%d' % (pb + 1), 'sg%d' % (j % 2)], writes=[('actT', j)])
        for s in range(4):
            t0 = tb * 512 + s * 128
            pm = [k.bank[5], k.bank[6]]
            for cb in range(2):
                for kk in range(NFC):
                    P.op('pe', lambda e, kk=kk, cb=cb: e.matmul(pm[cb], lhsT=k.actT[:, kk, s * 128:(s + 1) * 128],
                                                               rhs=k.wd_sb[:, kk, cb * 512:(cb + 1) * 512],
                                                               start=(kk == 0), stop=(kk == NFC - 1)),
                         reads=[('actT', kk), 'wd_sb'], writes=['bank%d' % (5 + cb)])
            post_norm_residual(k, pm, ['bank5', 'bank6'], 'ffn_post', k.xt[s], 'xt%d' % s)
            P.dma('sp', xdst[t0:t0 + 128, :], k.xt[s], reads=['xt%d' % s], writes=[('x', t0 // 128)], stream='xo')


def even_phase(k, l, xsrc, xdst, es):
    nc, P = k.nc, k.P
    e_ = l // 2
    ins = k.ins

    def sb(name, shape, dt):
        return es.enter_context(nc.sbuf_tensor('evs%d_' % l + name, list(shape), dt)).ap()

    w_in_sb = sb('w_in', [128, 8, 1536], BF16)
    w_out_sb = sb('w_out', [128, 8, 1024], BF16)
    poolw_sb = sb('poolw', [128, 4, 128], BF16)
    Dm_sb = sb('Dm', [128, 3, 4, 128], BF16)
    WmT = sb('WmT', [128, 4, 128], BF16)
    wsf = sb('wsf', [128, 4, 128], F32)
    wsb = sb('wsb', [128, 4, 128], BF16)
    tril = sb('tril', [128, 128], F32)
    Bt = sb('Bt', [128, 512], F32)
    lng = sb('lng', [128, 512], F32)
    lnb = sb('lnb', [128, 512], F32)
    psc = sb('psc', [128, 4], F32)
    a_sb = [sb('a%d' % i, [128, 512], BF16) for i in range(2)]
    uT_sb = sb('uT', [128, 4, 128], BF16)
    vg = sb('vg', [128, 512], F32)
    vn = sb('vn', [128, 512], F32)
    vln = sb('vln', [128, 512], BF16)
    diffT = sb('diffT', [128, 4, 128], BF16)
    yaT = sb('yaT', [128, 4, 128], BF16)
    ybT = sb('ybT', [128, 4, 128], BF16)
    mxb = sb('mxb', [128, 512], F32)
    hT1 = sb('hT1', [128, 8, 128], BF16)
    bst = sb('bst', [128, 6], F32)
    mv = sb('mv', [128, 2], F32)
    lrs = sb('lrs', [128, 1], F32)

    P.dma('sp', w_in_sb, k.wb[('ev_w_in', e_)].rearrange('(c p) n -> p c n', p=128), reads=[('wb', 'ev_w_in', e_)],
          writes=['w_in'], stream='w')
    P.dma('sp', w_out_sb, k.wb[('ev_w_out', e_)].rearrange('(c p) n -> p c n', p=128), reads=[('wb', 'ev_w_out', e_)],
          writes=['w_out'], stream='w')
    P.dma('sp', poolw_sb, k.wb[('ev_pool_w', e_)].rearrange('g c d -> c g d'), reads=[('wb', 'ev_pool_w', e_)],
          writes=['poolw'], stream='w')
    P.dma('sp', Dm_sb, ins['c_D'].rearrange('a g s t -> s a g t'), writes=['Dm'], stream='small')
    P.dma('sp', wsf, ins['ev_sgu_w'][e_].rearrange('g t s -> t g s'), writes=['wsf'], stream='small')
    P.dma('sp', Bt, ins['ev_sgu_b'][e_:e_ + 1].rearrange('o g t -> o (g t)').broadcast_to([128, 512]), writes=['Bt'],
          stream='small')
    P.dma('sp', lng, ins['ev_sgu_ln_g'][e_:e_ + 1, :].broadcast_to([128, 512]), writes=['lng'], stream='small')
    P.dma('sp', lnb, ins['ev_sgu_ln_b'][e_:e_ + 1, :].broadcast_to([128, 512]), writes=['lnb'], stream='small')
    with nc.allow_non_contiguous_dma(reason='tiny per-channel scale'):
        P.dma('sp', psc, ins['ev_pool_scale'][e_].rearrange('(g p) -> p g', p=128), writes=['psc'], stream='small')
    P.op('dve', lambda e: e.tensor_scalar(out=tril, in0=k.io_f, scalar1=k.iop[:, 0:1], scalar2=None, op0=ALU.is_le),
         reads=['io_f', 'iop'], writes=['tril'])
    P.op('dve', lambda e: e.tensor_tensor(out=wsb, in0=wsf, in1=tril.unsqueeze(1).broadcast_to([128, 4, 128]), op=ALU.mult),
         reads=['wsf', 'tril'], writes=['wsb'])
    tp = k.bankb[0]
    for g in range(4):
        P.op('pe', lambda e, g=g: e.transpose(out=tp[:, g * 128:(g + 1) * 128], in_=wsb[:, g, :], identity=k.ident),
             reads=['wsb', 'ident'], writes=['bank0'])
    P.op('act', lambda e: e.copy(out=WmT, in_=tp[:, 0:512].rearrange('p (g t) -> p g t', g=4)), reads=['bank0'],
         writes=['WmT'])

    for i in range(NT):
        xt = k.xt[i % 2]
        xr = 'xt%d' % (i % 2)
        P.dma('sp', xt, xsrc[i * 128:(i + 1) * 128, :], reads=[('x', i)], writes=[xr], stream='x')
        norm_to_hT(k, xt, xr, 'mix_pre', hT1, 'hT1')
        a_cur, a_prev = a_sb[i % 2], a_sb[(i + 1) % 2]
        ar, apr = 'a%d' % (i % 2), 'a%d' % ((i + 1) % 2)
        pa, pu, pv = k.bank[1], k.bank[2], k.bank[3]
        for kc in range(8):
            P.op('pe', lambda e, kc=kc: e.matmul(pa, lhsT=hT1[:, kc, :], rhs=w_in_sb[:, kc, 0:512], start=(kc == 0),
                                                stop=(kc == 7)), reads=['hT1', 'w_in'], writes=['bank1'])
        P.op('act', lambda e: e.copy(out=a_cur, in_=pa), reads=['bank1'], writes=[ar])
        for kc in range(8):
            P.op('pe', lambda e, kc=kc: e.matmul(pv, lhsT=hT1[:, kc, :], rhs=w_in_sb[:, kc, 1024:1536], start=(kc == 0),
                                                stop=(kc == 7)), reads=['hT1', 'w_in'], writes=['bank3'])
        P.op('act', lambda e: e.activation(out=vg, in_=pv, func=AF.Gelu_apprx_tanh), reads=['bank3'], writes=['vg'])
        for c in range(4):
            for kc in range(8):
                P.op('pe', lambda e, kc=kc, c=c: e.matmul(pu[:, c * 128:(c + 1) * 128],
                                                         lhsT=w_in_sb[:, kc, 512 + c * 128:512 + (c + 1) * 128],
                                                         rhs=hT1[:, kc, :], start=(kc == 0), stop=(kc == 7)),
                     reads=['hT1', 'w_in'], writes=['bank2'])
        P.op('act', lambda e: e.activation(out=uT_sb, in_=pu.rearrange('p (c t) -> p c t', c=4), func=AF.Gelu_apprx_tanh),
             reads=['bank2'], writes=['uT'])
        P.op('dve', lambda e: e.bn_stats(out=bst, in_=vg), reads=['vg'], writes=['bst'])
        P.op('dve', lambda e: e.bn_aggr(out=mv, in_=bst), reads=['bst'], writes=['mv'])
        P.op('dve', lambda e: e.tensor_scalar(out=lrs, in0=mv[:, 1:2], scalar1=1e-5, scalar2=None, op0=ALU.add),
             reads=['mv'], writes=['lrs'])
        P.op('act', lambda e: e.activation(out=lrs, in_=lrs, func=AF.Sqrt), reads=['lrs'], writes=['lrs'])
        P.op('dve', lambda e: e.reciprocal(out=lrs, in_=lrs), reads=['lrs'], writes=['lrs'])
        P.op('dve', lambda e: e.tensor_scalar(out=vn, in0=vg, scalar1=mv[:, 0:1], scalar2=lrs[:, 0:1], op0=ALU.subtract,
                                              op1=ALU.mult), reads=['vg', 'mv', 'lrs'], writes=['vn'])
        P.op('pool', lambda e: e.tensor_tensor(out=vn, in0=vn, in1=lng, op=ALU.mult), reads=['vn', 'lng'], writes=['vn'])
        P.op('pool', lambda e: e.tensor_tensor(out=vln, in0=vn, in1=lnb, op=ALU.add), reads=['vn', 'lnb'], writes=['vln'])
        pd_ = k.bank[4]
        for g in range(4):
            first = True
            sl = slice(g * 128, (g + 1) * 128)
            P.op('pe', lambda e, g=g, sl=sl: e.matmul(pd_[:, sl], lhsT=a_cur[:, sl], rhs=Dm_sb[:, 0 if i == 0 else 1, g, :],
                                                     start=True, stop=(i == 0)), reads=[ar, 'Dm'], writes=['bank4'])
            if i > 0:
                P.op('pe', lambda e, g=g, sl=sl: e.matmul(pd_[:, sl], lhsT=a_prev[:, sl], rhs=Dm_sb[:, 2, g, :],
                                                         start=False, stop=True), reads=[apr, 'Dm'], writes=['bank4'])
        P.op('dve', lambda e: e.tensor_copy(out=diffT, in_=pd_.rearrange('p (g t) -> p g t', g=4)), reads=['bank4'],
             writes=['diffT'])
        pya = k.bank[5]
        for g in range(4):
            P.op('pe', lambda e, g=g: e.matmul(pya[:, g * 128:(g + 1) * 128], lhsT=poolw_sb[:, g, :], rhs=diffT[:, g, :],
                                              start=True, stop=True), reads=['poolw', 'diffT'], writes=['bank5'])
        P.op('dve', lambda e: e.tensor_tensor(out=yaT, in0=pya.rearrange('p (g t) -> p g t', g=4),
                                              in1=psc.unsqueeze(2).broadcast_to([128, 4, 128]), op=ALU.mult),
             reads=['bank5', 'psc'], writes=['yaT'])
        pmx = k.bank[4]
        for g in range(4):
            P.op('pe', lambda e, g=g: e.matmul(pmx[:, g * 128:(g + 1) * 128], lhsT=vln[:, g * 128:(g + 1) * 128],
                                              rhs=WmT[:, g, :], start=True, stop=True), reads=['vln', 'WmT'],
                 writes=['bank4'])
        P.op('dve', lambda e: e.tensor_tensor(out=mxb, in0=pmx, in1=Bt, op=ALU.add), reads=['bank4', 'Bt'],
             writes=['mxb'])
        P.op('pool', lambda e: e.tensor_tensor(out=ybT, in0=mxb.rearrange('p (g t) -> p g t', g=4), in1=uT_sb, op=ALU.mult),
             reads=['mxb', 'uT'], writes=['ybT'])
        pm = [k.bank[6], k.bank[7]]
        for cb in range(2):
            for kc in range(8):
                lh = yaT[:, kc, :] if kc < 4 else ybT[:, kc - 4, :]
                P.op('pe', lambda e, kc=kc, cb=cb, lh=lh: e.matmul(pm[cb], lhsT=lh, rhs=w_out_sb[:, kc, cb * 512:(cb + 1) * 512],
                                                                  start=(kc == 0), stop=(kc == 7)),
                     reads=['yaT', 'ybT', 'w_out'], writes=['bank%d' % (6 + cb)])
        post_norm_residual(k, pm, ['bank6', 'bank7'], 'mix_post', xt, xr)
        P.dma('sp', xdst[i * 128:(i + 1) * 128, :], xt, reads=[xr], writes=[('x', i)], stream='xo')


def odd_phase(k, l, xsrc, xdst, es):
    from contextlib import ExitStack
    nc, P = k.nc, k.P
    o_ = l // 2
    ins = k.ins
    PI = 3.14159265358979

    def sbs(stack, name, shape, dt):
        return stack.enter_context(nc.sbuf_tensor('od%d_' % l + name, list(shape), dt)).ap()

    def sb(name, shape, dt):
        return sbs(es, name, shape, dt)

    qd = k.dram('qd%d' % l, [T, 512], BF16)
    ydd = k.dram('ydd%d' % l, [T, 512], BF16)
    KE = sb('KE', [128, 2, T], BF16)
    kwT = sb('kwT', [64, 2, T], BF16)
    kcvd = k.dram('kcvd%d' % l, [4, 64, T], BF16)
    ropeS = k.dram('ropeS%d' % l, [128, NT, 72], F32)
    ropeC = k.dram('ropeC%d' % l, [128, NT, 72], F32)
    vs_aug = sb('vs_aug', [128, NT, 2, 65], BF16)
    vw_aug = sb('vw_aug', [128, NT, 2, 65], BF16)
    gsig = sb('gsig', [128, NT, 24], F32)
    kcmpT = sb('kcmpT', [64, 2, 256], BF16)
    vcmp_aug = sb('vcmp', [128, 2, 2, 65], BF16)
    tri = sb('tri', [128, 128], F32)
    ntri = sb('ntri', [128, 128], F32)
    cm = sb('cm', [128, 128], F32)
    keep = sb('keep', [128, 128], F32)
    addc = sb('addc', [128, 128], F32)
    cts = sb('cts', [128, 2, 64], BF16)
    decT = sb('decT', [128, 512], F32)
    xi = sb('xi', [128, 512], F32)
    zeta = sb('zeta', [128, 4], F32)
    gch = sb('gch', [128, 4], F32)
    gng = sb('gng', [128, 512], F32)
    for nm, t_, src in [('tri', tri, 'c_tri'), ('cm', cm, 'c_cm'), ('keep', keep, 'c_keep'), ('addc', addc, 'c_add'),
                        ('decT', decT, 'c_decT'), ('xi', xi, 'c_xi'), ('zeta', zeta, 'c_zeta'), ('gch', gch, 'c_gch')]:
        P.dma('sp', t_, ins[src], writes=[nm], stream='small')
    P.dma('sp', cts, ins['c_cts'].rearrange('c n j -> n c j'), writes=['cts'], stream='small')
    P.dma('sp', KE[64:128, 0, :], ins['c_E'], writes=['KE'], stream='small')
    P.dma('sp', KE[64:128, 1, :], ins['c_E'], writes=['KE'], stream='small')
    P.dma('sp', gng, ins['od_ret_gn_g'][o_:o_ + 1, :].broadcast_to([128, 512]), writes=['gng'], stream='small')
    P.op('dve', lambda e: e.tensor_scalar(out=ntri, in0=tri, scalar1=-1.0, scalar2=1.0, op0=ALU.mult, op1=ALU.add),
         reads=['tri'], writes=['ntri'])
    P.op('pool', lambda e: e.memset(vs_aug, 1.0), writes=['vs_aug'])
    P.op('pool', lambda e: e.memset(vw_aug, 1.0), writes=['vw_aug'])
    P.op('pool', lambda e: e.memset(vcmp_aug, 1.0), writes=['vcmp'])
    with ExitStack() as ts:
        posi = sbs(ts, 'posi', [128, NT], I32)
        sinT = sbs(ts, 'sinT', [128, NT, 72], F32)
        cosT = sbs(ts, 'cosT', [128, NT, 72], F32)
        posf = sbs(ts, 'posf', [128, NT], F32)
        invf = sbs(ts, 'invf', [128, 72], F32)
        ang = sbs(ts, 'ang', [128, NT, 72], F32)
        arg = sbs(ts, 'arg', [128, NT, 72], F32)
        kf = sbs(ts, 'kf', [128, NT, 72], F32)
        ki = sbs(ts, 'ki', [128, NT, 72], I32)
        with nc.allow_non_contiguous_dma(reason='positions to token-on-partition layout'):
            P.dma('sp', posi, ins['positions'].rearrange('o (i p) -> p (o i)', p=128), writes=['posi'], stream='small')
        P.dma('sp', invf, ins['c_invf'].broadcast_to([128, 72]), writes=['invf'], stream='small')
        P.op('dve', lambda e: e.tensor_copy(out=posf, in_=posi), reads=['posi'], writes=['posf'])
        P.op('dve', lambda e: e.tensor_tensor(out=ang, in0=posf.unsqueeze(2).broadcast_to([128, NT, 72]),
                                              in1=invf.unsqueeze(1).broadcast_to([128, NT, 72]), op=ALU.mult),
             reads=['posf', 'invf'], writes=['ang'])
        for shift, dst, dn in [(0.0, sinT, 'sinT'), (PI / 2, cosT, 'cosT')]:
            P.op('dve', lambda e, shift=shift: e.tensor_scalar(out=arg, in0=ang, scalar1=shift, scalar2=None, op0=ALU.add),
                 reads=['ang'], writes=['arg'])
            P.op('dve', lambda e: e.tensor_scalar(out=kf, in0=arg, scalar1=1.0 / (2 * PI), scalar2=None, op0=ALU.mult),
                 reads=['arg'], writes=['kf'])
            P.op('dve', lambda e: e.tensor_copy(out=ki, in_=kf), reads=['kf'], writes=['ki'])
            P.op('dve', lambda e: e.tensor_copy(out=kf, in_=ki), reads=['ki'], writes=['kf'])
            P.op('dve', lambda e: e.scalar_tensor_tensor(out=arg, in0=kf, scalar=-6.28125, in1=arg, op0=ALU.mult, op1=ALU.add),
                 reads=['kf', 'arg'], writes=['arg'])
            P.op('dve', lambda e: e.scalar_tensor_tensor(out=arg, in0=kf, scalar=-(2 * PI - 6.28125), in1=arg, op0=ALU.mult,
                                                         op1=ALU.add), reads=['kf', 'arg'], writes=['arg'])
            P.op('dve', lambda e: e.tensor_scalar(out=arg, in0=arg, scalar1=3.1415925, scalar2=-3.1415925, op0=ALU.min,
                                                  op1=ALU.max), reads=['arg'], writes=['arg'])
            P.op('act', lambda e, dst=dst: e.activation(out=dst, in_=arg, func=AF.Sin), reads=['arg'], writes=[dn])
        P.dma('sp', ropeS, sinT, reads=['sinT'], writes=['ropeS'], stream='xo')
        P.dma('sp', ropeC, cosT, reads=['cosT'], writes=['ropeC'], stream='xo')
        P.barrier()

    with ExitStack() as s1:
        w_in_sb = sbs(s1, 'w_in', [128, 8, 3352], BF16)
        hT1 = sbs(s1, 'hT1', [128, 8, 128], BF16)
        csb = [sbs(s1, 'csb%d' % j, [128, 2, 72], F32) for j in range(2)]
        kcv_t = sbs(s1, 'kcv_t', [64, 4, 128], BF16)
        zq = sbs(s1, 'zq', [128, 512], F32)
        qb = sbs(s1, 'qb', [128, 512], BF16)
        zkv = sbs(s1, 'zkv', [128, 768], F32)
        kvb = sbs(s1, 'kvb', [128, 768], BF16)
        rt = [sbs(s1, 'rt%d' % j, [128, 512], F32) for j in range(4)]
        zrq = sbs(s1, 'zrq', [128, 512], F32)
        zrk = sbs(s1, 'zrk', [128, 512], F32)
        rqb = sbs(s1, 'rqb', [128, 512], BF16)
        rkb = sbs(s1, 'rkb', [128, 512], BF16)
        rkr = sbs(s1, 'rkr', [128, 512], F32)
        rkz = sbs(s1, 'rkz', [128, 512], BF16)
        rvb = sbs(s1, 'rvb', [128, 512], BF16)
        gsl = sbs(s1, 'gsl', [128, 512], F32)
        qkT = sbs(s1, 'qkT', [128, 8, 128], BF16)
        qxiT = sbs(s1, 'qxiT', [128, 512], BF16)
        STs = sbs(s1, 'STs', [128, 512], BF16)
        state = sbs(s1, 'state', [128, 512], F32)
        stbf = sbs(s1, 'stbf', [128, 512], BF16)
        bst4 = sbs(s1, 'bst4', [128, 4, 6], F32)
        mv4 = sbs(s1, 'mv4', [128, 4, 2], F32)
        rs4 = sbs(s1, 'rs4', [128, 4], F32)
        on = sbs(s1, 'on', [128, 512], F32)
        ydb = sbs(s1, 'ydb', [128, 512], BF16)
        P.dma('sp', w_in_sb, k.wb[('od_w_in', o_)].rearrange('(c p) n -> p c n', p=128), reads=[('wb', 'od_w_in', o_)],
              writes=['w_in'], stream='w')
        P.op('dve', lambda e: e.memset(state, 0.0), writes=['state'])

        def proj(bank, c0, c1):
            for kc in range(8):
                P.op('pe', lambda e, kc=kc: e.matmul(k.bank[bank][:, 0:c1 - c0], lhsT=hT1[:, kc, :], rhs=w_in_sb[:, kc, c0:c1],
                                                    start=(kc == 0), stop=(kc == 7)), reads=['hT1', 'w_in'],
                     writes=['bank%d' % bank])

        def rope(src, dst, nh, hd, half, c_, s_, csn, sname, dname):
            sv = src.rearrange('p (h d) -> p h d', h=nh)
            dv = dst.rearrange('p (h d) -> p h d', h=nh)
            x1, x2 = sv[:, :, 0:half], sv[:, :, half:2 * half]
            cb = c_.unsqueeze(1).broadcast_to([128, nh, half])
            sb_ = s_.unsqueeze(1).broadcast_to([128, nh, half])
            t = [r_[:, 0:nh * half].rearrange('p (h d) -> p h d', h=nh) for r_ in rt]
            P.op('dve', lambda e: e.tensor_tensor(out=t[0], in0=x1, in1=cb, op=ALU.mult), reads=[sname, csn], writes=['rt0'])
            P.op('pool', lambda e: e.tensor_tensor(out=t[1], in0=x2, in1=sb_, op=ALU.mult), reads=[sname, csn], writes=['rt1'])
            P.op('dve', lambda e: e.tensor_tensor(out=t[2], in0=x2, in1=cb, op=ALU.mult), reads=[sname, csn], writes=['rt2'])
            P.op('pool', lambda e: e.tensor_tensor(out=t[3], in0=x1, in1=sb_, op=ALU.mult), reads=[sname, csn], writes=['rt3'])
            if 2 * half < hd:
                P.op('act', lambda e: e.copy(out=dst, in_=src), reads=[sname], writes=[dname])
            P.op('dve', lambda e: e.tensor_tensor(out=dv[:, :, 0:half], in0=t[0], in1=t[1], op=ALU.subtract),
                 reads=['rt0', 'rt1'], writes=[dname])
            P.op('pool', lambda e: e.tensor_tensor(out=dv[:, :, half:2 * half], in0=t[2], in1=t[3], op=ALU.add),
                 reads=['rt2', 'rt3'], writes=[dname])

        for i in range(NT):
            xt = k.xt[i % 2]
            xr = 'xt%d' % (i % 2)
            P.dma('sp', xt, xsrc[i * 128:(i + 1) * 128, :], reads=[('x', i)], writes=[xr], stream='x')
            norm_to_hT(k, xt, xr, 'mix_pre', hT1, 'hT1')
            cs_ = csb[i % 2]
            csn = 'csb%d' % (i % 2)
            P.dma('sp', cs_[:, 0, :], ropeC[:, i, :], reads=['ropeC'], writes=[csn], stream='small')
            P.dma('sp', cs_[:, 1, :], ropeS[:, i, :], reads=['ropeS'], writes=[csn], stream='small')
            cn, sn = cs_[:, 0, 0:8], cs_[:, 1, 0:8]
            cr, sr = cs_[:, 0, 8:72], cs_[:, 1, 8:72]
            proj(1, 0, 512)
            P.op('act', lambda e: e.activation(out=zq, in_=k.bank[1], func=AF.Copy, scale=0.125), reads=['bank1'], writes=['zq'])
            rope(zq, qb, 8, 64, 8, cn, sn, csn, 'zq', 'qb')
            P.dma('sp', qd[i * 128:(i + 1) * 128, :], qb, reads=['qb'], writes=[('qd', i)], stream='xo')
            proj(2, 512, 1024)
            proj(3, 1024, 1304)
            P.op('act', lambda e: e.copy(out=zkv[:, 0:512], in_=k.bank[2]), reads=['bank2'], writes=['zkv'])
            P.op('act', lambda e: e.copy(out=zkv[:, 512:768], in_=k.bank[3][:, 0:256]), reads=['bank3'], writes=['zkv'])
            P.op('act', lambda e: e.activation(out=gsig[:, i, :], in_=k.bank[3][:, 256:280], func=AF.Sigmoid),
                 reads=['bank3'], writes=[('gsig', i)])
            kv4 = zkv.rearrange('p (a b g d) -> p a b g d', a=3, b=2, g=2)[:, :, 0, :, :]
            x1, x2 = kv4[:, :, :, 0:8], kv4[:, :, :, 8:16]
            cb = cn.unsqueeze(1).unsqueeze(1).broadcast_to([128, 3, 2, 8])
            sb_ = sn.unsqueeze(1).unsqueeze(1).broadcast_to([128, 3, 2, 8])
            t = [r_[:, 0:48].rearrange('p (a g d) -> p a g d', a=3, g=2) for r_ in rt]
            P.op('dve', lambda e: e.tensor_tensor(out=t[0], in0=x1, in1=cb, op=ALU.mult), reads=['zkv', csn], writes=['rt0'])
            P.op('dve', lambda e: e.tensor_tensor(out=t[1], in0=x2, in1=sb_, op=ALU.mult), reads=['zkv', csn], writes=['rt1'])
            P.op('dve', lambda e: e.tensor_tensor(out=t[2], in0=x2, in1=cb, op=ALU.mult), reads=['zkv', csn], writes=['rt2'])
            P.op('dve', lambda e: e.tensor_tensor(out=t[3], in0=x1, in1=sb_, op=ALU.mult), reads=['zkv', csn], writes=['rt3'])
            P.op('dve', lambda e: e.tensor_tensor(out=x1, in0=t[0], in1=t[1], op=ALU.subtract), reads=['rt0', 'rt1'], writes=['zkv'])
            P.op('dve', lambda e: e.tensor_tensor(out=x2, in0=t[2], in1=t[3], op=ALU.add), reads=['rt2', 'rt3'], writes=['zkv'])
            P.op('act', lambda e: e.copy(out=kvb, in_=zkv), reads=['zkv'], writes=['kvb'])
            P.op('pool', lambda e: e.tensor_copy(out=vs_aug[:, i, :, 0:64], in_=kvb[:, 384:512].rearrange('p (g d) -> p g d', g=2)),
                 reads=['kvb'], writes=['vs_aug'])
            P.op('pool', lambda e: e.tensor_copy(out=vw_aug[:, i, :, 0:64], in_=kvb[:, 640:768].rearrange('p (g d) -> p g d', g=2)),
                 reads=['kvb'], writes=['vw_aug'])
            tp = k.bankb[0]
            srcs = [0, 64, 128, 192, 512, 576, 256, 320]
            for j, c0 in enumerate(srcs):
                P.op('pe', lambda e, j=j, c0=c0: e.transpose(out=tp[0:64, j * 128:(j + 1) * 128], in_=kvb[:, c0:c0 + 64],
                                                            identity=k.ident), reads=['kvb', 'ident'], writes=['bank0'])
            P.op('act', lambda e: e.copy(out=kcv_t, in_=tp[0:64, 0:512].rearrange('p (j t) -> p j t', j=4)), reads=['bank0'],
                 writes=['kcv_t'])
            P.dma('sp', kcvd[:, :, i * 128:(i + 1) * 128].rearrange('j p t -> p j t'), kcv_t, reads=['kcv_t'], writes=['kcvd'],
                  stream='xo')
            P.op('act', lambda e: e.copy(out=kwT[:, :, i * 128:(i + 1) * 128],
                                         in_=tp[0:64, 512:768].rearrange('p (j t) -> p j t', j=2)), reads=['bank0'], writes=['kwT'])
            P.op('act', lambda e: e.copy(out=KE[0:64, :, i * 128:(i + 1) * 128],
                                         in_=tp[0:64, 768:1024].rearrange('p (j t) -> p j t', j=2)), reads=['bank0'], writes=['KE'])
            proj(4, 1304, 1816)
            proj(5, 1816, 2328)
            proj(6, 2328, 2840)
            proj(7, 2840, 3352)
            P.op('act', lambda e: e.copy(out=zrq, in_=k.bank[4]), reads=['bank4'], writes=['zrq'])
            P.op('act', lambda e: e.activation(out=zrk, in_=k.bank[5], func=AF.Copy, scale=128.0 ** -0.5), reads=['bank5'],
                 writes=['zrk'])
            P.op('act', lambda e: e.copy(out=rvb, in_=k.bank[6]), reads=['bank6'], writes=['rvb'])
            P.op('act', lambda e: e.activation(out=gsl, in_=k.bank[7], func=AF.Silu), reads=['bank7'], writes=['gsl'])
            rope(zrq, rqb, 4, 128, 64, cr, sr, csn, 'zrq', 'rqb')
            rope(zrk, rkr, 4, 128, 64, cr, sr, csn, 'zrk', 'rkr')
            P.op('act', lambda e: e.copy(out=rkb, in_=rkr), reads=['rkr'], writes=['rkb'])
            P.op('dve', lambda e: e.tensor_tensor(out=rkz.rearrange('p (h d) -> p h d', h=4),
                                                  in0=rkr.rearrange('p (h d) -> p h d', h=4),
                                                  in1=zeta.unsqueeze(2).broadcast_to([128, 4, 128]), op=ALU.mult),
                 reads=['rkr', 'zeta'], writes=['rkz'])
            for h in range(4):
                P.op('pe', lambda e, h=h: e.transpose(out=tp[:, h * 128:(h + 1) * 128], in_=rqb[:, h * 128:(h + 1) * 128],
                                                      identity=k.ident), reads=['rqb', 'ident'], writes=['bank0'])
            for h in range(4):
                P.op('pe', lambda e, h=h: e.transpose(out=tp[:, (4 + h) * 128:(5 + h) * 128], in_=rkb[:, h * 128:(h + 1) * 128],
                                                      identity=k.ident), reads=['rkb', 'ident'], writes=['bank0'])
            P.op('act', lambda e: e.copy(out=qkT, in_=tp.rearrange('p (c t) -> p c t', c=8)), reads=['bank0'], writes=['qkT'])
            for h in range(4):
                P.op('pe', lambda e, h=h: e.matmul(k.bank[1][:, h * 128:(h + 1) * 128], lhsT=qkT[:, 4 + h, :], rhs=qkT[:, h, :],
                                                  start=True, stop=True), reads=['qkT'], writes=['bank1'])
            P.op('dve', lambda e: e.tensor_tensor(out=STs, in0=k.bank[1], in1=decT, op=ALU.mult), reads=['bank1', 'decT'],
                 writes=['STs'])
            P.op('pool', lambda e: e.tensor_tensor(out=qxiT, in0=qkT[:, 0:4, :].rearrange('p h t -> p (h t)'), in1=xi, op=ALU.mult),
                 reads=['qkT', 'xi'], writes=['qxiT'])
            for h in range(4):
                hs = slice(h * 128, (h + 1) * 128)
                P.op('pe', lambda e, hs=hs: e.matmul(k.bank[2][:, hs], lhsT=STs[:, hs], rhs=rvb[:, hs], start=True, stop=(i == 0)),
                     reads=['STs', 'rvb'], writes=['bank2'])
                if i > 0:
                    P.op('pe', lambda e, hs=hs: e.matmul(k.bank[2][:, hs], lhsT=qxiT[:, hs], rhs=stbf[:, hs], start=False, stop=True),
                         reads=['qxiT', 'stbf'], writes=['bank2'])
            for h in range(4):
                hs = slice(h * 128, (h + 1) * 128)
                P.op('pe', lambda e, hs=hs: e.matmul(k.bank[3][:, hs], lhsT=rkz[:, hs], rhs=rvb[:, hs], start=True, stop=True),
                     reads=['rkz', 'rvb'], writes=['bank3'])
            P.op('dve', lambda e: e.tensor_tensor(out=state.rearrange('p (h d) -> p h d', h=4),
                                                  in0=state.rearrange('p (h d) -> p h d', h=4),
                                                  in1=gch.unsqueeze(2).broadcast_to([128, 4, 128]), op=ALU.mult),
                 reads=['state', 'gch'], writes=['state'])
            P.op('dve', lambda e: e.tensor_tensor(out=state, in0=k.bank[3], in1=state, op=ALU.add), reads=['bank3', 'state'],
                 writes=['state'])
            P.op('act', lambda e: e.copy(out=stbf, in_=state), reads=['state'], writes=['stbf'])
            for h in range(4):
                P.op('dve', lambda e, h=h: e.bn_stats(out=bst4[:, h, :], in_=k.bank[2][:, h * 128:(h + 1) * 128]),
                     reads=['bank2'], writes=['bst4'])
            for h in range(4):
                P.op('dve', lambda e, h=h: e.bn_aggr(out=mv4[:, h, :], in_=bst4[:, h, :]), reads=['bst4'], writes=['mv4'])
            P.op('dve', lambda e: e.tensor_scalar(out=rs4, in0=mv4[:, :, 1], scalar1=1e-5, scalar2=None, op0=ALU.add),
                 reads=['mv4'], writes=['rs4'])
            P.op('act', lambda e: e.activation(out=rs4, in_=rs4, func=AF.Sqrt), reads=['rs4'], writes=['rs4'])
            P.op('dve', lambda e: e.reciprocal(out=rs4, in_=rs4), reads=['rs4'], writes=['rs4'])
            for h in range(4):
                hs = slice(h * 128, (h + 1) * 128)
                P.op('dve', lambda e, h=h, hs=hs: e.tensor_scalar(out=on[:, hs], in0=k.bank[2][:, hs], scalar1=mv4[:, h, 0:1],
                                                                  scalar2=rs4[:, h:h + 1], op0=ALU.subtract, op1=ALU.mult),
                     reads=['bank2', 'mv4', 'rs4'], writes=['on'])
            P.op('pool', lambda e: e.tensor_tensor(out=on, in0=on, in1=gng, op=ALU.mult), reads=['on', 'gng'], writes=['on'])
            P.op('pool', lambda e: e.tensor_tensor(out=ydb, in0=on, in1=gsl, op=ALU.mult), reads=['on', 'gsl'], writes=['ydb'])
            P.dma('sp', ydd[i * 128:(i + 1) * 128, :], ydb, reads=['ydb'], writes=[('ydd', i)], stream='xo')

        w1 = sbs(s1, 'w1', [64, 32, 128], BF16)
        w2 = sbs(s1, 'w2', [128, 64], BF16)
        posf32 = sbs(s1, 'posf32', [64, 32], F32)
        posT = sbs(s1, 'posT', [64, 32], BF16)
        cbias = sbs(s1, 'cbias', [128, 1], F32)
        ghT = sbs(s1, 'ghT', [128, 256], BF16)
        csrc = sbs(s1, 'csrc', [64, T], BF16)
        for kind, (n1, n2, npos) in enumerate([('od_cmp_k_w1', 'od_cmp_k_w2', 'od_cmp_k_pos'),
                                               ('od_cmp_v_w1', 'od_cmp_v_w2', 'od_cmp_v_pos')]):
            P.dma('sp', w1, k.wb[(n1, o_)].rearrange('(l d) j -> d l j', d=64), reads=[('wb', n1, o_)], writes=['w1'], stream='w')
            P.dma('sp', w2, k.wb[(n2, o_)], reads=[('wb', n2, o_)], writes=['w2'], stream='w')
            with nc.allow_non_contiguous_dma(reason='tiny pos-emb transpose'):
                P.dma('sp', posf32, ins[npos][o_].rearrange('l d -> d l'), writes=['posf32'], stream='small')
            P.op('dve', lambda e: e.tensor_copy(out=posT, in_=posf32), reads=['posf32'], writes=['posT'])
            for g in range(2):
                P.dma('sp', csrc, kcvd[2 * kind + g], reads=['kcvd'], writes=['csrc'], stream='w')
                srcv = csrc.rearrange('p (n s) -> p n s', s=16)
                hb_ = k.bank[1]
                for l_ in range(32):
                    P.op('pe', lambda e, l_=l_: e.matmul(hb_[:, 0:255], lhsT=w1[:, l_, :],
                                                        rhs=(srcv[:, 0:255, l_] if l_ < 16 else srcv[:, 1:256, l_ - 16]),
                                                        start=(l_ == 0), stop=(l_ == 31)),
                         reads=['w1', 'csrc'], writes=['bank1'])
                for l_ in range(32):
                    P.op('pe', lambda e, l_=l_: e.matmul(hb_[:, 256:257], lhsT=w1[:, l_, :], rhs=posT[:, l_:l_ + 1],
                                                        start=(l_ == 0), stop=(l_ == 31)), reads=['w1', 'posT'], writes=['bank1'])
                P.op('dve', lambda e: e.tensor_copy(out=cbias, in_=hb_[:, 256:257]), reads=['bank1'], writes=['cbias'])
                P.op('dve', lambda e: e.memset(ghT[:, 255:256], 0.0), writes=['ghT'])
                P.op('act', lambda e: e.activation(out=ghT[:, 0:255], in_=hb_[:, 0:255], func=AF.Gelu_apprx_tanh,
                                                   bias=cbias[:, 0:1]), reads=['bank1', 'cbias'], writes=['ghT'])
                if kind == 0:
                    P.op('pe', lambda e: e.matmul(k.bank[2][0:64, 0:256], lhsT=w2, rhs=ghT, start=True, stop=True),
                         reads=['w2', 'ghT'], writes=['bank2'])
                    P.op('act', lambda e, g=g: e.copy(out=kcmpT[:, g, :], in_=k.bank[2][0:64, 0:256]), reads=['bank2'],
                         writes=['kcmpT'])
                else:
                    for c in range(2):
                        P.op('pe', lambda e, c=c: e.matmul(k.bank[2][:, c * 64:(c + 1) * 64], lhsT=ghT[:, c * 128:(c + 1) * 128],
                                                          rhs=w2, start=True, stop=True), reads=['w2', 'ghT'], writes=['bank2'])
                    P.op('act', lambda e, g=g: e.copy(out=vcmp_aug[:, :, g, 0:64],
                                                      in_=k.bank[2][:, 0:128].rearrange('p (c d) -> p c d', c=2)),
                         reads=['bank2'], writes=['vcmp'])
        P.barrier()

    with ExitStack() as s2:
        w_out_sb = sbs(s2, 'w_out', [128, 8, 1024], BF16)
        PT = sbs(s2, 'PT', [128, NT, 512], BF16)
        PW = sbs(s2, 'PW', [128, 5, 512], BF16)
        Pc = sbs(s2, 'Pc', [128, 2, 512], BF16)
        QN = [sbs(s2, 'QN%d' % g, [128, 512], BF16) for g in range(2)]
        qt = [sbs(s2, 'qt%d' % j, [128, 512], BF16) for j in range(2)]
        ydt = sbs(s2, 'ydt', [128, 512], BF16)
        negt = sbs(s2, 'negt', [128, 128], BF16)
        expf = sbs(s2, 'expf', [128, 512], F32)
        score = sbs(s2, 'score', [128, 64], F32)
        sc2 = sbs(s2, 'sc2', [128, 64], F32)
        m8a = sbs(s2, 'm8a', [128, 8], F32)
        m8b = sbs(s2, 'm8b', [128, 8], F32)
        thr = sbs(s2, 'thr', [128, 1], F32)
        psl = sbs(s2, 'psl', [128, 64], F32)
        rden = sbs(s2, 'rden', [128, 12], F32)
        coef = sbs(s2, 'coef', [128, 12], F32)
        yc = sbs(s2, 'yc', [128, 512], F32)
        ycb = sbs(s2, 'ycb', [128, 512], BF16)
        yT = sbs(s2, 'yT', [128, 8, 128], BF16)
        P.dma('sp', w_out_sb, k.wb[('od_w_out', o_)].rearrange('(c p) n -> p c n', p=128), reads=[('wb', 'od_w_out', o_)],
              writes=['w_out'], stream='w')
        P.op('dve', lambda e: e.memset(negt, 0.0), writes=['negt'])
        tp = k.bankb[0]
        sbank = [0]

        def next_sbank():
            sbank[0] += 1
            return 1 + (sbank[0] % 2)

        def den_view(bank):
            return k.bank[bank][:, 0:260].rearrange('p (m e) -> p m e', e=65)[:, :, 64]

        def masked_exp(b, nn, dst, mask_ap, dname):
            if mask_ap is None:
                P.op('act', lambda e: e.activation(out=dst[0:nn], in_=k.bank[b][0:nn, :], func=AF.Exp), reads=['bank%d' % b],
                     writes=[dname])
            else:
                P.op('act', lambda e: e.activation(out=expf[0:nn], in_=k.bank[b][0:nn, :], func=AF.Exp), reads=['bank%d' % b],
                     writes=['expf'])
                P.op('pool', lambda e: e.tensor_tensor(out=dst[0:nn].rearrange('p (m q) -> p m q', m=4),
                                                       in0=expf[0:nn].rearrange('p (m q) -> p m q', m=4),
                                                       in1=mask_ap.unsqueeze(1).broadcast_to([nn, 4, 128]), op=ALU.mult),
                     reads=['expf', 'tri', 'ntri'], writes=[dname])

        for i in range(NT):
            xt = k.xt[i % 2]
            xr = 'xt%d' % (i % 2)
            P.dma('sp', xt, xsrc[i * 128:(i + 1) * 128, :], reads=[('x', i)], writes=[xr], stream='x')
            qti = qt[i % 2]
            qtn = 'qt%d' % (i % 2)
            P.dma('sp', qti, qd[i * 128:(i + 1) * 128, :], reads=[('qd', i)], writes=[qtn], stream='x')
            P.dma('sp', ydt, ydd[i * 128:(i + 1) * 128, :], reads=[('ydd', i)], writes=['ydt'], stream='x')
            off = 62 - 2 * i
            for g in range(2):
                Q = QN[g]
                qn = 'QN%d' % g
                for m in range(4):
                    h = 4 * g + m
                    P.op('pe', lambda e, m=m, h=h: e.transpose(out=tp[0:64, m * 128:(m + 1) * 128], in_=qti[:, h * 64:(h + 1) * 64],
                                                              identity=k.ident), reads=[qtn, 'ident'], writes=['bank0'])
                P.op('act', lambda e: e.copy(out=Q[0:64, :], in_=tp[0:64, 0:512]), reads=['bank0'], writes=[qn])
                chunks = [(0, 128)] + ([(1, 127)] if 8 * i + 6 >= 128 else [])
                for (c, nn) in chunks:
                    b = next_sbank()
                    P.op('pe', lambda e, c=c, nn=nn, b=b: e.matmul(k.bank[b][0:nn, :], lhsT=kcmpT[:, g, c * 128:c * 128 + nn],
                                                                  rhs=Q[0:64, :], start=True, stop=True),
                         reads=['kcmpT', qn], writes=['bank%d' % b])
                    full = (16 * (c * 128 + nn - 1) + 31 <= 128 * i)
                    if full:
                        P.op('act', lambda e, c=c, nn=nn, b=b: e.activation(out=Pc[0:nn, c, :], in_=k.bank[b][0:nn, :], func=AF.Exp),
                             reads=['bank%d' % b], writes=['Pc'])
                    else:
                        tv = float(128 * i - 31 - 2048 * c)
                        P.op('act', lambda e, nn=nn, b=b: e.activation(out=expf[0:nn], in_=k.bank[b][0:nn, :], func=AF.Exp),
                             reads=['bank%d' % b], writes=['expf'])
                        P.op('dve', lambda e, c=c, nn=nn, tv=tv: e.scalar_tensor_tensor(
                            out=Pc[0:nn, c, :].rearrange('p (m q) -> p m q', m=4),
                            in0=cm[0:nn].unsqueeze(1).broadcast_to([nn, 4, 128]), scalar=tv,
                            in1=expf[0:nn].rearrange('p (m q) -> p m q', m=4), op0=ALU.is_le, op1=ALU.mult),
                             reads=['expf', 'cm'], writes=['Pc'])
                for m in range(4):
                    for ci, (c, nn) in enumerate(chunks):
                        P.op('pe', lambda e, m=m, c=c, nn=nn, ci=ci: e.matmul(k.bank[3][:, m * 65:(m + 1) * 65],
                                                                            lhsT=Pc[0:nn, c, m * 128:(m + 1) * 128],
                                                                            rhs=vcmp_aug[0:nn, c, g, :], start=(ci == 0),
                                                                            stop=(ci == len(chunks) - 1)),
                             reads=['Pc', 'vcmp'], writes=['bank3'])
                for m in range(4):
                    for ci, (c, nn) in enumerate(chunks):
                        P.op('pe', lambda e, m=m, c=c, nn=nn, ci=ci: e.matmul(k.bank[4][:, m * 64:(m + 1) * 64],
                                                                            lhsT=Pc[0:nn, c, m * 128:(m + 1) * 128],
                                                                            rhs=cts[0:nn, c, :], start=(ci == 0),
                                                                            stop=(ci == len(chunks) - 1)),
                             reads=['Pc', 'cts'], writes=['bank4'])
                P.op('dve', lambda e: e.tensor_scalar(out=rden[:, 0:4], in0=den_view(3), scalar1=1e-30, scalar2=None, op0=ALU.max),
                     reads=['bank3'], writes=['rden'])
                P.op('dve', lambda e: e.reciprocal(out=rden[:, 0:4], in_=rden[:, 0:4]), reads=['rden'], writes=['rden'])
                P.op('dve', lambda e: e.tensor_scalar(out=psl, in0=k.bank[4][:, 0:64], scalar1=rden[:, 0:1], scalar2=None,
                                                      op0=ALU.mult), reads=['bank4', 'rden'], writes=['psl'])
                for m in range(1, 4):
                    P.op('dve', lambda e, m=m: e.scalar_tensor_tensor(out=psl, in0=k.bank[4][:, m * 64:(m + 1) * 64],
                                                                      scalar=rden[:, m:m + 1], in1=psl, op0=ALU.mult, op1=ALU.add),
                         reads=['bank4', 'rden', 'psl'], writes=['psl'])
                P.op('dve', lambda e: e.tensor_tensor(out=score, in0=psl, in1=keep[:, off:off + 64], op=ALU.mult),
                     reads=['psl', 'keep'], writes=['score'])
                P.op('dve', lambda e: e.tensor_tensor(out=score, in0=score, in1=addc[:, off:off + 64], op=ALU.add),
                     reads=['score', 'addc'], writes=['score'])
                P.op('dve', lambda e: e.memset(score[:, 0:1], 1.0e4), reads=['score'], writes=['score'])
                P.op('dve', lambda e: e.max(out=m8a, in_=score), reads=['score'], writes=['m8a'])
                P.op('dve', lambda e: e.match_replace(out=sc2, in_to_replace=m8a, in_values=score, imm_value=-2.0),
                     reads=['score', 'm8a'], writes=['sc2'])
                P.op('dve', lambda e: e.max(out=m8b, in_=sc2), reads=['sc2'], writes=['m8b'])
                P.op('dve', lambda e: e.tensor_scalar(out=thr, in0=m8b[:, 7:8], scalar1=0.0, scalar2=None, op0=ALU.max),
                     reads=['m8b'], writes=['thr'])
                P.op('dve', lambda e: e.tensor_scalar(out=negt[:, 64:128], in0=score, scalar1=thr[:, 0:1], scalar2=-30000.0,
                                                      op0=ALU.is_lt, op1=ALU.mult), reads=['score', 'thr'], writes=['negt'])
                P.op('pe', lambda e: e.transpose(out=tp[:, 512:640], in_=negt, identity=k.ident), reads=['negt', 'ident'],
                     writes=['bank0'])
                P.op('act', lambda e: e.copy(out=Q[64:128, :].rearrange('p (m q) -> p m q', m=4),
                                             in_=tp[64:128, 512:640].unsqueeze(1).broadcast_to([64, 4, 128])),
                     reads=['bank0'], writes=[qn])
                for jt in range(i + 1):
                    b = next_sbank()
                    P.op('pe', lambda e, jt=jt, b=b: e.matmul(k.bank[b], lhsT=KE[:, g, jt * 128:(jt + 1) * 128], rhs=Q, start=True,
                                                             stop=True), reads=['KE', qn], writes=['bank%d' % b])
                    masked_exp(b, 128, PT[:, jt, :], tri if jt == i else None, ('PT', jt))
                for m in range(4):
                    for jt in range(i + 1):
                        P.op('pe', lambda e, m=m, jt=jt: e.matmul(k.bank[5][:, m * 65:(m + 1) * 65],
                                                                 lhsT=PT[:, jt, m * 128:(m + 1) * 128], rhs=vs_aug[:, jt, g, :],
                                                                 start=(jt == 0), stop=(jt == i)),
                             reads=[('PT', jt), 'vs_aug'], writes=['bank5'])
                jts = list(range(max(0, i - 4), i + 1))
                for sl_, jt in enumerate(jts):
                    b = next_sbank()
                    P.op('pe', lambda e, jt=jt, b=b: e.matmul(k.bank[b], lhsT=kwT[:, g, jt * 128:(jt + 1) * 128], rhs=Q[0:64, :],
                                                             start=True, stop=True), reads=['kwT', qn], writes=['bank%d' % b])
                    mk = tri if jt == i else (ntri if jt == i - 4 else None)
                    masked_exp(b, 128, PW[:, sl_, :], mk, ('PW', sl_))
                for m in range(4):
                    for sl_, jt in enumerate(jts):
                        P.op('pe', lambda e, m=m, jt=jt, sl_=sl_: e.matmul(k.bank[6][:, m * 65:(m + 1) * 65],
                                                                          lhsT=PW[:, sl_, m * 128:(m + 1) * 128],
                                                                          rhs=vw_aug[:, jt, g, :], start=(sl_ == 0),
                                                                          stop=(sl_ == len(jts) - 1)),
                             reads=[('PW', sl_), 'vw_aug'], writes=['bank6'])
                P.op('dve', lambda e: e.tensor_scalar(out=rden[:, 4:8], in0=den_view(5), scalar1=1e-30, scalar2=None, op0=ALU.max),
                     reads=['bank5'], writes=['rden'])
                P.op('dve', lambda e: e.tensor_scalar(out=rden[:, 8:12], in0=den_view(6), scalar1=1e-30, scalar2=None, op0=ALU.max),
                     reads=['bank6'], writes=['rden'])
                P.op('dve', lambda e: e.reciprocal(out=rden[:, 4:12], in_=rden[:, 4:12]), reads=['rden'], writes=['rden'])
                P.op('dve', lambda e: e.tensor_tensor(out=coef.rearrange('p (b m) -> p b m', b=3),
                                                      in0=rden.rearrange('p (b m) -> p b m', b=3),
                                                      in1=gsig[:, i, g * 12:(g + 1) * 12].rearrange('p (m b) -> p b m', b=3),
                                                      op=ALU.mult), reads=['rden', ('gsig', i)], writes=['coef'])
                for m in range(4):
                    h = 4 * g + m
                    ym = yc[:, h * 64:(h + 1) * 64]
                    P.op('dve', lambda e, m=m, ym=ym: e.tensor_scalar(out=ym, in0=k.bank[3][:, m * 65:m * 65 + 64],
                                                                      scalar1=coef[:, m:m + 1], scalar2=None, op0=ALU.mult),
                         reads=['bank3', 'coef'], writes=['yc'])
                    P.op('dve', lambda e, m=m, ym=ym: e.scalar_tensor_tensor(out=ym, in0=k.bank[5][:, m * 65:m * 65 + 64],
                                                                             scalar=coef[:, 4 + m:5 + m], in1=ym, op0=ALU.mult,
                                                                             op1=ALU.add), reads=['bank5', 'coef', 'yc'], writes=['yc'])
                    P.op('dve', lambda e, m=m, ym=ym: e.scalar_tensor_tensor(out=ym, in0=k.bank[6][:, m * 65:m * 65 + 64],
                                                                             scalar=coef[:, 8 + m:9 + m], in1=ym, op0=ALU.mult,
                                                                             op1=ALU.add), reads=['bank6', 'coef', 'yc'], writes=['yc'])
            P.op('act', lambda e: e.copy(out=ycb, in_=yc), reads=['yc'], writes=['ycb'])
            for c in range(4):
                P.op('pe', lambda e, c=c: e.transpose(out=tp[:, c * 128:(c + 1) * 128], in_=ycb[:, c * 128:(c + 1) * 128],
                                                      identity=k.ident), reads=['ycb', 'ident'], writes=['bank0'])
            for c in range(4):
                P.op('pe', lambda e, c=c: e.transpose(out=tp[:, (4 + c) * 128:(5 + c) * 128], in_=ydt[:, c * 128:(c + 1) * 128],
                                                      identity=k.ident), reads=['ydt', 'ident'], writes=['bank0'])
            P.op('act', lambda e: e.copy(out=yT, in_=tp.rearrange('p (c t) -> p c t', c=8)), reads=['bank0'], writes=['yT'])
            pm = [k.bank[3], k.bank[4]]
            for cb in range(2):
                for kc in range(8):
                    P.op('pe', lambda e, kc=kc, cb=cb: e.matmul(pm[cb], lhsT=yT[:, kc, :], rhs=w_out_sb[:, kc, cb * 512:(cb + 1) * 512],
                                                               start=(kc == 0), stop=(kc == 7)), reads=['yT', 'w_out'],
                         writes=['bank%d' % (3 + cb)])
            post_norm_residual(k, pm, ['bank3', 'bank4'], 'mix_post', xt, xr)
            P.dma('sp', xdst[i * 128:(i + 1) * 128, :], xt, reads=[xr], writes=[('x', i)], stream='xo')


def make_consts():
    c = {}
    Dm = np.zeros((3, 4, 128, 128), np.float32)
    for g, w in enumerate(POOL_WINDOWS):
        for t in range(128):
            lo = max(t + 1 - w, 0)
            for s in range(lo, t + 1):
                Dm[0, g, s, t] += 1.0 / (t + 1 - lo)
            Dm[0, g, t, t] -= 1.0
            for s in range(t + 1 - w, t + 1):
                if s >= 0:
                    Dm[1, g, s, t] += 1.0 / w
                else:
                    Dm[2, g, s + 128, t] += 1.0 / w
            Dm[1, g, t, t] -= 1.0
    c['c_D'] = Dm.astype(ml_dtypes.bfloat16)
    bf = ml_dtypes.bfloat16
    invf = np.concatenate([1.0 / (500000.0 ** (np.arange(0, 16, 2, dtype=np.float32) / 16)),
                           1.0 / (10000.0 ** (np.arange(0, 128, 2, dtype=np.float32) / 128))]).astype(np.float32)
    c['c_invf'] = invf.reshape(1, 72)
    lg = np.log1p(-np.exp2(-5.0 - np.arange(4, dtype=np.float64)))
    idx = np.arange(128, dtype=np.float64)
    rel = idx[None, :] - idx[:, None]
    dec = np.where((rel >= 0)[:, None, :], np.exp(np.maximum(rel, 0)[:, None, :] * lg[None, :, None]), 0.0)
    c['c_decT'] = dec.reshape(128, 512).astype(np.float32)
    xi = np.exp((idx + 1.0)[None, :] * lg[:, None])
    c['c_xi'] = np.broadcast_to(xi.reshape(1, 512), (128, 512)).astype(np.float32).copy()
    c['c_zeta'] = np.exp((127 - idx)[:, None] * lg[None, :]).astype(np.float32)
    c['c_gch'] = np.broadcast_to(np.exp(128 * lg)[None, :], (128, 4)).astype(np.float32).copy()
    p = np.arange(128)
    c['c_tri'] = (p[:, None] <= p[None, :]).astype(np.float32)
    c['c_cm'] = (16.0 * p[:, None] - p[None, :]).astype(np.float32)
    cq = (p >= 64).astype(np.int64)[:, None]
    jj = np.arange(128)[None, :]
    c['c_keep'] = (jj <= 60 + cq).astype(np.float32)
    add = np.zeros((128, 128), np.float32)
    add[(jj == 61 + cq) | (jj == 62 + cq)] = 1.0e4
    add[jj > 62 + cq] = -1.0
    c['c_add'] = add
    n = np.arange(256)
    cs = n * 16
    ss = np.arange(64) * 64
    cts = ((cs[:, None] < ss[None, :] + 64) & (cs[:, None] + 32 > ss[None, :])).astype(np.float32)
    cts[255] = 0
    c['c_cts'] = cts.reshape(2, 128, 64).astype(bf)
    c['c_E'] = (np.arange(4096)[None, :] // 64 == np.arange(64)[:, None]).astype(bf)
    return c


INPUT_NAMES = ["x", "positions", "ln_mix_pre", "ln_mix_post", "ln_ffn_pre", "ln_ffn_post", "ffn_w_gate", "ffn_w_up",
               "ffn_w_down", "ev_w_in", "ev_pool_w", "ev_pool_scale", "ev_sgu_ln_g", "ev_sgu_ln_b", "ev_sgu_w",
               "ev_sgu_b", "ev_w_out", "od_w_in", "od_cmp_k_pos", "od_cmp_k_w1", "od_cmp_k_w2", "od_cmp_v_pos",
               "od_cmp_v_w1", "od_cmp_v_w2", "od_ret_gn_g", "od_w_out"]


def build(shapes, consts, layers=(0, 1, 2, 3), phases=('mix', 'ffn')):
    from contextlib import ExitStack
    nc = bass.Bass("TRN2", target_bir_lowering=False)
    ins = {}
    for n in INPUT_NAMES:
        shp = list(shapes[n])
        if n == 'x':
            shp = [T, D]
        if n == 'positions':
            shp = [1, T]
        ins[n] = nc.dram_tensor(n, shp, I32 if n == 'positions' else F32, kind="ExternalInput").ap()
    for n, v in consts.items():
        ins[n] = nc.dram_tensor(n, list(v.shape), BF16 if v.dtype == ml_dtypes.bfloat16 else F32, kind="ExternalInput").ap()
    y = nc.dram_tensor("y", [T, D], F32, kind="ExternalOutput").ap()
    k = K(nc, layers)
    setup_common(k, ins)
    k.sb2 = k.sb('ssa', [128, 2], F32)
    P = k.P
    xsrc = ins['x']
    for l in layers:
        load_gains(k, l)
        if 'mix' in phases:
            with ExitStack() as es:
                if l % 2 == 0:
                    even_phase(k, l, xsrc, y, es)
                else:
                    odd_phase(k, l, xsrc, y, es)
                P.barrier()
            xsrc = y
        if 'ffn' in phases:
            with ExitStack() as es:
                def sb(name, shape, dt):
                    return es.enter_context(nc.sbuf_tensor('ffs%d_' % l + name, list(shape), dt)).ap()
                k.wd_sb = sb('wd', [128, NFC, 1024], BF16)
                k.xt = k.xt[:2] + [sb('xt2', [128, D], F32), sb('xt3', [128, D], F32)]
                k.wgu = [sb('wgu%d' % i, [128, 2, 8, 512], BF16) for i in range(2)]
                k.hT = sb('hT', [128, 8, 512], BF16)
                k.actT = sb('actT', [128, NFC, 512], BF16)
                k.sg = [sb('sg%d' % i, [128, 512], F32) for i in range(2)]
                ffn_phase(k, l, xsrc, y)
                P.barrier()
            xsrc = y
    P.finish()
    return nc


_CACHE = {}


def kernel(**inputs):
    consts = make_consts()
    shapes = {n: inputs[n].shape for n in INPUT_NAMES}
    if 'nc' not in _CACHE:
        _CACHE['nc'] = build(shapes, consts)
    nc = _CACHE['nc']
    in_maps = []
    for c in range(4):
        b = c % 4
        m = {n: np.ascontiguousarray(inputs[n]) for n in INPUT_NAMES if n not in ('x', 'positions')}
        m['x'] = np.ascontiguousarray(inputs['x'][b])
        m['positions'] = np.ascontiguousarray(inputs['positions'][b:b + 1]).astype(np.int32)
        m.update(consts)
        in_maps.append(m)
    res = run_bass_kernel_spmd(nc, in_maps, core_ids=list(range(4)))
    out = np.stack([res.results[b]["y"] for b in range(4)], axis=0)
    return out.astype(np.float32)
```

```python
import numpy as np
import ml_dtypes
import concourse.bass as bass
import concourse.mybir as mybir
from concourse.bass_utils import run_bass_kernel_spmd

F32 = mybir.dt.float32
BF16 = mybir.dt.bfloat16
I32 = mybir.dt.int32
AF = mybir.ActivationFunctionType
ALU = mybir.AluOpType
AX = mybir.AxisListType

import os
SAME_ENG_SYNC = os.environ.get("SES", "1") == "1"


class Prog:
    def __init__(self, nc):
        self.nc = nc
        self.E = {'pe': nc.tensor, 'dve': nc.vector, 'act': nc.scalar, 'pool': nc.gpsimd, 'sp': nc.sync}
        self.semh = {k: nc.alloc_semaphore('s_' + k) for k in ['pe', 'dve', 'act', 'pool']}
        self.cnt = {k: 0 for k in self.semh}
        self.seen = {e: {} for e in self.E}
        self.lastw = {}
        self.readers = {}
        self.nwait = 0
        self.dslot = 0
        self.NSLOT = 32
        self.nins = 0

    def _deps(self, reads, writes):
        deps = {}
        for r in reads:
            w = self.lastw.get(r)
            if w and deps.get(w[0], 0) < w[1]:
                deps[w[0]] = w[1]
        for w_ in writes:
            w = self.lastw.get(w_)
            if w and deps.get(w[0], 0) < w[1]:
                deps[w[0]] = w[1]
            for k, v in self.readers.get(w_, {}).items():
                if deps.get(k, 0) < v:
                    deps[k] = v
        return deps

    def _wait(self, eng, deps):
        e = self.E[eng]
        seen = self.seen[eng]
        for k, v in deps.items():
            if k == eng and (eng == 'pe' or not SAME_ENG_SYNC):
                continue
            if seen.get(k, 0) >= v:
                continue
            e.wait_ge(self.semh[k], v)
            seen[k] = v
            self.nwait += 1

    def _record(self, tag, reads, writes):
        for w in writes:
            self.lastw[w] = tag
            self.readers[w] = {}
        for r in reads:
            d = self.readers.setdefault(r, {})
            if d.get(tag[0], 0) < tag[1]:
                d[tag[0]] = tag[1]

    def op(self, eng, fn, reads=(), writes=()):
        self._wait(eng, self._deps(reads, writes))
        ins = fn(self.E[eng])
        self.cnt[eng] += 1
        ins.then_inc(self.semh[eng], 1)
        self.nins += 1
        self._record((eng, self.cnt[eng]), reads, writes)

    def dma(self, q, out, in_, reads=(), writes=(), stream='d0', **kw):
        slot = self.dslot % self.NSLOT
        self.dslot += 1
        key = 'd:%d' % slot
        if key not in self.semh:
            self.semh[key] = self.nc.alloc_semaphore('sd_%d' % slot)
            self.cnt[key] = 0
        e = self.E[q]
        if self.cnt[key] > 0 and self.seen[q].get(key, 0) < self.cnt[key]:
            e.wait_ge(self.semh[key], self.cnt[key])
            self.seen[q][key] = self.cnt[key]
        self._wait(q, self._deps(reads, writes))
        e.dma_start(out=out, in_=in_, **kw).then_inc(self.semh[key], 16)
        self.cnt[key] += 16
        self.nins += 1
        self._record((key, self.cnt[key]), reads, writes)

    def barrier(self):
        for eng in self.E:
            deps = {k: v for k, v in self.cnt.items() if v > 0}
            e = self.E[eng]
            for k, v in deps.items():
                if k == eng and eng == 'pe':
                    continue
                if self.seen[eng].get(k, 0) >= v:
                    continue
                e.wait_ge(self.semh[k], v)
                self.seen[eng][k] = v
        self.lastw = {}
        self.readers = {}

    def finish(self, eng='sp'):
        deps = {k: v for k, v in self.cnt.items() if v > 0}
        e = self.E[eng]
        for k, v in deps.items():
            e.wait_ge(self.semh[k], v)


T = 4096
D = 1024
NT = T // 128
FH = 2816
NFC = FH // 128
DEPTH = 4
POOL_WINDOWS = (2, 4, 8, 16)
EPS = 1e-6


def _flat2(ap, c=1024):
    n = len(ap.shape)
    names = ' '.join('d%d' % i for i in range(n))
    f = ap.rearrange('%s -> (%s)' % (names, names)) if n > 1 else ap
    return f.rearrange('(r c) -> r c', c=c)


class K:
    def __init__(self, nc, layers=(0, 1, 2, 3), Tn=T):
        self.nc = nc
        self.P = Prog(nc)
        self.layers = layers
        self.uid = 0

    def sb(self, name, shape, dt):
        return self.nc.alloc_sbuf_tensor(name, list(shape), dt).ap()

    def dram(self, name, shape, dt, kind="Internal"):
        return self.nc.dram_tensor(name, list(shape), dt, kind=kind).ap()


def cast_copy(k, dst, src, res):
    d2 = _flat2(dst)
    s2 = _flat2(src)
    rows = d2.shape[0]
    r0 = 0
    while r0 < rows:
        r1 = min(rows, r0 + 2048)
        k.P.dma('pool', d2[r0:r1, :], s2[r0:r1, :], writes=[res], stream='cast')
        r0 = r1


def cast_layer(k, l):
    ins = k.ins
    order = []
    if l % 2 == 0:
        order += [('ev_w_in', l // 2), ('ev_pool_w', l // 2), ('ev_w_out', l // 2)]
    else:
        order += [('od_w_in', l // 2), ('od_cmp_k_w1', l // 2), ('od_cmp_k_w2', l // 2), ('od_cmp_v_w1', l // 2),
                  ('od_cmp_v_w2', l // 2), ('od_w_out', l // 2)]
    order += [('ffn_w_gate', l), ('ffn_w_up', l), ('ffn_w_down', l)]
    for name, idx in order:
        src = ins[name][idx]
        dst = k.dram('wb_%s_%d' % (name, idx), src.shape, BF16)
        k.wb[(name, idx)] = dst
        cast_copy(k, dst, src, ('wb', name, idx))


def rstd_from_ssq(k, ssq, rstd, tmp, n, eps, tag):
    P = k.P
    P.op('dve', lambda e: e.tensor_scalar(out=tmp, in0=ssq, scalar1=1.0 / n, scalar2=eps, op0=ALU.mult, op1=ALU.add),
         reads=[tag + 'ssq'], writes=[tag + 'tmp'])
    P.op('act', lambda e: e.activation(out=tmp, in_=tmp, func=AF.Sqrt), reads=[tag + 'tmp'], writes=[tag + 'tmp'])
    P.op('dve', lambda e: e.reciprocal(out=rstd, in_=tmp), reads=[tag + 'tmp'], writes=[tag + 'rstd'])


def setup_common(k, ins):
    nc, P = k.nc, k.P
    k.ins = ins
    k.bank = [nc.alloc_psum_tensor('bank%d' % i, [128, 512], F32).ap() for i in range(8)]
    k.bankb = [b.bitcast(BF16) for b in k.bank]
    k.io_f = k.sb('io_f', [128, 128], F32)
    k.iop = k.sb('iop', [128, 1], F32)
    k.ident = k.sb('ident', [128, 128], BF16)
    k.ones_row = k.sb('ones_row', [1, 128], BF16)
    P.op('pool', lambda e: e.iota(k.io_f, pattern=[[1, 128]], base=0, channel_multiplier=0,
                                  allow_small_or_imprecise_dtypes=True), writes=['io_f'])
    P.op('pool', lambda e: e.iota(k.iop, pattern=[[1, 1]], base=0, channel_multiplier=1,
                                  allow_small_or_imprecise_dtypes=True), writes=['iop'])
    P.op('dve', lambda e: e.tensor_scalar(out=k.ident, in0=k.io_f, scalar1=k.iop[:, 0:1], scalar2=None,
                                          op0=ALU.is_equal), reads=['io_f', 'iop'], writes=['ident'])
    P.op('dve', lambda e: e.memset(k.ones_row, 1.0), writes=['ones_row'])
    k.wb = {}
    cast_layer(k, k.layers[0])
    k.xt = [k.sb('xt%d' % s, [128, D], F32) for s in range(2)]
    k.hbs = [k.sb('hb%d' % i, [128, D], BF16) for i in range(2)]
    k.junk = k.sb('junk', [128, D], BF16)
    k.tmpf = k.sb('tmpf', [128, D], F32)
    k.G = {n: k.sb('G_' + n, [128, D], F32) for n in ['mix_pre', 'mix_post', 'ffn_pre', 'ffn_post']}
    k.st = {n: k.sb('st_' + n, [128, 1], F32) for n in ['ssq', 'tmp', 'rstd', 'ssq2', 'tmp2', 'rstd2']}


def load_gains(k, l):
    for n in ['mix_pre', 'mix_post', 'ffn_pre', 'ffn_post']:
        k.P.dma('sp', k.G[n], k.ins['ln_' + n][l:l + 1, :].broadcast_to([128, D]), writes=['G_' + n], stream='small')


def norm_a(k, xt_ap, xres, gname, hbi):
    P = k.P
    st = k.st
    hb = k.hbs[hbi]
    P.op('act', lambda e: e.activation(out=k.junk, in_=xt_ap, func=AF.Square, accum_out=st['ssq']),
         reads=[xres], writes=['junk', 'ssq'])
    rstd_from_ssq(k, st['ssq'], st['rstd'], st['tmp'], D, EPS, '')
    P.op('dve', lambda e: e.scalar_tensor_tensor(out=hb, in0=xt_ap, scalar=st['rstd'][:, 0:1], in1=k.G[gname],
                                                 op0=ALU.mult, op1=ALU.mult),
         reads=[xres, 'rstd', 'G_' + gname], writes=['hb%d' % hbi])


def trans_t(k, hbi, hT_dst, hTres):
    P = k.P
    hb = k.hbs[hbi]
    tp = k.bankb[0]
    for c in range(8):
        P.op('pe', lambda e, c=c: e.transpose(out=tp[:, c * 128:(c + 1) * 128], in_=hb[:, c * 128:(c + 1) * 128],
                                              identity=k.ident), reads=['hb%d' % hbi, 'ident'], writes=['bank0'])
    P.op('act', lambda e: e.copy(out=hT_dst, in_=tp.rearrange('p (c t) -> p c t', c=8)), reads=['bank0'],
         writes=[hTres])


def post_norm_residual(k, pm, pmres, gname, xt_ap, xres):
    P = k.P
    st = k.st
    ssa = k.sb2
    for cb in range(2):
        P.op('act', lambda e, cb=cb: e.activation(out=k.junk[:, cb * 512:(cb + 1) * 512], in_=pm[cb], func=AF.Square,
                                                  accum_out=ssa[:, cb:cb + 1]),
             reads=[pmres[cb]], writes=['junk', 'ssa'])
    P.op('dve', lambda e: e.tensor_tensor(out=st['ssq2'], in0=ssa[:, 0:1], in1=ssa[:, 1:2], op=ALU.add),
         reads=['ssa'], writes=['2ssq'])
    rstd_from_ssq(k, st['ssq2'], st['rstd2'], st['tmp2'], D, EPS, '2')
    for cb in range(2):
        sl = slice(cb * 512, (cb + 1) * 512)
        P.op('dve', lambda e, cb=cb, sl=sl: e.scalar_tensor_tensor(out=k.tmpf[:, sl], in0=pm[cb], scalar=st['rstd2'][:, 0:1],
                                                                   in1=k.G[gname][:, sl], op0=ALU.mult, op1=ALU.mult),
             reads=[pmres[cb], '2rstd', 'G_' + gname], writes=['tmpf'])
    P.op('pool', lambda e: e.tensor_tensor(out=xt_ap, in0=xt_ap, in1=k.tmpf, op=ALU.add), reads=['tmpf', xres],
         writes=[xres])


def ffn_phase(k, l, xsrc, xdst):
    nc, P = k.nc, k.P
    wg, wu, wd = k.wb[('ffn_w_gate', l)], k.wb[('ffn_w_up', l)], k.wb[('ffn_w_down', l)]
    P.dma('sp', k.wd_sb, wd.rearrange('(c p) n -> p c n', p=128), reads=[('wb', 'ffn_w_down', l)], writes=['wd_sb'],
          stream='w')
    groups = [(0, 4), (4, 4), (8, 4), (12, 4), (16, 4), (20, 2)]
    wgv = wg.rearrange('(c p) n -> p c n', p=128)
    wuv = wu.rearrange('(c p) n -> p c n', p=128)
    gi = 0
    NB = T // 512

    def xtile(tb, s):
        j = (tb % 2) * 4 + s
        return k.xt8[j], 'xt8_%d' % j

    def front_a(tb, s):
        t0 = tb * 512 + s * 128
        xt, xr = xtile(tb, s)
        P.dma('sp', xt, xsrc[t0:t0 + 128, :], reads=[('x', t0 // 128)], writes=[xr], stream='x')
        norm_a(k, xt, xr, 'ffn_pre', s % 2)

    def front_t(tb, s):
        trans_t(k, s % 2, k.hT2[tb % 2][:, :, s * 128:(s + 1) * 128], ('hT', tb % 2))

    for s in range(4):
        front_a(0, s)
        front_t(0, s)
    for tb in range(NB):
        hT = k.hT2[tb % 2]
        hTr = ('hT', tb % 2)
        for gidx, (j0, nj) in enumerate(groups):
            pre = gidx < 4 and tb + 1 < NB
            if pre:
                front_a(tb + 1, gidx)
            buf = gi % 2
            gi += 1
            wt = k.wgu[buf]
            P.dma('sp', wt[:, 0, :, 0:nj * 128], wgv[:, :, j0 * 128:(j0 + nj) * 128], reads=[('wb', 'ffn_w_gate', l)],
                  writes=['wgu%d' % buf], stream='w')
            P.dma('sp', wt[:, 1, :, 0:nj * 128], wuv[:, :, j0 * 128:(j0 + nj) * 128], reads=[('wb', 'ffn_w_up', l)],
                  writes=['wgu%d' % buf], stream='w')
            for jj in range(nj):
                j = j0 + jj
                pb = 1 + 2 * (j % 2)
                pg, pu = k.bank[pb], k.bank[pb + 1]
                for kc in range(8):
                    P.op('pe', lambda e, kc=kc, jj=jj: e.matmul(pg, lhsT=wt[:, 0, kc, jj * 128:(jj + 1) * 128], rhs=hT[:, kc, :],
                                                               start=(kc == 0), stop=(kc == 7)),
                         reads=['wgu%d' % buf, hTr], writes=['bank%d' % pb])
                for kc in range(8):
                    P.op('pe', lambda e, kc=kc, jj=jj: e.matmul(pu, lhsT=wt[:, 1, kc, jj * 128:(jj + 1) * 128], rhs=hT[:, kc, :],
                                                               start=(kc == 0), stop=(kc == 7)),
                         reads=['wgu%d' % buf, hTr], writes=['bank%d' % (pb + 1)])
                sg = k.sg[j % 2]
                P.op('act', lambda e: e.activation(out=sg, in_=pg, func=AF.Silu), reads=['bank%d' % pb],
                     writes=['sg%d' % (j % 2)])
                P.op('dve', lambda e, j=j: e.tensor_tensor(out=k.actT[:, j, :], in0=pu, in1=sg, op=ALU.mult),
                     reads=['bank%d' % (pb + 1), 'sg%d' % (j % 2)], writes=[('actT', j)])
            if pre:
                front_t(tb + 1, gidx)
        for s in range(4):
            t0 = tb * 512 + s * 128
            pm = [k.bank[5], k.bank[6]]
            for cb in range(2):
                for kk in range(NFC):
                    P.op('pe', lambda e, kk=kk, cb=cb: e.matmul(pm[cb], lhsT=k.actT[:, kk, s * 128:(s + 1) * 128],
                                                               rhs=k.wd_sb[:, kk, cb * 512:(cb + 1) * 512],
                                                               start=(kk == 0), stop=(kk == NFC - 1)),
                         reads=[('actT', kk), 'wd_sb'], writes=['bank%d' % (5 + cb)])
            xt_, xr_ = xtile(tb, s)
            post_norm_residual(k, pm, ['bank5', 'bank6'], 'ffn_post', xt_, xr_)
            P.dma('sp', xdst[t0:t0 + 128, :], xt_, reads=[xr_], writes=[('x', t0 // 128)], stream='xo')


def even_phase(k, l, xsrc, xdst, es):
    nc, P = k.nc, k.P
    e_ = l // 2
    ins = k.ins

    def sb(name, shape, dt):
        return es.enter_context(nc.sbuf_tensor('evs%d_' % l + name, list(shape), dt)).ap()

    w_in_sb = sb('w_in', [128, 8, 1536], BF16)
    w_out_sb = sb('w_out', [128, 8, 1024], BF16)
    poolw_sb = sb('poolw', [128, 4, 128], BF16)
    Dm_sb = sb('Dm', [128, 3, 4, 128], BF16)
    WmT = sb('WmT', [128, 4, 128], BF16)
    wsf = sb('wsf', [128, 4, 128], F32)
    wsb = sb('wsb', [128, 4, 128], BF16)
    tril = sb('tril', [128, 128], F32)
    Bt = sb('Bt', [128, 512], F32)
    lng = sb('lng', [128, 512], F32)
    lnb = sb('lnb', [128, 512], F32)
    psc = sb('psc', [128, 4], F32)
    a_sb = [sb('a%d' % i, [128, 512], BF16) for i in range(2)]
    uT_sb = sb('uT', [128, 4, 128], BF16)
    vg = sb('vg', [128, 512], F32)
    vn = sb('vn', [128, 512], F32)
    vln = sb('vln', [128, 512], BF16)
    diffT = sb('diffT', [128, 4, 128], BF16)
    yaT = sb('yaT', [128, 4, 128], BF16)
    ybT = sb('ybT', [128, 4, 128], BF16)
    mxb = sb('mxb', [128, 512], F32)
    hT1s = [sb('hT1_%d' % i, [128, 8, 128], BF16) for i in range(2)]
    bst = sb('bst', [128, 6], F32)
    mv = sb('mv', [128, 2], F32)
    lrs = sb('lrs', [128, 1], F32)

    P.dma('sp', w_in_sb, k.wb[('ev_w_in', e_)].rearrange('(c p) n -> p c n', p=128), reads=[('wb', 'ev_w_in', e_)],
          writes=['w_in'], stream='w')
    P.dma('sp', w_out_sb, k.wb[('ev_w_out', e_)].rearrange('(c p) n -> p c n', p=128), reads=[('wb', 'ev_w_out', e_)],
          writes=['w_out'], stream='w')
    P.dma('sp', poolw_sb, k.wb[('ev_pool_w', e_)].rearrange('g c d -> c g d'), reads=[('wb', 'ev_pool_w', e_)],
          writes=['poolw'], stream='w')
    P.dma('sp', Dm_sb, ins['c_D'].rearrange('a g s t -> s a g t'), writes=['Dm'], stream='small')
    P.dma('sp', wsf, ins['ev_sgu_w'][e_].rearrange('g t s -> t g s'), writes=['wsf'], stream='small')
    P.dma('sp', Bt, ins['ev_sgu_b'][e_:e_ + 1].rearrange('o g t -> o (g t)').broadcast_to([128, 512]), writes=['Bt'],
          stream='small')
    P.dma('sp', lng, ins['ev_sgu_ln_g'][e_:e_ + 1, :].broadcast_to([128, 512]), writes=['lng'], stream='small')
    P.dma('sp', lnb, ins['ev_sgu_ln_b'][e_:e_ + 1, :].broadcast_to([128, 512]), writes=['lnb'], stream='small')
    with nc.allow_non_contiguous_dma(reason='tiny per-channel scale'):
        P.dma('sp', psc, ins['ev_pool_scale'][e_].rearrange('(g p) -> p g', p=128), writes=['psc'], stream='small')
    P.op('dve', lambda e: e.tensor_scalar(out=tril, in0=k.io_f, scalar1=k.iop[:, 0:1], scalar2=None, op0=ALU.is_le),
         reads=['io_f', 'iop'], writes=['tril'])
    P.op('dve', lambda e: e.tensor_tensor(out=wsb, in0=wsf, in1=tril.unsqueeze(1).broadcast_to([128, 4, 128]), op=ALU.mult),
         reads=['wsf', 'tril'], writes=['wsb'])
    tp = k.bankb[0]
    for g in range(4):
        P.op('pe', lambda e, g=g: e.transpose(out=tp[:, g * 128:(g + 1) * 128], in_=wsb[:, g, :], identity=k.ident),
             reads=['wsb', 'ident'], writes=['bank0'])
    P.op('act', lambda e: e.copy(out=WmT, in_=tp[:, 0:512].rearrange('p (g t) -> p g t', g=4)), reads=['bank0'],
         writes=['WmT'])

    def front_a(i):
        P.dma('sp', k.xt[i % 2], xsrc[i * 128:(i + 1) * 128, :], reads=[('x', i)], writes=['xt%d' % (i % 2)], stream='x')
        norm_a(k, k.xt[i % 2], 'xt%d' % (i % 2), 'mix_pre', i % 2)

    def front_t(i):
        trans_t(k, i % 2, hT1s[i % 2], 'hT1_%d' % (i % 2))

    front_a(0)
    front_t(0)
    for i in range(NT):
        xt = k.xt[i % 2]
        xr = 'xt%d' % (i % 2)
        hT1 = hT1s[i % 2]
        hT1n = 'hT1_%d' % (i % 2)
        a_cur, a_prev = a_sb[i % 2], a_sb[(i + 1) % 2]
        ar, apr = 'a%d' % (i % 2), 'a%d' % ((i + 1) % 2)
        if i + 1 < NT and i >= 1:
            pass
        pa, pu, pv = k.bank[1], k.bank[2], k.bank[3]
        for kc in range(8):
            P.op('pe', lambda e, kc=kc: e.matmul(pa, lhsT=hT1[:, kc, :], rhs=w_in_sb[:, kc, 0:512], start=(kc == 0),
                                                stop=(kc == 7)), reads=[hT1n, 'w_in'], writes=['bank1'])
        P.op('act', lambda e: e.copy(out=a_cur, in_=pa), reads=['bank1'], writes=[ar])
        for kc in range(8):
            P.op('pe', lambda e, kc=kc: e.matmul(pv, lhsT=hT1[:, kc, :], rhs=w_in_sb[:, kc, 1024:1536], start=(kc == 0),
                                                stop=(kc == 7)), reads=[hT1n, 'w_in'], writes=['bank3'])
        P.op('act', lambda e: e.activation(out=vg, in_=pv, func=AF.Gelu_apprx_tanh), reads=['bank3'], writes=['vg'])
        for c in range(4):
            for kc in range(8):
                P.op('pe', lambda e, kc=kc, c=c: e.matmul(pu[:, c * 128:(c + 1) * 128],
                                                         lhsT=w_in_sb[:, kc, 512 + c * 128:512 + (c + 1) * 128],
                                                         rhs=hT1[:, kc, :], start=(kc == 0), stop=(kc == 7)),
                     reads=[hT1n, 'w_in'], writes=['bank2'])
        P.op('act', lambda e: e.activation(out=uT_sb, in_=pu.rearrange('p (c t) -> p c t', c=4), func=AF.Gelu_apprx_tanh),
             reads=['bank2'], writes=['uT'])
        if i + 1 < NT:
            front_a(i + 1)
        P.op('dve', lambda e: e.bn_stats(out=bst, in_=vg), reads=['vg'], writes=['bst'])
        P.op('dve', lambda e: e.bn_aggr(out=mv, in_=bst), reads=['bst'], writes=['mv'])
        P.op('dve', lambda e: e.tensor_scalar(out=lrs, in0=mv[:, 1:2], scalar1=1e-5, scalar2=None, op0=ALU.add),
             reads=['mv'], writes=['lrs'])
        P.op('act', lambda e: e.activation(out=lrs, in_=lrs, func=AF.Sqrt), reads=['lrs'], writes=['lrs'])
        P.op('dve', lambda e: e.reciprocal(out=lrs, in_=lrs), reads=['lrs'], writes=['lrs'])
        P.op('dve', lambda e: e.tensor_scalar(out=vn, in0=vg, scalar1=mv[:, 0:1], scalar2=lrs[:, 0:1], op0=ALU.subtract,
                                              op1=ALU.mult), reads=['vg', 'mv', 'lrs'], writes=['vn'])
        P.op('pool', lambda e: e.tensor_tensor(out=vn, in0=vn, in1=lng, op=ALU.mult), reads=['vn', 'lng'], writes=['vn'])
        P.op('pool', lambda e: e.tensor_tensor(out=vln, in0=vn, in1=lnb, op=ALU.add), reads=['vn', 'lnb'], writes=['vln'])
        pd_ = k.bank[4]
        for g in range(4):
            first = True
            sl = slice(g * 128, (g + 1) * 128)
            P.op('pe', lambda e, g=g, sl=sl: e.matmul(pd_[:, sl], lhsT=a_cur[:, sl], rhs=Dm_sb[:, 0 if i == 0 else 1, g, :],
                                                     start=True, stop=(i == 0)), reads=[ar, 'Dm'], writes=['bank4'])
            if i > 0:
                P.op('pe', lambda e, g=g, sl=sl: e.matmul(pd_[:, sl], lhsT=a_prev[:, sl], rhs=Dm_sb[:, 2, g, :],
                                                         start=False, stop=True), reads=[apr, 'Dm'], writes=['bank4'])
        P.op('dve', lambda e: e.tensor_copy(out=diffT, in_=pd_.rearrange('p (g t) -> p g t', g=4)), reads=['bank4'],
             writes=['diffT'])
        pya = k.bank[5]
        for g in range(4):
            P.op('pe', lambda e, g=g: e.matmul(pya[:, g * 128:(g + 1) * 128], lhsT=poolw_sb[:, g, :], rhs=diffT[:, g, :],
                                              start=True, stop=True), reads=['poolw', 'diffT'], writes=['bank5'])
        P.op('dve', lambda e: e.tensor_tensor(out=yaT, in0=pya.rearrange('p (g t) -> p g t', g=4),
                                              in1=psc.unsqueeze(2).broadcast_to([128, 4, 128]), op=ALU.mult),
             reads=['bank5', 'psc'], writes=['yaT'])
        pmx = k.bank[4]
        for g in range(4):
            P.op('pe', lambda e, g=g: e.matmul(pmx[:, g * 128:(g + 1) * 128], lhsT=vln[:, g * 128:(g + 1) * 128],
                                              rhs=WmT[:, g, :], start=True, stop=True), reads=['vln', 'WmT'],
                 writes=['bank4'])
        P.op('dve', lambda e: e.tensor_tensor(out=mxb, in0=pmx, in1=Bt, op=ALU.add), reads=['bank4', 'Bt'],
             writes=['mxb'])
        P.op('pool', lambda e: e.tensor_tensor(out=ybT, in0=mxb.rearrange('p (g t) -> p g t', g=4), in1=uT_sb, op=ALU.mult),
             reads=['mxb', 'uT'], writes=['ybT'])
        if i + 1 < NT:
            front_t(i + 1)
        pm = [k.bank[6], k.bank[7]]
        for cb in range(2):
            for kc in range(8):
                lh = yaT[:, kc, :] if kc < 4 else ybT[:, kc - 4, :]
                P.op('pe', lambda e, kc=kc, cb=cb, lh=lh: e.matmul(pm[cb], lhsT=lh, rhs=w_out_sb[:, kc, cb * 512:(cb + 1) * 512],
                                                                  start=(kc == 0), stop=(kc == 7)),
                     reads=['yaT', 'ybT', 'w_out'], writes=['bank%d' % (6 + cb)])
        post_norm_residual(k, pm, ['bank6', 'bank7'], 'mix_post', xt, xr)
        P.dma('sp', xdst[i * 128:(i + 1) * 128, :], xt, reads=[xr], writes=[('x', i)], stream='xo')


def odd_phase(k, l, xsrc, xdst, es):
    from contextlib import ExitStack
    nc, P = k.nc, k.P
    o_ = l // 2
    ins = k.ins
    PI = 3.14159265358979

    def sbs(stack, name, shape, dt):
        return stack.enter_context(nc.sbuf_tensor('od%d_' % l + name, list(shape), dt)).ap()

    def sb(name, shape, dt):
        return sbs(es, name, shape, dt)

    qd = k.dram('qd%d' % l, [T, 512], BF16)
    ydd = k.dram('ydd%d' % l, [T, 512], BF16)
    KE = sb('KE', [128, 2, T], BF16)
    kwT = sb('kwT', [64, 2, T], BF16)
    kcvd = k.dram('kcvd%d' % l, [4, 64, T], BF16)
    ropeS = k.dram('ropeS%d' % l, [128, NT, 72], F32)
    ropeC = k.dram('ropeC%d' % l, [128, NT, 72], F32)
    vs_aug = sb('vs_aug', [128, NT, 2, 65], BF16)
    vw_aug = sb('vw_aug', [128, NT, 2, 65], BF16)
    gsig = sb('gsig', [128, NT, 24], F32)
    kcmpT = sb('kcmpT', [64, 2, 256], BF16)
    vcmp_aug = sb('vcmp', [128, 2, 2, 65], BF16)
    tri = sb('tri', [128, 128], F32)
    ntri = sb('ntri', [128, 128], F32)
    cm = sb('cm', [128, 128], F32)
    keep = sb('keep', [128, 128], F32)
    addc = sb('addc', [128, 128], F32)
    cts = sb('cts', [128, 2, 64], BF16)
    decT = sb('decT', [128, 512], F32)
    xi = sb('xi', [128, 512], F32)
    zeta = sb('zeta', [128, 4], F32)
    gch = sb('gch', [128, 4], F32)
    gng = sb('gng', [128, 512], F32)
    for nm, t_, src in [('tri', tri, 'c_tri'), ('cm', cm, 'c_cm'), ('keep', keep, 'c_keep'), ('addc', addc, 'c_add'),
                        ('decT', decT, 'c_decT'), ('xi', xi, 'c_xi'), ('zeta', zeta, 'c_zeta'), ('gch', gch, 'c_gch')]:
        P.dma('sp', t_, ins[src], writes=[nm], stream='small')
    P.dma('sp', cts, ins['c_cts'].rearrange('c n j -> n c j'), writes=['cts'], stream='small')
    P.dma('sp', KE[64:128, 0, :], ins['c_E'], writes=['KE'], stream='small')
    P.dma('sp', KE[64:128, 1, :], ins['c_E'], writes=['KE'], stream='small')
    P.dma('sp', gng, ins['od_ret_gn_g'][o_:o_ + 1, :].broadcast_to([128, 512]), writes=['gng'], stream='small')
    P.op('dve', lambda e: e.tensor_scalar(out=ntri, in0=tri, scalar1=-1.0, scalar2=1.0, op0=ALU.mult, op1=ALU.add),
         reads=['tri'], writes=['ntri'])
    P.op('pool', lambda e: e.memset(vs_aug, 1.0), writes=['vs_aug'])
    P.op('pool', lambda e: e.memset(vw_aug, 1.0), writes=['vw_aug'])
    P.op('pool', lambda e: e.memset(vcmp_aug, 1.0), writes=['vcmp'])
    with ExitStack() as ts:
        posi = sbs(ts, 'posi', [128, NT], I32)
        sinT = sbs(ts, 'sinT', [128, NT, 72], F32)
        cosT = sbs(ts, 'cosT', [128, NT, 72], F32)
        posf = sbs(ts, 'posf', [128, NT], F32)
        invf = sbs(ts, 'invf', [128, 72], F32)
        ang = sbs(ts, 'ang', [128, NT, 72], F32)
        arg = sbs(ts, 'arg', [128, NT, 72], F32)
        kf = sbs(ts, 'kf', [128, NT, 72], F32)
        ki = sbs(ts, 'ki', [128, NT, 72], I32)
        with nc.allow_non_contiguous_dma(reason='positions to token-on-partition layout'):
            P.dma('sp', posi, ins['positions'].rearrange('o (i p) -> p (o i)', p=128), writes=['posi'], stream='small')
        P.dma('sp', invf, ins['c_invf'].broadcast_to([128, 72]), writes=['invf'], stream='small')
        P.op('dve', lambda e: e.tensor_copy(out=posf, in_=posi), reads=['posi'], writes=['posf'])
        P.op('dve', lambda e: e.tensor_tensor(out=ang, in0=posf.unsqueeze(2).broadcast_to([128, NT, 72]),
                                              in1=invf.unsqueeze(1).broadcast_to([128, NT, 72]), op=ALU.mult),
             reads=['posf', 'invf'], writes=['ang'])
        for shift, dst, dn in [(0.0, sinT, 'sinT'), (PI / 2, cosT, 'cosT')]:
            P.op('dve', lambda e, shift=shift: e.tensor_scalar(out=arg, in0=ang, scalar1=shift, scalar2=None, op0=ALU.add),
                 reads=['ang'], writes=['arg'])
            P.op('dve', lambda e: e.tensor_scalar(out=kf, in0=arg, scalar1=1.0 / (2 * PI), scalar2=None, op0=ALU.mult),
                 reads=['arg'], writes=['kf'])
            P.op('dve', lambda e: e.tensor_copy(out=ki, in_=kf), reads=['kf'], writes=['ki'])
            P.op('dve', lambda e: e.tensor_copy(out=kf, in_=ki), reads=['ki'], writes=['kf'])
            P.op('dve', lambda e: e.scalar_tensor_tensor(out=arg, in0=kf, scalar=-6.28125, in1=arg, op0=ALU.mult, op1=ALU.add),
                 reads=['kf', 'arg'], writes=['arg'])
            P.op('dve', lambda e: e.scalar_tensor_tensor(out=arg, in0=kf, scalar=-(2 * PI - 6.28125), in1=arg, op0=ALU.mult,
                                                         op1=ALU.add), reads=['kf', 'arg'], writes=['arg'])
            P.op('dve', lambda e: e.tensor_scalar(out=arg, in0=arg, scalar1=3.1415925, scalar2=-3.1415925, op0=ALU.min,
                                                  op1=ALU.max), reads=['arg'], writes=['arg'])
            P.op('act', lambda e, dst=dst: e.activation(out=dst, in_=arg, func=AF.Sin), reads=['arg'], writes=[dn])
        P.dma('sp', ropeS, sinT, reads=['sinT'], writes=['ropeS'], stream='xo')
        P.dma('sp', ropeC, cosT, reads=['cosT'], writes=['ropeC'], stream='xo')
        P.barrier()

    with ExitStack() as s1:
        w_in_sb = sbs(s1, 'w_in', [128, 8, 3352], BF16)
        hT1s = [sbs(s1, 'hT1_%d' % j, [128, 8, 128], BF16) for j in range(2)]
        cur = {}
        csb = [sbs(s1, 'csb%d' % j, [128, 2, 72], F32) for j in range(2)]
        kcv_t = sbs(s1, 'kcv_t', [64, 4, 128], BF16)
        zq = sbs(s1, 'zq', [128, 512], F32)
        qb = sbs(s1, 'qb', [128, 512], BF16)
        zkv = sbs(s1, 'zkv', [128, 768], F32)
        kvb = sbs(s1, 'kvb', [128, 768], BF16)
        rt = [sbs(s1, 'rt%d' % j, [128, 512], F32) for j in range(4)]
        zrq = sbs(s1, 'zrq', [128, 512], F32)
        zrk = sbs(s1, 'zrk', [128, 512], F32)
        rqb = sbs(s1, 'rqb', [128, 512], BF16)
        rkb = sbs(s1, 'rkb', [128, 512], BF16)
        rkr = sbs(s1, 'rkr', [128, 512], F32)
        rkz = sbs(s1, 'rkz', [128, 512], BF16)
        rvb = sbs(s1, 'rvb', [128, 512], BF16)
        gsl = sbs(s1, 'gsl', [128, 512], F32)
        qkT = sbs(s1, 'qkT', [128, 8, 128], BF16)
        qxiT = sbs(s1, 'qxiT', [128, 512], BF16)
        STs = sbs(s1, 'STs', [128, 512], BF16)
        state = sbs(s1, 'state', [128, 512], F32)
        stbf = sbs(s1, 'stbf', [128, 512], BF16)
        bst4 = sbs(s1, 'bst4', [128, 4, 6], F32)
        mv4 = sbs(s1, 'mv4', [128, 4, 2], F32)
        rs4 = sbs(s1, 'rs4', [128, 4], F32)
        on = sbs(s1, 'on', [128, 512], F32)
        ydb = sbs(s1, 'ydb', [128, 512], BF16)
        P.dma('sp', w_in_sb, k.wb[('od_w_in', o_)].rearrange('(c p) n -> p c n', p=128), reads=[('wb', 'od_w_in', o_)],
              writes=['w_in'], stream='w')
        P.op('dve', lambda e: e.memset(state, 0.0), writes=['state'])

        def proj(bank, c0, c1):
            for kc in range(8):
                P.op('pe', lambda e, kc=kc: e.matmul(k.bank[bank][:, 0:c1 - c0], lhsT=cur['hT'][:, kc, :], rhs=w_in_sb[:, kc, c0:c1],
                                                    start=(kc == 0), stop=(kc == 7)), reads=[cur['hTn'], 'w_in'],
                     writes=['bank%d' % bank])

        def rope(src, dst, nh, hd, half, c_, s_, csn, sname, dname):
            sv = src.rearrange('p (h d) -> p h d', h=nh)
            dv = dst.rearrange('p (h d) -> p h d', h=nh)
            x1, x2 = sv[:, :, 0:half], sv[:, :, half:2 * half]
            cb = c_.unsqueeze(1).broadcast_to([128, nh, half])
            sb_ = s_.unsqueeze(1).broadcast_to([128, nh, half])
            t = [r_[:, 0:nh * half].rearrange('p (h d) -> p h d', h=nh) for r_ in rt]
            P.op('dve', lambda e: e.tensor_tensor(out=t[0], in0=x1, in1=cb, op=ALU.mult), reads=[sname, csn], writes=['rt0'])
            P.op('pool', lambda e: e.tensor_tensor(out=t[1], in0=x2, in1=sb_, op=ALU.mult), reads=[sname, csn], writes=['rt1'])
            P.op('dve', lambda e: e.tensor_tensor(out=t[2], in0=x2, in1=cb, op=ALU.mult), reads=[sname, csn], writes=['rt2'])
            P.op('pool', lambda e: e.tensor_tensor(out=t[3], in0=x1, in1=sb_, op=ALU.mult), reads=[sname, csn], writes=['rt3'])
            if 2 * half < hd:
                P.op('act', lambda e: e.copy(out=dst, in_=src), reads=[sname], writes=[dname])
            P.op('dve', lambda e: e.tensor_tensor(out=dv[:, :, 0:half], in0=t[0], in1=t[1], op=ALU.subtract),
                 reads=['rt0', 'rt1'], writes=[dname])
            P.op('pool', lambda e: e.tensor_tensor(out=dv[:, :, half:2 * half], in0=t[2], in1=t[3], op=ALU.add),
                 reads=['rt2', 'rt3'], writes=[dname])

        def o1_front_a(i):
            P.dma('sp', k.xt[i % 2], xsrc[i * 128:(i + 1) * 128, :], reads=[('x', i)], writes=['xt%d' % (i % 2)], stream='x')
            norm_a(k, k.xt[i % 2], 'xt%d' % (i % 2), 'mix_pre', i % 2)

        def o1_front_t(i):
            trans_t(k, i % 2, hT1s[i % 2], 'hT1_%d' % (i % 2))

        o1_front_a(0)
        o1_front_t(0)
        for i in range(NT):
            cur['hT'] = hT1s[i % 2]
            cur['hTn'] = 'hT1_%d' % (i % 2)
            cs_ = csb[i % 2]
            csn = 'csb%d' % (i % 2)
            P.dma('sp', cs_[:, 0, :], ropeC[:, i, :], reads=['ropeC'], writes=[csn], stream='small')
            P.dma('sp', cs_[:, 1, :], ropeS[:, i, :], reads=['ropeS'], writes=[csn], stream='small')
            cn, sn = cs_[:, 0, 0:8], cs_[:, 1, 0:8]
            cr, sr = cs_[:, 0, 8:72], cs_[:, 1, 8:72]
            proj(1, 0, 512)
            P.op('act', lambda e: e.activation(out=zq, in_=k.bank[1], func=AF.Copy, scale=0.125), reads=['bank1'], writes=['zq'])
            rope(zq, qb, 8, 64, 8, cn, sn, csn, 'zq', 'qb')
            P.dma('sp', qd[i * 128:(i + 1) * 128, :], qb, reads=['qb'], writes=[('qd', i)], stream='xo')
            proj(2, 512, 1024)
            proj(3, 1024, 1304)
            P.op('act', lambda e: e.copy(out=zkv[:, 0:512], in_=k.bank[2]), reads=['bank2'], writes=['zkv'])
            P.op('act', lambda e: e.copy(out=zkv[:, 512:768], in_=k.bank[3][:, 0:256]), reads=['bank3'], writes=['zkv'])
            P.op('act', lambda e: e.activation(out=gsig[:, i, :], in_=k.bank[3][:, 256:280], func=AF.Sigmoid),
                 reads=['bank3'], writes=[('gsig', i)])
            kv4 = zkv.rearrange('p (a b g d) -> p a b g d', a=3, b=2, g=2)[:, :, 0, :, :]
            x1, x2 = kv4[:, :, :, 0:8], kv4[:, :, :, 8:16]
            cb = cn.unsqueeze(1).unsqueeze(1).broadcast_to([128, 3, 2, 8])
            sb_ = sn.unsqueeze(1).unsqueeze(1).broadcast_to([128, 3, 2, 8])
            t = [r_[:, 0:48].rearrange('p (a g d) -> p a g d', a=3, g=2) for r_ in rt]
            P.op('dve', lambda e: e.tensor_tensor(out=t[0], in0=x1, in1=cb, op=ALU.mult), reads=['zkv', csn], writes=['rt0'])
            P.op('dve', lambda e: e.tensor_tensor(out=t[1], in0=x2, in1=sb_, op=ALU.mult), reads=['zkv', csn], writes=['rt1'])
            P.op('dve', lambda e: e.tensor_tensor(out=t[2], in0=x2, in1=cb, op=ALU.mult), reads=['zkv', csn], writes=['rt2'])
            P.op('dve', lambda e: e.tensor_tensor(out=t[3], in0=x1, in1=sb_, op=ALU.mult), reads=['zkv', csn], writes=['rt3'])
            P.op('dve', lambda e: e.tensor_tensor(out=x1, in0=t[0], in1=t[1], op=ALU.subtract), reads=['rt0', 'rt1'], writes=['zkv'])
            P.op('dve', lambda e: e.tensor_tensor(out=x2, in0=t[2], in1=t[3], op=ALU.add), reads=['rt2', 'rt3'], writes=['zkv'])
            P.op('act', lambda e: e.copy(out=kvb, in_=zkv), reads=['zkv'], writes=['kvb'])
            P.op('pool', lambda e: e.tensor_copy(out=vs_aug[:, i, :, 0:64], in_=kvb[:, 384:512].rearrange('p (g d) -> p g d', g=2)),
                 reads=['kvb'], writes=['vs_aug'])
            P.op('pool', lambda e: e.tensor_copy(out=vw_aug[:, i, :, 0:64], in_=kvb[:, 640:768].rearrange('p (g d) -> p g d', g=2)),
                 reads=['kvb'], writes=['vw_aug'])
            tp = k.bankb[0]
            srcs = [0, 64, 128, 192, 512, 576, 256, 320]
            for j, c0 in enumerate(srcs):
                P.op('pe', lambda e, j=j, c0=c0: e.transpose(out=tp[0:64, j * 128:(j + 1) * 128], in_=kvb[:, c0:c0 + 64],
                                                            identity=k.ident), reads=['kvb', 'ident'], writes=['bank0'])
            P.op('act', lambda e: e.copy(out=kcv_t, in_=tp[0:64, 0:512].rearrange('p (j t) -> p j t', j=4)), reads=['bank0'],
                 writes=['kcv_t'])
            P.dma('sp', kcvd[:, :, i * 128:(i + 1) * 128].rearrange('j p t -> p j t'), kcv_t, reads=['kcv_t'], writes=['kcvd'],
                  stream='xo')
            P.op('act', lambda e: e.copy(out=kwT[:, :, i * 128:(i + 1) * 128],
                                         in_=tp[0:64, 512:768].rearrange('p (j t) -> p j t', j=2)), reads=['bank0'], writes=['kwT'])
            P.op('act', lambda e: e.copy(out=KE[0:64, :, i * 128:(i + 1) * 128],
                                         in_=tp[0:64, 768:1024].rearrange('p (j t) -> p j t', j=2)), reads=['bank0'], writes=['KE'])
            proj(4, 1304, 1816)
            proj(5, 1816, 2328)
            proj(6, 2328, 2840)
            proj(7, 2840, 3352)
            if i + 1 < NT:
                o1_front_a(i + 1)
            P.op('act', lambda e: e.copy(out=zrq, in_=k.bank[4]), reads=['bank4'], writes=['zrq'])
            P.op('act', lambda e: e.activation(out=zrk, in_=k.bank[5], func=AF.Copy, scale=128.0 ** -0.5), reads=['bank5'],
                 writes=['zrk'])
            P.op('act', lambda e: e.copy(out=rvb, in_=k.bank[6]), reads=['bank6'], writes=['rvb'])
            P.op('act', lambda e: e.activation(out=gsl, in_=k.bank[7], func=AF.Silu), reads=['bank7'], writes=['gsl'])
            rope(zrq, rqb, 4, 128, 64, cr, sr, csn, 'zrq', 'rqb')
            rope(zrk, rkr, 4, 128, 64, cr, sr, csn, 'zrk', 'rkr')
            P.op('act', lambda e: e.copy(out=rkb, in_=rkr), reads=['rkr'], writes=['rkb'])
            P.op('dve', lambda e: e.tensor_tensor(out=rkz.rearrange('p (h d) -> p h d', h=4),
                                                  in0=rkr.rearrange('p (h d) -> p h d', h=4),
                                                  in1=zeta.unsqueeze(2).broadcast_to([128, 4, 128]), op=ALU.mult),
                 reads=['rkr', 'zeta'], writes=['rkz'])
            for h in range(4):
                P.op('pe', lambda e, h=h: e.transpose(out=tp[:, h * 128:(h + 1) * 128], in_=rqb[:, h * 128:(h + 1) * 128],
                                                      identity=k.ident), reads=['rqb', 'ident'], writes=['bank0'])
            for h in range(4):
                P.op('pe', lambda e, h=h: e.transpose(out=tp[:, (4 + h) * 128:(5 + h) * 128], in_=rkb[:, h * 128:(h + 1) * 128],
                                                      identity=k.ident), reads=['rkb', 'ident'], writes=['bank0'])
            P.op('act', lambda e: e.copy(out=qkT, in_=tp.rearrange('p (c t) -> p c t', c=8)), reads=['bank0'], writes=['qkT'])
            for h in range(4):
                P.op('pe', lambda e, h=h: e.matmul(k.bank[1][:, h * 128:(h + 1) * 128], lhsT=qkT[:, 4 + h, :], rhs=qkT[:, h, :],
                                                  start=True, stop=True), reads=['qkT'], writes=['bank1'])
            P.op('dve', lambda e: e.tensor_tensor(out=STs, in0=k.bank[1], in1=decT, op=ALU.mult), reads=['bank1', 'decT'],
                 writes=['STs'])
            P.op('pool', lambda e: e.tensor_tensor(out=qxiT, in0=qkT[:, 0:4, :].rearrange('p h t -> p (h t)'), in1=xi, op=ALU.mult),
                 reads=['qkT', 'xi'], writes=['qxiT'])
            for h in range(4):
                hs = slice(h * 128, (h + 1) * 128)
                P.op('pe', lambda e, hs=hs: e.matmul(k.bank[2][:, hs], lhsT=STs[:, hs], rhs=rvb[:, hs], start=True, stop=(i == 0)),
                     reads=['STs', 'rvb'], writes=['bank2'])
                if i > 0:
                    P.op('pe', lambda e, hs=hs: e.matmul(k.bank[2][:, hs], lhsT=qxiT[:, hs], rhs=stbf[:, hs], start=False, stop=True),
                         reads=['qxiT', 'stbf'], writes=['bank2'])
            for h in range(4):
                hs = slice(h * 128, (h + 1) * 128)
                P.op('pe', lambda e, hs=hs: e.matmul(k.bank[3][:, hs], lhsT=rkz[:, hs], rhs=rvb[:, hs], start=True, stop=True),
                     reads=['rkz', 'rvb'], writes=['bank3'])
            P.op('dve', lambda e: e.tensor_tensor(out=state.rearrange('p (h d) -> p h d', h=4),
                                                  in0=state.rearrange('p (h d) -> p h d', h=4),
                                                  in1=gch.unsqueeze(2).broadcast_to([128, 4, 128]), op=ALU.mult),
                 reads=['state', 'gch'], writes=['state'])
            P.op('dve', lambda e: e.tensor_tensor(out=state, in0=k.bank[3], in1=state, op=ALU.add), reads=['bank3', 'state'],
                 writes=['state'])
            P.op('act', lambda e: e.copy(out=stbf, in_=state), reads=['state'], writes=['stbf'])
            if i + 1 < NT:
                o1_front_t(i + 1)
            for h in range(4):
                P.op('dve', lambda e, h=h: e.bn_stats(out=bst4[:, h, :], in_=k.bank[2][:, h * 128:(h + 1) * 128]),
                     reads=['bank2'], writes=['bst4'])
            for h in range(4):
                P.op('dve', lambda e, h=h: e.bn_aggr(out=mv4[:, h, :], in_=bst4[:, h, :]), reads=['bst4'], writes=['mv4'])
            P.op('dve', lambda e: e.tensor_scalar(out=rs4, in0=mv4[:, :, 1], scalar1=1e-5, scalar2=None, op0=ALU.add),
                 reads=['mv4'], writes=['rs4'])
            P.op('act', lambda e: e.activation(out=rs4, in_=rs4, func=AF.Sqrt), reads=['rs4'], writes=['rs4'])
            P.op('dve', lambda e: e.reciprocal(out=rs4, in_=rs4), reads=['rs4'], writes=['rs4'])
            for h in range(4):
                hs = slice(h * 128, (h + 1) * 128)
                P.op('dve', lambda e, h=h, hs=hs: e.tensor_scalar(out=on[:, hs], in0=k.bank[2][:, hs], scalar1=mv4[:, h, 0:1],
                                                                  scalar2=rs4[:, h:h + 1], op0=ALU.subtract, op1=ALU.mult),
                     reads=['bank2', 'mv4', 'rs4'], writes=['on'])
            P.op('pool', lambda e: e.tensor_tensor(out=on, in0=on, in1=gng, op=ALU.mult), reads=['on', 'gng'], writes=['on'])
            P.op('pool', lambda e: e.tensor_tensor(out=ydb, in0=on, in1=gsl, op=ALU.mult), reads=['on', 'gsl'], writes=['ydb'])
            P.dma('sp', ydd[i * 128:(i + 1) * 128, :], ydb, reads=['ydb'], writes=[('ydd', i)], stream='xo')

        P.barrier()
        s1.close()
        s1c = ExitStack()
        w1 = sbs(s1c, 'w1', [64, 32, 128], BF16)
        w2 = sbs(s1c, 'w2', [128, 64], BF16)
        posf32 = sbs(s1c, 'posf32', [64, 32], F32)
        posT = sbs(s1c, 'posT', [64, 32], BF16)
        cbias = sbs(s1c, 'cbias', [128, 1], F32)
        ghT = sbs(s1c, 'ghT', [128, 256], BF16)
        csrc = sbs(s1c, 'csrc', [64, T], BF16)
        for kind, (n1, n2, npos) in enumerate([('od_cmp_k_w1', 'od_cmp_k_w2', 'od_cmp_k_pos'),
                                               ('od_cmp_v_w1', 'od_cmp_v_w2', 'od_cmp_v_pos')]):
            P.dma('sp', w1, k.wb[(n1, o_)].rearrange('(l d) j -> d l j', d=64), reads=[('wb', n1, o_)], writes=['w1'], stream='w')
            P.dma('sp', w2, k.wb[(n2, o_)], reads=[('wb', n2, o_)], writes=['w2'], stream='w')
            with nc.allow_non_contiguous_dma(reason='tiny pos-emb transpose'):
                P.dma('sp', posf32, ins[npos][o_].rearrange('l d -> d l'), writes=['posf32'], stream='small')
            P.op('dve', lambda e: e.tensor_copy(out=posT, in_=posf32), reads=['posf32'], writes=['posT'])
            for g in range(2):
                P.dma('sp', csrc, kcvd[2 * kind + g], reads=['kcvd'], writes=['csrc'], stream='w')
                srcv = csrc.rearrange('p (n s) -> p n s', s=16)
                hb_ = k.bank[1]
                for l_ in range(32):
                    P.op('pe', lambda e, l_=l_: e.matmul(hb_[:, 0:255], lhsT=w1[:, l_, :],
                                                        rhs=(srcv[:, 0:255, l_] if l_ < 16 else srcv[:, 1:256, l_ - 16]),
                                                        start=(l_ == 0), stop=(l_ == 31)),
                         reads=['w1', 'csrc'], writes=['bank1'])
                for l_ in range(32):
                    P.op('pe', lambda e, l_=l_: e.matmul(hb_[:, 256:257], lhsT=w1[:, l_, :], rhs=posT[:, l_:l_ + 1],
                                                        start=(l_ == 0), stop=(l_ == 31)), reads=['w1', 'posT'], writes=['bank1'])
                P.op('dve', lambda e: e.tensor_copy(out=cbias, in_=hb_[:, 256:257]), reads=['bank1'], writes=['cbias'])
                P.op('dve', lambda e: e.memset(ghT[:, 255:256], 0.0), writes=['ghT'])
                P.op('act', lambda e: e.activation(out=ghT[:, 0:255], in_=hb_[:, 0:255], func=AF.Gelu_apprx_tanh,
                                                   bias=cbias[:, 0:1]), reads=['bank1', 'cbias'], writes=['ghT'])
                if kind == 0:
                    P.op('pe', lambda e: e.matmul(k.bank[2][0:64, 0:256], lhsT=w2, rhs=ghT, start=True, stop=True),
                         reads=['w2', 'ghT'], writes=['bank2'])
                    P.op('act', lambda e, g=g: e.copy(out=kcmpT[:, g, :], in_=k.bank[2][0:64, 0:256]), reads=['bank2'],
                         writes=['kcmpT'])
                else:
                    for c in range(2):
                        P.op('pe', lambda e, c=c: e.matmul(k.bank[2][:, c * 64:(c + 1) * 64], lhsT=ghT[:, c * 128:(c + 1) * 128],
                                                          rhs=w2, start=True, stop=True), reads=['w2', 'ghT'], writes=['bank2'])
                    P.op('act', lambda e, g=g: e.copy(out=vcmp_aug[:, :, g, 0:64],
                                                      in_=k.bank[2][:, 0:128].rearrange('p (c d) -> p c d', c=2)),
                         reads=['bank2'], writes=['vcmp'])
        P.barrier()
        s1c.close()

    with ExitStack() as s2:
        w_out_sb = sbs(s2, 'w_out', [128, 8, 1024], BF16)
        PT = sbs(s2, 'PT', [128, NT, 512], BF16)
        PW = sbs(s2, 'PW', [128, 5, 512], BF16)
        Pc = sbs(s2, 'Pc', [128, 2, 512], BF16)
        QN = [sbs(s2, 'QN%d' % g, [128, 512], BF16) for g in range(2)]
        qt = [sbs(s2, 'qt%d' % j, [128, 512], BF16) for j in range(2)]
        ydts = [sbs(s2, 'ydt%d' % j, [128, 512], BF16) for j in range(2)]
        negt = sbs(s2, 'negt', [128, 128], BF16)
        expf = sbs(s2, 'expf', [128, 512], F32)
        score = sbs(s2, 'score', [128, 64], F32)
        sc2 = sbs(s2, 'sc2', [128, 64], F32)
        m8a = sbs(s2, 'm8a', [128, 8], F32)
        m8b = sbs(s2, 'm8b', [128, 8], F32)
        thr = sbs(s2, 'thr', [128, 1], F32)
        psl = sbs(s2, 'psl', [128, 64], F32)
        rden = sbs(s2, 'rden', [128, 12], F32)
        coef = sbs(s2, 'coef', [128, 12], F32)
        yc = sbs(s2, 'yc', [128, 512], F32)
        ycb = sbs(s2, 'ycb', [128, 512], BF16)
        yT = sbs(s2, 'yT', [128, 8, 128], BF16)
        P.dma('sp', w_out_sb, k.wb[('od_w_out', o_)].rearrange('(c p) n -> p c n', p=128), reads=[('wb', 'od_w_out', o_)],
              writes=['w_out'], stream='w')
        P.op('dve', lambda e: e.memset(negt, 0.0), writes=['negt'])
        tp = k.bankb[0]
        sbank = [0]

        def next_sbank():
            sbank[0] += 1
            return 1 + (sbank[0] % 2)

        def den_view(bank):
            return k.bank[bank][:, 0:260].rearrange('p (m e) -> p m e', e=65)[:, :, 64]

        def masked_exp(b, nn, dst, mask_ap, dname):
            if mask_ap is None:
                P.op('act', lambda e: e.activation(out=dst[0:nn], in_=k.bank[b][0:nn, :], func=AF.Exp), reads=['bank%d' % b],
                     writes=[dname])
            else:
                P.op('act', lambda e: e.activation(out=expf[0:nn], in_=k.bank[b][0:nn, :], func=AF.Exp), reads=['bank%d' % b],
                     writes=['expf'])
                P.op('pool', lambda e: e.tensor_tensor(out=dst[0:nn].rearrange('p (m q) -> p m q', m=4),
                                                       in0=expf[0:nn].rearrange('p (m q) -> p m q', m=4),
                                                       in1=mask_ap.unsqueeze(1).broadcast_to([nn, 4, 128]), op=ALU.mult),
                     reads=['expf', 'tri', 'ntri'], writes=[dname])

        def o2_loads(i):
            P.dma('sp', k.xt[i % 2], xsrc[i * 128:(i + 1) * 128, :], reads=[('x', i)], writes=['xt%d' % (i % 2)], stream='x')
            P.dma('sp', qt[i % 2], qd[i * 128:(i + 1) * 128, :], reads=[('qd', i)], writes=['qt%d' % (i % 2)], stream='x')
            P.dma('sp', ydts[i % 2], ydd[i * 128:(i + 1) * 128, :], reads=[('ydd', i)], writes=['ydt%d' % (i % 2)], stream='x')

        o2_loads(0)
        for i in range(NT):
            xt = k.xt[i % 2]
            xr = 'xt%d' % (i % 2)
            qti = qt[i % 2]
            qtn = 'qt%d' % (i % 2)
            ydt = ydts[i % 2]
            ydn = 'ydt%d' % (i % 2)
            if i + 1 < NT:
                o2_loads(i + 1)
            off = 62 - 2 * i
            for g in range(2):
                Q = QN[g]
                qn = 'QN%d' % g
                for m in range(4):
                    h = 4 * g + m
                    P.op('pe', lambda e, m=m, h=h: e.transpose(out=tp[0:64, m * 128:(m + 1) * 128], in_=qti[:, h * 64:(h + 1) * 64],
                                                              identity=k.ident), reads=[qtn, 'ident'], writes=['bank0'])
                P.op('act', lambda e: e.copy(out=Q[0:64, :], in_=tp[0:64, 0:512]), reads=['bank0'], writes=[qn + 'lo'])
                chunks = [(0, 128)] + ([(1, 127)] if 8 * i + 6 >= 128 else [])
                for (c, nn) in chunks:
                    b = next_sbank()
                    P.op('pe', lambda e, c=c, nn=nn, b=b: e.matmul(k.bank[b][0:nn, :], lhsT=kcmpT[:, g, c * 128:c * 128 + nn],
                                                                  rhs=Q[0:64, :], start=True, stop=True),
                         reads=['kcmpT', qn + 'lo'], writes=['bank%d' % b])
                    full = (16 * (c * 128 + nn - 1) + 31 <= 128 * i)
                    if full:
                        P.op('act', lambda e, c=c, nn=nn, b=b: e.activation(out=Pc[0:nn, c, :], in_=k.bank[b][0:nn, :], func=AF.Exp),
                             reads=['bank%d' % b], writes=['Pc'])
                    else:
                        tv = float(128 * i - 31 - 2048 * c)
                        P.op('act', lambda e, nn=nn, b=b: e.activation(out=expf[0:nn], in_=k.bank[b][0:nn, :], func=AF.Exp),
                             reads=['bank%d' % b], writes=['expf'])
                        P.op('dve', lambda e, c=c, nn=nn, tv=tv: e.scalar_tensor_tensor(
                            out=Pc[0:nn, c, :].rearrange('p (m q) -> p m q', m=4),
                            in0=cm[0:nn].unsqueeze(1).broadcast_to([nn, 4, 128]), scalar=tv,
                            in1=expf[0:nn].rearrange('p (m q) -> p m q', m=4), op0=ALU.is_le, op1=ALU.mult),
                             reads=['expf', 'cm'], writes=['Pc'])
                for m in range(4):
                    for ci, (c, nn) in enumerate(chunks):
                        P.op('pe', lambda e, m=m, c=c, nn=nn, ci=ci: e.matmul(k.bank[3][:, m * 65:(m + 1) * 65],
                                                                            lhsT=Pc[0:nn, c, m * 128:(m + 1) * 128],
                                                                            rhs=vcmp_aug[0:nn, c, g, :], start=(ci == 0),
                                                                            stop=(ci == len(chunks) - 1)),
                             reads=['Pc', 'vcmp'], writes=['bank3'])
                for m in range(4):
                    for ci, (c, nn) in enumerate(chunks):
                        P.op('pe', lambda e, m=m, c=c, nn=nn, ci=ci: e.matmul(k.bank[4][:, m * 64:(m + 1) * 64],
                                                                            lhsT=Pc[0:nn, c, m * 128:(m + 1) * 128],
                                                                            rhs=cts[0:nn, c, :], start=(ci == 0),
                                                                            stop=(ci == len(chunks) - 1)),
                             reads=['Pc', 'cts'], writes=['bank4'])
                jts = list(range(max(0, i - 4), i + 1))
                for sl_, jt in enumerate(jts):
                    b = next_sbank()
                    P.op('pe', lambda e, jt=jt, b=b: e.matmul(k.bank[b], lhsT=kwT[:, g, jt * 128:(jt + 1) * 128], rhs=Q[0:64, :],
                                                             start=True, stop=True), reads=['kwT', qn + 'lo'], writes=['bank%d' % b])
                    mk = tri if jt == i else (ntri if jt == i - 4 else None)
                    masked_exp(b, 128, PW[:, sl_, :], mk, ('PW', sl_))
                for m in range(4):
                    for sl_, jt in enumerate(jts):
                        P.op('pe', lambda e, m=m, jt=jt, sl_=sl_: e.matmul(k.bank[6][:, m * 65:(m + 1) * 65],
                                                                          lhsT=PW[:, sl_, m * 128:(m + 1) * 128],
                                                                          rhs=vw_aug[:, jt, g, :], start=(sl_ == 0),
                                                                          stop=(sl_ == len(jts) - 1)),
                             reads=[('PW', sl_), 'vw_aug'], writes=['bank6'])
                P.op('dve', lambda e: e.tensor_scalar(out=rden[:, 0:4], in0=den_view(3), scalar1=1e-30, scalar2=None, op0=ALU.max),
                     reads=['bank3'], writes=['rden'])
                P.op('dve', lambda e: e.reciprocal(out=rden[:, 0:4], in_=rden[:, 0:4]), reads=['rden'], writes=['rden'])
                P.op('dve', lambda e: e.tensor_scalar(out=psl, in0=k.bank[4][:, 0:64], scalar1=rden[:, 0:1], scalar2=None,
                                                      op0=ALU.mult), reads=['bank4', 'rden'], writes=['psl'])
                for m in range(1, 4):
                    P.op('dve', lambda e, m=m: e.scalar_tensor_tensor(out=psl, in0=k.bank[4][:, m * 64:(m + 1) * 64],
                                                                      scalar=rden[:, m:m + 1], in1=psl, op0=ALU.mult, op1=ALU.add),
                         reads=['bank4', 'rden', 'psl'], writes=['psl'])
                P.op('dve', lambda e: e.tensor_tensor(out=score, in0=psl, in1=keep[:, off:off + 64], op=ALU.mult),
                     reads=['psl', 'keep'], writes=['score'])
                P.op('dve', lambda e: e.tensor_tensor(out=score, in0=score, in1=addc[:, off:off + 64], op=ALU.add),
                     reads=['score', 'addc'], writes=['score'])
                P.op('dve', lambda e: e.memset(score[:, 0:1], 1.0e4), reads=['score'], writes=['score'])
                P.op('dve', lambda e: e.max(out=m8a, in_=score), reads=['score'], writes=['m8a'])
                P.op('dve', lambda e: e.match_replace(out=sc2, in_to_replace=m8a, in_values=score, imm_value=-2.0),
                     reads=['score', 'm8a'], writes=['sc2'])
                P.op('dve', lambda e: e.max(out=m8b, in_=sc2), reads=['sc2'], writes=['m8b'])
                P.op('dve', lambda e: e.tensor_scalar(out=thr, in0=m8b[:, 7:8], scalar1=0.0, scalar2=None, op0=ALU.max),
                     reads=['m8b'], writes=['thr'])
                P.op('dve', lambda e: e.tensor_scalar(out=negt[:, 64:128], in0=score, scalar1=thr[:, 0:1], scalar2=-30000.0,
                                                      op0=ALU.is_lt, op1=ALU.mult), reads=['score', 'thr'], writes=['negt'])
                P.op('pe', lambda e: e.transpose(out=tp[:, 512:640], in_=negt, identity=k.ident), reads=['negt', 'ident'],
                     writes=['bank0'])
                P.op('act', lambda e: e.copy(out=Q[64:128, :].rearrange('p (m q) -> p m q', m=4),
                                             in_=tp[64:128, 512:640].unsqueeze(1).broadcast_to([64, 4, 128])),
                     reads=['bank0'], writes=[qn + 'hi'])
                for jt in range(i + 1):
                    b = next_sbank()
                    P.op('pe', lambda e, jt=jt, b=b: e.matmul(k.bank[b], lhsT=KE[:, g, jt * 128:(jt + 1) * 128], rhs=Q, start=True,
                                                             stop=True), reads=['KE', qn + 'lo', qn + 'hi'], writes=['bank%d' % b])
                    masked_exp(b, 128, PT[:, jt, :], tri if jt == i else None, ('PT', jt))
                for m in range(4):
                    for jt in range(i + 1):
                        P.op('pe', lambda e, m=m, jt=jt: e.matmul(k.bank[5][:, m * 65:(m + 1) * 65],
                                                                 lhsT=PT[:, jt, m * 128:(m + 1) * 128], rhs=vs_aug[:, jt, g, :],
                                                                 start=(jt == 0), stop=(jt == i)),
                             reads=[('PT', jt), 'vs_aug'], writes=['bank5'])
                P.op('dve', lambda e: e.tensor_scalar(out=rden[:, 4:8], in0=den_view(5), scalar1=1e-30, scalar2=None, op0=ALU.max),
                     reads=['bank5'], writes=['rden'])
                P.op('dve', lambda e: e.tensor_scalar(out=rden[:, 8:12], in0=den_view(6), scalar1=1e-30, scalar2=None, op0=ALU.max),
                     reads=['bank6'], writes=['rden'])
                P.op('dve', lambda e: e.reciprocal(out=rden[:, 4:12], in_=rden[:, 4:12]), reads=['rden'], writes=['rden'])
                P.op('dve', lambda e: e.tensor_tensor(out=coef.rearrange('p (b m) -> p b m', b=3),
                                                      in0=rden.rearrange('p (b m) -> p b m', b=3),
                                                      in1=gsig[:, i, g * 12:(g + 1) * 12].rearrange('p (m b) -> p b m', b=3),
                                                      op=ALU.mult), reads=['rden', ('gsig', i)], writes=['coef'])
                for m in range(4):
                    h = 4 * g + m
                    ym = yc[:, h * 64:(h + 1) * 64]
                    P.op('dve', lambda e, m=m, ym=ym: e.tensor_scalar(out=ym, in0=k.bank[3][:, m * 65:m * 65 + 64],
                                                                      scalar1=coef[:, m:m + 1], scalar2=None, op0=ALU.mult),
                         reads=['bank3', 'coef'], writes=['yc'])
                    P.op('dve', lambda e, m=m, ym=ym: e.scalar_tensor_tensor(out=ym, in0=k.bank[5][:, m * 65:m * 65 + 64],
                                                                             scalar=coef[:, 4 + m:5 + m], in1=ym, op0=ALU.mult,
                                                                             op1=ALU.add), reads=['bank5', 'coef', 'yc'], writes=['yc'])
                    P.op('dve', lambda e, m=m, ym=ym: e.scalar_tensor_tensor(out=ym, in0=k.bank[6][:, m * 65:m * 65 + 64],
                                                                             scalar=coef[:, 8 + m:9 + m], in1=ym, op0=ALU.mult,
                                                                             op1=ALU.add), reads=['bank6', 'coef', 'yc'], writes=['yc'])
            P.op('act', lambda e: e.copy(out=ycb, in_=yc), reads=['yc'], writes=['ycb'])
            for c in range(4):
                P.op('pe', lambda e, c=c: e.transpose(out=tp[:, c * 128:(c + 1) * 128], in_=ycb[:, c * 128:(c + 1) * 128],
                                                      identity=k.ident), reads=['ycb', 'ident'], writes=['bank0'])
            for c in range(4):
                P.op('pe', lambda e, c=c: e.transpose(out=tp[:, (4 + c) * 128:(5 + c) * 128], in_=ydt[:, c * 128:(c + 1) * 128],
                                                      identity=k.ident), reads=[ydn, 'ident'], writes=['bank0'])
            P.op('act', lambda e: e.copy(out=yT, in_=tp.rearrange('p (c t) -> p c t', c=8)), reads=['bank0'], writes=['yT'])
            pm = [k.bank[3], k.bank[4]]
            for cb in range(2):
                for kc in range(8):
                    P.op('pe', lambda e, kc=kc, cb=cb: e.matmul(pm[cb], lhsT=yT[:, kc, :], rhs=w_out_sb[:, kc, cb * 512:(cb + 1) * 512],
                                                               start=(kc == 0), stop=(kc == 7)), reads=['yT', 'w_out'],
                         writes=['bank%d' % (3 + cb)])
            post_norm_residual(k, pm, ['bank3', 'bank4'], 'mix_post', xt, xr)
            P.dma('sp', xdst[i * 128:(i + 1) * 128, :], xt, reads=[xr], writes=[('x', i)], stream='xo')


def make_consts():
    c = {}
    Dm = np.zeros((3, 4, 128, 128), np.float32)
    for g, w in enumerate(POOL_WINDOWS):
        for t in range(128):
            lo = max(t + 1 - w, 0)
            for s in range(lo, t + 1):
                Dm[0, g, s, t] += 1.0 / (t + 1 - lo)
            Dm[0, g, t, t] -= 1.0
            for s in range(t + 1 - w, t + 1):
                if s >= 0:
                    Dm[1, g, s, t] += 1.0 / w
                else:
                    Dm[2, g, s + 128, t] += 1.0 / w
            Dm[1, g, t, t] -= 1.0
    c['c_D'] = Dm.astype(ml_dtypes.bfloat16)
    bf = ml_dtypes.bfloat16
    invf = np.concatenate([1.0 / (500000.0 ** (np.arange(0, 16, 2, dtype=np.float32) / 16)),
                           1.0 / (10000.0 ** (np.arange(0, 128, 2, dtype=np.float32) / 128))]).astype(np.float32)
    c['c_invf'] = invf.reshape(1, 72)
    lg = np.log1p(-np.exp2(-5.0 - np.arange(4, dtype=np.float64)))
    idx = np.arange(128, dtype=np.float64)
    rel = idx[None, :] - idx[:, None]
    dec = np.where((rel >= 0)[:, None, :], np.exp(np.maximum(rel, 0)[:, None, :] * lg[None, :, None]), 0.0)
    c['c_decT'] = dec.reshape(128, 512).astype(np.float32)
    xi = np.exp((idx + 1.0)[None, :] * lg[:, None])
    c['c_xi'] = np.broadcast_to(xi.reshape(1, 512), (128, 512)).astype(np.float32).copy()
    c['c_zeta'] = np.exp((127 - idx)[:, None] * lg[None, :]).astype(np.float32)
    c['c_gch'] = np.broadcast_to(np.exp(128 * lg)[None, :], (128, 4)).astype(np.float32).copy()
    p = np.arange(128)
    c['c_tri'] = (p[:, None] <= p[None, :]).astype(np.float32)
    c['c_cm'] = (16.0 * p[:, None] - p[None, :]).astype(np.float32)
    cq = (p >= 64).astype(np.int64)[:, None]
    jj = np.arange(128)[None, :]
    c['c_keep'] = (jj <= 60 + cq).astype(np.float32)
    add = np.zeros((128, 128), np.float32)
    add[(jj == 61 + cq) | (jj == 62 + cq)] = 1.0e4
    add[jj > 62 + cq] = -1.0
    c['c_add'] = add
    n = np.arange(256)
    cs = n * 16
    ss = np.arange(64) * 64
    cts = ((cs[:, None] < ss[None, :] + 64) & (cs[:, None] + 32 > ss[None, :])).astype(np.float32)
    cts[255] = 0
    c['c_cts'] = cts.reshape(2, 128, 64).astype(bf)
    c['c_E'] = (np.arange(4096)[None, :] // 64 == np.arange(64)[:, None]).astype(bf)
    return c


INPUT_NAMES = ["x", "positions", "ln_mix_pre", "ln_mix_post", "ln_ffn_pre", "ln_ffn_post", "ffn_w_gate", "ffn_w_up",
               "ffn_w_down", "ev_w_in", "ev_pool_w", "ev_pool_scale", "ev_sgu_ln_g", "ev_sgu_ln_b", "ev_sgu_w",
               "ev_sgu_b", "ev_w_out", "od_w_in", "od_cmp_k_pos", "od_cmp_k_w1", "od_cmp_k_w2", "od_cmp_v_pos",
               "od_cmp_v_w1", "od_cmp_v_w2", "od_ret_gn_g", "od_w_out"]


def build(shapes, consts, layers=(0, 1, 2, 3), phases=('mix', 'ffn')):
    from contextlib import ExitStack
    nc = bass.Bass("TRN2", target_bir_lowering=False)
    ins = {}
    for n in INPUT_NAMES:
        shp = list(shapes[n])
        if n == 'x':
            shp = [T, D]
        if n == 'positions':
            shp = [1, T]
        ins[n] = nc.dram_tensor(n, shp, I32 if n == 'positions' else F32, kind="ExternalInput").ap()
    for n, v in consts.items():
        ins[n] = nc.dram_tensor(n, list(v.shape), BF16 if v.dtype == ml_dtypes.bfloat16 else F32, kind="ExternalInput").ap()
    y = nc.dram_tensor("y", [T, D], F32, kind="ExternalOutput").ap()
    k = K(nc, layers)
    setup_common(k, ins)
    k.sb2 = k.sb('ssa', [128, 2], F32)
    P = k.P
    xsrc = ins['x']
    for li, l in enumerate(layers):
        load_gains(k, l)
        if li + 1 < len(layers):
            cast_layer(k, layers[li + 1])
        if 'mix' in phases:
            with ExitStack() as es:
                if l % 2 == 0:
                    even_phase(k, l, xsrc, y, es)
                else:
                    odd_phase(k, l, xsrc, y, es)
                P.barrier()
            xsrc = y
        if 'ffn' in phases:
            with ExitStack() as es:
                def sb(name, shape, dt):
                    return es.enter_context(nc.sbuf_tensor('ffs%d_' % l + name, list(shape), dt)).ap()
                k.wd_sb = sb('wd', [128, NFC, 1024], BF16)
                k.xt8 = [sb('xt8_%d' % i, [128, D], F32) for i in range(8)]
                k.wgu = [sb('wgu%d' % i, [128, 2, 8, 512], BF16) for i in range(2)]
                k.hT2 = [sb('hT%d' % i, [128, 8, 512], BF16) for i in range(2)]
                k.actT = sb('actT', [128, NFC, 512], BF16)
                k.sg = [sb('sg%d' % i, [128, 512], F32) for i in range(2)]
                ffn_phase(k, l, xsrc, y)
                P.barrier()
            xsrc = y
    P.finish()
    return nc


_CACHE = {}


def kernel(**inputs):
    consts = make_consts()
    shapes = {n: inputs[n].shape for n in INPUT_NAMES}
    if 'nc' not in _CACHE:
        _CACHE['nc'] = build(shapes, consts)
    nc = _CACHE['nc']
    in_maps = []
    for c in range(4):
        b = c % 4
        m = {n: np.ascontiguousarray(inputs[n]) for n in INPUT_NAMES if n not in ('x', 'positions')}
        m['x'] = np.ascontiguousarray(inputs['x'][b])
        m['positions'] = np.ascontiguousarray(inputs['positions'][b:b + 1]).astype(np.int32)
        m.update(consts)
        in_maps.append(m)
    res = run_bass_kernel_spmd(nc, in_maps, core_ids=list(range(4)))
    out = np.stack([res.results[b]["y"] for b in range(4)], axis=0)
    return out.astype(np.float32)
```

```python
import numpy as np
import ml_dtypes
import concourse.bass as bass
import concourse.mybir as mybir
from concourse.bass_utils import run_bass_kernel_spmd

F32 = mybir.dt.float32
BF16 = mybir.dt.bfloat16
I32 = mybir.dt.int32
AF = mybir.ActivationFunctionType
ALU = mybir.AluOpType
AX = mybir.AxisListType

import os
SAME_ENG_SYNC = os.environ.get("SES", "1") == "1"


class Prog:
    def __init__(self, nc):
        self.nc = nc
        self.E = {'pe': nc.tensor, 'dve': nc.vector, 'act': nc.scalar, 'pool': nc.gpsimd, 'sp': nc.sync}
        self.semh = {k: nc.alloc_semaphore('s_' + k) for k in ['pe', 'dve', 'act', 'pool']}
        self.cnt = {k: 0 for k in self.semh}
        self.seen = {e: {} for e in self.E}
        self.lastw = {}
        self.readers = {}
        self.nwait = 0
        self.dslot = 0
        self.NSLOT = 32
        self.nins = 0

    def _deps(self, reads, writes):
        deps = {}
        for r in reads:
            w = self.lastw.get(r)
            if w and deps.get(w[0], 0) < w[1]:
                deps[w[0]] = w[1]
        for w_ in writes:
            w = self.lastw.get(w_)
            if w and deps.get(w[0], 0) < w[1]:
                deps[w[0]] = w[1]
            for k, v in self.readers.get(w_, {}).items():
                if deps.get(k, 0) < v:
                    deps[k] = v
        return deps

    def _wait(self, eng, deps):
        e = self.E[eng]
        seen = self.seen[eng]
        for k, v in deps.items():
            if k == eng and (eng == 'pe' or not SAME_ENG_SYNC):
                continue
            if seen.get(k, 0) >= v:
                continue
            e.wait_ge(self.semh[k], v)
            seen[k] = v
            self.nwait += 1

    def _record(self, tag, reads, writes):
        for w in writes:
            self.lastw[w] = tag
            self.readers[w] = {}
        for r in reads:
            d = self.readers.setdefault(r, {})
            if d.get(tag[0], 0) < tag[1]:
                d[tag[0]] = tag[1]

    def op(self, eng, fn, reads=(), writes=()):
        self._wait(eng, self._deps(reads, writes))
        ins = fn(self.E[eng])
        self.cnt[eng] += 1
        ins.then_inc(self.semh[eng], 1)
        self.nins += 1
        self._record((eng, self.cnt[eng]), reads, writes)

    def dma(self, q, out, in_, reads=(), writes=(), stream='d0', **kw):
        slot = self.dslot % self.NSLOT
        self.dslot += 1
        key = 'd:%d' % slot
        if key not in self.semh:
            self.semh[key] = self.nc.alloc_semaphore('sd_%d' % slot)
            self.cnt[key] = 0
        e = self.E[q]
        if self.cnt[key] > 0 and self.seen[q].get(key, 0) < self.cnt[key]:
            e.wait_ge(self.semh[key], self.cnt[key])
            self.seen[q][key] = self.cnt[key]
        self._wait(q, self._deps(reads, writes))
        e.dma_start(out=out, in_=in_, **kw).then_inc(self.semh[key], 16)
        self.cnt[key] += 16
        self.nins += 1
        self._record((key, self.cnt[key]), reads, writes)

    def barrier(self):
        for eng in self.E:
            deps = {k: v for k, v in self.cnt.items() if v > 0}
            e = self.E[eng]
            for k, v in deps.items():
                if k == eng and eng == 'pe':
                    continue
                if self.seen[eng].get(k, 0) >= v:
                    continue
                e.wait_ge(self.semh[k], v)
                self.seen[eng][k] = v
        self.lastw = {}
        self.readers = {}

    def finish(self, eng='sp'):
        deps = {k: v for k, v in self.cnt.items() if v > 0}
        e = self.E[eng]
        for k, v in deps.items():
            e.wait_ge(self.semh[k], v)


T = 4096
D = 1024
NT = T // 128
FH = 2816
NFC = FH // 128
DEPTH = 4
POOL_WINDOWS = (2, 4, 8, 16)
EPS = 1e-6


def _flat2(ap, c=1024):
    n = len(ap.shape)
    names = ' '.join('d%d' % i for i in range(n))
    f = ap.rearrange('%s -> (%s)' % (names, names)) if n > 1 else ap
    return f.rearrange('(r c) -> r c', c=c)


class K:
    def __init__(self, nc, layers=(0, 1, 2, 3), Tn=T):
        self.nc = nc
        self.P = Prog(nc)
        self.layers = layers
        self.uid = 0

    def sb(self, name, shape, dt):
        return self.nc.alloc_sbuf_tensor(name, list(shape), dt).ap()

    def dram(self, name, shape, dt, kind="Internal"):
        return self.nc.dram_tensor(name, list(shape), dt, kind=kind).ap()


def cast_copy(k, dst, src, res):
    d2 = _flat2(dst)
    s2 = _flat2(src)
    rows = d2.shape[0]
    r0 = 0
    while r0 < rows:
        r1 = min(rows, r0 + 2048)
        k.P.dma('pool', d2[r0:r1, :], s2[r0:r1, :], writes=[res], stream='cast')
        r0 = r1


def cast_layer(k, l):
    ins = k.ins
    order = []
    if l % 2 == 0:
        order += [('ev_w_in', l // 2), ('ev_pool_w', l // 2), ('ev_w_out', l // 2)]
    else:
        order += [('od_w_in', l // 2), ('od_cmp_k_w1', l // 2), ('od_cmp_k_w2', l // 2), ('od_cmp_v_w1', l // 2),
                  ('od_cmp_v_w2', l // 2), ('od_w_out', l // 2)]
    order += [('ffn_w_gate', l), ('ffn_w_up', l), ('ffn_w_down', l)]
    for name, idx in order:
        src = ins[name][idx]
        dst = k.dram('wb_%s_%d' % (name, idx), src.shape, BF16)
        k.wb[(name, idx)] = dst
        cast_copy(k, dst, src, ('wb', name, idx))


def rstd_from_ssq(k, ssq, rstd, tmp, n, eps, tag):
    P = k.P
    P.op('dve', lambda e: e.tensor_scalar(out=tmp, in0=ssq, scalar1=1.0 / n, scalar2=eps, op0=ALU.mult, op1=ALU.add),
         reads=[tag + 'ssq'], writes=[tag + 'tmp'])
    P.op('act', lambda e: e.activation(out=tmp, in_=tmp, func=AF.Sqrt), reads=[tag + 'tmp'], writes=[tag + 'tmp'])
    P.op('dve', lambda e: e.reciprocal(out=rstd, in_=tmp), reads=[tag + 'tmp'], writes=[tag + 'rstd'])


def setup_common(k, ins):
    nc, P = k.nc, k.P
    k.ins = ins
    k.bank = [nc.alloc_psum_tensor('bank%d' % i, [128, 512], F32).ap() for i in range(8)]
    k.bankb = [b.bitcast(BF16) for b in k.bank]
    k.io_f = k.sb('io_f', [128, 128], F32)
    k.iop = k.sb('iop', [128, 1], F32)
    k.ident = k.sb('ident', [128, 128], BF16)
    k.ones_row = k.sb('ones_row', [1, 128], BF16)
    P.op('pool', lambda e: e.iota(k.io_f, pattern=[[1, 128]], base=0, channel_multiplier=0,
                                  allow_small_or_imprecise_dtypes=True), writes=['io_f'])
    P.op('pool', lambda e: e.iota(k.iop, pattern=[[1, 1]], base=0, channel_multiplier=1,
                                  allow_small_or_imprecise_dtypes=True), writes=['iop'])
    P.op('dve', lambda e: e.tensor_scalar(out=k.ident, in0=k.io_f, scalar1=k.iop[:, 0:1], scalar2=None,
                                          op0=ALU.is_equal), reads=['io_f', 'iop'], writes=['ident'])
    P.op('dve', lambda e: e.memset(k.ones_row, 1.0), writes=['ones_row'])
    k.wb = {}
    cast_layer(k, k.layers[0])
    k.xt = [k.sb('xt%d' % s, [128, D], F32) for s in range(2)]
    k.hbs = [k.sb('hb%d' % i, [128, D], BF16) for i in range(2)]
    k.junk = k.sb('junk', [128, D], BF16)
    k.tmpf = k.sb('tmpf', [128, D], F32)
    k.G = {n: k.sb('G_' + n, [128, D], F32) for n in ['mix_pre', 'mix_post', 'ffn_pre', 'ffn_post']}
    k.st = {n: k.sb('st_' + n, [128, 1], F32) for n in ['ssq', 'tmp', 'rstd', 'ssq2', 'tmp2', 'rstd2']}


def load_gains(k, l):
    for n in ['mix_pre', 'mix_post', 'ffn_pre', 'ffn_post']:
        k.P.dma('sp', k.G[n], k.ins['ln_' + n][l:l + 1, :].broadcast_to([128, D]), writes=['G_' + n], stream='small')


def norm_a(k, xt_ap, xres, gname, hbi):
    P = k.P
    st = k.st
    hb = k.hbs[hbi]
    P.op('act', lambda e: e.activation(out=k.junk, in_=xt_ap, func=AF.Square, accum_out=st['ssq']),
         reads=[xres], writes=['junk', 'ssq'])
    rstd_from_ssq(k, st['ssq'], st['rstd'], st['tmp'], D, EPS, '')
    P.op('dve', lambda e: e.scalar_tensor_tensor(out=hb, in0=xt_ap, scalar=st['rstd'][:, 0:1], in1=k.G[gname],
                                                 op0=ALU.mult, op1=ALU.mult),
         reads=[xres, 'rstd', 'G_' + gname], writes=['hb%d' % hbi])


def trans_t(k, hbi, hT_dst, hTres):
    P = k.P
    hb = k.hbs[hbi]
    tp = k.bankb[0]
    for c in range(8):
        P.op('pe', lambda e, c=c: e.transpose(out=tp[:, c * 128:(c + 1) * 128], in_=hb[:, c * 128:(c + 1) * 128],
                                              identity=k.ident), reads=['hb%d' % hbi, 'ident'], writes=['bank0'])
    P.op('act', lambda e: e.copy(out=hT_dst, in_=tp.rearrange('p (c t) -> p c t', c=8)), reads=['bank0'],
         writes=[hTres])


def post_norm_residual(k, pm, pmres, gname, xt_ap, xres):
    P = k.P
    st = k.st
    ssa = k.sb2
    for cb in range(2):
        P.op('act', lambda e, cb=cb: e.activation(out=k.junk[:, cb * 512:(cb + 1) * 512], in_=pm[cb], func=AF.Square,
                                                  accum_out=ssa[:, cb:cb + 1]),
             reads=[pmres[cb]], writes=['junk', 'ssa'])
    P.op('dve', lambda e: e.tensor_tensor(out=st['ssq2'], in0=ssa[:, 0:1], in1=ssa[:, 1:2], op=ALU.add),
         reads=['ssa'], writes=['2ssq'])
    rstd_from_ssq(k, st['ssq2'], st['rstd2'], st['tmp2'], D, EPS, '2')
    for cb in range(2):
        sl = slice(cb * 512, (cb + 1) * 512)
        P.op('dve', lambda e, cb=cb, sl=sl: e.scalar_tensor_tensor(out=k.tmpf[:, sl], in0=pm[cb], scalar=st['rstd2'][:, 0:1],
                                                                   in1=k.G[gname][:, sl], op0=ALU.mult, op1=ALU.mult),
             reads=[pmres[cb], '2rstd', 'G_' + gname], writes=['tmpf'])
    P.op('pool', lambda e: e.tensor_tensor(out=xt_ap, in0=xt_ap, in1=k.tmpf, op=ALU.add), reads=['tmpf', xres],
         writes=[xres])


def ffn_phase(k, l, xsrc, xdst):
    nc, P = k.nc, k.P
    wg, wu, wd = k.wb[('ffn_w_gate', l)], k.wb[('ffn_w_up', l)], k.wb[('ffn_w_down', l)]
    P.dma('sp', k.wd_sb, wd.rearrange('(c p) n -> p c n', p=128), reads=[('wb', 'ffn_w_down', l)], writes=['wd_sb'],
          stream='w')
    groups = [(0, 4), (4, 4), (8, 4), (12, 4), (16, 4), (20, 2)]
    wgv = wg.rearrange('(c p) n -> p c n', p=128)
    wuv = wu.rearrange('(c p) n -> p c n', p=128)
    gi = 0
    NB = T // 512

    def xtile(tb, s):
        j = (tb % 2) * 4 + s
        return k.xt8[j], 'xt8_%d' % j

    def front_a(tb, s):
        t0 = tb * 512 + s * 128
        xt, xr = xtile(tb, s)
        P.dma('sp', xt, xsrc[t0:t0 + 128, :], reads=[('x', t0 // 128)], writes=[xr], stream='x')
        norm_a(k, xt, xr, 'ffn_pre', s % 2)

    def front_t(tb, s):
        trans_t(k, s % 2, k.hT2[tb % 2][:, :, s * 128:(s + 1) * 128], ('hT', tb % 2))

    for s in range(4):
        front_a(0, s)
        front_t(0, s)
    for tb in range(NB):
        hT = k.hT2[tb % 2]
        hTr = ('hT', tb % 2)
        for gidx, (j0, nj) in enumerate(groups):
            pre = gidx < 4 and tb + 1 < NB
            if pre:
                front_a(tb + 1, gidx)
            buf = gi % 2
            gi += 1
            wt = k.wgu[buf]
            P.dma('sp', wt[:, 0, :, 0:nj * 128], wgv[:, :, j0 * 128:(j0 + nj) * 128], reads=[('wb', 'ffn_w_gate', l)],
                  writes=['wgu%d' % buf], stream='w')
            P.dma('sp', wt[:, 1, :, 0:nj * 128], wuv[:, :, j0 * 128:(j0 + nj) * 128], reads=[('wb', 'ffn_w_up', l)],
                  writes=['wgu%d' % buf], stream='w')
            for jj in range(nj):
                j = j0 + jj
                pb = 1 + 2 * (j % 2)
                pg, pu = k.bank[pb], k.bank[pb + 1]
                for kc in range(8):
                    P.op('pe', lambda e, kc=kc, jj=jj: e.matmul(pg, lhsT=wt[:, 0, kc, jj * 128:(jj + 1) * 128], rhs=hT[:, kc, :],
                                                               start=(kc == 0), stop=(kc == 7)),
                         reads=['wgu%d' % buf, hTr], writes=['bank%d' % pb])
                for kc in range(8):
                    P.op('pe', lambda e, kc=kc, jj=jj: e.matmul(pu, lhsT=wt[:, 1, kc, jj * 128:(jj + 1) * 128], rhs=hT[:, kc, :],
                                                               start=(kc == 0), stop=(kc == 7)),
                         reads=['wgu%d' % buf, hTr], writes=['bank%d' % (pb + 1)])
                sg = k.sg[j % 2]
                P.op('act', lambda e: e.activation(out=sg, in_=pg, func=AF.Silu), reads=['bank%d' % pb],
                     writes=['sg%d' % (j % 2)])
                P.op('dve', lambda e, j=j: e.tensor_tensor(out=k.actT[:, j, :], in0=pu, in1=sg, op=ALU.mult),
                     reads=['bank%d' % (pb + 1), 'sg%d' % (j % 2)], writes=[('actT', j)])
            if pre:
                front_t(tb + 1, gidx)
        for s in range(4):
            t0 = tb * 512 + s * 128
            pm = [k.bank[5], k.bank[6]]
            for cb in range(2):
                for kk in range(NFC):
                    P.op('pe', lambda e, kk=kk, cb=cb: e.matmul(pm[cb], lhsT=k.actT[:, kk, s * 128:(s + 1) * 128],
                                                               rhs=k.wd_sb[:, kk, cb * 512:(cb + 1) * 512],
                                                               start=(kk == 0), stop=(kk == NFC - 1)),
                         reads=[('actT', kk), 'wd_sb'], writes=['bank%d' % (5 + cb)])
            xt_, xr_ = xtile(tb, s)
            post_norm_residual(k, pm, ['bank5', 'bank6'], 'ffn_post', xt_, xr_)
            P.dma('sp', xdst[t0:t0 + 128, :], xt_, reads=[xr_], writes=[('x', t0 // 128)], stream='xo')


def even_phase(k, l, xsrc, xdst, es):
    nc, P = k.nc, k.P
    e_ = l // 2
    ins = k.ins

    def sb(name, shape, dt):
        return es.enter_context(nc.sbuf_tensor('evs%d_' % l + name, list(shape), dt)).ap()

    w_in_sb = sb('w_in', [128, 8, 1536], BF16)
    w_out_sb = sb('w_out', [128, 8, 1024], BF16)
    poolw_sb = sb('poolw', [128, 4, 128], BF16)
    Dm_sb = sb('Dm', [128, 3, 4, 128], BF16)
    WmT = sb('WmT', [128, 4, 128], BF16)
    wsf = sb('wsf', [128, 4, 128], F32)
    wsb = sb('wsb', [128, 4, 128], BF16)
    tril = sb('tril', [128, 128], F32)
    Bt = sb('Bt', [128, 512], F32)
    lng = sb('lng', [128, 512], F32)
    lnb = sb('lnb', [128, 512], F32)
    psc = sb('psc', [128, 4], F32)
    a_sb = [sb('a%d' % i, [128, 512], BF16) for i in range(2)]
    uT_sb = sb('uT', [128, 4, 128], BF16)
    vg = sb('vg', [128, 512], F32)
    vn = sb('vn', [128, 512], F32)
    vln = sb('vln', [128, 512], BF16)
    diffT = sb('diffT', [128, 4, 128], BF16)
    yaT = sb('yaT', [128, 4, 128], BF16)
    ybT = sb('ybT', [128, 4, 128], BF16)
    mxb = sb('mxb', [128, 512], F32)
    hT1s = [sb('hT1_%d' % i, [128, 8, 128], BF16) for i in range(2)]
    bst = sb('bst', [128, 6], F32)
    mv = sb('mv', [128, 2], F32)
    lrs = sb('lrs', [128, 1], F32)

    P.dma('sp', w_in_sb, k.wb[('ev_w_in', e_)].rearrange('(c p) n -> p c n', p=128), reads=[('wb', 'ev_w_in', e_)],
          writes=['w_in'], stream='w')
    P.dma('sp', w_out_sb, k.wb[('ev_w_out', e_)].rearrange('(c p) n -> p c n', p=128), reads=[('wb', 'ev_w_out', e_)],
          writes=['w_out'], stream='w')
    P.dma('sp', poolw_sb, k.wb[('ev_pool_w', e_)].rearrange('g c d -> c g d'), reads=[('wb', 'ev_pool_w', e_)],
          writes=['poolw'], stream='w')
    P.dma('sp', Dm_sb, ins['c_D'].rearrange('a g s t -> s a g t'), writes=['Dm'], stream='small')
    P.dma('sp', wsf, ins['ev_sgu_w'][e_].rearrange('g t s -> t g s'), writes=['wsf'], stream='small')
    P.dma('sp', Bt, ins['ev_sgu_b'][e_:e_ + 1].rearrange('o g t -> o (g t)').broadcast_to([128, 512]), writes=['Bt'],
          stream='small')
    P.dma('sp', lng, ins['ev_sgu_ln_g'][e_:e_ + 1, :].broadcast_to([128, 512]), writes=['lng'], stream='small')
    P.dma('sp', lnb, ins['ev_sgu_ln_b'][e_:e_ + 1, :].broadcast_to([128, 512]), writes=['lnb'], stream='small')
    with nc.allow_non_contiguous_dma(reason='tiny per-channel scale'):
        P.dma('sp', psc, ins['ev_pool_scale'][e_].rearrange('(g p) -> p g', p=128), writes=['psc'], stream='small')
    P.op('dve', lambda e: e.tensor_scalar(out=tril, in0=k.io_f, scalar1=k.iop[:, 0:1], scalar2=None, op0=ALU.is_le),
         reads=['io_f', 'iop'], writes=['tril'])
    P.op('dve', lambda e: e.tensor_tensor(out=wsb, in0=wsf, in1=tril.unsqueeze(1).broadcast_to([128, 4, 128]), op=ALU.mult),
         reads=['wsf', 'tril'], writes=['wsb'])
    tp = k.bankb[0]
    for g in range(4):
        P.op('pe', lambda e, g=g: e.transpose(out=tp[:, g * 128:(g + 1) * 128], in_=wsb[:, g, :], identity=k.ident),
             reads=['wsb', 'ident'], writes=['bank0'])
    P.op('act', lambda e: e.copy(out=WmT, in_=tp[:, 0:512].rearrange('p (g t) -> p g t', g=4)), reads=['bank0'],
         writes=['WmT'])

    def front_a(i):
        P.dma('sp', k.xt[i % 2], xsrc[i * 128:(i + 1) * 128, :], reads=[('x', i)], writes=['xt%d' % (i % 2)], stream='x')
        norm_a(k, k.xt[i % 2], 'xt%d' % (i % 2), 'mix_pre', i % 2)

    def front_t(i):
        trans_t(k, i % 2, hT1s[i % 2], 'hT1_%d' % (i % 2))

    front_a(0)
    front_t(0)
    for i in range(NT):
        xt = k.xt[i % 2]
        xr = 'xt%d' % (i % 2)
        hT1 = hT1s[i % 2]
        hT1n = 'hT1_%d' % (i % 2)
        a_cur, a_prev = a_sb[i % 2], a_sb[(i + 1) % 2]
        ar, apr = 'a%d' % (i % 2), 'a%d' % ((i + 1) % 2)
        if i + 1 < NT and i >= 1:
            pass
        pa, pu, pv = k.bank[1], k.bank[2], k.bank[3]
        for kc in range(8):
            P.op('pe', lambda e, kc=kc: e.matmul(pa, lhsT=hT1[:, kc, :], rhs=w_in_sb[:, kc, 0:512], start=(kc == 0),
                                                stop=(kc == 7)), reads=[hT1n, 'w_in'], writes=['bank1'])
        P.op('act', lambda e: e.copy(out=a_cur, in_=pa), reads=['bank1'], writes=[ar])
        for kc in range(8):
            P.op('pe', lambda e, kc=kc: e.matmul(pv, lhsT=hT1[:, kc, :], rhs=w_in_sb[:, kc, 1024:1536], start=(kc == 0),
                                                stop=(kc == 7)), reads=[hT1n, 'w_in'], writes=['bank3'])
        P.op('act', lambda e: e.activation(out=vg, in_=pv, func=AF.Gelu_apprx_tanh), reads=['bank3'], writes=['vg'])
        for c in range(4):
            for kc in range(8):
                P.op('pe', lambda e, kc=kc, c=c: e.matmul(pu[:, c * 128:(c + 1) * 128],
                                                         lhsT=w_in_sb[:, kc, 512 + c * 128:512 + (c + 1) * 128],
                                                         rhs=hT1[:, kc, :], start=(kc == 0), stop=(kc == 7)),
                     reads=[hT1n, 'w_in'], writes=['bank2'])
        P.op('act', lambda e: e.activation(out=uT_sb, in_=pu.rearrange('p (c t) -> p c t', c=4), func=AF.Gelu_apprx_tanh),
             reads=['bank2'], writes=['uT'])
        if i + 1 < NT:
            front_a(i + 1)
        P.op('dve', lambda e: e.bn_stats(out=bst, in_=vg), reads=['vg'], writes=['bst'])
        P.op('dve', lambda e: e.bn_aggr(out=mv, in_=bst), reads=['bst'], writes=['mv'])
        P.op('dve', lambda e: e.tensor_scalar(out=lrs, in0=mv[:, 1:2], scalar1=1e-5, scalar2=None, op0=ALU.add),
             reads=['mv'], writes=['lrs'])
        P.op('act', lambda e: e.activation(out=lrs, in_=lrs, func=AF.Sqrt), reads=['lrs'], writes=['lrs'])
        P.op('dve', lambda e: e.reciprocal(out=lrs, in_=lrs), reads=['lrs'], writes=['lrs'])
        P.op('dve', lambda e: e.tensor_scalar(out=vn, in0=vg, scalar1=mv[:, 0:1], scalar2=lrs[:, 0:1], op0=ALU.subtract,
                                              op1=ALU.mult), reads=['vg', 'mv', 'lrs'], writes=['vn'])
        P.op('pool', lambda e: e.tensor_tensor(out=vn, in0=vn, in1=lng, op=ALU.mult), reads=['vn', 'lng'], writes=['vn'])
        P.op('pool', lambda e: e.tensor_tensor(out=vln, in0=vn, in1=lnb, op=ALU.add), reads=['vn', 'lnb'], writes=['vln'])
        pd_ = k.bank[4]
        for g in range(4):
            first = True
            sl = slice(g * 128, (g + 1) * 128)
            P.op('pe', lambda e, g=g, sl=sl: e.matmul(pd_[:, sl], lhsT=a_cur[:, sl], rhs=Dm_sb[:, 0 if i == 0 else 1, g, :],
                                                     start=True, stop=(i == 0)), reads=[ar, 'Dm'], writes=['bank4'])
            if i > 0:
                P.op('pe', lambda e, g=g, sl=sl: e.matmul(pd_[:, sl], lhsT=a_prev[:, sl], rhs=Dm_sb[:, 2, g, :],
                                                         start=False, stop=True), reads=[apr, 'Dm'], writes=['bank4'])
        P.op('dve', lambda e: e.tensor_copy(out=diffT, in_=pd_.rearrange('p (g t) -> p g t', g=4)), reads=['bank4'],
             writes=['diffT'])
        pya = k.bank[5]
        for g in range(4):
            P.op('pe', lambda e, g=g: e.matmul(pya[:, g * 128:(g + 1) * 128], lhsT=poolw_sb[:, g, :], rhs=diffT[:, g, :],
                                              start=True, stop=True), reads=['poolw', 'diffT'], writes=['bank5'])
        P.op('dve', lambda e: e.tensor_tensor(out=yaT, in0=pya.rearrange('p (g t) -> p g t', g=4),
                                              in1=psc.unsqueeze(2).broadcast_to([128, 4, 128]), op=ALU.mult),
             reads=['bank5', 'psc'], writes=['yaT'])
        pmx = k.bank[4]
        for g in range(4):
            P.op('pe', lambda e, g=g: e.matmul(pmx[:, g * 128:(g + 1) * 128], lhsT=vln[:, g * 128:(g + 1) * 128],
                                              rhs=WmT[:, g, :], start=True, stop=True), reads=['vln', 'WmT'],
                 writes=['bank4'])
        P.op('dve', lambda e: e.tensor_tensor(out=mxb, in0=pmx, in1=Bt, op=ALU.add), reads=['bank4', 'Bt'],
             writes=['mxb'])
        P.op('pool', lambda e: e.tensor_tensor(out=ybT, in0=mxb.rearrange('p (g t) -> p g t', g=4), in1=uT_sb, op=ALU.mult),
             reads=['mxb', 'uT'], writes=['ybT'])
        if i + 1 < NT:
            front_t(i + 1)
        pm = [k.bank[6], k.bank[7]]
        for cb in range(2):
            for kc in range(8):
                lh = yaT[:, kc, :] if kc < 4 else ybT[:, kc - 4, :]
                P.op('pe', lambda e, kc=kc, cb=cb, lh=lh: e.matmul(pm[cb], lhsT=lh, rhs=w_out_sb[:, kc, cb * 512:(cb + 1) * 512],
                                                                  start=(kc == 0), stop=(kc == 7)),
                     reads=['yaT', 'ybT', 'w_out'], writes=['bank%d' % (6 + cb)])
        post_norm_residual(k, pm, ['bank6', 'bank7'], 'mix_post', xt, xr)
        P.dma('sp', xdst[i * 128:(i + 1) * 128, :], xt, reads=[xr], writes=[('x', i)], stream='xo')


def odd_phase(k, l, xsrc, xdst, es):
    from contextlib import ExitStack
    nc, P = k.nc, k.P
    o_ = l // 2
    ins = k.ins
    PI = 3.14159265358979

    def sbs(stack, name, shape, dt):
        return stack.enter_context(nc.sbuf_tensor('od%d_' % l + name, list(shape), dt)).ap()

    def sb(name, shape, dt):
        return sbs(es, name, shape, dt)

    qd = k.dram('qd%d' % l, [T, 512], BF16)
    ydd = k.dram('ydd%d' % l, [T, 512], BF16)
    KE = sb('KE', [128, 2, T], BF16)
    kwT = sb('kwT', [64, 2, T], BF16)
    kcvd = k.dram('kcvd%d' % l, [4, 64, T], BF16)
    ropeS = k.dram('ropeS%d' % l, [128, NT, 72], F32)
    ropeC = k.dram('ropeC%d' % l, [128, NT, 72], F32)
    vs_aug = sb('vs_aug', [128, NT, 2, 65], BF16)
    vw_aug = sb('vw_aug', [128, NT, 2, 65], BF16)
    gsig = sb('gsig', [128, NT, 24], F32)
    kcmpT = sb('kcmpT', [64, 2, 256], BF16)
    vcmp_aug = sb('vcmp', [128, 2, 2, 65], BF16)
    tri = sb('tri', [128, 128], F32)
    ntri = sb('ntri', [128, 128], F32)
    cm = sb('cm', [128, 128], F32)
    keep = sb('keep', [128, 128], F32)
    addc = sb('addc', [128, 128], F32)
    cts = sb('cts', [128, 2, 64], BF16)
    decT = sb('decT', [128, 512], F32)
    xi = sb('xi', [128, 512], F32)
    zeta = sb('zeta', [128, 4], F32)
    gch = sb('gch', [128, 4], F32)
    gng = sb('gng', [128, 512], F32)
    for nm, t_, src in [('tri', tri, 'c_tri'), ('cm', cm, 'c_cm'), ('keep', keep, 'c_keep'), ('addc', addc, 'c_add'),
                        ('decT', decT, 'c_decT'), ('xi', xi, 'c_xi'), ('zeta', zeta, 'c_zeta'), ('gch', gch, 'c_gch')]:
        P.dma('sp', t_, ins[src], writes=[nm], stream='small')
    P.dma('sp', cts, ins['c_cts'].rearrange('c n j -> n c j'), writes=['cts'], stream='small')
    P.dma('sp', KE[64:128, 0, :], ins['c_E'], writes=['KE'], stream='small')
    P.dma('sp', KE[64:128, 1, :], ins['c_E'], writes=['KE'], stream='small')
    P.dma('sp', gng, ins['od_ret_gn_g'][o_:o_ + 1, :].broadcast_to([128, 512]), writes=['gng'], stream='small')
    P.op('dve', lambda e: e.tensor_scalar(out=ntri, in0=tri, scalar1=-1.0, scalar2=1.0, op0=ALU.mult, op1=ALU.add),
         reads=['tri'], writes=['ntri'])
    P.op('pool', lambda e: e.memset(vs_aug, 1.0), writes=['vs_aug'])
    P.op('pool', lambda e: e.memset(vw_aug, 1.0), writes=['vw_aug'])
    P.op('pool', lambda e: e.memset(vcmp_aug, 1.0), writes=['vcmp'])
    with ExitStack() as ts:
        posi = sbs(ts, 'posi', [128, NT], I32)
        sinT = sbs(ts, 'sinT', [128, NT, 72], F32)
        cosT = sbs(ts, 'cosT', [128, NT, 72], F32)
        posf = sbs(ts, 'posf', [128, NT], F32)
        invf = sbs(ts, 'invf', [128, 72], F32)
        ang = sbs(ts, 'ang', [128, NT, 72], F32)
        arg = sbs(ts, 'arg', [128, NT, 72], F32)
        kf = sbs(ts, 'kf', [128, NT, 72], F32)
        ki = sbs(ts, 'ki', [128, NT, 72], I32)
        with nc.allow_non_contiguous_dma(reason='positions to token-on-partition layout'):
            P.dma('sp', posi, ins['positions'].rearrange('o (i p) -> p (o i)', p=128), writes=['posi'], stream='small')
        P.dma('sp', invf, ins['c_invf'].broadcast_to([128, 72]), writes=['invf'], stream='small')
        P.op('dve', lambda e: e.tensor_copy(out=posf, in_=posi), reads=['posi'], writes=['posf'])
        P.op('dve', lambda e: e.tensor_tensor(out=ang, in0=posf.unsqueeze(2).broadcast_to([128, NT, 72]),
                                              in1=invf.unsqueeze(1).broadcast_to([128, NT, 72]), op=ALU.mult),
             reads=['posf', 'invf'], writes=['ang'])
        for shift, dst, dn in [(0.0, sinT, 'sinT'), (PI / 2, cosT, 'cosT')]:
            P.op('dve', lambda e, shift=shift: e.tensor_scalar(out=arg, in0=ang, scalar1=shift, scalar2=None, op0=ALU.add),
                 reads=['ang'], writes=['arg'])
            P.op('dve', lambda e: e.tensor_scalar(out=kf, in0=arg, scalar1=1.0 / (2 * PI), scalar2=None, op0=ALU.mult),
                 reads=['arg'], writes=['kf'])
            P.op('dve', lambda e: e.tensor_copy(out=ki, in_=kf), reads=['kf'], writes=['ki'])
            P.op('dve', lambda e: e.tensor_copy(out=kf, in_=ki), reads=['ki'], writes=['kf'])
            P.op('dve', lambda e: e.scalar_tensor_tensor(out=arg, in0=kf, scalar=-6.28125, in1=arg, op0=ALU.mult, op1=ALU.add),
                 reads=['kf', 'arg'], writes=['arg'])
            P.op('dve', lambda e: e.scalar_tensor_tensor(out=arg, in0=kf, scalar=-(2 * PI - 6.28125), in1=arg, op0=ALU.mult,
                                                         op1=ALU.add), reads=['kf', 'arg'], writes=['arg'])
            P.op('dve', lambda e: e.tensor_scalar(out=arg, in0=arg, scalar1=3.1415925, scalar2=-3.1415925, op0=ALU.min,
                                                  op1=ALU.max), reads=['arg'], writes=['arg'])
            P.op('act', lambda e, dst=dst: e.activation(out=dst, in_=arg, func=AF.Sin), reads=['arg'], writes=[dn])
        P.dma('sp', ropeS, sinT, reads=['sinT'], writes=['ropeS'], stream='xo')
        P.dma('sp', ropeC, cosT, reads=['cosT'], writes=['ropeC'], stream='xo')
        P.barrier()

    with ExitStack() as s1:
        w_in_sb = sbs(s1, 'w_in', [128, 8, 3352], BF16)
        hT1s = [sbs(s1, 'hT1_%d' % j, [128, 8, 128], BF16) for j in range(2)]
        cur = {}
        csb = [sbs(s1, 'csb%d' % j, [128, 2, 72], F32) for j in range(2)]
        kcv_t = sbs(s1, 'kcv_t', [64, 4, 128], BF16)
        zq = sbs(s1, 'zq', [128, 512], F32)
        qb = sbs(s1, 'qb', [128, 512], BF16)
        zkv = sbs(s1, 'zkv', [128, 768], F32)
        kvb = sbs(s1, 'kvb', [128, 768], BF16)
        rt = [sbs(s1, 'rt%d' % j, [128, 512], F32) for j in range(4)]
        zrq = sbs(s1, 'zrq', [128, 512], F32)
        zrk = sbs(s1, 'zrk', [128, 512], F32)
        rqb = sbs(s1, 'rqb', [128, 512], BF16)
        rkb = sbs(s1, 'rkb', [128, 512], BF16)
        rkr = sbs(s1, 'rkr', [128, 512], F32)
        rkz = sbs(s1, 'rkz', [128, 512], BF16)
        rvb = sbs(s1, 'rvb', [128, 512], BF16)
        gsl = sbs(s1, 'gsl', [128, 512], F32)
        qkT = sbs(s1, 'qkT', [128, 8, 128], BF16)
        qxiT = sbs(s1, 'qxiT', [128, 512], BF16)
        STs = sbs(s1, 'STs', [128, 512], BF16)
        state = sbs(s1, 'state', [128, 512], F32)
        stbf = sbs(s1, 'stbf', [128, 512], BF16)
        bst4 = sbs(s1, 'bst4', [128, 4, 6], F32)
        mv4 = sbs(s1, 'mv4', [128, 4, 2], F32)
        rs4 = sbs(s1, 'rs4', [128, 4], F32)
        on = sbs(s1, 'on', [128, 512], F32)
        ydb = sbs(s1, 'ydb', [128, 512], BF16)
        P.dma('sp', w_in_sb, k.wb[('od_w_in', o_)].rearrange('(c p) n -> p c n', p=128), reads=[('wb', 'od_w_in', o_)],
              writes=['w_in'], stream='w')
        P.op('dve', lambda e: e.memset(state, 0.0), writes=['state'])

        def proj(bank, c0, c1):
            for kc in range(8):
                P.op('pe', lambda e, kc=kc: e.matmul(k.bank[bank][:, 0:c1 - c0], lhsT=cur['hT'][:, kc, :], rhs=w_in_sb[:, kc, c0:c1],
                                                    start=(kc == 0), stop=(kc == 7)), reads=[cur['hTn'], 'w_in'],
                     writes=['bank%d' % bank])

        def rope(src, dst, nh, hd, half, c_, s_, csn, sname, dname):
            sv = src.rearrange('p (h d) -> p h d', h=nh)
            dv = dst.rearrange('p (h d) -> p h d', h=nh)
            x1, x2 = sv[:, :, 0:half], sv[:, :, half:2 * half]
            cb = c_.unsqueeze(1).broadcast_to([128, nh, half])
            sb_ = s_.unsqueeze(1).broadcast_to([128, nh, half])
            t = [r_[:, 0:nh * half].rearrange('p (h d) -> p h d', h=nh) for r_ in rt]
            P.op('dve', lambda e: e.tensor_tensor(out=t[0], in0=x1, in1=cb, op=ALU.mult), reads=[sname, csn], writes=['rt0'])
            P.op('pool', lambda e: e.tensor_tensor(out=t[1], in0=x2, in1=sb_, op=ALU.mult), reads=[sname, csn], writes=['rt1'])
            P.op('dve', lambda e: e.tensor_tensor(out=t[2], in0=x2, in1=cb, op=ALU.mult), reads=[sname, csn], writes=['rt2'])
            P.op('pool', lambda e: e.tensor_tensor(out=t[3], in0=x1, in1=sb_, op=ALU.mult), reads=[sname, csn], writes=['rt3'])
            if 2 * half < hd:
                P.op('act', lambda e: e.copy(out=dst, in_=src), reads=[sname], writes=[dname])
            P.op('dve', lambda e: e.tensor_tensor(out=dv[:, :, 0:half], in0=t[0], in1=t[1], op=ALU.subtract),
                 reads=['rt0', 'rt1'], writes=[dname])
            P.op('pool', lambda e: e.tensor_tensor(out=dv[:, :, half:2 * half], in0=t[2], in1=t[3], op=ALU.add),
                 reads=['rt2', 'rt3'], writes=[dname])

        def o1_front_a(i):
            P.dma('sp', k.xt[i % 2], xsrc[i * 128:(i + 1) * 128, :], reads=[('x', i)], writes=['xt%d' % (i % 2)], stream='x')
            norm_a(k, k.xt[i % 2], 'xt%d' % (i % 2), 'mix_pre', i % 2)

        def o1_front_t(i):
            trans_t(k, i % 2, hT1s[i % 2], 'hT1_%d' % (i % 2))

        o1_front_a(0)
        o1_front_t(0)
        for i in range(NT):
            cur['hT'] = hT1s[i % 2]
            cur['hTn'] = 'hT1_%d' % (i % 2)
            cs_ = csb[i % 2]
            csn = 'csb%d' % (i % 2)
            P.dma('sp', cs_[:, 0, :], ropeC[:, i, :], reads=['ropeC'], writes=[csn], stream='small')
            P.dma('sp', cs_[:, 1, :], ropeS[:, i, :], reads=['ropeS'], writes=[csn], stream='small')
            cn, sn = cs_[:, 0, 0:8], cs_[:, 1, 0:8]
            cr, sr = cs_[:, 0, 8:72], cs_[:, 1, 8:72]
            proj(1, 0, 512)
            P.op('act', lambda e: e.activation(out=zq, in_=k.bank[1], func=AF.Copy, scale=0.125), reads=['bank1'], writes=['zq'])
            rope(zq, qb, 8, 64, 8, cn, sn, csn, 'zq', 'qb')
            P.dma('sp', qd[i * 128:(i + 1) * 128, :], qb, reads=['qb'], writes=[('qd', i)], stream='xo')
            proj(2, 512, 1024)
            proj(3, 1024, 1304)
            P.op('act', lambda e: e.copy(out=zkv[:, 0:512], in_=k.bank[2]), reads=['bank2'], writes=['zkv'])
            P.op('act', lambda e: e.copy(out=zkv[:, 512:768], in_=k.bank[3][:, 0:256]), reads=['bank3'], writes=['zkv'])
            P.op('act', lambda e: e.activation(out=gsig[:, i, :], in_=k.bank[3][:, 256:280], func=AF.Sigmoid),
                 reads=['bank3'], writes=[('gsig', i)])
            kv4 = zkv.rearrange('p (a b g d) -> p a b g d', a=3, b=2, g=2)[:, :, 0, :, :]
            x1, x2 = kv4[:, :, :, 0:8], kv4[:, :, :, 8:16]
            cb = cn.unsqueeze(1).unsqueeze(1).broadcast_to([128, 3, 2, 8])
            sb_ = sn.unsqueeze(1).unsqueeze(1).broadcast_to([128, 3, 2, 8])
            t = [r_[:, 0:48].rearrange('p (a g d) -> p a g d', a=3, g=2) for r_ in rt]
            P.op('dve', lambda e: e.tensor_tensor(out=t[0], in0=x1, in1=cb, op=ALU.mult), reads=['zkv', csn], writes=['rt0'])
            P.op('dve', lambda e: e.tensor_tensor(out=t[1], in0=x2, in1=sb_, op=ALU.mult), reads=['zkv', csn], writes=['rt1'])
            P.op('dve', lambda e: e.tensor_tensor(out=t[2], in0=x2, in1=cb, op=ALU.mult), reads=['zkv', csn], writes=['rt2'])
            P.op('dve', lambda e: e.tensor_tensor(out=t[3], in0=x1, in1=sb_, op=ALU.mult), reads=['zkv', csn], writes=['rt3'])
            P.op('dve', lambda e: e.tensor_tensor(out=x1, in0=t[0], in1=t[1], op=ALU.subtract), reads=['rt0', 'rt1'], writes=['zkv'])
            P.op('dve', lambda e: e.tensor_tensor(out=x2, in0=t[2], in1=t[3], op=ALU.add), reads=['rt2', 'rt3'], writes=['zkv'])
            P.op('act', lambda e: e.copy(out=kvb, in_=zkv), reads=['zkv'], writes=['kvb'])
            P.op('pool', lambda e: e.tensor_copy(out=vs_aug[:, i, :, 0:64], in_=kvb[:, 384:512].rearrange('p (g d) -> p g d', g=2)),
                 reads=['kvb'], writes=['vs_aug'])
            P.op('pool', lambda e: e.tensor_copy(out=vw_aug[:, i, :, 0:64], in_=kvb[:, 640:768].rearrange('p (g d) -> p g d', g=2)),
                 reads=['kvb'], writes=['vw_aug'])
            tp = k.bankb[0]
            srcs = [0, 64, 128, 192, 512, 576, 256, 320]
            for j, c0 in enumerate(srcs):
                P.op('pe', lambda e, j=j, c0=c0: e.transpose(out=tp[0:64, j * 128:(j + 1) * 128], in_=kvb[:, c0:c0 + 64],
                                                            identity=k.ident), reads=['kvb', 'ident'], writes=['bank0'])
            P.op('act', lambda e: e.copy(out=kcv_t, in_=tp[0:64, 0:512].rearrange('p (j t) -> p j t', j=4)), reads=['bank0'],
                 writes=['kcv_t'])
            P.dma('sp', kcvd[:, :, i * 128:(i + 1) * 128].rearrange('j p t -> p j t'), kcv_t, reads=['kcv_t'], writes=['kcvd'],
                  stream='xo')
            P.op('act', lambda e: e.copy(out=kwT[:, :, i * 128:(i + 1) * 128],
                                         in_=tp[0:64, 512:768].rearrange('p (j t) -> p j t', j=2)), reads=['bank0'], writes=['kwT'])
            P.op('act', lambda e: e.copy(out=KE[0:64, :, i * 128:(i + 1) * 128],
                                         in_=tp[0:64, 768:1024].rearrange('p (j t) -> p j t', j=2)), reads=['bank0'], writes=['KE'])
            proj(4, 1304, 1816)
            proj(5, 1816, 2328)
            proj(6, 2328, 2840)
            proj(7, 2840, 3352)
            if i + 1 < NT:
                o1_front_a(i + 1)
            P.op('act', lambda e: e.copy(out=zrq, in_=k.bank[4]), reads=['bank4'], writes=['zrq'])
            P.op('act', lambda e: e.activation(out=zrk, in_=k.bank[5], func=AF.Copy, scale=128.0 ** -0.5), reads=['bank5'],
                 writes=['zrk'])
            P.op('act', lambda e: e.copy(out=rvb, in_=k.bank[6]), reads=['bank6'], writes=['rvb'])
            P.op('act', lambda e: e.activation(out=gsl, in_=k.bank[7], func=AF.Silu), reads=['bank7'], writes=['gsl'])
            rope(zrq, rqb, 4, 128, 64, cr, sr, csn, 'zrq', 'rqb')
            rope(zrk, rkr, 4, 128, 64, cr, sr, csn, 'zrk', 'rkr')
            P.op('act', lambda e: e.copy(out=rkb, in_=rkr), reads=['rkr'], writes=['rkb'])
            P.op('dve', lambda e: e.tensor_tensor(out=rkz.rearrange('p (h d) -> p h d', h=4),
                                                  in0=rkr.rearrange('p (h d) -> p h d', h=4),
                                                  in1=zeta.unsqueeze(2).broadcast_to([128, 4, 128]), op=ALU.mult),
                 reads=['rkr', 'zeta'], writes=['rkz'])
            for h in range(4):
                P.op('pe', lambda e, h=h: e.transpose(out=tp[:, h * 128:(h + 1) * 128], in_=rqb[:, h * 128:(h + 1) * 128],
                                                      identity=k.ident), reads=['rqb', 'ident'], writes=['bank0'])
            for h in range(4):
                P.op('pe', lambda e, h=h: e.transpose(out=tp[:, (4 + h) * 128:(5 + h) * 128], in_=rkb[:, h * 128:(h + 1) * 128],
                                                      identity=k.ident), reads=['rkb', 'ident'], writes=['bank0'])
            P.op('act', lambda e: e.copy(out=qkT, in_=tp.rearrange('p (c t) -> p c t', c=8)), reads=['bank0'], writes=['qkT'])
            for h in range(4):
                P.op('pe', lambda e, h=h: e.matmul(k.bank[1][:, h * 128:(h + 1) * 128], lhsT=qkT[:, 4 + h, :], rhs=qkT[:, h, :],
                                                  start=True, stop=True), reads=['qkT'], writes=['bank1'])
            P.op('dve', lambda e: e.tensor_tensor(out=STs, in0=k.bank[1], in1=decT, op=ALU.mult), reads=['bank1', 'decT'],
                 writes=['STs'])
            P.op('pool', lambda e: e.tensor_tensor(out=qxiT, in0=qkT[:, 0:4, :].rearrange('p h t -> p (h t)'), in1=xi, op=ALU.mult),
                 reads=['qkT', 'xi'], writes=['qxiT'])
            for h in range(4):
                hs = slice(h * 128, (h + 1) * 128)
                P.op('pe', lambda e, hs=hs: e.matmul(k.bank[2][:, hs], lhsT=STs[:, hs], rhs=rvb[:, hs], start=True, stop=(i == 0)),
                     reads=['STs', 'rvb'], writes=['bank2'])
                if i > 0:
                    P.op('pe', lambda e, hs=hs: e.matmul(k.bank[2][:, hs], lhsT=qxiT[:, hs], rhs=stbf[:, hs], start=False, stop=True),
                         reads=['qxiT', 'stbf'], writes=['bank2'])
            for h in range(4):
                hs = slice(h * 128, (h + 1) * 128)
                P.op('pe', lambda e, hs=hs: e.matmul(k.bank[3][:, hs], lhsT=rkz[:, hs], rhs=rvb[:, hs], start=True, stop=True),
                     reads=['rkz', 'rvb'], writes=['bank3'])
            P.op('dve', lambda e: e.tensor_tensor(out=state.rearrange('p (h d) -> p h d', h=4),
                                                  in0=state.rearrange('p (h d) -> p h d', h=4),
                                                  in1=gch.unsqueeze(2).broadcast_to([128, 4, 128]), op=ALU.mult),
                 reads=['state', 'gch'], writes=['state'])
            P.op('dve', lambda e: e.tensor_tensor(out=state, in0=k.bank[3], in1=state, op=ALU.add), reads=['bank3', 'state'],
                 writes=['state'])
            P.op('act', lambda e: e.copy(out=stbf, in_=state), reads=['state'], writes=['stbf'])
            if i + 1 < NT:
                o1_front_t(i + 1)
            for h in range(4):
                P.op('dve', lambda e, h=h: e.bn_stats(out=bst4[:, h, :], in_=k.bank[2][:, h * 128:(h + 1) * 128]),
                     reads=['bank2'], writes=['bst4'])
            for h in range(4):
                P.op('dve', lambda e, h=h: e.bn_aggr(out=mv4[:, h, :], in_=bst4[:, h, :]), reads=['bst4'], writes=['mv4'])
            P.op('dve', lambda e: e.tensor_scalar(out=rs4, in0=mv4[:, :, 1], scalar1=1e-5, scalar2=None, op0=ALU.add),
                 reads=['mv4'], writes=['rs4'])
            P.op('act', lambda e: e.activation(out=rs4, in_=rs4, func=AF.Sqrt), reads=['rs4'], writes=['rs4'])
            P.op('dve', lambda e: e.reciprocal(out=rs4, in_=rs4), reads=['rs4'], writes=['rs4'])
            for h in range(4):
                hs = slice(h * 128, (h + 1) * 128)
                P.op('dve', lambda e, h=h, hs=hs: e.tensor_scalar(out=on[:, hs], in0=k.bank[2][:, hs], scalar1=mv4[:, h, 0:1],
                                                                  scalar2=rs4[:, h:h + 1], op0=ALU.subtract, op1=ALU.mult),
                     reads=['bank2', 'mv4', 'rs4'], writes=['on'])
            P.op('pool', lambda e: e.tensor_tensor(out=on, in0=on, in1=gng, op=ALU.mult), reads=['on', 'gng'], writes=['on'])
            P.op('pool', lambda e: e.tensor_tensor(out=ydb, in0=on, in1=gsl, op=ALU.mult), reads=['on', 'gsl'], writes=['ydb'])
            P.dma('sp', ydd[i * 128:(i + 1) * 128, :], ydb, reads=['ydb'], writes=[('ydd', i)], stream='xo')

        P.barrier()
        s1.close()
        s1c = ExitStack()
        w1 = sbs(s1c, 'w1', [64, 32, 128], BF16)
        w2 = sbs(s1c, 'w2', [128, 64], BF16)
        posf32 = sbs(s1c, 'posf32', [64, 32], F32)
        posT = sbs(s1c, 'posT', [64, 32], BF16)
        cbias = sbs(s1c, 'cbias', [128, 1], F32)
        ghT = sbs(s1c, 'ghT', [128, 256], BF16)
        csrc = sbs(s1c, 'csrc', [64, T], BF16)
        for kind, (n1, n2, npos) in enumerate([('od_cmp_k_w1', 'od_cmp_k_w2', 'od_cmp_k_pos'),
                                               ('od_cmp_v_w1', 'od_cmp_v_w2', 'od_cmp_v_pos')]):
            P.dma('sp', w1, k.wb[(n1, o_)].rearrange('(l d) j -> d l j', d=64), reads=[('wb', n1, o_)], writes=['w1'], stream='w')
            P.dma('sp', w2, k.wb[(n2, o_)], reads=[('wb', n2, o_)], writes=['w2'], stream='w')
            with nc.allow_non_contiguous_dma(reason='tiny pos-emb transpose'):
                P.dma('sp', posf32, ins[npos][o_].rearrange('l d -> d l'), writes=['posf32'], stream='small')
            P.op('dve', lambda e: e.tensor_copy(out=posT, in_=posf32), reads=['posf32'], writes=['posT'])
            for g in range(2):
                P.dma('sp', csrc, kcvd[2 * kind + g], reads=['kcvd'], writes=['csrc'], stream='w')
                srcv = csrc.rearrange('p (n s) -> p n s', s=16)
                hb_ = k.bank[1]
                for l_ in range(32):
                    P.op('pe', lambda e, l_=l_: e.matmul(hb_[:, 0:255], lhsT=w1[:, l_, :],
                                                        rhs=(srcv[:, 0:255, l_] if l_ < 16 else srcv[:, 1:256, l_ - 16]),
                                                        start=(l_ == 0), stop=(l_ == 31)),
                         reads=['w1', 'csrc'], writes=['bank1'])
                for l_ in range(32):
                    P.op('pe', lambda e, l_=l_: e.matmul(hb_[:, 256:257], lhsT=w1[:, l_, :], rhs=posT[:, l_:l_ + 1],
                                                        start=(l_ == 0), stop=(l_ == 31)), reads=['w1', 'posT'], writes=['bank1'])
                P.op('dve', lambda e: e.tensor_copy(out=cbias, in_=hb_[:, 256:257]), reads=['bank1'], writes=['cbias'])
                P.op('dve', lambda e: e.memset(ghT[:, 255:256], 0.0), writes=['ghT'])
                P.op('act', lambda e: e.activation(out=ghT[:, 0:255], in_=hb_[:, 0:255], func=AF.Gelu_apprx_tanh,
                                                   bias=cbias[:, 0:1]), reads=['bank1', 'cbias'], writes=['ghT'])
                if kind == 0:
                    P.op('pe', lambda e: e.matmul(k.bank[2][0:64, 0:256], lhsT=w2, rhs=ghT, start=True, stop=True),
                         reads=['w2', 'ghT'], writes=['bank2'])
                    P.op('act', lambda e, g=g: e.copy(out=kcmpT[:, g, :], in_=k.bank[2][0:64, 0:256]), reads=['bank2'],
                         writes=['kcmpT'])
                else:
                    for c in range(2):
                        P.op('pe', lambda e, c=c: e.matmul(k.bank[2][:, c * 64:(c + 1) * 64], lhsT=ghT[:, c * 128:(c + 1) * 128],
                                                          rhs=w2, start=True, stop=True), reads=['w2', 'ghT'], writes=['bank2'])
                    P.op('act', lambda e, g=g: e.copy(out=vcmp_aug[:, :, g, 0:64],
                                                      in_=k.bank[2][:, 0:128].rearrange('p (c d) -> p c d', c=2)),
                         reads=['bank2'], writes=['vcmp'])
        P.barrier()
        s1c.close()

    with ExitStack() as s2:
        w_out_sb = sbs(s2, 'w_out', [128, 8, 1024], BF16)
        PT = sbs(s2, 'PT', [128, NT, 512], BF16)
        PW = sbs(s2, 'PW', [128, 5, 512], BF16)
        Pc = sbs(s2, 'Pc', [128, 2, 512], BF16)
        QN = [sbs(s2, 'QN%d' % g, [128, 512], BF16) for g in range(2)]
        qt = [sbs(s2, 'qt%d' % j, [128, 512], BF16) for j in range(2)]
        ydts = [sbs(s2, 'ydt%d' % j, [128, 512], BF16) for j in range(2)]
        negt = sbs(s2, 'negt', [128, 128], BF16)
        expf = sbs(s2, 'expf', [128, 512], F32)
        score = sbs(s2, 'score', [128, 64], F32)
        sc2 = sbs(s2, 'sc2', [128, 64], F32)
        m8a = sbs(s2, 'm8a', [128, 8], F32)
        m8b = sbs(s2, 'm8b', [128, 8], F32)
        thr = sbs(s2, 'thr', [128, 1], F32)
        psl = sbs(s2, 'psl', [128, 64], F32)
        rdenA = [sbs(s2, 'rdenA%d' % g, [128, 4], F32) for g in range(2)]
        rdenB = [sbs(s2, 'rdenB%d' % g, [128, 12], F32) for g in range(2)]
        coefs = [sbs(s2, 'coef%d' % g, [128, 12], F32) for g in range(2)]
        ocw = [sbs(s2, 'ocw%d' % g, [128, 2, 260], F32) for g in range(2)]
        yc = sbs(s2, 'yc', [128, 512], F32)
        ycb = sbs(s2, 'ycb', [128, 512], BF16)
        yT = sbs(s2, 'yT', [128, 8, 128], BF16)
        P.dma('sp', w_out_sb, k.wb[('od_w_out', o_)].rearrange('(c p) n -> p c n', p=128), reads=[('wb', 'od_w_out', o_)],
              writes=['w_out'], stream='w')
        P.op('dve', lambda e: e.memset(negt, 0.0), writes=['negt'])
        tp = k.bankb[0]
        sbank = [0]

        def next_sbank():
            sbank[0] += 1
            return 1 + (sbank[0] % 2)

        def den_view(ap260):
            return ap260.rearrange('p (m e) -> p m e', e=65)[:, :, 64]

        def masked_exp(b, nn, dst, mask_ap, dname):
            if mask_ap is None:
                P.op('act', lambda e: e.activation(out=dst[0:nn], in_=k.bank[b][0:nn, :], func=AF.Exp), reads=['bank%d' % b],
                     writes=[dname])
            else:
                P.op('act', lambda e: e.activation(out=expf[0:nn], in_=k.bank[b][0:nn, :], func=AF.Exp), reads=['bank%d' % b],
                     writes=['expf'])
                P.op('pool', lambda e: e.tensor_tensor(out=dst[0:nn].rearrange('p (m q) -> p m q', m=4),
                                                       in0=expf[0:nn].rearrange('p (m q) -> p m q', m=4),
                                                       in1=mask_ap.unsqueeze(1).broadcast_to([nn, 4, 128]), op=ALU.mult),
                     reads=['expf', 'tri', 'ntri'], writes=[dname])

        def o2_loads(i):
            P.dma('sp', k.xt[i % 2], xsrc[i * 128:(i + 1) * 128, :], reads=[('x', i)], writes=['xt%d' % (i % 2)], stream='x')
            P.dma('sp', qt[i % 2], qd[i * 128:(i + 1) * 128, :], reads=[('qd', i)], writes=['qt%d' % (i % 2)], stream='x')
            P.dma('sp', ydts[i % 2], ydd[i * 128:(i + 1) * 128, :], reads=[('ydd', i)], writes=['ydt%d' % (i % 2)], stream='x')

        def stage_a(i, g):
            qti = qt[i % 2]
            qtn = 'qt%d' % (i % 2)
            off = 62 - 2 * i
            Q = QN[g]
            qn = 'QN%d' % g
            rdA = rdenA[g]
            rdAn = 'rdenA%d' % g
            for m in range(4):
                h = 4 * g + m
                P.op('pe', lambda e, m=m, h=h: e.transpose(out=tp[0:64, m * 128:(m + 1) * 128], in_=qti[:, h * 64:(h + 1) * 64],
                                                          identity=k.ident), reads=[qtn, 'ident'], writes=['bank0'])
            P.op('act', lambda e: e.copy(out=Q[0:64, :], in_=tp[0:64, 0:512]), reads=['bank0'], writes=[qn + 'lo'])
            chunks = [(0, 128)] + ([(1, 127)] if 8 * i + 6 >= 128 else [])
            for (c, nn) in chunks:
                b = next_sbank()
                P.op('pe', lambda e, c=c, nn=nn, b=b: e.matmul(k.bank[b][0:nn, :], lhsT=kcmpT[:, g, c * 128:c * 128 + nn],
                                                              rhs=Q[0:64, :], start=True, stop=True),
                     reads=['kcmpT', qn + 'lo'], writes=['bank%d' % b])
                full = (16 * (c * 128 + nn - 1) + 31 <= 128 * i)
                if full:
                    P.op('act', lambda e, c=c, nn=nn, b=b: e.activation(out=Pc[0:nn, c, :], in_=k.bank[b][0:nn, :], func=AF.Exp),
                         reads=['bank%d' % b], writes=['Pc'])
                else:
                    tv = float(128 * i - 31 - 2048 * c)
                    P.op('act', lambda e, nn=nn, b=b: e.activation(out=expf[0:nn], in_=k.bank[b][0:nn, :], func=AF.Exp),
                         reads=['bank%d' % b], writes=['expf'])
                    P.op('dve', lambda e, c=c, nn=nn, tv=tv: e.scalar_tensor_tensor(
                        out=Pc[0:nn, c, :].rearrange('p (m q) -> p m q', m=4),
                        in0=cm[0:nn].unsqueeze(1).broadcast_to([nn, 4, 128]), scalar=tv,
                        in1=expf[0:nn].rearrange('p (m q) -> p m q', m=4), op0=ALU.is_le, op1=ALU.mult),
                         reads=['expf', 'cm'], writes=['Pc'])
            for m in range(4):
                for ci, (c, nn) in enumerate(chunks):
                    P.op('pe', lambda e, m=m, c=c, nn=nn, ci=ci: e.matmul(k.bank[3][:, m * 65:(m + 1) * 65],
                                                                        lhsT=Pc[0:nn, c, m * 128:(m + 1) * 128],
                                                                        rhs=vcmp_aug[0:nn, c, g, :], start=(ci == 0),
                                                                        stop=(ci == len(chunks) - 1)),
                         reads=['Pc', 'vcmp'], writes=['bank3'])
            for m in range(4):
                for ci, (c, nn) in enumerate(chunks):
                    P.op('pe', lambda e, m=m, c=c, nn=nn, ci=ci: e.matmul(k.bank[4][:, m * 64:(m + 1) * 64],
                                                                        lhsT=Pc[0:nn, c, m * 128:(m + 1) * 128],
                                                                        rhs=cts[0:nn, c, :], start=(ci == 0),
                                                                        stop=(ci == len(chunks) - 1)),
                         reads=['Pc', 'cts'], writes=['bank4'])
            jts = list(range(max(0, i - 4), i + 1))
            for sl_, jt in enumerate(jts):
                b = next_sbank()
                P.op('pe', lambda e, jt=jt, b=b: e.matmul(k.bank[b], lhsT=kwT[:, g, jt * 128:(jt + 1) * 128], rhs=Q[0:64, :],
                                                         start=True, stop=True), reads=['kwT', qn + 'lo'], writes=['bank%d' % b])
                mk = tri if jt == i else (ntri if jt == i - 4 else None)
                masked_exp(b, 128, PW[:, sl_, :], mk, ('PW', sl_))
            for m in range(4):
                for sl_, jt in enumerate(jts):
                    P.op('pe', lambda e, m=m, jt=jt, sl_=sl_: e.matmul(k.bank[6][:, m * 65:(m + 1) * 65],
                                                                      lhsT=PW[:, sl_, m * 128:(m + 1) * 128],
                                                                      rhs=vw_aug[:, jt, g, :], start=(sl_ == 0),
                                                                      stop=(sl_ == len(jts) - 1)),
                         reads=[('PW', sl_), 'vw_aug'], writes=['bank6'])
            P.op('dve', lambda e: e.tensor_scalar(out=rdA, in0=den_view(k.bank[3][:, 0:260]), scalar1=1e-30, scalar2=None,
                                                  op0=ALU.max), reads=['bank3'], writes=[rdAn])
            P.op('dve', lambda e: e.reciprocal(out=rdA, in_=rdA), reads=[rdAn], writes=[rdAn])
            P.op('dve', lambda e: e.tensor_scalar(out=psl, in0=k.bank[4][:, 0:64], scalar1=rdA[:, 0:1], scalar2=None,
                                                  op0=ALU.mult), reads=['bank4', rdAn], writes=['psl'])
            for m in range(1, 4):
                P.op('dve', lambda e, m=m: e.scalar_tensor_tensor(out=psl, in0=k.bank[4][:, m * 64:(m + 1) * 64],
                                                                  scalar=rdA[:, m:m + 1], in1=psl, op0=ALU.mult, op1=ALU.add),
                     reads=['bank4', rdAn, 'psl'], writes=['psl'])
            P.op('act', lambda e: e.copy(out=ocw[g][:, 0, :], in_=k.bank[3][:, 0:260]), reads=['bank3'], writes=['ocw%d' % g])
            P.op('act', lambda e: e.copy(out=ocw[g][:, 1, :], in_=k.bank[6][:, 0:260]), reads=['bank6'], writes=['ocw%d' % g])
            P.op('dve', lambda e: e.tensor_tensor(out=score, in0=psl, in1=keep[:, off:off + 64], op=ALU.mult),
                 reads=['psl', 'keep'], writes=['score'])
            P.op('dve', lambda e: e.tensor_tensor(out=score, in0=score, in1=addc[:, off:off + 64], op=ALU.add),
                 reads=['score', 'addc'], writes=['score'])
            P.op('dve', lambda e: e.memset(score[:, 0:1], 1.0e4), reads=['score'], writes=['score'])
            P.op('dve', lambda e: e.max(out=m8a, in_=score), reads=['score'], writes=['m8a'])
            P.op('dve', lambda e: e.match_replace(out=sc2, in_to_replace=m8a, in_values=score, imm_value=-2.0),
                 reads=['score', 'm8a'], writes=['sc2'])
            P.op('dve', lambda e: e.max(out=m8b, in_=sc2), reads=['sc2'], writes=['m8b'])
            P.op('dve', lambda e: e.tensor_scalar(out=thr, in0=m8b[:, 7:8], scalar1=0.0, scalar2=None, op0=ALU.max),
                 reads=['m8b'], writes=['thr'])
            P.op('dve', lambda e: e.tensor_scalar(out=negt[:, 64:128], in0=score, scalar1=thr[:, 0:1], scalar2=-30000.0,
                                                  op0=ALU.is_lt, op1=ALU.mult), reads=['score', 'thr'], writes=['negt'])
            P.op('pe', lambda e: e.transpose(out=tp[:, 512:640], in_=negt, identity=k.ident), reads=['negt', 'ident'],
                 writes=['bank0'])
            P.op('act', lambda e: e.copy(out=Q[64:128, :].rearrange('p (m q) -> p m q', m=4),
                                         in_=tp[64:128, 512:640].unsqueeze(1).broadcast_to([64, 4, 128])),
                 reads=['bank0'], writes=[qn + 'hi'])

        def stage_b(i, g):
            Q = QN[g]
            qn = 'QN%d' % g
            ob = 5 if g == 0 else 7
            obn = 'bank%d' % ob
            rdB = rdenB[g]
            rdBn = 'rdenB%d' % g
            coef = coefs[g]
            cfn = 'coef%d' % g
            for jt in range(i + 1):
                b = next_sbank()
                P.op('pe', lambda e, jt=jt, b=b: e.matmul(k.bank[b], lhsT=KE[:, g, jt * 128:(jt + 1) * 128], rhs=Q, start=True,
                                                         stop=True), reads=['KE', qn + 'lo', qn + 'hi'], writes=['bank%d' % b])
                masked_exp(b, 128, PT[:, jt, :], tri if jt == i else None, ('PT', jt))
            for m in range(4):
                for jt in range(i + 1):
                    P.op('pe', lambda e, m=m, jt=jt: e.matmul(k.bank[ob][:, m * 65:(m + 1) * 65],
                                                             lhsT=PT[:, jt, m * 128:(m + 1) * 128], rhs=vs_aug[:, jt, g, :],
                                                             start=(jt == 0), stop=(jt == i)),
                         reads=[('PT', jt), 'vs_aug'], writes=[obn])
            P.op('dve', lambda e: e.tensor_copy(out=rdB[:, 0:4], in_=rdenA[g]), reads=['rdenA%d' % g], writes=[rdBn])
            P.op('dve', lambda e: e.tensor_scalar(out=rdB[:, 4:8], in0=den_view(k.bank[ob][:, 0:260]), scalar1=1e-30, scalar2=None,
                                                  op0=ALU.max), reads=[obn], writes=[rdBn])
            P.op('dve', lambda e: e.tensor_scalar(out=rdB[:, 8:12], in0=den_view(ocw[g][:, 1, :]), scalar1=1e-30, scalar2=None,
                                                  op0=ALU.max), reads=['ocw%d' % g], writes=[rdBn])
            P.op('dve', lambda e: e.reciprocal(out=rdB[:, 4:12], in_=rdB[:, 4:12]), reads=[rdBn], writes=[rdBn])
            P.op('dve', lambda e: e.tensor_tensor(out=coef.rearrange('p (b m) -> p b m', b=3),
                                                  in0=rdB.rearrange('p (b m) -> p b m', b=3),
                                                  in1=gsig[:, i, g * 12:(g + 1) * 12].rearrange('p (m b) -> p b m', b=3),
                                                  op=ALU.mult), reads=[rdBn, ('gsig', i)], writes=[cfn])
            for m in range(4):
                h = 4 * g + m
                ym = yc[:, h * 64:(h + 1) * 64]
                P.op('pool', lambda e, m=m, ym=ym: e.tensor_scalar(out=ym, in0=ocw[g][:, 0, m * 65:m * 65 + 64],
                                                                   scalar1=coef[:, m:m + 1], scalar2=0.0, op0=ALU.mult, op1=ALU.add),
                     reads=['ocw%d' % g, cfn], writes=[('yc', h)])
                P.op('dve', lambda e, m=m, ym=ym: e.scalar_tensor_tensor(out=ym, in0=k.bank[ob][:, m * 65:m * 65 + 64],
                                                                         scalar=coef[:, 4 + m:5 + m], in1=ym, op0=ALU.mult,
                                                                         op1=ALU.add), reads=[obn, cfn, ('yc', h)],
                     writes=[('yc', h)])
                P.op('dve', lambda e, m=m, ym=ym: e.scalar_tensor_tensor(out=ym, in0=ocw[g][:, 1, m * 65:m * 65 + 64],
                                                                         scalar=coef[:, 8 + m:9 + m], in1=ym, op0=ALU.mult,
                                                                         op1=ALU.add), reads=['ocw%d' % g, cfn, ('yc', h)],
                     writes=[('yc', h)])

        def out_proj(i):
            xt = k.xt[i % 2]
            xr = 'xt%d' % (i % 2)
            ydt = ydts[i % 2]
            ydn = 'ydt%d' % (i % 2)
            P.op('act', lambda e: e.copy(out=ycb, in_=yc), reads=[('yc', h) for h in range(8)], writes=['ycb'])
            for c in range(4):
                P.op('pe', lambda e, c=c: e.transpose(out=tp[:, c * 128:(c + 1) * 128], in_=ycb[:, c * 128:(c + 1) * 128],
                                                      identity=k.ident), reads=['ycb', 'ident'], writes=['bank0'])
            for c in range(4):
                P.op('pe', lambda e, c=c: e.transpose(out=tp[:, (4 + c) * 128:(5 + c) * 128], in_=ydt[:, c * 128:(c + 1) * 128],
                                                      identity=k.ident), reads=[ydn, 'ident'], writes=['bank0'])
            P.op('act', lambda e: e.copy(out=yT, in_=tp.rearrange('p (c t) -> p c t', c=8)), reads=['bank0'], writes=['yT'])
            pm = [k.bank[3], k.bank[4]]
            for cb in range(2):
                for kc in range(8):
                    P.op('pe', lambda e, kc=kc, cb=cb: e.matmul(pm[cb], lhsT=yT[:, kc, :], rhs=w_out_sb[:, kc, cb * 512:(cb + 1) * 512],
                                                               start=(kc == 0), stop=(kc == 7)), reads=['yT', 'w_out'],
                         writes=['bank%d' % (3 + cb)])
            post_norm_residual(k, pm, ['bank3', 'bank4'], 'mix_post', xt, xr)
            P.dma('sp', xdst[i * 128:(i + 1) * 128, :], xt, reads=[xr], writes=[('x', i)], stream='xo')

        units = [(i, g) for i in range(NT) for g in range(2)]
        o2_loads(0)
        stage_a(*units[0])
        for n, (i, g) in enumerate(units):
            if n + 1 < len(units):
                ni, ng = units[n + 1]
                if ng == 0:
                    o2_loads(ni)
                stage_a(ni, ng)
            stage_b(i, g)
            if g == 1:
                out_proj(i)

def make_consts():
    c = {}
    Dm = np.zeros((3, 4, 128, 128), np.float32)
    for g, w in enumerate(POOL_WINDOWS):
        for t in range(128):
            lo = max(t + 1 - w, 0)
            for s in range(lo, t + 1):
                Dm[0, g, s, t] += 1.0 / (t + 1 - lo)
            Dm[0, g, t, t] -= 1.0
            for s in range(t + 1 - w, t + 1):
                if s >= 0:
                    Dm[1, g, s, t] += 1.0 / w
                else:
                    Dm[2, g, s + 128, t] += 1.0 / w
            Dm[1, g, t, t] -= 1.0
    c['c_D'] = Dm.astype(ml_dtypes.bfloat16)
    bf = ml_dtypes.bfloat16
    invf = np.concatenate([1.0 / (500000.0 ** (np.arange(0, 16, 2, dtype=np.float32) / 16)),
                           1.0 / (10000.0 ** (np.arange(0, 128, 2, dtype=np.float32) / 128))]).astype(np.float32)
    c['c_invf'] = invf.reshape(1, 72)
    lg = np.log1p(-np.exp2(-5.0 - np.arange(4, dtype=np.float64)))
    idx = np.arange(128, dtype=np.float64)
    rel = idx[None, :] - idx[:, None]
    dec = np.where((rel >= 0)[:, None, :], np.exp(np.maximum(rel, 0)[:, None, :] * lg[None, :, None]), 0.0)
    c['c_decT'] = dec.reshape(128, 512).astype(np.float32)
    xi = np.exp((idx + 1.0)[None, :] * lg[:, None])
    c['c_xi'] = np.broadcast_to(xi.reshape(1, 512), (128, 512)).astype(np.float32).copy()
    c['c_zeta'] = np.exp((127 - idx)[:, None] * lg[None, :]).astype(np.float32)
    c['c_gch'] = np.broadcast_to(np.exp(128 * lg)[None, :], (128, 4)).astype(np.float32).copy()
    p = np.arange(128)
    c['c_tri'] = (p[:, None] <= p[None, :]).astype(np.float32)
    c['c_cm'] = (16.0 * p[:, None] - p[None, :]).astype(np.float32)
    cq = (p >= 64).astype(np.int64)[:, None]
    jj = np.arange(128)[None, :]
    c['c_keep'] = (jj <= 60 + cq).astype(np.float32)
    add = np.zeros((128, 128), np.float32)
    add[(jj == 61 + cq) | (jj == 62 + cq)] = 1.0e4
    add[jj > 62 + cq] = -1.0
    c['c_add'] = add
    n = np.arange(256)
    cs = n * 16
    ss = np.arange(64) * 64
    cts = ((cs[:, None] < ss[None, :] + 64) & (cs[:, None] + 32 > ss[None, :])).astype(np.float32)
    cts[255] = 0
    c['c_cts'] = cts.reshape(2, 128, 64).astype(bf)
    c['c_E'] = (np.arange(4096)[None, :] // 64 == np.arange(64)[:, None]).astype(bf)
    return c


INPUT_NAMES = ["x", "positions", "ln_mix_pre", "ln_mix_post", "ln_ffn_pre", "ln_ffn_post", "ffn_w_gate", "ffn_w_up",
               "ffn_w_down", "ev_w_in", "ev_pool_w", "ev_pool_scale", "ev_sgu_ln_g", "ev_sgu_ln_b", "ev_sgu_w",
               "ev_sgu_b", "ev_w_out", "od_w_in", "od_cmp_k_pos", "od_cmp_k_w1", "od_cmp_k_w2", "od_cmp_v_pos",
               "od_cmp_v_w1", "od_cmp_v_w2", "od_ret_gn_g", "od_w_out"]


def build(shapes, consts, layers=(0, 1, 2, 3), phases=('mix', 'ffn')):
    from contextlib import ExitStack
    nc = bass.Bass("TRN2", target_bir_lowering=False)
    ins = {}
    for n in INPUT_NAMES:
        shp = list(shapes[n])
        if n == 'x':
            shp = [T, D]
        if n == 'positions':
            shp = [1, T]
        ins[n] = nc.dram_tensor(n, shp, I32 if n == 'positions' else F32, kind="ExternalInput").ap()
    for n, v in consts.items():
        ins[n] = nc.dram_tensor(n, list(v.shape), BF16 if v.dtype == ml_dtypes.bfloat16 else F32, kind="ExternalInput").ap()
    y = nc.dram_tensor("y", [T, D], F32, kind="ExternalOutput").ap()
    k = K(nc, layers)
    setup_common(k, ins)
    k.sb2 = k.sb('ssa', [128, 2], F32)
    P = k.P
    xsrc = ins['x']
    for li, l in enumerate(layers):
        load_gains(k, l)
        if li + 1 < len(layers):
            cast_layer(k, layers[li + 1])
        if 'mix' in phases:
            with ExitStack() as es:
                if l % 2 == 0:
                    even_phase(k, l, xsrc, y, es)
                else:
                    odd_phase(k, l, xsrc, y, es)
                P.barrier()
            xsrc = y
        if 'ffn' in phases:
            with ExitStack() as es:
                def sb(name, shape, dt):
                    return es.enter_context(nc.sbuf_tensor('ffs%d_' % l + name, list(shape), dt)).ap()
                k.wd_sb = sb('wd', [128, NFC, 1024], BF16)
                k.xt8 = [sb('xt8_%d' % i, [128, D], F32) for i in range(8)]
                k.wgu = [sb('wgu%d' % i, [128, 2, 8, 512], BF16) for i in range(2)]
                k.hT2 = [sb('hT%d' % i, [128, 8, 512], BF16) for i in range(2)]
                k.actT = sb('actT', [128, NFC, 512], BF16)
                k.sg = [sb('sg%d' % i, [128, 512], F32) for i in range(2)]
                ffn_phase(k, l, xsrc, y)
                P.barrier()
            xsrc = y
    P.finish()
    return nc


_CACHE = {}


def kernel(**inputs):
    consts = make_consts()
    shapes = {n: inputs[n].shape for n in INPUT_NAMES}
    if 'nc' not in _CACHE:
        _CACHE['nc'] = build(shapes, consts)
    nc = _CACHE['nc']
    in_maps = []
    for c in range(4):
        b = c % 4
        m = {n: np.ascontiguousarray(inputs[n]) for n in INPUT_NAMES if n not in ('x', 'positions')}
        m['x'] = np.ascontiguousarray(inputs['x'][b])
        m['positions'] = np.ascontiguousarray(inputs['positions'][b:b + 1]).astype(np.int32)
        m.update(consts)
        in_maps.append(m)
    res = run_bass_kernel_spmd(nc, in_maps, core_ids=list(range(4)))
    out = np.stack([res.results[b]["y"] for b in range(4)], axis=0)
    return out.astype(np.float32)
```

```python
import numpy as np
import ml_dtypes
import concourse.bass as bass
import concourse.mybir as mybir
from concourse.bass_utils import run_bass_kernel_spmd

F32 = mybir.dt.float32
BF16 = mybir.dt.bfloat16
I32 = mybir.dt.int32
AF = mybir.ActivationFunctionType
ALU = mybir.AluOpType
AX = mybir.AxisListType

import os
SAME_ENG_SYNC = os.environ.get("SES", "1") == "1"


class Prog:
    def __init__(self, nc):
        self.nc = nc
        self.E = {'pe': nc.tensor, 'dve': nc.vector, 'act': nc.scalar, 'pool': nc.gpsimd, 'sp': nc.sync}
        self.semh = {k: nc.alloc_semaphore('s_' + k) for k in ['pe', 'dve', 'act', 'pool']}
        self.cnt = {k: 0 for k in self.semh}
        self.seen = {e: {} for e in self.E}
        self.lastw = {}
        self.readers = {}
        self.nwait = 0
        self.dslot = 0
        self.NSLOT = 32
        self.nins = 0

    def _deps(self, reads, writes):
        deps = {}
        for r in reads:
            w = self.lastw.get(r)
            if w and deps.get(w[0], 0) < w[1]:
                deps[w[0]] = w[1]
            if isinstance(r, str) and r.startswith('bank'):
                for k_, v in self.readers.get(r, {}).items():
                    if deps.get(k_, 0) < v:
                        deps[k_] = v
        for w_ in writes:
            w = self.lastw.get(w_)
            if w and deps.get(w[0], 0) < w[1]:
                deps[w[0]] = w[1]
            for k, v in self.readers.get(w_, {}).items():
                if deps.get(k, 0) < v:
                    deps[k] = v
        return deps

    def _wait(self, eng, deps):
        e = self.E[eng]
        seen = self.seen[eng]
        for k, v in deps.items():
            if k == eng and (eng == 'pe' or not SAME_ENG_SYNC):
                continue
            if seen.get(k, 0) >= v:
                continue
            e.wait_ge(self.semh[k], v)
            seen[k] = v
            self.nwait += 1

    def _record(self, tag, reads, writes):
        for w in writes:
            self.lastw[w] = tag
            self.readers[w] = {}
        for r in reads:
            d = self.readers.setdefault(r, {})
            if d.get(tag[0], 0) < tag[1]:
                d[tag[0]] = tag[1]

    def op(self, eng, fn, reads=(), writes=()):
        self._wait(eng, self._deps(reads, writes))
        ins = fn(self.E[eng])
        self.cnt[eng] += 1
        ins.then_inc(self.semh[eng], 1)
        self.nins += 1
        self._record((eng, self.cnt[eng]), reads, writes)

    def dma(self, q, out, in_, reads=(), writes=(), stream='d0', **kw):
        slot = self.dslot % self.NSLOT
        self.dslot += 1
        key = 'd:%d' % slot
        if key not in self.semh:
            self.semh[key] = self.nc.alloc_semaphore('sd_%d' % slot)
            self.cnt[key] = 0
        e = self.E[q]
        if self.cnt[key] > 0 and self.seen[q].get(key, 0) < self.cnt[key]:
            e.wait_ge(self.semh[key], self.cnt[key])
            self.seen[q][key] = self.cnt[key]
        self._wait(q, self._deps(reads, writes))
        e.dma_start(out=out, in_=in_, **kw).then_inc(self.semh[key], 16)
        self.cnt[key] += 16
        self.nins += 1
        self._record((key, self.cnt[key]), reads, writes)

    def barrier(self):
        for eng in self.E:
            deps = {k: v for k, v in self.cnt.items() if v > 0}
            e = self.E[eng]
            for k, v in deps.items():
                if k == eng and eng == 'pe':
                    continue
                if self.seen[eng].get(k, 0) >= v:
                    continue
                e.wait_ge(self.semh[k], v)
                self.seen[eng][k] = v
        self.lastw = {}
        self.readers = {}

    def finish(self, eng='sp'):
        deps = {k: v for k, v in self.cnt.items() if v > 0}
        e = self.E[eng]
        for k, v in deps.items():
            e.wait_ge(self.semh[k], v)


T = 4096
D = 1024
NT = T // 128
FH = 2816
NFC = FH // 128
DEPTH = 4
POOL_WINDOWS = (2, 4, 8, 16)
EPS = 1e-6


def _flat2(ap, c=1024):
    n = len(ap.shape)
    names = ' '.join('d%d' % i for i in range(n))
    f = ap.rearrange('%s -> (%s)' % (names, names)) if n > 1 else ap
    return f.rearrange('(r c) -> r c', c=c)


class K:
    def __init__(self, nc, layers=(0, 1, 2, 3), Tn=T):
        self.nc = nc
        self.P = Prog(nc)
        self.layers = layers
        self.uid = 0

    def sb(self, name, shape, dt):
        return self.nc.alloc_sbuf_tensor(name, list(shape), dt).ap()

    def dram(self, name, shape, dt, kind="Internal"):
        return self.nc.dram_tensor(name, list(shape), dt, kind=kind).ap()


def cast_copy(k, dst, src, res):
    d2 = _flat2(dst)
    s2 = _flat2(src)
    rows = d2.shape[0]
    r0 = 0
    while r0 < rows:
        r1 = min(rows, r0 + 2048)
        k.P.dma('pool', d2[r0:r1, :], s2[r0:r1, :], writes=[res], stream='cast')
        r0 = r1


def cast_layer(k, l):
    ins = k.ins
    order = []
    if l % 2 == 0:
        order += [('ev_w_in', l // 2), ('ev_pool_w', l // 2), ('ev_w_out', l // 2)]
    else:
        order += [('od_w_in', l // 2), ('od_cmp_k_w1', l // 2), ('od_cmp_k_w2', l // 2), ('od_cmp_v_w1', l // 2),
                  ('od_cmp_v_w2', l // 2), ('od_w_out', l // 2)]
    order += [('ffn_w_gate', l), ('ffn_w_up', l), ('ffn_w_down', l)]
    for name, idx in order:
        src = ins[name][idx]
        dst = k.dram('wb_%s_%d' % (name, idx), src.shape, BF16)
        k.wb[(name, idx)] = dst
        cast_copy(k, dst, src, ('wb', name, idx))


def rstd_from_ssq(k, ssq, rstd, tmp, n, eps, tag):
    P = k.P
    P.op('dve', lambda e: e.tensor_scalar(out=tmp, in0=ssq, scalar1=1.0 / n, scalar2=eps, op0=ALU.mult, op1=ALU.add),
         reads=[tag + 'ssq'], writes=[tag + 'tmp'])
    P.op('act', lambda e: e.activation(out=tmp, in_=tmp, func=AF.Sqrt), reads=[tag + 'tmp'], writes=[tag + 'tmp'])
    P.op('dve', lambda e: e.reciprocal(out=rstd, in_=tmp), reads=[tag + 'tmp'], writes=[tag + 'rstd'])


def setup_common(k, ins):
    nc, P = k.nc, k.P
    k.ins = ins
    k.bank = [nc.alloc_psum_tensor('bank%d' % i, [128, 512], F32).ap() for i in range(8)]
    k.bankb = [b.bitcast(BF16) for b in k.bank]
    k.io_f = k.sb('io_f', [128, 128], F32)
    k.iop = k.sb('iop', [128, 1], F32)
    k.ident = k.sb('ident', [128, 128], BF16)
    k.ones_row = k.sb('ones_row', [1, 128], BF16)
    P.op('pool', lambda e: e.iota(k.io_f, pattern=[[1, 128]], base=0, channel_multiplier=0,
                                  allow_small_or_imprecise_dtypes=True), writes=['io_f'])
    P.op('pool', lambda e: e.iota(k.iop, pattern=[[1, 1]], base=0, channel_multiplier=1,
                                  allow_small_or_imprecise_dtypes=True), writes=['iop'])
    P.op('dve', lambda e: e.tensor_scalar(out=k.ident, in0=k.io_f, scalar1=k.iop[:, 0:1], scalar2=None,
                                          op0=ALU.is_equal), reads=['io_f', 'iop'], writes=['ident'])
    P.op('dve', lambda e: e.memset(k.ones_row, 1.0), writes=['ones_row'])
    k.wb = {}
    cast_layer(k, k.layers[0])
    k.xt = [k.sb('xt%d' % s, [128, D], F32) for s in range(2)]
    k.hbs = [k.sb('hb%d' % i, [128, D], BF16) for i in range(2)]
    k.junk = k.sb('junk', [128, D], BF16)
    k.tmpf = k.sb('tmpf', [128, D], F32)
    k.G = {n: k.sb('G_' + n, [128, D], F32) for n in ['mix_pre', 'mix_post', 'ffn_pre', 'ffn_post']}
    k.st = {n: k.sb('st_' + n, [128, 1], F32) for n in ['ssq', 'tmp', 'rstd', 'ssq2', 'tmp2', 'rstd2']}


def load_gains(k, l):
    for n in ['mix_pre', 'mix_post', 'ffn_pre', 'ffn_post']:
        k.P.dma('sp', k.G[n], k.ins['ln_' + n][l:l + 1, :].broadcast_to([128, D]), writes=['G_' + n], stream='small')


def norm_a(k, xt_ap, xres, gname, hbi):
    P = k.P
    st = k.st
    hb = k.hbs[hbi]
    P.op('act', lambda e: e.activation(out=k.junk, in_=xt_ap, func=AF.Square, accum_out=st['ssq']),
         reads=[xres], writes=['junk', 'ssq'])
    rstd_from_ssq(k, st['ssq'], st['rstd'], st['tmp'], D, EPS, '')
    P.op('dve', lambda e: e.scalar_tensor_tensor(out=hb, in0=xt_ap, scalar=st['rstd'][:, 0:1], in1=k.G[gname],
                                                 op0=ALU.mult, op1=ALU.mult),
         reads=[xres, 'rstd', 'G_' + gname], writes=['hb%d' % hbi])


def trans_t(k, hbi, hT_dst, hTres):
    P = k.P
    hb = k.hbs[hbi]
    tp = k.bankb[0]
    for c in range(8):
        P.op('pe', lambda e, c=c: e.transpose(out=tp[:, c * 128:(c + 1) * 128], in_=hb[:, c * 128:(c + 1) * 128],
                                              identity=k.ident), reads=['hb%d' % hbi, 'ident'], writes=['bank0'])
    P.op('act', lambda e: e.copy(out=hT_dst, in_=tp.rearrange('p (c t) -> p c t', c=8)), reads=['bank0'],
         writes=[hTres])


def post_norm_residual(k, pm, pmres, gname, xt_ap, xres):
    P = k.P
    st = k.st
    ssa = k.sb2
    for cb in range(2):
        P.op('act', lambda e, cb=cb: e.activation(out=k.junk[:, cb * 512:(cb + 1) * 512], in_=pm[cb], func=AF.Square,
                                                  accum_out=ssa[:, cb:cb + 1]),
             reads=[pmres[cb]], writes=['junk', 'ssa'])
    P.op('dve', lambda e: e.tensor_tensor(out=st['ssq2'], in0=ssa[:, 0:1], in1=ssa[:, 1:2], op=ALU.add),
         reads=['ssa'], writes=['2ssq'])
    rstd_from_ssq(k, st['ssq2'], st['rstd2'], st['tmp2'], D, EPS, '2')
    for cb in range(2):
        sl = slice(cb * 512, (cb + 1) * 512)
        P.op('dve', lambda e, cb=cb, sl=sl: e.scalar_tensor_tensor(out=k.tmpf[:, sl], in0=pm[cb], scalar=st['rstd2'][:, 0:1],
                                                                   in1=k.G[gname][:, sl], op0=ALU.mult, op1=ALU.mult),
             reads=[pmres[cb], '2rstd', 'G_' + gname], writes=['tmpf'])
    P.op('pool', lambda e: e.tensor_tensor(out=xt_ap, in0=xt_ap, in1=k.tmpf, op=ALU.add), reads=['tmpf', xres],
         writes=[xres])


def ffn_phase(k, l, xsrc, xdst):
    nc, P = k.nc, k.P
    wg, wu, wd = k.wb[('ffn_w_gate', l)], k.wb[('ffn_w_up', l)], k.wb[('ffn_w_down', l)]
    P.dma('sp', k.wd_sb, wd.rearrange('(c p) n -> p c n', p=128), reads=[('wb', 'ffn_w_down', l)], writes=['wd_sb'],
          stream='w')
    groups = [(0, 4), (4, 4), (8, 4), (12, 4), (16, 4), (20, 2)]
    wgv = wg.rearrange('(c p) n -> p c n', p=128)
    wuv = wu.rearrange('(c p) n -> p c n', p=128)
    gi = 0
    NB = T // 512

    def xtile(tb, s):
        j = (tb % 2) * 4 + s
        return k.xt8[j], 'xt8_%d' % j

    def front_a(tb, s):
        t0 = tb * 512 + s * 128
        xt, xr = xtile(tb, s)
        P.dma('sp', xt, xsrc[t0:t0 + 128, :], reads=[('x', t0 // 128)], writes=[xr], stream='x')
        norm_a(k, xt, xr, 'ffn_pre', s % 2)

    def front_t(tb, s):
        trans_t(k, s % 2, k.hT2[tb % 2][:, :, s * 128:(s + 1) * 128], ('hT', tb % 2))

    for s in range(4):
        front_a(0, s)
        front_t(0, s)
    for tb in range(NB):
        hT = k.hT2[tb % 2]
        hTr = ('hT', tb % 2)
        for gidx, (j0, nj) in enumerate(groups):
            pre = gidx < 4 and tb + 1 < NB
            if pre:
                front_a(tb + 1, gidx)
            buf = gi % 2
            gi += 1
            wt = k.wgu[buf]
            P.dma('sp', wt[:, 0, :, 0:nj * 128], wgv[:, :, j0 * 128:(j0 + nj) * 128], reads=[('wb', 'ffn_w_gate', l)],
                  writes=['wgu%d' % buf], stream='w')
            P.dma('sp', wt[:, 1, :, 0:nj * 128], wuv[:, :, j0 * 128:(j0 + nj) * 128], reads=[('wb', 'ffn_w_up', l)],
                  writes=['wgu%d' % buf], stream='w')
            for jj in range(nj):
                j = j0 + jj
                pb = 1 + 2 * (j % 2)
                pg, pu = k.bank[pb], k.bank[pb + 1]
                for kc in range(8):
                    P.op('pe', lambda e, kc=kc, jj=jj: e.matmul(pg, lhsT=wt[:, 0, kc, jj * 128:(jj + 1) * 128], rhs=hT[:, kc, :],
                                                               start=(kc == 0), stop=(kc == 7)),
                         reads=['wgu%d' % buf, hTr], writes=['bank%d' % pb])
                for kc in range(8):
                    P.op('pe', lambda e, kc=kc, jj=jj: e.matmul(pu, lhsT=wt[:, 1, kc, jj * 128:(jj + 1) * 128], rhs=hT[:, kc, :],
                                                               start=(kc == 0), stop=(kc == 7)),
                         reads=['wgu%d' % buf, hTr], writes=['bank%d' % (pb + 1)])
                sg = k.sg[j % 2]
                P.op('act', lambda e: e.activation(out=sg, in_=pg, func=AF.Silu), reads=['bank%d' % pb],
                     writes=['sg%d' % (j % 2)])
                P.op('dve', lambda e, j=j: e.tensor_tensor(out=k.actT[:, j, :], in0=pu, in1=sg, op=ALU.mult),
                     reads=['bank%d' % (pb + 1), 'sg%d' % (j % 2)], writes=[('actT', j)])
            if pre:
                front_t(tb + 1, gidx)
        for s in range(4):
            t0 = tb * 512 + s * 128
            pm = [k.bank[5], k.bank[6]]
            for cb in range(2):
                for kk in range(NFC):
                    P.op('pe', lambda e, kk=kk, cb=cb: e.matmul(pm[cb], lhsT=k.actT[:, kk, s * 128:(s + 1) * 128],
                                                               rhs=k.wd_sb[:, kk, cb * 512:(cb + 1) * 512],
                                                               start=(kk == 0), stop=(kk == NFC - 1)),
                         reads=[('actT', kk), 'wd_sb'], writes=['bank%d' % (5 + cb)])
            xt_, xr_ = xtile(tb, s)
            post_norm_residual(k, pm, ['bank5', 'bank6'], 'ffn_post', xt_, xr_)
            P.dma('sp', xdst[t0:t0 + 128, :], xt_, reads=[xr_], writes=[('x', t0 // 128)], stream='xo')


def even_phase(k, l, xsrc, xdst, es):
    nc, P = k.nc, k.P
    e_ = l // 2
    ins = k.ins

    def sb(name, shape, dt):
        return es.enter_context(nc.sbuf_tensor('evs%d_' % l + name, list(shape), dt)).ap()

    w_in_sb = sb('w_in', [128, 8, 1536], BF16)
    w_out_sb = sb('w_out', [128, 8, 1024], BF16)
    poolw_sb = sb('poolw', [128, 4, 128], BF16)
    Dm_sb = sb('Dm', [128, 3, 4, 128], BF16)
    WmT = sb('WmT', [128, 4, 128], BF16)
    wsf = sb('wsf', [128, 4, 128], F32)
    wsb = sb('wsb', [128, 4, 128], BF16)
    tril = sb('tril', [128, 128], F32)
    Bt = sb('Bt', [128, 512], F32)
    lng = sb('lng', [128, 512], F32)
    lnb = sb('lnb', [128, 512], F32)
    psc = sb('psc', [128, 4], F32)
    a_sb = [sb('a%d' % i, [128, 512], BF16) for i in range(2)]
    uT_sb = sb('uT', [128, 4, 128], BF16)
    vg = sb('vg', [128, 512], F32)
    vn = sb('vn', [128, 512], F32)
    vln = sb('vln', [128, 512], BF16)
    diffT = sb('diffT', [128, 4, 128], BF16)
    yaT = sb('yaT', [128, 4, 128], BF16)
    ybT = sb('ybT', [128, 4, 128], BF16)
    mxb = sb('mxb', [128, 512], F32)
    hT1s = [sb('hT1_%d' % i, [128, 8, 128], BF16) for i in range(2)]
    bst = sb('bst', [128, 6], F32)
    mv = sb('mv', [128, 2], F32)
    lrs = sb('lrs', [128, 1], F32)

    P.dma('sp', w_in_sb, k.wb[('ev_w_in', e_)].rearrange('(c p) n -> p c n', p=128), reads=[('wb', 'ev_w_in', e_)],
          writes=['w_in'], stream='w')
    P.dma('sp', w_out_sb, k.wb[('ev_w_out', e_)].rearrange('(c p) n -> p c n', p=128), reads=[('wb', 'ev_w_out', e_)],
          writes=['w_out'], stream='w')
    P.dma('sp', poolw_sb, k.wb[('ev_pool_w', e_)].rearrange('g c d -> c g d'), reads=[('wb', 'ev_pool_w', e_)],
          writes=['poolw'], stream='w')
    P.dma('sp', Dm_sb, ins['c_D'].rearrange('a g s t -> s a g t'), writes=['Dm'], stream='small')
    P.dma('sp', wsf, ins['ev_sgu_w'][e_].rearrange('g t s -> t g s'), writes=['wsf'], stream='small')
    P.dma('sp', Bt, ins['ev_sgu_b'][e_:e_ + 1].rearrange('o g t -> o (g t)').broadcast_to([128, 512]), writes=['Bt'],
          stream='small')
    P.dma('sp', lng, ins['ev_sgu_ln_g'][e_:e_ + 1, :].broadcast_to([128, 512]), writes=['lng'], stream='small')
    P.dma('sp', lnb, ins['ev_sgu_ln_b'][e_:e_ + 1, :].broadcast_to([128, 512]), writes=['lnb'], stream='small')
    with nc.allow_non_contiguous_dma(reason='tiny per-channel scale'):
        P.dma('sp', psc, ins['ev_pool_scale'][e_].rearrange('(g p) -> p g', p=128), writes=['psc'], stream='small')
    P.op('dve', lambda e: e.tensor_scalar(out=tril, in0=k.io_f, scalar1=k.iop[:, 0:1], scalar2=None, op0=ALU.is_le),
         reads=['io_f', 'iop'], writes=['tril'])
    P.op('dve', lambda e: e.tensor_tensor(out=wsb, in0=wsf, in1=tril.unsqueeze(1).broadcast_to([128, 4, 128]), op=ALU.mult),
         reads=['wsf', 'tril'], writes=['wsb'])
    tp = k.bankb[0]
    for g in range(4):
        P.op('pe', lambda e, g=g: e.transpose(out=tp[:, g * 128:(g + 1) * 128], in_=wsb[:, g, :], identity=k.ident),
             reads=['wsb', 'ident'], writes=['bank0'])
    P.op('act', lambda e: e.copy(out=WmT, in_=tp[:, 0:512].rearrange('p (g t) -> p g t', g=4)), reads=['bank0'],
         writes=['WmT'])

    def front_a(i):
        P.dma('sp', k.xt[i % 2], xsrc[i * 128:(i + 1) * 128, :], reads=[('x', i)], writes=['xt%d' % (i % 2)], stream='x')
        norm_a(k, k.xt[i % 2], 'xt%d' % (i % 2), 'mix_pre', i % 2)

    def front_t(i):
        trans_t(k, i % 2, hT1s[i % 2], 'hT1_%d' % (i % 2))

    front_a(0)
    front_t(0)
    for i in range(NT):
        xt = k.xt[i % 2]
        xr = 'xt%d' % (i % 2)
        hT1 = hT1s[i % 2]
        hT1n = 'hT1_%d' % (i % 2)
        a_cur, a_prev = a_sb[i % 2], a_sb[(i + 1) % 2]
        ar, apr = 'a%d' % (i % 2), 'a%d' % ((i + 1) % 2)
        if i + 1 < NT and i >= 1:
            pass
        pa, pu, pv = k.bank[1], k.bank[2], k.bank[3]
        for kc in range(8):
            P.op('pe', lambda e, kc=kc: e.matmul(pa, lhsT=hT1[:, kc, :], rhs=w_in_sb[:, kc, 0:512], start=(kc == 0),
                                                stop=(kc == 7)), reads=[hT1n, 'w_in'], writes=['bank1'])
        P.op('act', lambda e: e.copy(out=a_cur, in_=pa), reads=['bank1'], writes=[ar])
        for kc in range(8):
            P.op('pe', lambda e, kc=kc: e.matmul(pv, lhsT=hT1[:, kc, :], rhs=w_in_sb[:, kc, 1024:1536], start=(kc == 0),
                                                stop=(kc == 7)), reads=[hT1n, 'w_in'], writes=['bank3'])
        P.op('act', lambda e: e.activation(out=vg, in_=pv, func=AF.Gelu_apprx_tanh), reads=['bank3'], writes=['vg'])
        for c in range(4):
            for kc in range(8):
                P.op('pe', lambda e, kc=kc, c=c: e.matmul(pu[:, c * 128:(c + 1) * 128],
                                                         lhsT=w_in_sb[:, kc, 512 + c * 128:512 + (c + 1) * 128],
                                                         rhs=hT1[:, kc, :], start=(kc == 0), stop=(kc == 7)),
                     reads=[hT1n, 'w_in'], writes=['bank2'])
        P.op('act', lambda e: e.activation(out=uT_sb, in_=pu.rearrange('p (c t) -> p c t', c=4), func=AF.Gelu_apprx_tanh),
             reads=['bank2'], writes=['uT'])
        if i + 1 < NT:
            front_a(i + 1)
        P.op('dve', lambda e: e.bn_stats(out=bst, in_=vg), reads=['vg'], writes=['bst'])
        P.op('dve', lambda e: e.bn_aggr(out=mv, in_=bst), reads=['bst'], writes=['mv'])
        P.op('dve', lambda e: e.tensor_scalar(out=lrs, in0=mv[:, 1:2], scalar1=1e-5, scalar2=None, op0=ALU.add),
             reads=['mv'], writes=['lrs'])
        P.op('act', lambda e: e.activation(out=lrs, in_=lrs, func=AF.Sqrt), reads=['lrs'], writes=['lrs'])
        P.op('dve', lambda e: e.reciprocal(out=lrs, in_=lrs), reads=['lrs'], writes=['lrs'])
        P.op('dve', lambda e: e.tensor_scalar(out=vn, in0=vg, scalar1=mv[:, 0:1], scalar2=lrs[:, 0:1], op0=ALU.subtract,
                                              op1=ALU.mult), reads=['vg', 'mv', 'lrs'], writes=['vn'])
        P.op('pool', lambda e: e.tensor_tensor(out=vn, in0=vn, in1=lng, op=ALU.mult), reads=['vn', 'lng'], writes=['vn'])
        P.op('pool', lambda e: e.tensor_tensor(out=vln, in0=vn, in1=lnb, op=ALU.add), reads=['vn', 'lnb'], writes=['vln'])
        pd_ = k.bank[4]
        for g in range(4):
            first = True
            sl = slice(g * 128, (g + 1) * 128)
            P.op('pe', lambda e, g=g, sl=sl: e.matmul(pd_[:, sl], lhsT=a_cur[:, sl], rhs=Dm_sb[:, 0 if i == 0 else 1, g, :],
                                                     start=True, stop=(i == 0)), reads=[ar, 'Dm'], writes=['bank4'])
            if i > 0:
                P.op('pe', lambda e, g=g, sl=sl: e.matmul(pd_[:, sl], lhsT=a_prev[:, sl], rhs=Dm_sb[:, 2, g, :],
                                                         start=False, stop=True), reads=[apr, 'Dm'], writes=['bank4'])
        P.op('dve', lambda e: e.tensor_copy(out=diffT, in_=pd_.rearrange('p (g t) -> p g t', g=4)), reads=['bank4'],
             writes=['diffT'])
        pya = k.bank[5]
        for g in range(4):
            P.op('pe', lambda e, g=g: e.matmul(pya[:, g * 128:(g + 1) * 128], lhsT=poolw_sb[:, g, :], rhs=diffT[:, g, :],
                                              start=True, stop=True), reads=['poolw', 'diffT'], writes=['bank5'])
        P.op('dve', lambda e: e.tensor_tensor(out=yaT, in0=pya.rearrange('p (g t) -> p g t', g=4),
                                              in1=psc.unsqueeze(2).broadcast_to([128, 4, 128]), op=ALU.mult),
             reads=['bank5', 'psc'], writes=['yaT'])
        pmx = k.bank[4]
        for g in range(4):
            P.op('pe', lambda e, g=g: e.matmul(pmx[:, g * 128:(g + 1) * 128], lhsT=vln[:, g * 128:(g + 1) * 128],
                                              rhs=WmT[:, g, :], start=True, stop=True), reads=['vln', 'WmT'],
                 writes=['bank4'])
        P.op('dve', lambda e: e.tensor_tensor(out=mxb, in0=pmx, in1=Bt, op=ALU.add), reads=['bank4', 'Bt'],
             writes=['mxb'])
        P.op('pool', lambda e: e.tensor_tensor(out=ybT, in0=mxb.rearrange('p (g t) -> p g t', g=4), in1=uT_sb, op=ALU.mult),
             reads=['mxb', 'uT'], writes=['ybT'])
        if i + 1 < NT:
            front_t(i + 1)
        pm = [k.bank[6], k.bank[7]]
        for cb in range(2):
            for kc in range(8):
                lh = yaT[:, kc, :] if kc < 4 else ybT[:, kc - 4, :]
                P.op('pe', lambda e, kc=kc, cb=cb, lh=lh: e.matmul(pm[cb], lhsT=lh, rhs=w_out_sb[:, kc, cb * 512:(cb + 1) * 512],
                                                                  start=(kc == 0), stop=(kc == 7)),
                     reads=['yaT', 'ybT', 'w_out'], writes=['bank%d' % (6 + cb)])
        post_norm_residual(k, pm, ['bank6', 'bank7'], 'mix_post', xt, xr)
        P.dma('sp', xdst[i * 128:(i + 1) * 128, :], xt, reads=[xr], writes=[('x', i)], stream='xo')


def odd_phase(k, l, xsrc, xdst, es):
    from contextlib import ExitStack
    nc, P = k.nc, k.P
    o_ = l // 2
    ins = k.ins
    PI = 3.14159265358979

    def sbs(stack, name, shape, dt):
        return stack.enter_context(nc.sbuf_tensor('od%d_' % l + name, list(shape), dt)).ap()

    def sb(name, shape, dt):
        return sbs(es, name, shape, dt)

    qd = k.dram('qd%d' % l, [T, 512], BF16)
    ydd = k.dram('ydd%d' % l, [T, 512], BF16)
    KE = sb('KE', [128, 2, T], BF16)
    kwT = sb('kwT', [64, 2, T], BF16)
    kcvd = k.dram('kcvd%d' % l, [4, 64, T], BF16)
    ropeS = k.dram('ropeS%d' % l, [128, NT, 72], F32)
    ropeC = k.dram('ropeC%d' % l, [128, NT, 72], F32)
    vs_aug = sb('vs_aug', [128, NT, 2, 65], BF16)
    vw_aug = sb('vw_aug', [128, NT, 2, 65], BF16)
    gsig = sb('gsig', [128, NT, 24], F32)
    kcmpT = sb('kcmpT', [64, 2, 256], BF16)
    vcmp_aug = sb('vcmp', [128, 2, 2, 65], BF16)
    tri = sb('tri', [128, 128], F32)
    ntri = sb('ntri', [128, 128], F32)
    cm = sb('cm', [128, 128], F32)
    keep = sb('keep', [128, 128], F32)
    addc = sb('addc', [128, 128], F32)
    cts = sb('cts', [128, 2, 64], BF16)
    decT = sb('decT', [128, 512], F32)
    xi = sb('xi', [128, 512], F32)
    zeta = sb('zeta', [128, 4], F32)
    gch = sb('gch', [128, 4], F32)
    gng = sb('gng', [128, 512], F32)
    for nm, t_, src in [('tri', tri, 'c_tri'), ('cm', cm, 'c_cm'), ('keep', keep, 'c_keep'), ('addc', addc, 'c_add'),
                        ('decT', decT, 'c_decT'), ('xi', xi, 'c_xi'), ('zeta', zeta, 'c_zeta'), ('gch', gch, 'c_gch')]:
        P.dma('sp', t_, ins[src], writes=[nm], stream='small')
    P.dma('sp', cts, ins['c_cts'].rearrange('c n j -> n c j'), writes=['cts'], stream='small')
    P.dma('sp', KE[64:128, 0, :], ins['c_E'], writes=['KE'], stream='small')
    P.dma('sp', KE[64:128, 1, :], ins['c_E'], writes=['KE'], stream='small')
    P.dma('sp', gng, ins['od_ret_gn_g'][o_:o_ + 1, :].broadcast_to([128, 512]), writes=['gng'], stream='small')
    P.op('dve', lambda e: e.tensor_scalar(out=ntri, in0=tri, scalar1=-1.0, scalar2=1.0, op0=ALU.mult, op1=ALU.add),
         reads=['tri'], writes=['ntri'])
    P.op('pool', lambda e: e.memset(vs_aug, 1.0), writes=['vs_aug'])
    P.op('pool', lambda e: e.memset(vw_aug, 1.0), writes=['vw_aug'])
    P.op('pool', lambda e: e.memset(vcmp_aug, 1.0), writes=['vcmp'])
    with ExitStack() as ts:
        posi = sbs(ts, 'posi', [128, NT], I32)
        sinT = sbs(ts, 'sinT', [128, NT, 72], F32)
        cosT = sbs(ts, 'cosT', [128, NT, 72], F32)
        posf = sbs(ts, 'posf', [128, NT], F32)
        invf = sbs(ts, 'invf', [128, 72], F32)
        ang = sbs(ts, 'ang', [128, NT, 72], F32)
        arg = sbs(ts, 'arg', [128, NT, 72], F32)
        kf = sbs(ts, 'kf', [128, NT, 72], F32)
        ki = sbs(ts, 'ki', [128, NT, 72], I32)
        with nc.allow_non_contiguous_dma(reason='positions to token-on-partition layout'):
            P.dma('sp', posi, ins['positions'].rearrange('o (i p) -> p (o i)', p=128), writes=['posi'], stream='small')
        P.dma('sp', invf, ins['c_invf'].broadcast_to([128, 72]), writes=['invf'], stream='small')
        P.op('dve', lambda e: e.tensor_copy(out=posf, in_=posi), reads=['posi'], writes=['posf'])
        P.op('dve', lambda e: e.tensor_tensor(out=ang, in0=posf.unsqueeze(2).broadcast_to([128, NT, 72]),
                                              in1=invf.unsqueeze(1).broadcast_to([128, NT, 72]), op=ALU.mult),
             reads=['posf', 'invf'], writes=['ang'])
        for shift, dst, dn in [(0.0, sinT, 'sinT'), (PI / 2, cosT, 'cosT')]:
            P.op('dve', lambda e, shift=shift: e.tensor_scalar(out=arg, in0=ang, scalar1=shift, scalar2=None, op0=ALU.add),
                 reads=['ang'], writes=['arg'])
            P.op('dve', lambda e: e.tensor_scalar(out=kf, in0=arg, scalar1=1.0 / (2 * PI), scalar2=None, op0=ALU.mult),
                 reads=['arg'], writes=['kf'])
            P.op('dve', lambda e: e.tensor_copy(out=ki, in_=kf), reads=['kf'], writes=['ki'])
            P.op('dve', lambda e: e.tensor_copy(out=kf, in_=ki), reads=['ki'], writes=['kf'])
            P.op('dve', lambda e: e.scalar_tensor_tensor(out=arg, in0=kf, scalar=-6.28125, in1=arg, op0=ALU.mult, op1=ALU.add),
                 reads=['kf', 'arg'], writes=['arg'])
            P.op('dve', lambda e: e.scalar_tensor_tensor(out=arg, in0=kf, scalar=-(2 * PI - 6.28125), in1=arg, op0=ALU.mult,
                                                         op1=ALU.add), reads=['kf', 'arg'], writes=['arg'])
            P.op('dve', lambda e: e.tensor_scalar(out=arg, in0=arg, scalar1=3.1415925, scalar2=-3.1415925, op0=ALU.min,
                                                  op1=ALU.max), reads=['arg'], writes=['arg'])
            P.op('act', lambda e, dst=dst: e.activation(out=dst, in_=arg, func=AF.Sin), reads=['arg'], writes=[dn])
        P.dma('sp', ropeS, sinT, reads=['sinT'], writes=['ropeS'], stream='xo')
        P.dma('sp', ropeC, cosT, reads=['cosT'], writes=['ropeC'], stream='xo')
        P.barrier()

    with ExitStack() as s1:
        w_in_sb = sbs(s1, 'w_in', [128, 8, 3352], BF16)
        hT1s = [sbs(s1, 'hT1_%d' % j, [128, 8, 128], BF16) for j in range(2)]
        cur = {}
        csb = [sbs(s1, 'csb%d' % j, [128, 2, 72], F32) for j in range(2)]
        kcv_t = sbs(s1, 'kcv_t', [64, 4, 128], BF16)
        zq = sbs(s1, 'zq', [128, 512], F32)
        qb = sbs(s1, 'qb', [128, 512], BF16)
        zkv = sbs(s1, 'zkv', [128, 768], F32)
        kvb = sbs(s1, 'kvb', [128, 768], BF16)
        rt = [sbs(s1, 'rt%d' % j, [128, 512], F32) for j in range(4)]
        zrq = sbs(s1, 'zrq', [128, 512], F32)
        zrk = sbs(s1, 'zrk', [128, 512], F32)
        rqb = sbs(s1, 'rqb', [128, 512], BF16)
        rkb = sbs(s1, 'rkb', [128, 512], BF16)
        rkr = sbs(s1, 'rkr', [128, 512], F32)
        rkz = sbs(s1, 'rkz', [128, 512], BF16)
        rvb = sbs(s1, 'rvb', [128, 512], BF16)
        gsl = sbs(s1, 'gsl', [128, 512], F32)
        qkT = sbs(s1, 'qkT', [128, 8, 128], BF16)
        qxiT = sbs(s1, 'qxiT', [128, 512], BF16)
        STs = sbs(s1, 'STs', [128, 512], BF16)
        state = sbs(s1, 'state', [128, 512], F32)
        stbf = sbs(s1, 'stbf', [128, 512], BF16)
        bst4 = sbs(s1, 'bst4', [128, 4, 6], F32)
        mv4 = sbs(s1, 'mv4', [128, 4, 2], F32)
        rs4 = sbs(s1, 'rs4', [128, 4], F32)
        on = sbs(s1, 'on', [128, 512], F32)
        ydb = sbs(s1, 'ydb', [128, 512], BF16)
        P.dma('sp', w_in_sb, k.wb[('od_w_in', o_)].rearrange('(c p) n -> p c n', p=128), reads=[('wb', 'od_w_in', o_)],
              writes=['w_in'], stream='w')
        P.op('dve', lambda e: e.memset(state, 0.0), writes=['state'])

        def proj(bank, c0, c1):
            for kc in range(8):
                P.op('pe', lambda e, kc=kc: e.matmul(k.bank[bank][:, 0:c1 - c0], lhsT=cur['hT'][:, kc, :], rhs=w_in_sb[:, kc, c0:c1],
                                                    start=(kc == 0), stop=(kc == 7)), reads=[cur['hTn'], 'w_in'],
                     writes=['bank%d' % bank])

        def rope(src, dst, nh, hd, half, c_, s_, csn, sname, dname):
            sv = src.rearrange('p (h d) -> p h d', h=nh)
            dv = dst.rearrange('p (h d) -> p h d', h=nh)
            x1, x2 = sv[:, :, 0:half], sv[:, :, half:2 * half]
            cb = c_.unsqueeze(1).broadcast_to([128, nh, half])
            sb_ = s_.unsqueeze(1).broadcast_to([128, nh, half])
            t = [r_[:, 0:nh * half].rearrange('p (h d) -> p h d', h=nh) for r_ in rt]
            P.op('dve', lambda e: e.tensor_tensor(out=t[0], in0=x1, in1=cb, op=ALU.mult), reads=[sname, csn], writes=['rt0'])
            P.op('pool', lambda e: e.tensor_tensor(out=t[1], in0=x2, in1=sb_, op=ALU.mult), reads=[sname, csn], writes=['rt1'])
            P.op('dve', lambda e: e.tensor_tensor(out=t[2], in0=x2, in1=cb, op=ALU.mult), reads=[sname, csn], writes=['rt2'])
            P.op('pool', lambda e: e.tensor_tensor(out=t[3], in0=x1, in1=sb_, op=ALU.mult), reads=[sname, csn], writes=['rt3'])
            if 2 * half < hd:
                P.op('act', lambda e: e.copy(out=dst, in_=src), reads=[sname], writes=[dname])
            P.op('dve', lambda e: e.tensor_tensor(out=dv[:, :, 0:half], in0=t[0], in1=t[1], op=ALU.subtract),
                 reads=['rt0', 'rt1'], writes=[dname])
            P.op('pool', lambda e: e.tensor_tensor(out=dv[:, :, half:2 * half], in0=t[2], in1=t[3], op=ALU.add),
                 reads=['rt2', 'rt3'], writes=[dname])

        def o1_front_a(i):
            P.dma('sp', k.xt[i % 2], xsrc[i * 128:(i + 1) * 128, :], reads=[('x', i)], writes=['xt%d' % (i % 2)], stream='x')
            norm_a(k, k.xt[i % 2], 'xt%d' % (i % 2), 'mix_pre', i % 2)

        def o1_front_t(i):
            trans_t(k, i % 2, hT1s[i % 2], 'hT1_%d' % (i % 2))

        o1_front_a(0)
        o1_front_t(0)
        for i in range(NT):
            cur['hT'] = hT1s[i % 2]
            cur['hTn'] = 'hT1_%d' % (i % 2)
            cs_ = csb[i % 2]
            csn = 'csb%d' % (i % 2)
            P.dma('sp', cs_[:, 0, :], ropeC[:, i, :], reads=['ropeC'], writes=[csn], stream='small')
            P.dma('sp', cs_[:, 1, :], ropeS[:, i, :], reads=['ropeS'], writes=[csn], stream='small')
            cn, sn = cs_[:, 0, 0:8], cs_[:, 1, 0:8]
            cr, sr = cs_[:, 0, 8:72], cs_[:, 1, 8:72]
            proj(1, 0, 512)
            P.op('act', lambda e: e.activation(out=zq, in_=k.bank[1], func=AF.Copy, scale=0.125), reads=['bank1'], writes=['zq'])
            rope(zq, qb, 8, 64, 8, cn, sn, csn, 'zq', 'qb')
            P.dma('sp', qd[i * 128:(i + 1) * 128, :], qb, reads=['qb'], writes=[('qd', i)], stream='xo')
            proj(2, 512, 1024)
            proj(3, 1024, 1304)
            P.op('act', lambda e: e.copy(out=zkv[:, 0:512], in_=k.bank[2]), reads=['bank2'], writes=['zkv'])
            P.op('act', lambda e: e.copy(out=zkv[:, 512:768], in_=k.bank[3][:, 0:256]), reads=['bank3'], writes=['zkv'])
            P.op('act', lambda e: e.activation(out=gsig[:, i, :], in_=k.bank[3][:, 256:280], func=AF.Sigmoid),
                 reads=['bank3'], writes=[('gsig', i)])
            kv4 = zkv.rearrange('p (a b g d) -> p a b g d', a=3, b=2, g=2)[:, :, 0, :, :]
            x1, x2 = kv4[:, :, :, 0:8], kv4[:, :, :, 8:16]
            cb = cn.unsqueeze(1).unsqueeze(1).broadcast_to([128, 3, 2, 8])
            sb_ = sn.unsqueeze(1).unsqueeze(1).broadcast_to([128, 3, 2, 8])
            t = [r_[:, 0:48].rearrange('p (a g d) -> p a g d', a=3, g=2) for r_ in rt]
            P.op('dve', lambda e: e.tensor_tensor(out=t[0], in0=x1, in1=cb, op=ALU.mult), reads=['zkv', csn], writes=['rt0'])
            P.op('dve', lambda e: e.tensor_tensor(out=t[1], in0=x2, in1=sb_, op=ALU.mult), reads=['zkv', csn], writes=['rt1'])
            P.op('dve', lambda e: e.tensor_tensor(out=t[2], in0=x2, in1=cb, op=ALU.mult), reads=['zkv', csn], writes=['rt2'])
            P.op('dve', lambda e: e.tensor_tensor(out=t[3], in0=x1, in1=sb_, op=ALU.mult), reads=['zkv', csn], writes=['rt3'])
            P.op('dve', lambda e: e.tensor_tensor(out=x1, in0=t[0], in1=t[1], op=ALU.subtract), reads=['rt0', 'rt1'], writes=['zkv'])
            P.op('dve', lambda e: e.tensor_tensor(out=x2, in0=t[2], in1=t[3], op=ALU.add), reads=['rt2', 'rt3'], writes=['zkv'])
            P.op('act', lambda e: e.copy(out=kvb, in_=zkv), reads=['zkv'], writes=['kvb'])
            P.op('pool', lambda e: e.tensor_copy(out=vs_aug[:, i, :, 0:64], in_=kvb[:, 384:512].rearrange('p (g d) -> p g d', g=2)),
                 reads=['kvb'], writes=['vs_aug'])
            P.op('pool', lambda e: e.tensor_copy(out=vw_aug[:, i, :, 0:64], in_=kvb[:, 640:768].rearrange('p (g d) -> p g d', g=2)),
                 reads=['kvb'], writes=['vw_aug'])
            tp = k.bankb[0]
            srcs = [0, 64, 128, 192, 512, 576, 256, 320]
            for j, c0 in enumerate(srcs):
                P.op('pe', lambda e, j=j, c0=c0: e.transpose(out=tp[0:64, j * 128:(j + 1) * 128], in_=kvb[:, c0:c0 + 64],
                                                            identity=k.ident), reads=['kvb', 'ident'], writes=['bank0'])
            P.op('act', lambda e: e.copy(out=kcv_t, in_=tp[0:64, 0:512].rearrange('p (j t) -> p j t', j=4)), reads=['bank0'],
                 writes=['kcv_t'])
            P.dma('sp', kcvd[:, :, i * 128:(i + 1) * 128].rearrange('j p t -> p j t'), kcv_t, reads=['kcv_t'], writes=['kcvd'],
                  stream='xo')
            P.op('act', lambda e: e.copy(out=kwT[:, :, i * 128:(i + 1) * 128],
                                         in_=tp[0:64, 512:768].rearrange('p (j t) -> p j t', j=2)), reads=['bank0'], writes=['kwT'])
            P.op('act', lambda e: e.copy(out=KE[0:64, :, i * 128:(i + 1) * 128],
                                         in_=tp[0:64, 768:1024].rearrange('p (j t) -> p j t', j=2)), reads=['bank0'], writes=['KE'])
            proj(4, 1304, 1816)
            proj(5, 1816, 2328)
            proj(6, 2328, 2840)
            proj(7, 2840, 3352)
            if i + 1 < NT:
                o1_front_a(i + 1)
            P.op('act', lambda e: e.copy(out=zrq, in_=k.bank[4]), reads=['bank4'], writes=['zrq'])
            P.op('act', lambda e: e.activation(out=zrk, in_=k.bank[5], func=AF.Copy, scale=128.0 ** -0.5), reads=['bank5'],
                 writes=['zrk'])
            P.op('act', lambda e: e.copy(out=rvb, in_=k.bank[6]), reads=['bank6'], writes=['rvb'])
            P.op('act', lambda e: e.activation(out=gsl, in_=k.bank[7], func=AF.Silu), reads=['bank7'], writes=['gsl'])
            rope(zrq, rqb, 4, 128, 64, cr, sr, csn, 'zrq', 'rqb')
            rope(zrk, rkr, 4, 128, 64, cr, sr, csn, 'zrk', 'rkr')
            P.op('act', lambda e: e.copy(out=rkb, in_=rkr), reads=['rkr'], writes=['rkb'])
            P.op('dve', lambda e: e.tensor_tensor(out=rkz.rearrange('p (h d) -> p h d', h=4),
                                                  in0=rkr.rearrange('p (h d) -> p h d', h=4),
                                                  in1=zeta.unsqueeze(2).broadcast_to([128, 4, 128]), op=ALU.mult),
                 reads=['rkr', 'zeta'], writes=['rkz'])
            for h in range(4):
                P.op('pe', lambda e, h=h: e.transpose(out=tp[:, h * 128:(h + 1) * 128], in_=rqb[:, h * 128:(h + 1) * 128],
                                                      identity=k.ident), reads=['rqb', 'ident'], writes=['bank0'])
            for h in range(4):
                P.op('pe', lambda e, h=h: e.transpose(out=tp[:, (4 + h) * 128:(5 + h) * 128], in_=rkb[:, h * 128:(h + 1) * 128],
                                                      identity=k.ident), reads=['rkb', 'ident'], writes=['bank0'])
            P.op('act', lambda e: e.copy(out=qkT, in_=tp.rearrange('p (c t) -> p c t', c=8)), reads=['bank0'], writes=['qkT'])
            for h in range(4):
                P.op('pe', lambda e, h=h: e.matmul(k.bank[1][:, h * 128:(h + 1) * 128], lhsT=qkT[:, 4 + h, :], rhs=qkT[:, h, :],
                                                  start=True, stop=True), reads=['qkT'], writes=['bank1'])
            P.op('dve', lambda e: e.tensor_tensor(out=STs, in0=k.bank[1], in1=decT, op=ALU.mult), reads=['bank1', 'decT'],
                 writes=['STs'])
            P.op('pool', lambda e: e.tensor_tensor(out=qxiT, in0=qkT[:, 0:4, :].rearrange('p h t -> p (h t)'), in1=xi, op=ALU.mult),
                 reads=['qkT', 'xi'], writes=['qxiT'])
            for h in range(4):
                hs = slice(h * 128, (h + 1) * 128)
                P.op('pe', lambda e, hs=hs: e.matmul(k.bank[2][:, hs], lhsT=STs[:, hs], rhs=rvb[:, hs], start=True, stop=(i == 0)),
                     reads=['STs', 'rvb'], writes=['bank2'])
                if i > 0:
                    P.op('pe', lambda e, hs=hs: e.matmul(k.bank[2][:, hs], lhsT=qxiT[:, hs], rhs=stbf[:, hs], start=False, stop=True),
                         reads=['qxiT', 'stbf'], writes=['bank2'])
            for h in range(4):
                hs = slice(h * 128, (h + 1) * 128)
                P.op('pe', lambda e, hs=hs: e.matmul(k.bank[3][:, hs], lhsT=rkz[:, hs], rhs=rvb[:, hs], start=True, stop=True),
                     reads=['rkz', 'rvb'], writes=['bank3'])
            P.op('dve', lambda e: e.tensor_tensor(out=state.rearrange('p (h d) -> p h d', h=4),
                                                  in0=state.rearrange('p (h d) -> p h d', h=4),
                                                  in1=gch.unsqueeze(2).broadcast_to([128, 4, 128]), op=ALU.mult),
                 reads=['state', 'gch'], writes=['state'])
            P.op('dve', lambda e: e.tensor_tensor(out=state, in0=k.bank[3], in1=state, op=ALU.add), reads=['bank3', 'state'],
                 writes=['state'])
            P.op('act', lambda e: e.copy(out=stbf, in_=state), reads=['state'], writes=['stbf'])
            if i + 1 < NT:
                o1_front_t(i + 1)
            for h in range(4):
                P.op('dve', lambda e, h=h: e.bn_stats(out=bst4[:, h, :], in_=k.bank[2][:, h * 128:(h + 1) * 128]),
                     reads=['bank2'], writes=['bst4'])
            for h in range(4):
                P.op('dve', lambda e, h=h: e.bn_aggr(out=mv4[:, h, :], in_=bst4[:, h, :]), reads=['bst4'], writes=['mv4'])
            P.op('dve', lambda e: e.tensor_scalar(out=rs4, in0=mv4[:, :, 1], scalar1=1e-5, scalar2=None, op0=ALU.add),
                 reads=['mv4'], writes=['rs4'])
            P.op('act', lambda e: e.activation(out=rs4, in_=rs4, func=AF.Sqrt), reads=['rs4'], writes=['rs4'])
            P.op('dve', lambda e: e.reciprocal(out=rs4, in_=rs4), reads=['rs4'], writes=['rs4'])
            for h in range(4):
                hs = slice(h * 128, (h + 1) * 128)
                P.op('dve', lambda e, h=h, hs=hs: e.tensor_scalar(out=on[:, hs], in0=k.bank[2][:, hs], scalar1=mv4[:, h, 0:1],
                                                                  scalar2=rs4[:, h:h + 1], op0=ALU.subtract, op1=ALU.mult),
                     reads=['bank2', 'mv4', 'rs4'], writes=['on'])
            P.op('pool', lambda e: e.tensor_tensor(out=on, in0=on, in1=gng, op=ALU.mult), reads=['on', 'gng'], writes=['on'])
            P.op('pool', lambda e: e.tensor_tensor(out=ydb, in0=on, in1=gsl, op=ALU.mult), reads=['on', 'gsl'], writes=['ydb'])
            P.dma('sp', ydd[i * 128:(i + 1) * 128, :], ydb, reads=['ydb'], writes=[('ydd', i)], stream='xo')

        P.barrier()
        s1.close()
        s1c = ExitStack()
        w1 = sbs(s1c, 'w1', [64, 32, 128], BF16)
        w2 = sbs(s1c, 'w2', [128, 64], BF16)
        posf32 = sbs(s1c, 'posf32', [64, 32], F32)
        posT = sbs(s1c, 'posT', [64, 32], BF16)
        cbias = sbs(s1c, 'cbias', [128, 1], F32)
        ghT = sbs(s1c, 'ghT', [128, 256], BF16)
        csrc = sbs(s1c, 'csrc', [64, T], BF16)
        for kind, (n1, n2, npos) in enumerate([('od_cmp_k_w1', 'od_cmp_k_w2', 'od_cmp_k_pos'),
                                               ('od_cmp_v_w1', 'od_cmp_v_w2', 'od_cmp_v_pos')]):
            P.dma('sp', w1, k.wb[(n1, o_)].rearrange('(l d) j -> d l j', d=64), reads=[('wb', n1, o_)], writes=['w1'], stream='w')
            P.dma('sp', w2, k.wb[(n2, o_)], reads=[('wb', n2, o_)], writes=['w2'], stream='w')
            with nc.allow_non_contiguous_dma(reason='tiny pos-emb transpose'):
                P.dma('sp', posf32, ins[npos][o_].rearrange('l d -> d l'), writes=['posf32'], stream='small')
            P.op('dve', lambda e: e.tensor_copy(out=posT, in_=posf32), reads=['posf32'], writes=['posT'])
            for g in range(2):
                P.dma('sp', csrc, kcvd[2 * kind + g], reads=['kcvd'], writes=['csrc'], stream='w')
                srcv = csrc.rearrange('p (n s) -> p n s', s=16)
                hb_ = k.bank[1]
                for l_ in range(32):
                    P.op('pe', lambda e, l_=l_: e.matmul(hb_[:, 0:255], lhsT=w1[:, l_, :],
                                                        rhs=(srcv[:, 0:255, l_] if l_ < 16 else srcv[:, 1:256, l_ - 16]),
                                                        start=(l_ == 0), stop=(l_ == 31)),
                         reads=['w1', 'csrc'], writes=['bank1'])
                for l_ in range(32):
                    P.op('pe', lambda e, l_=l_: e.matmul(hb_[:, 256:257], lhsT=w1[:, l_, :], rhs=posT[:, l_:l_ + 1],
                                                        start=(l_ == 0), stop=(l_ == 31)), reads=['w1', 'posT'], writes=['bank1'])
                P.op('dve', lambda e: e.tensor_copy(out=cbias, in_=hb_[:, 256:257]), reads=['bank1'], writes=['cbias'])
                P.op('dve', lambda e: e.memset(ghT[:, 255:256], 0.0), writes=['ghT'])
                P.op('act', lambda e: e.activation(out=ghT[:, 0:255], in_=hb_[:, 0:255], func=AF.Gelu_apprx_tanh,
                                                   bias=cbias[:, 0:1]), reads=['bank1', 'cbias'], writes=['ghT'])
                if kind == 0:
                    P.op('pe', lambda e: e.matmul(k.bank[2][0:64, 0:256], lhsT=w2, rhs=ghT, start=True, stop=True),
                         reads=['w2', 'ghT'], writes=['bank2'])
                    P.op('act', lambda e, g=g: e.copy(out=kcmpT[:, g, :], in_=k.bank[2][0:64, 0:256]), reads=['bank2'],
                         writes=['kcmpT'])
                else:
                    for c in range(2):
                        P.op('pe', lambda e, c=c: e.matmul(k.bank[2][:, c * 64:(c + 1) * 64], lhsT=ghT[:, c * 128:(c + 1) * 128],
                                                          rhs=w2, start=True, stop=True), reads=['w2', 'ghT'], writes=['bank2'])
                    P.op('act', lambda e, g=g: e.copy(out=vcmp_aug[:, :, g, 0:64],
                                                      in_=k.bank[2][:, 0:128].rearrange('p (c d) -> p c d', c=2)),
                         reads=['bank2'], writes=['vcmp'])
        P.barrier()
        s1c.close()

    with ExitStack() as s2:
        w_out_sb = sbs(s2, 'w_out', [128, 8, 1024], BF16)
        PT = sbs(s2, 'PT', [128, NT, 512], BF16)
        PW = sbs(s2, 'PW', [128, 5, 512], BF16)
        Pc = sbs(s2, 'Pc', [128, 2, 512], BF16)
        QN = [sbs(s2, 'QN%d' % g, [128, 512], BF16) for g in range(2)]
        qt = [sbs(s2, 'qt%d' % j, [128, 512], BF16) for j in range(2)]
        ydts = [sbs(s2, 'ydt%d' % j, [128, 512], BF16) for j in range(2)]
        negt = sbs(s2, 'negt', [128, 128], BF16)
        expf = sbs(s2, 'expf', [128, 512], F32)
        score = sbs(s2, 'score', [128, 64], F32)
        sc2 = sbs(s2, 'sc2', [128, 64], F32)
        m8a = sbs(s2, 'm8a', [128, 8], F32)
        m8b = sbs(s2, 'm8b', [128, 8], F32)
        thr = sbs(s2, 'thr', [128, 1], F32)
        psl = sbs(s2, 'psl', [128, 64], F32)
        rdenA = [sbs(s2, 'rdenA%d' % g, [128, 4], F32) for g in range(2)]
        rdenB = [sbs(s2, 'rdenB%d' % g, [128, 12], F32) for g in range(2)]
        coefs = [sbs(s2, 'coef%d' % g, [128, 12], F32) for g in range(2)]
        ocw = [sbs(s2, 'ocw%d' % g, [128, 2, 260], F32) for g in range(2)]
        yc = sbs(s2, 'yc', [128, 512], F32)
        ycb = sbs(s2, 'ycb', [128, 512], BF16)
        yT = sbs(s2, 'yT', [128, 8, 128], BF16)
        P.dma('sp', w_out_sb, k.wb[('od_w_out', o_)].rearrange('(c p) n -> p c n', p=128), reads=[('wb', 'od_w_out', o_)],
              writes=['w_out'], stream='w')
        P.op('dve', lambda e: e.memset(negt, 0.0), writes=['negt'])
        tp = k.bankb[0]
        sbank = [0]

        def next_sbank():
            sbank[0] += 1
            return 1 + (sbank[0] % 2)

        def den_view(ap260):
            return ap260.rearrange('p (m e) -> p m e', e=65)[:, :, 64]

        def masked_exp(b, nn, dst, mask_ap, dname):
            if mask_ap is None:
                P.op('act', lambda e: e.activation(out=dst[0:nn], in_=k.bank[b][0:nn, :], func=AF.Exp), reads=['bank%d' % b],
                     writes=[dname])
            else:
                P.op('act', lambda e: e.activation(out=expf[0:nn], in_=k.bank[b][0:nn, :], func=AF.Exp), reads=['bank%d' % b],
                     writes=['expf'])
                P.op('pool', lambda e: e.tensor_tensor(out=dst[0:nn].rearrange('p (m q) -> p m q', m=4),
                                                       in0=expf[0:nn].rearrange('p (m q) -> p m q', m=4),
                                                       in1=mask_ap.unsqueeze(1).broadcast_to([nn, 4, 128]), op=ALU.mult),
                     reads=['expf', 'tri', 'ntri'], writes=[dname])

        def o2_loads(i):
            P.dma('sp', k.xt[i % 2], xsrc[i * 128:(i + 1) * 128, :], reads=[('x', i)], writes=['xt%d' % (i % 2)], stream='x')
            P.dma('sp', qt[i % 2], qd[i * 128:(i + 1) * 128, :], reads=[('qd', i)], writes=['qt%d' % (i % 2)], stream='x')
            P.dma('sp', ydts[i % 2], ydd[i * 128:(i + 1) * 128, :], reads=[('ydd', i)], writes=['ydt%d' % (i % 2)], stream='x')

        def stage_a(i, g):
            qti = qt[i % 2]
            qtn = 'qt%d' % (i % 2)
            off = 62 - 2 * i
            Q = QN[g]
            qn = 'QN%d' % g
            rdA = rdenA[g]
            rdAn = 'rdenA%d' % g
            for m in range(4):
                h = 4 * g + m
                P.op('pe', lambda e, m=m, h=h: e.transpose(out=tp[0:64, m * 128:(m + 1) * 128], in_=qti[:, h * 64:(h + 1) * 64],
                                                          identity=k.ident), reads=[qtn, 'ident'], writes=['bank0'])
            P.op('act', lambda e: e.copy(out=Q[0:64, :], in_=tp[0:64, 0:512]), reads=['bank0'], writes=[qn + 'lo'])
            chunks = [(0, 128)] + ([(1, 127)] if 8 * i + 6 >= 128 else [])
            for (c, nn) in chunks:
                b = next_sbank()
                P.op('pe', lambda e, c=c, nn=nn, b=b: e.matmul(k.bank[b][0:nn, :], lhsT=kcmpT[:, g, c * 128:c * 128 + nn],
                                                              rhs=Q[0:64, :], start=True, stop=True),
                     reads=['kcmpT', qn + 'lo'], writes=['bank%d' % b])
                full = (16 * (c * 128 + nn - 1) + 31 <= 128 * i)
                if full:
                    P.op('act', lambda e, c=c, nn=nn, b=b: e.activation(out=Pc[0:nn, c, :], in_=k.bank[b][0:nn, :], func=AF.Exp),
                         reads=['bank%d' % b], writes=['Pc'])
                else:
                    tv = float(128 * i - 31 - 2048 * c)
                    P.op('act', lambda e, nn=nn, b=b: e.activation(out=expf[0:nn], in_=k.bank[b][0:nn, :], func=AF.Exp),
                         reads=['bank%d' % b], writes=['expf'])
                    P.op('dve', lambda e, c=c, nn=nn, tv=tv: e.scalar_tensor_tensor(
                        out=Pc[0:nn, c, :].rearrange('p (m q) -> p m q', m=4),
                        in0=cm[0:nn].unsqueeze(1).broadcast_to([nn, 4, 128]), scalar=tv,
                        in1=expf[0:nn].rearrange('p (m q) -> p m q', m=4), op0=ALU.is_le, op1=ALU.mult),
                         reads=['expf', 'cm'], writes=['Pc'])
            for m in range(4):
                for ci, (c, nn) in enumerate(chunks):
                    P.op('pe', lambda e, m=m, c=c, nn=nn, ci=ci: e.matmul(k.bank[3][:, m * 65:(m + 1) * 65],
                                                                        lhsT=Pc[0:nn, c, m * 128:(m + 1) * 128],
                                                                        rhs=vcmp_aug[0:nn, c, g, :], start=(ci == 0),
                                                                        stop=(ci == len(chunks) - 1)),
                         reads=['Pc', 'vcmp'], writes=['bank3'])
            for m in range(4):
                for ci, (c, nn) in enumerate(chunks):
                    P.op('pe', lambda e, m=m, c=c, nn=nn, ci=ci: e.matmul(k.bank[4][:, m * 64:(m + 1) * 64],
                                                                        lhsT=Pc[0:nn, c, m * 128:(m + 1) * 128],
                                                                        rhs=cts[0:nn, c, :], start=(ci == 0),
                                                                        stop=(ci == len(chunks) - 1)),
                         reads=['Pc', 'cts'], writes=['bank4'])
            jts = list(range(max(0, i - 4), i + 1))
            for sl_, jt in enumerate(jts):
                b = next_sbank()
                P.op('pe', lambda e, jt=jt, b=b: e.matmul(k.bank[b], lhsT=kwT[:, g, jt * 128:(jt + 1) * 128], rhs=Q[0:64, :],
                                                         start=True, stop=True), reads=['kwT', qn + 'lo'], writes=['bank%d' % b])
                mk = tri if jt == i else (ntri if jt == i - 4 else None)
                masked_exp(b, 128, PW[:, sl_, :], mk, ('PW', sl_))
            for m in range(4):
                for sl_, jt in enumerate(jts):
                    P.op('pe', lambda e, m=m, jt=jt, sl_=sl_: e.matmul(k.bank[6][:, m * 65:(m + 1) * 65],
                                                                      lhsT=PW[:, sl_, m * 128:(m + 1) * 128],
                                                                      rhs=vw_aug[:, jt, g, :], start=(sl_ == 0),
                                                                      stop=(sl_ == len(jts) - 1)),
                         reads=[('PW', sl_), 'vw_aug'], writes=['bank6'])
            P.op('dve', lambda e: e.tensor_scalar(out=rdA, in0=den_view(k.bank[3][:, 0:260]), scalar1=1e-30, scalar2=None,
                                                  op0=ALU.max), reads=['bank3'], writes=[rdAn])
            P.op('dve', lambda e: e.reciprocal(out=rdA, in_=rdA), reads=[rdAn], writes=[rdAn])
            P.op('dve', lambda e: e.tensor_scalar(out=psl, in0=k.bank[4][:, 0:64], scalar1=rdA[:, 0:1], scalar2=None,
                                                  op0=ALU.mult), reads=['bank4', rdAn], writes=['psl'])
            for m in range(1, 4):
                P.op('dve', lambda e, m=m: e.scalar_tensor_tensor(out=psl, in0=k.bank[4][:, m * 64:(m + 1) * 64],
                                                                  scalar=rdA[:, m:m + 1], in1=psl, op0=ALU.mult, op1=ALU.add),
                     reads=['bank4', rdAn, 'psl'], writes=['psl'])
            P.op('act', lambda e: e.copy(out=ocw[g][:, 0, :], in_=k.bank[3][:, 0:260]), reads=['bank3'], writes=['ocw%d' % g])
            P.op('act', lambda e: e.copy(out=ocw[g][:, 1, :], in_=k.bank[6][:, 0:260]), reads=['bank6'], writes=['ocw%d' % g])
            P.op('dve', lambda e: e.tensor_tensor(out=score, in0=psl, in1=keep[:, off:off + 64], op=ALU.mult),
                 reads=['psl', 'keep'], writes=['score'])
            P.op('dve', lambda e: e.tensor_tensor(out=score, in0=score, in1=addc[:, off:off + 64], op=ALU.add),
                 reads=['score', 'addc'], writes=['score'])
            P.op('dve', lambda e: e.memset(score[:, 0:1], 1.0e4), reads=['score'], writes=['score'])
            P.op('dve', lambda e: e.max(out=m8a, in_=score), reads=['score'], writes=['m8a'])
            P.op('dve', lambda e: e.match_replace(out=sc2, in_to_replace=m8a, in_values=score, imm_value=-2.0),
                 reads=['score', 'm8a'], writes=['sc2'])
            P.op('dve', lambda e: e.max(out=m8b, in_=sc2), reads=['sc2'], writes=['m8b'])
            P.op('dve', lambda e: e.tensor_scalar(out=thr, in0=m8b[:, 7:8], scalar1=0.0, scalar2=None, op0=ALU.max),
                 reads=['m8b'], writes=['thr'])
            P.op('dve', lambda e: e.tensor_scalar(out=negt[:, 64:128], in0=score, scalar1=thr[:, 0:1], scalar2=-30000.0,
                                                  op0=ALU.is_lt, op1=ALU.mult), reads=['score', 'thr'], writes=['negt'])
            P.op('pe', lambda e: e.transpose(out=tp[:, 512:640], in_=negt, identity=k.ident), reads=['negt', 'ident'],
                 writes=['bank0'])
            P.op('act', lambda e: e.copy(out=Q[64:128, :].rearrange('p (m q) -> p m q', m=4),
                                         in_=tp[64:128, 512:640].unsqueeze(1).broadcast_to([64, 4, 128])),
                 reads=['bank0'], writes=[qn + 'hi'])

        def stage_b(i, g):
            Q = QN[g]
            qn = 'QN%d' % g
            ob = 5 if g == 0 else 7
            obn = 'bank%d' % ob
            rdB = rdenB[g]
            rdBn = 'rdenB%d' % g
            coef = coefs[g]
            cfn = 'coef%d' % g
            for jt in range(i + 1):
                b = next_sbank()
                P.op('pe', lambda e, jt=jt, b=b: e.matmul(k.bank[b], lhsT=KE[:, g, jt * 128:(jt + 1) * 128], rhs=Q, start=True,
                                                         stop=True), reads=['KE', qn + 'lo', qn + 'hi'], writes=['bank%d' % b])
                masked_exp(b, 128, PT[:, jt, :], tri if jt == i else None, ('PT', jt))
            for m in range(4):
                for jt in range(i + 1):
                    P.op('pe', lambda e, m=m, jt=jt: e.matmul(k.bank[ob][:, m * 65:(m + 1) * 65],
                                                             lhsT=PT[:, jt, m * 128:(m + 1) * 128], rhs=vs_aug[:, jt, g, :],
                                                             start=(jt == 0), stop=(jt == i)),
                         reads=[('PT', jt), 'vs_aug'], writes=[obn])
            P.op('dve', lambda e: e.tensor_copy(out=rdB[:, 0:4], in_=rdenA[g]), reads=['rdenA%d' % g], writes=[rdBn])
            P.op('dve', lambda e: e.tensor_scalar(out=rdB[:, 4:8], in0=den_view(k.bank[ob][:, 0:260]), scalar1=1e-30, scalar2=None,
                                                  op0=ALU.max), reads=[obn], writes=[rdBn])
            P.op('dve', lambda e: e.tensor_scalar(out=rdB[:, 8:12], in0=den_view(ocw[g][:, 1, :]), scalar1=1e-30, scalar2=None,
                                                  op0=ALU.max), reads=['ocw%d' % g], writes=[rdBn])
            P.op('dve', lambda e: e.reciprocal(out=rdB[:, 4:12], in_=rdB[:, 4:12]), reads=[rdBn], writes=[rdBn])
            P.op('dve', lambda e: e.tensor_tensor(out=coef.rearrange('p (b m) -> p b m', b=3),
                                                  in0=rdB.rearrange('p (b m) -> p b m', b=3),
                                                  in1=gsig[:, i, g * 12:(g + 1) * 12].rearrange('p (m b) -> p b m', b=3),
                                                  op=ALU.mult), reads=[rdBn, ('gsig', i)], writes=[cfn])
            for m in range(4):
                h = 4 * g + m
                ym = yc[:, h * 64:(h + 1) * 64]
                P.op('pool', lambda e, m=m, ym=ym: e.tensor_scalar(out=ym, in0=ocw[g][:, 0, m * 65:m * 65 + 64],
                                                                   scalar1=coef[:, m:m + 1], scalar2=0.0, op0=ALU.mult, op1=ALU.add),
                     reads=['ocw%d' % g, cfn], writes=[('yc', h)])
                P.op('dve', lambda e, m=m, ym=ym: e.scalar_tensor_tensor(out=ym, in0=k.bank[ob][:, m * 65:m * 65 + 64],
                                                                         scalar=coef[:, 4 + m:5 + m], in1=ym, op0=ALU.mult,
                                                                         op1=ALU.add), reads=[obn, cfn, ('yc', h)],
                     writes=[('yc', h)])
                P.op('dve', lambda e, m=m, ym=ym: e.scalar_tensor_tensor(out=ym, in0=ocw[g][:, 1, m * 65:m * 65 + 64],
                                                                         scalar=coef[:, 8 + m:9 + m], in1=ym, op0=ALU.mult,
                                                                         op1=ALU.add), reads=['ocw%d' % g, cfn, ('yc', h)],
                     writes=[('yc', h)])

        def out_proj(i):
            xt = k.xt[i % 2]
            xr = 'xt%d' % (i % 2)
            ydt = ydts[i % 2]
            ydn = 'ydt%d' % (i % 2)
            P.op('act', lambda e: e.copy(out=ycb, in_=yc), reads=[('yc', h) for h in range(8)], writes=['ycb'])
            for c in range(4):
                P.op('pe', lambda e, c=c: e.transpose(out=tp[:, c * 128:(c + 1) * 128], in_=ycb[:, c * 128:(c + 1) * 128],
                                                      identity=k.ident), reads=['ycb', 'ident'], writes=['bank0'])
            for c in range(4):
                P.op('pe', lambda e, c=c: e.transpose(out=tp[:, (4 + c) * 128:(5 + c) * 128], in_=ydt[:, c * 128:(c + 1) * 128],
                                                      identity=k.ident), reads=[ydn, 'ident'], writes=['bank0'])
            P.op('act', lambda e: e.copy(out=yT, in_=tp.rearrange('p (c t) -> p c t', c=8)), reads=['bank0'], writes=['yT'])
            pm = [k.bank[3], k.bank[4]]
            for cb in range(2):
                for kc in range(8):
                    P.op('pe', lambda e, kc=kc, cb=cb: e.matmul(pm[cb], lhsT=yT[:, kc, :], rhs=w_out_sb[:, kc, cb * 512:(cb + 1) * 512],
                                                               start=(kc == 0), stop=(kc == 7)), reads=['yT', 'w_out'],
                         writes=['bank%d' % (3 + cb)])
            post_norm_residual(k, pm, ['bank3', 'bank4'], 'mix_post', xt, xr)
            P.dma('sp', xdst[i * 128:(i + 1) * 128, :], xt, reads=[xr], writes=[('x', i)], stream='xo')

        units = [(i, g) for i in range(NT) for g in range(2)]
        o2_loads(0)
        stage_a(*units[0])
        for n, (i, g) in enumerate(units):
            if n + 1 < len(units):
                ni, ng = units[n + 1]
                if ng == 0:
                    o2_loads(ni)
                stage_a(ni, ng)
            stage_b(i, g)
            if g == 1:
                out_proj(i)

def make_consts():
    c = {}
    Dm = np.zeros((3, 4, 128, 128), np.float32)
    for g, w in enumerate(POOL_WINDOWS):
        for t in range(128):
            lo = max(t + 1 - w, 0)
            for s in range(lo, t + 1):
                Dm[0, g, s, t] += 1.0 / (t + 1 - lo)
            Dm[0, g, t, t] -= 1.0
            for s in range(t + 1 - w, t + 1):
                if s >= 0:
                    Dm[1, g, s, t] += 1.0 / w
                else:
                    Dm[2, g, s + 128, t] += 1.0 / w
            Dm[1, g, t, t] -= 1.0
    c['c_D'] = Dm.astype(ml_dtypes.bfloat16)
    bf = ml_dtypes.bfloat16
    invf = np.concatenate([1.0 / (500000.0 ** (np.arange(0, 16, 2, dtype=np.float32) / 16)),
                           1.0 / (10000.0 ** (np.arange(0, 128, 2, dtype=np.float32) / 128))]).astype(np.float32)
    c['c_invf'] = invf.reshape(1, 72)
    lg = np.log1p(-np.exp2(-5.0 - np.arange(4, dtype=np.float64)))
    idx = np.arange(128, dtype=np.float64)
    rel = idx[None, :] - idx[:, None]
    dec = np.where((rel >= 0)[:, None, :], np.exp(np.maximum(rel, 0)[:, None, :] * lg[None, :, None]), 0.0)
    c['c_decT'] = dec.reshape(128, 512).astype(np.float32)
    xi = np.exp((idx + 1.0)[None, :] * lg[:, None])
    c['c_xi'] = np.broadcast_to(xi.reshape(1, 512), (128, 512)).astype(np.float32).copy()
    c['c_zeta'] = np.exp((127 - idx)[:, None] * lg[None, :]).astype(np.float32)
    c['c_gch'] = np.broadcast_to(np.exp(128 * lg)[None, :], (128, 4)).astype(np.float32).copy()
    p = np.arange(128)
    c['c_tri'] = (p[:, None] <= p[None, :]).astype(np.float32)
    c['c_cm'] = (16.0 * p[:, None] - p[None, :]).astype(np.float32)
    cq = (p >= 64).astype(np.int64)[:, None]
    jj = np.arange(128)[None, :]
    c['c_keep'] = (jj <= 60 + cq).astype(np.float32)
    add = np.zeros((128, 128), np.float32)
    add[(jj == 61 + cq) | (jj == 62 + cq)] = 1.0e4
    add[jj > 62 + cq] = -1.0
    c['c_add'] = add
    n = np.arange(256)
    cs = n * 16
    ss = np.arange(64) * 64
    cts = ((cs[:, None] < ss[None, :] + 64) & (cs[:, None] + 32 > ss[None, :])).astype(np.float32)
    cts[255] = 0
    c['c_cts'] = cts.reshape(2, 128, 64).astype(bf)
    c['c_E'] = (np.arange(4096)[None, :] // 64 == np.arange(64)[:, None]).astype(bf)
    return c


INPUT_NAMES = ["x", "positions", "ln_mix_pre", "ln_mix_post", "ln_ffn_pre", "ln_ffn_post", "ffn_w_gate", "ffn_w_up",
               "ffn_w_down", "ev_w_in", "ev_pool_w", "ev_pool_scale", "ev_sgu_ln_g", "ev_sgu_ln_b", "ev_sgu_w",
               "ev_sgu_b", "ev_w_out", "od_w_in", "od_cmp_k_pos", "od_cmp_k_w1", "od_cmp_k_w2", "od_cmp_v_pos",
               "od_cmp_v_w1", "od_cmp_v_w2", "od_ret_gn_g", "od_w_out"]


def build(shapes, consts, layers=(0, 1, 2, 3), phases=('mix', 'ffn')):
    from contextlib import ExitStack
    nc = bass.Bass("TRN2", target_bir_lowering=False)
    ins = {}
    for n in INPUT_NAMES:
        shp = list(shapes[n])
        if n == 'x':
            shp = [T, D]
        if n == 'positions':
            shp = [1, T]
        ins[n] = nc.dram_tensor(n, shp, I32 if n == 'positions' else F32, kind="ExternalInput").ap()
    for n, v in consts.items():
        ins[n] = nc.dram_tensor(n, list(v.shape), BF16 if v.dtype == ml_dtypes.bfloat16 else F32, kind="ExternalInput").ap()
    y = nc.dram_tensor("y", [T, D], F32, kind="ExternalOutput").ap()
    k = K(nc, layers)
    setup_common(k, ins)
    k.sb2 = k.sb('ssa', [128, 2], F32)
    P = k.P
    xsrc = ins['x']
    for li, l in enumerate(layers):
        load_gains(k, l)
        if li + 1 < len(layers):
            cast_layer(k, layers[li + 1])
        if 'mix' in phases:
            with ExitStack() as es:
                if l % 2 == 0:
                    even_phase(k, l, xsrc, y, es)
                else:
                    odd_phase(k, l, xsrc, y, es)
                P.barrier()
            xsrc = y
        if 'ffn' in phases:
            with ExitStack() as es:
                def sb(name, shape, dt):
                    return es.enter_context(nc.sbuf_tensor('ffs%d_' % l + name, list(shape), dt)).ap()
                k.wd_sb = sb('wd', [128, NFC, 1024], BF16)
                k.xt8 = [sb('xt8_%d' % i, [128, D], F32) for i in range(8)]
                k.wgu = [sb('wgu%d' % i, [128, 2, 8, 512], BF16) for i in range(2)]
                k.hT2 = [sb('hT%d' % i, [128, 8, 512], BF16) for i in range(2)]
                k.actT = sb('actT', [128, NFC, 512], BF16)
                k.sg = [sb('sg%d' % i, [128, 512], F32) for i in range(2)]
                ffn_phase(k, l, xsrc, y)
                P.barrier()
            xsrc = y
    P.finish()
    return nc


_CACHE = {}


def kernel(**inputs):
    consts = make_consts()
    shapes = {n: inputs[n].shape for n in INPUT_NAMES}
    if 'nc' not in _CACHE:
        _CACHE['nc'] = build(shapes, consts)
    nc = _CACHE['nc']
    in_maps = []
    for c in range(4):
        b = c % 4
        m = {n: np.ascontiguousarray(inputs[n]) for n in INPUT_NAMES if n not in ('x', 'positions')}
        m['x'] = np.ascontiguousarray(inputs['x'][b])
        m['positions'] = np.ascontiguousarray(inputs['positions'][b:b + 1]).astype(np.int32)
        m.update(consts)
        in_maps.append(m)
    res = run_bass_kernel_spmd(nc, in_maps, core_ids=list(range(4)))
    out = np.stack([res.results[b]["y"] for b in range(4)], axis=0)
    return out.astype(np.float32)
```

```python
import numpy as np
import ml_dtypes
import concourse.bass as bass
import concourse.mybir as mybir
from concourse.bass_utils import run_bass_kernel_spmd

F32 = mybir.dt.float32
BF16 = mybir.dt.bfloat16
I32 = mybir.dt.int32
AF = mybir.ActivationFunctionType
ALU = mybir.AluOpType
AX = mybir.AxisListType

import os
SAME_ENG_SYNC = os.environ.get("SES", "1") == "1"


class Prog:
    def __init__(self, nc):
        self.nc = nc
        self.E = {'pe': nc.tensor, 'dve': nc.vector, 'act': nc.scalar, 'pool': nc.gpsimd, 'sp': nc.sync}
        self.semh = {k: nc.alloc_semaphore('s_' + k) for k in ['pe', 'dve', 'act', 'pool']}
        self.cnt = {k: 0 for k in self.semh}
        self.seen = {e: {} for e in self.E}
        self.lastw = {}
        self.readers = {}
        self.nwait = 0
        self.dslot = 0
        self.NSLOT = 32
        self.nins = 0

    def _deps(self, reads, writes):
        deps = {}
        for r in reads:
            w = self.lastw.get(r)
            if w and deps.get(w[0], 0) < w[1]:
                deps[w[0]] = w[1]
            if isinstance(r, str) and r.startswith('bank'):
                for k_, v in self.readers.get(r, {}).items():
                    if deps.get(k_, 0) < v:
                        deps[k_] = v
        for w_ in writes:
            w = self.lastw.get(w_)
            if w and deps.get(w[0], 0) < w[1]:
                deps[w[0]] = w[1]
            for k, v in self.readers.get(w_, {}).items():
                if deps.get(k, 0) < v:
                    deps[k] = v
        return deps

    def _wait(self, eng, deps):
        e = self.E[eng]
        seen = self.seen[eng]
        for k, v in deps.items():
            if k == eng and (eng == 'pe' or not SAME_ENG_SYNC):
                continue
            if seen.get(k, 0) >= v:
                continue
            e.wait_ge(self.semh[k], v)
            seen[k] = v
            self.nwait += 1

    def _record(self, tag, reads, writes):
        for w in writes:
            self.lastw[w] = tag
            self.readers[w] = {}
        for r in reads:
            d = self.readers.setdefault(r, {})
            if d.get(tag[0], 0) < tag[1]:
                d[tag[0]] = tag[1]

    def op(self, eng, fn, reads=(), writes=()):
        self._wait(eng, self._deps(reads, writes))
        ins = fn(self.E[eng])
        self.cnt[eng] += 1
        ins.then_inc(self.semh[eng], 1)
        self.nins += 1
        self._record((eng, self.cnt[eng]), reads, writes)

    def dma(self, q, out, in_, reads=(), writes=(), stream='d0', **kw):
        slot = self.dslot % self.NSLOT
        self.dslot += 1
        key = 'd:%d' % slot
        if key not in self.semh:
            self.semh[key] = self.nc.alloc_semaphore('sd_%d' % slot)
            self.cnt[key] = 0
        e = self.E[q]
        if self.cnt[key] > 0 and self.seen[q].get(key, 0) < self.cnt[key]:
            e.wait_ge(self.semh[key], self.cnt[key])
            self.seen[q][key] = self.cnt[key]
        self._wait(q, self._deps(reads, writes))
        e.dma_start(out=out, in_=in_, **kw).then_inc(self.semh[key], 16)
        self.cnt[key] += 16
        self.nins += 1
        self._record((key, self.cnt[key]), reads, writes)

    def barrier(self):
        for eng in self.E:
            deps = {k: v for k, v in self.cnt.items() if v > 0}
            e = self.E[eng]
            for k, v in deps.items():
                if k == eng and eng == 'pe':
                    continue
                if self.seen[eng].get(k, 0) >= v:
                    continue
                e.wait_ge(self.semh[k], v)
                self.seen[eng][k] = v
        self.lastw = {}
        self.readers = {}

    def finish(self, eng='sp'):
        deps = {k: v for k, v in self.cnt.items() if v > 0}
        e = self.E[eng]
        for k, v in deps.items():
            e.wait_ge(self.semh[k], v)


T = 4096
D = 1024
NT = T // 128
FH = 2816
NFC = FH // 128
DEPTH = 4
POOL_WINDOWS = (2, 4, 8, 16)
EPS = 1e-6


def _flat2(ap, c=1024):
    n = len(ap.shape)
    names = ' '.join('d%d' % i for i in range(n))
    f = ap.rearrange('%s -> (%s)' % (names, names)) if n > 1 else ap
    return f.rearrange('(r c) -> r c', c=c)


class K:
    def __init__(self, nc, layers=(0, 1, 2, 3), Tn=T):
        self.nc = nc
        self.P = Prog(nc)
        self.layers = layers
        self.uid = 0

    def sb(self, name, shape, dt):
        return self.nc.alloc_sbuf_tensor(name, list(shape), dt).ap()

    def dram(self, name, shape, dt, kind="Internal"):
        return self.nc.dram_tensor(name, list(shape), dt, kind=kind).ap()


def cast_copy(k, dst, src, res):
    d2 = _flat2(dst)
    s2 = _flat2(src)
    rows = d2.shape[0]
    r0 = 0
    while r0 < rows:
        r1 = min(rows, r0 + 2048)
        k.P.dma('pool', d2[r0:r1, :], s2[r0:r1, :], writes=[res], stream='cast')
        r0 = r1


def cast_layer(k, l):
    ins = k.ins
    order = []
    if l % 2 == 0:
        order += [('ev_w_in', l // 2), ('ev_pool_w', l // 2), ('ev_w_out', l // 2)]
    else:
        order += [('od_w_in', l // 2), ('od_cmp_k_w1', l // 2), ('od_cmp_k_w2', l // 2), ('od_cmp_v_w1', l // 2),
                  ('od_cmp_v_w2', l // 2), ('od_w_out', l // 2)]
    order += [('ffn_w_gate', l), ('ffn_w_up', l), ('ffn_w_down', l)]
    for name, idx in order:
        src = ins[name][idx]
        dst = k.dram('wb_%s_%d' % (name, idx), src.shape, BF16)
        k.wb[(name, idx)] = dst
        cast_copy(k, dst, src, ('wb', name, idx))


def rstd_from_ssq(k, ssq, rstd, tmp, n, eps, tag):
    P = k.P
    P.op('dve', lambda e: e.tensor_scalar(out=tmp, in0=ssq, scalar1=1.0 / n, scalar2=eps, op0=ALU.mult, op1=ALU.add),
         reads=[tag + 'ssq'], writes=[tag + 'tmp'])
    P.op('act', lambda e: e.activation(out=tmp, in_=tmp, func=AF.Sqrt), reads=[tag + 'tmp'], writes=[tag + 'tmp'])
    P.op('dve', lambda e: e.reciprocal(out=rstd, in_=tmp), reads=[tag + 'tmp'], writes=[tag + 'rstd'])


def setup_common(k, ins):
    nc, P = k.nc, k.P
    k.ins = ins
    k.bank = [nc.alloc_psum_tensor('bank%d' % i, [128, 512], F32).ap() for i in range(8)]
    k.bankb = [b.bitcast(BF16) for b in k.bank]
    k.io_f = k.sb('io_f', [128, 128], F32)
    k.iop = k.sb('iop', [128, 1], F32)
    k.ident = k.sb('ident', [128, 128], BF16)
    k.ones_row = k.sb('ones_row', [1, 128], BF16)
    P.op('pool', lambda e: e.iota(k.io_f, pattern=[[1, 128]], base=0, channel_multiplier=0,
                                  allow_small_or_imprecise_dtypes=True), writes=['io_f'])
    P.op('pool', lambda e: e.iota(k.iop, pattern=[[1, 1]], base=0, channel_multiplier=1,
                                  allow_small_or_imprecise_dtypes=True), writes=['iop'])
    P.op('dve', lambda e: e.tensor_scalar(out=k.ident, in0=k.io_f, scalar1=k.iop[:, 0:1], scalar2=None,
                                          op0=ALU.is_equal), reads=['io_f', 'iop'], writes=['ident'])
    P.op('dve', lambda e: e.memset(k.ones_row, 1.0), writes=['ones_row'])
    k.wb = {}
    cast_layer(k, k.layers[0])
    k.xt = [k.sb('xt%d' % s, [128, D], F32) for s in range(3)]
    k.hbs = [k.sb('hb%d' % i, [128, D], BF16) for i in range(2)]
    k.junk = k.sb('junk', [128, D], BF16)
    k.tmpf = k.sb('tmpf', [128, D], F32)
    k.G = {n: k.sb('G_' + n, [128, D], F32) for n in ['mix_pre', 'mix_post', 'ffn_pre', 'ffn_post']}
    k.st = {n: k.sb('st_' + n, [128, 1], F32) for n in ['ssq', 'tmp', 'rstd', 'ssq2', 'tmp2', 'rstd2']}


def load_gains(k, l):
    for n in ['mix_pre', 'mix_post', 'ffn_pre', 'ffn_post']:
        k.P.dma('sp', k.G[n], k.ins['ln_' + n][l:l + 1, :].broadcast_to([128, D]), writes=['G_' + n], stream='small')


def norm_a(k, xt_ap, xres, gname, hbi):
    P = k.P
    st = k.st
    hb = k.hbs[hbi]
    P.op('act', lambda e: e.activation(out=k.junk, in_=xt_ap, func=AF.Square, accum_out=st['ssq']),
         reads=[xres], writes=['junk', 'ssq'])
    rstd_from_ssq(k, st['ssq'], st['rstd'], st['tmp'], D, EPS, '')
    P.op('dve', lambda e: e.scalar_tensor_tensor(out=hb, in0=xt_ap, scalar=st['rstd'][:, 0:1], in1=k.G[gname],
                                                 op0=ALU.mult, op1=ALU.mult),
         reads=[xres, 'rstd', 'G_' + gname], writes=['hb%d' % hbi])


def trans_t(k, hbi, hT_dst, hTres):
    P = k.P
    hb = k.hbs[hbi]
    tp = k.bankb[0]
    for c in range(8):
        P.op('pe', lambda e, c=c: e.transpose(out=tp[:, c * 128:(c + 1) * 128], in_=hb[:, c * 128:(c + 1) * 128],
                                              identity=k.ident), reads=['hb%d' % hbi, 'ident'], writes=['bank0'])
    P.op('act', lambda e: e.copy(out=hT_dst, in_=tp.rearrange('p (c t) -> p c t', c=8)), reads=['bank0'],
         writes=[hTres])


def post_norm_residual(k, pm, pmres, gname, xt_ap, xres):
    P = k.P
    st = k.st
    ssa = k.sb2
    for cb in range(2):
        P.op('act', lambda e, cb=cb: e.activation(out=k.junk[:, cb * 512:(cb + 1) * 512], in_=pm[cb], func=AF.Square,
                                                  accum_out=ssa[:, cb:cb + 1]),
             reads=[pmres[cb]], writes=['junk', 'ssa'])
    P.op('dve', lambda e: e.tensor_tensor(out=st['ssq2'], in0=ssa[:, 0:1], in1=ssa[:, 1:2], op=ALU.add),
         reads=['ssa'], writes=['2ssq'])
    rstd_from_ssq(k, st['ssq2'], st['rstd2'], st['tmp2'], D, EPS, '2')
    for cb in range(2):
        sl = slice(cb * 512, (cb + 1) * 512)
        P.op('dve', lambda e, cb=cb, sl=sl: e.scalar_tensor_tensor(out=k.tmpf[:, sl], in0=pm[cb], scalar=st['rstd2'][:, 0:1],
                                                                   in1=k.G[gname][:, sl], op0=ALU.mult, op1=ALU.mult),
             reads=[pmres[cb], '2rstd', 'G_' + gname], writes=['tmpf'])
    P.op('pool', lambda e: e.tensor_tensor(out=xt_ap, in0=xt_ap, in1=k.tmpf, op=ALU.add), reads=['tmpf', xres],
         writes=[xres])


def ffn_phase(k, l, xsrc, xdst):
    nc, P = k.nc, k.P
    wg, wu, wd = k.wb[('ffn_w_gate', l)], k.wb[('ffn_w_up', l)], k.wb[('ffn_w_down', l)]
    P.dma('sp', k.wd_sb, wd.rearrange('(c p) n -> p c n', p=128), reads=[('wb', 'ffn_w_down', l)], writes=['wd_sb'],
          stream='w')
    groups = [(0, 4), (4, 4), (8, 4), (12, 4), (16, 4), (20, 2)]
    wgv = wg.rearrange('(c p) n -> p c n', p=128)
    wuv = wu.rearrange('(c p) n -> p c n', p=128)
    gi = 0
    NB = T // 512

    def xtile(tb, s):
        j = (tb % 2) * 4 + s
        return k.xt8[j], 'xt8_%d' % j

    def front_a(tb, s):
        t0 = tb * 512 + s * 128
        xt, xr = xtile(tb, s)
        P.dma('sp', xt, xsrc[t0:t0 + 128, :], reads=[('x', t0 // 128)], writes=[xr], stream='x')
        norm_a(k, xt, xr, 'ffn_pre', s % 2)

    def front_t(tb, s):
        trans_t(k, s % 2, k.hT2[tb % 2][:, :, s * 128:(s + 1) * 128], ('hT', tb % 2))

    for s in range(4):
        front_a(0, s)
        front_t(0, s)
    for tb in range(NB):
        hT = k.hT2[tb % 2]
        hTr = ('hT', tb % 2)
        for gidx, (j0, nj) in enumerate(groups):
            pre = gidx < 4 and tb + 1 < NB
            if pre:
                front_a(tb + 1, gidx)
            buf = gi % 2
            gi += 1
            wt = k.wgu[buf]
            P.dma('sp', wt[:, 0, :, 0:nj * 128], wgv[:, :, j0 * 128:(j0 + nj) * 128], reads=[('wb', 'ffn_w_gate', l)],
                  writes=['wgu%d' % buf], stream='w')
            P.dma('sp', wt[:, 1, :, 0:nj * 128], wuv[:, :, j0 * 128:(j0 + nj) * 128], reads=[('wb', 'ffn_w_up', l)],
                  writes=['wgu%d' % buf], stream='w')
            for jj in range(nj):
                j = j0 + jj
                pb = 1 + 2 * (j % 2)
                pg, pu = k.bank[pb], k.bank[pb + 1]
                for kc in range(8):
                    P.op('pe', lambda e, kc=kc, jj=jj: e.matmul(pg, lhsT=wt[:, 0, kc, jj * 128:(jj + 1) * 128], rhs=hT[:, kc, :],
                                                               start=(kc == 0), stop=(kc == 7)),
                         reads=['wgu%d' % buf, hTr], writes=['bank%d' % pb])
                for kc in range(8):
                    P.op('pe', lambda e, kc=kc, jj=jj: e.matmul(pu, lhsT=wt[:, 1, kc, jj * 128:(jj + 1) * 128], rhs=hT[:, kc, :],
                                                               start=(kc == 0), stop=(kc == 7)),
                         reads=['wgu%d' % buf, hTr], writes=['bank%d' % (pb + 1)])
                sg = k.sg[j % 2]
                P.op('act', lambda e: e.activation(out=sg, in_=pg, func=AF.Silu), reads=['bank%d' % pb],
                     writes=['sg%d' % (j % 2)])
                P.op('dve', lambda e, j=j: e.tensor_tensor(out=k.actT[:, j, :], in0=pu, in1=sg, op=ALU.mult),
                     reads=['bank%d' % (pb + 1), 'sg%d' % (j % 2)], writes=[('actT', j)])
            if pre:
                front_t(tb + 1, gidx)
        for s in range(4):
            t0 = tb * 512 + s * 128
            pm = [k.bank[5], k.bank[6]]
            for cb in range(2):
                for kk in range(NFC):
                    P.op('pe', lambda e, kk=kk, cb=cb: e.matmul(pm[cb], lhsT=k.actT[:, kk, s * 128:(s + 1) * 128],
                                                               rhs=k.wd_sb[:, kk, cb * 512:(cb + 1) * 512],
                                                               start=(kk == 0), stop=(kk == NFC - 1)),
                         reads=[('actT', kk), 'wd_sb'], writes=['bank%d' % (5 + cb)])
            xt_, xr_ = xtile(tb, s)
            post_norm_residual(k, pm, ['bank5', 'bank6'], 'ffn_post', xt_, xr_)
            P.dma('sp', xdst[t0:t0 + 128, :], xt_, reads=[xr_], writes=[('x', t0 // 128)], stream='xo')


def even_phase(k, l, xsrc, xdst, es):
    nc, P = k.nc, k.P
    e_ = l // 2
    ins = k.ins

    def sb(name, shape, dt):
        return es.enter_context(nc.sbuf_tensor('evs%d_' % l + name, list(shape), dt)).ap()

    w_in_sb = sb('w_in', [128, 8, 1536], BF16)
    w_out_sb = sb('w_out', [128, 8, 1024], BF16)
    poolw_sb = sb('poolw', [128, 4, 128], BF16)
    Dm_sb = sb('Dm', [128, 3, 4, 128], BF16)
    WmT = sb('WmT', [128, 4, 128], BF16)
    wsf = sb('wsf', [128, 4, 128], F32)
    wsb = sb('wsb', [128, 4, 128], BF16)
    tril = sb('tril', [128, 128], F32)
    Bt = sb('Bt', [128, 512], F32)
    lng = sb('lng', [128, 512], F32)
    lnb = sb('lnb', [128, 512], F32)
    psc = sb('psc', [128, 4], F32)
    a_sb = [sb('a%d' % i, [128, 512], BF16) for i in range(2)]
    uT_sb = sb('uT', [128, 4, 128], BF16)
    vg = sb('vg', [128, 512], F32)
    vn = sb('vn', [128, 512], F32)
    vln = sb('vln', [128, 512], BF16)
    diffT = sb('diffT', [128, 4, 128], BF16)
    yaT = sb('yaT', [128, 4, 128], BF16)
    ybT = sb('ybT', [128, 4, 128], BF16)
    mxb = sb('mxb', [128, 512], F32)
    hT1s = [sb('hT1_%d' % i, [128, 8, 128], BF16) for i in range(2)]
    bst = sb('bst', [128, 6], F32)
    mv = sb('mv', [128, 2], F32)
    lrs = sb('lrs', [128, 1], F32)

    P.dma('sp', w_in_sb, k.wb[('ev_w_in', e_)].rearrange('(c p) n -> p c n', p=128), reads=[('wb', 'ev_w_in', e_)],
          writes=['w_in'], stream='w')
    P.dma('sp', w_out_sb, k.wb[('ev_w_out', e_)].rearrange('(c p) n -> p c n', p=128), reads=[('wb', 'ev_w_out', e_)],
          writes=['w_out'], stream='w')
    P.dma('sp', poolw_sb, k.wb[('ev_pool_w', e_)].rearrange('g c d -> c g d'), reads=[('wb', 'ev_pool_w', e_)],
          writes=['poolw'], stream='w')
    P.dma('sp', Dm_sb, ins['c_D'].rearrange('a g s t -> s a g t'), writes=['Dm'], stream='small')
    P.dma('sp', wsf, ins['ev_sgu_w'][e_].rearrange('g t s -> t g s'), writes=['wsf'], stream='small')
    P.dma('sp', Bt, ins['ev_sgu_b'][e_:e_ + 1].rearrange('o g t -> o (g t)').broadcast_to([128, 512]), writes=['Bt'],
          stream='small')
    P.dma('sp', lng, ins['ev_sgu_ln_g'][e_:e_ + 1, :].broadcast_to([128, 512]), writes=['lng'], stream='small')
    P.dma('sp', lnb, ins['ev_sgu_ln_b'][e_:e_ + 1, :].broadcast_to([128, 512]), writes=['lnb'], stream='small')
    with nc.allow_non_contiguous_dma(reason='tiny per-channel scale'):
        P.dma('sp', psc, ins['ev_pool_scale'][e_].rearrange('(g p) -> p g', p=128), writes=['psc'], stream='small')
    P.op('dve', lambda e: e.tensor_scalar(out=tril, in0=k.io_f, scalar1=k.iop[:, 0:1], scalar2=None, op0=ALU.is_le),
         reads=['io_f', 'iop'], writes=['tril'])
    P.op('dve', lambda e: e.tensor_tensor(out=wsb, in0=wsf, in1=tril.unsqueeze(1).broadcast_to([128, 4, 128]), op=ALU.mult),
         reads=['wsf', 'tril'], writes=['wsb'])
    tp = k.bankb[0]
    for g in range(4):
        P.op('pe', lambda e, g=g: e.transpose(out=tp[:, g * 128:(g + 1) * 128], in_=wsb[:, g, :], identity=k.ident),
             reads=['wsb', 'ident'], writes=['bank0'])
    P.op('act', lambda e: e.copy(out=WmT, in_=tp[:, 0:512].rearrange('p (g t) -> p g t', g=4)), reads=['bank0'],
         writes=['WmT'])

    def front_a(i):
        P.dma('sp', k.xt[i % 3], xsrc[i * 128:(i + 1) * 128, :], reads=[('x', i)], writes=['xt%d' % (i % 3)], stream='x')
        norm_a(k, k.xt[i % 3], 'xt%d' % (i % 3), 'mix_pre', i % 2)

    def front_t(i):
        trans_t(k, i % 2, hT1s[i % 2], 'hT1_%d' % (i % 2))

    front_a(0)
    front_t(0)
    for i in range(NT):
        xt = k.xt[i % 3]
        xr = 'xt%d' % (i % 3)
        hT1 = hT1s[i % 2]
        hT1n = 'hT1_%d' % (i % 2)
        a_cur, a_prev = a_sb[i % 2], a_sb[(i + 1) % 2]
        ar, apr = 'a%d' % (i % 2), 'a%d' % ((i + 1) % 2)
        if i + 1 < NT and i >= 1:
            pass
        pa, pu, pv = k.bank[1], k.bank[2], k.bank[3]
        for kc in range(8):
            P.op('pe', lambda e, kc=kc: e.matmul(pa, lhsT=hT1[:, kc, :], rhs=w_in_sb[:, kc, 0:512], start=(kc == 0),
                                                stop=(kc == 7)), reads=[hT1n, 'w_in'], writes=['bank1'])
        P.op('act', lambda e: e.copy(out=a_cur, in_=pa), reads=['bank1'], writes=[ar])
        for kc in range(8):
            P.op('pe', lambda e, kc=kc: e.matmul(pv, lhsT=hT1[:, kc, :], rhs=w_in_sb[:, kc, 1024:1536], start=(kc == 0),
                                                stop=(kc == 7)), reads=[hT1n, 'w_in'], writes=['bank3'])
        P.op('act', lambda e: e.activation(out=vg, in_=pv, func=AF.Gelu_apprx_tanh), reads=['bank3'], writes=['vg'])
        for c in range(4):
            for kc in range(8):
                P.op('pe', lambda e, kc=kc, c=c: e.matmul(pu[:, c * 128:(c + 1) * 128],
                                                         lhsT=w_in_sb[:, kc, 512 + c * 128:512 + (c + 1) * 128],
                                                         rhs=hT1[:, kc, :], start=(kc == 0), stop=(kc == 7)),
                     reads=[hT1n, 'w_in'], writes=['bank2'])
        P.op('act', lambda e: e.activation(out=uT_sb, in_=pu.rearrange('p (c t) -> p c t', c=4), func=AF.Gelu_apprx_tanh),
             reads=['bank2'], writes=['uT'])
        if i + 1 < NT:
            front_a(i + 1)
        P.op('dve', lambda e: e.bn_stats(out=bst, in_=vg), reads=['vg'], writes=['bst'])
        P.op('dve', lambda e: e.bn_aggr(out=mv, in_=bst), reads=['bst'], writes=['mv'])
        P.op('dve', lambda e: e.tensor_scalar(out=lrs, in0=mv[:, 1:2], scalar1=1e-5, scalar2=None, op0=ALU.add),
             reads=['mv'], writes=['lrs'])
        P.op('act', lambda e: e.activation(out=lrs, in_=lrs, func=AF.Sqrt), reads=['lrs'], writes=['lrs'])
        P.op('dve', lambda e: e.reciprocal(out=lrs, in_=lrs), reads=['lrs'], writes=['lrs'])
        P.op('dve', lambda e: e.tensor_scalar(out=vn, in0=vg, scalar1=mv[:, 0:1], scalar2=lrs[:, 0:1], op0=ALU.subtract,
                                              op1=ALU.mult), reads=['vg', 'mv', 'lrs'], writes=['vn'])
        P.op('pool', lambda e: e.tensor_tensor(out=vn, in0=vn, in1=lng, op=ALU.mult), reads=['vn', 'lng'], writes=['vn'])
        P.op('pool', lambda e: e.tensor_tensor(out=vln, in0=vn, in1=lnb, op=ALU.add), reads=['vn', 'lnb'], writes=['vln'])
        pd_ = k.bank[4]
        for g in range(4):
            first = True
            sl = slice(g * 128, (g + 1) * 128)
            P.op('pe', lambda e, g=g, sl=sl: e.matmul(pd_[:, sl], lhsT=a_cur[:, sl], rhs=Dm_sb[:, 0 if i == 0 else 1, g, :],
                                                     start=True, stop=(i == 0)), reads=[ar, 'Dm'], writes=['bank4'])
            if i > 0:
                P.op('pe', lambda e, g=g, sl=sl: e.matmul(pd_[:, sl], lhsT=a_prev[:, sl], rhs=Dm_sb[:, 2, g, :],
                                                         start=False, stop=True), reads=[apr, 'Dm'], writes=['bank4'])
        P.op('dve', lambda e: e.tensor_copy(out=diffT, in_=pd_.rearrange('p (g t) -> p g t', g=4)), reads=['bank4'],
             writes=['diffT'])
        pya = k.bank[5]
        for g in range(4):
            P.op('pe', lambda e, g=g: e.matmul(pya[:, g * 128:(g + 1) * 128], lhsT=poolw_sb[:, g, :], rhs=diffT[:, g, :],
                                              start=True, stop=True), reads=['poolw', 'diffT'], writes=['bank5'])
        P.op('dve', lambda e: e.tensor_tensor(out=yaT, in0=pya.rearrange('p (g t) -> p g t', g=4),
                                              in1=psc.unsqueeze(2).broadcast_to([128, 4, 128]), op=ALU.mult),
             reads=['bank5', 'psc'], writes=['yaT'])
        pmx = k.bank[4]
        for g in range(4):
            P.op('pe', lambda e, g=g: e.matmul(pmx[:, g * 128:(g + 1) * 128], lhsT=vln[:, g * 128:(g + 1) * 128],
                                              rhs=WmT[:, g, :], start=True, stop=True), reads=['vln', 'WmT'],
                 writes=['bank4'])
        P.op('dve', lambda e: e.tensor_tensor(out=mxb, in0=pmx, in1=Bt, op=ALU.add), reads=['bank4', 'Bt'],
             writes=['mxb'])
        P.op('pool', lambda e: e.tensor_tensor(out=ybT, in0=mxb.rearrange('p (g t) -> p g t', g=4), in1=uT_sb, op=ALU.mult),
             reads=['mxb', 'uT'], writes=['ybT'])
        if i + 1 < NT:
            front_t(i + 1)
        pm = [k.bank[6], k.bank[7]]
        for cb in range(2):
            for kc in range(8):
                lh = yaT[:, kc, :] if kc < 4 else ybT[:, kc - 4, :]
                P.op('pe', lambda e, kc=kc, cb=cb, lh=lh: e.matmul(pm[cb], lhsT=lh, rhs=w_out_sb[:, kc, cb * 512:(cb + 1) * 512],
                                                                  start=(kc == 0), stop=(kc == 7)),
                     reads=['yaT', 'ybT', 'w_out'], writes=['bank%d' % (6 + cb)])
        post_norm_residual(k, pm, ['bank6', 'bank7'], 'mix_post', xt, xr)
        P.dma('sp', xdst[i * 128:(i + 1) * 128, :], xt, reads=[xr], writes=[('x', i)], stream='xo')


def odd_phase(k, l, xsrc, xdst, es):
    from contextlib import ExitStack
    nc, P = k.nc, k.P
    o_ = l // 2
    ins = k.ins
    PI = 3.14159265358979

    def sbs(stack, name, shape, dt):
        return stack.enter_context(nc.sbuf_tensor('od%d_' % l + name, list(shape), dt)).ap()

    def sb(name, shape, dt):
        return sbs(es, name, shape, dt)

    qd = k.dram('qd%d' % l, [T, 512], BF16)
    ydd = k.dram('ydd%d' % l, [T, 512], BF16)
    KE = sb('KE', [128, 2, T], BF16)
    kwT = sb('kwT', [64, 2, T], BF16)
    kcvd = k.dram('kcvd%d' % l, [4, 64, T], BF16)
    ropeS = k.dram('ropeS%d' % l, [128, NT, 72], F32)
    ropeC = k.dram('ropeC%d' % l, [128, NT, 72], F32)
    vs_aug = sb('vs_aug', [128, NT, 2, 65], BF16)
    vw_aug = sb('vw_aug', [128, NT, 2, 65], BF16)
    gsig = sb('gsig', [128, NT, 24], F32)
    kcmpT = sb('kcmpT', [64, 2, 256], BF16)
    vcmp_aug = sb('vcmp', [128, 2, 2, 65], BF16)
    tri = sb('tri', [128, 128], F32)
    ntri = sb('ntri', [128, 128], F32)
    cm = sb('cm', [128, 128], F32)
    keep = sb('keep', [128, 128], F32)
    addc = sb('addc', [128, 128], F32)
    cts = sb('cts', [128, 2, 64], BF16)
    decT = sb('decT', [128, 512], F32)
    xi = sb('xi', [128, 512], F32)
    zeta = sb('zeta', [128, 4], F32)
    gch = sb('gch', [128, 4], F32)
    gng = sb('gng', [128, 512], F32)
    for nm, t_, src in [('tri', tri, 'c_tri'), ('cm', cm, 'c_cm'), ('keep', keep, 'c_keep'), ('addc', addc, 'c_add'),
                        ('decT', decT, 'c_decT'), ('xi', xi, 'c_xi'), ('zeta', zeta, 'c_zeta'), ('gch', gch, 'c_gch')]:
        P.dma('sp', t_, ins[src], writes=[nm], stream='small')
    P.dma('sp', cts, ins['c_cts'].rearrange('c n j -> n c j'), writes=['cts'], stream='small')
    P.dma('sp', KE[64:128, 0, :], ins['c_E'], writes=['KE'], stream='small')
    P.dma('sp', KE[64:128, 1, :], ins['c_E'], writes=['KE'], stream='small')
    P.dma('sp', gng, ins['od_ret_gn_g'][o_:o_ + 1, :].broadcast_to([128, 512]), writes=['gng'], stream='small')
    P.op('dve', lambda e: e.tensor_scalar(out=ntri, in0=tri, scalar1=-1.0, scalar2=1.0, op0=ALU.mult, op1=ALU.add),
         reads=['tri'], writes=['ntri'])
    P.op('pool', lambda e: e.memset(vs_aug, 1.0), writes=['vs_aug'])
    P.op('pool', lambda e: e.memset(vw_aug, 1.0), writes=['vw_aug'])
    P.op('pool', lambda e: e.memset(vcmp_aug, 1.0), writes=['vcmp'])
    with ExitStack() as ts:
        posi = sbs(ts, 'posi', [128, NT], I32)
        sinT = sbs(ts, 'sinT', [128, NT, 72], F32)
        cosT = sbs(ts, 'cosT', [128, NT, 72], F32)
        posf = sbs(ts, 'posf', [128, NT], F32)
        invf = sbs(ts, 'invf', [128, 72], F32)
        ang = sbs(ts, 'ang', [128, NT, 72], F32)
        arg = sbs(ts, 'arg', [128, NT, 72], F32)
        kf = sbs(ts, 'kf', [128, NT, 72], F32)
        ki = sbs(ts, 'ki', [128, NT, 72], I32)
        with nc.allow_non_contiguous_dma(reason='positions to token-on-partition layout'):
            P.dma('sp', posi, ins['positions'].rearrange('o (i p) -> p (o i)', p=128), writes=['posi'], stream='small')
        P.dma('sp', invf, ins['c_invf'].broadcast_to([128, 72]), writes=['invf'], stream='small')
        P.op('dve', lambda e: e.tensor_copy(out=posf, in_=posi), reads=['posi'], writes=['posf'])
        P.op('dve', lambda e: e.tensor_tensor(out=ang, in0=posf.unsqueeze(2).broadcast_to([128, NT, 72]),
                                              in1=invf.unsqueeze(1).broadcast_to([128, NT, 72]), op=ALU.mult),
             reads=['posf', 'invf'], writes=['ang'])
        for shift, dst, dn in [(0.0, sinT, 'sinT'), (PI / 2, cosT, 'cosT')]:
            P.op('dve', lambda e, shift=shift: e.tensor_scalar(out=arg, in0=ang, scalar1=shift, scalar2=None, op0=ALU.add),
                 reads=['ang'], writes=['arg'])
            P.op('dve', lambda e: e.tensor_scalar(out=kf, in0=arg, scalar1=1.0 / (2 * PI), scalar2=None, op0=ALU.mult),
                 reads=['arg'], writes=['kf'])
            P.op('dve', lambda e: e.tensor_copy(out=ki, in_=kf), reads=['kf'], writes=['ki'])
            P.op('dve', lambda e: e.tensor_copy(out=kf, in_=ki), reads=['ki'], writes=['kf'])
            P.op('dve', lambda e: e.scalar_tensor_tensor(out=arg, in0=kf, scalar=-6.28125, in1=arg, op0=ALU.mult, op1=ALU.add),
                 reads=['kf', 'arg'], writes=['arg'])
            P.op('dve', lambda e: e.scalar_tensor_tensor(out=arg, in0=kf, scalar=-(2 * PI - 6.28125), in1=arg, op0=ALU.mult,
                                                         op1=ALU.add), reads=['kf', 'arg'], writes=['arg'])
            P.op('dve', lambda e: e.tensor_scalar(out=arg, in0=arg, scalar1=3.1415925, scalar2=-3.1415925, op0=ALU.min,
                                                  op1=ALU.max), reads=['arg'], writes=['arg'])
            P.op('act', lambda e, dst=dst: e.activation(out=dst, in_=arg, func=AF.Sin), reads=['arg'], writes=[dn])
        P.dma('sp', ropeS, sinT, reads=['sinT'], writes=['ropeS'], stream='xo')
        P.dma('sp', ropeC, cosT, reads=['cosT'], writes=['ropeC'], stream='xo')
        P.barrier()

    with ExitStack() as s1:
        w_in_sb = sbs(s1, 'w_in', [128, 8, 3352], BF16)
        hT1s = [sbs(s1, 'hT1_%d' % j, [128, 8, 128], BF16) for j in range(2)]
        cur = {}
        csb = [sbs(s1, 'csb%d' % j, [128, 2, 72], F32) for j in range(2)]
        kcv_t = sbs(s1, 'kcv_t', [64, 4, 128], BF16)
        zq = sbs(s1, 'zq', [128, 512], F32)
        qb = sbs(s1, 'qb', [128, 512], BF16)
        zkv = sbs(s1, 'zkv', [128, 768], F32)
        kvb = sbs(s1, 'kvb', [128, 768], BF16)
        rt = [sbs(s1, 'rt%d' % j, [128, 512], F32) for j in range(4)]
        zrq = sbs(s1, 'zrq', [128, 512], F32)
        zrk = sbs(s1, 'zrk', [128, 512], F32)
        rqb = sbs(s1, 'rqb', [128, 512], BF16)
        rkb = sbs(s1, 'rkb', [128, 512], BF16)
        rkr = sbs(s1, 'rkr', [128, 512], F32)
        rkz = sbs(s1, 'rkz', [128, 512], BF16)
        rvb = sbs(s1, 'rvb', [128, 512], BF16)
        gsl = sbs(s1, 'gsl', [128, 512], F32)
        qkT = sbs(s1, 'qkT', [128, 8, 128], BF16)
        qxiT = sbs(s1, 'qxiT', [128, 512], BF16)
        STs = sbs(s1, 'STs', [128, 512], BF16)
        state = sbs(s1, 'state', [128, 512], F32)
        stbf = sbs(s1, 'stbf', [128, 512], BF16)
        bst4 = sbs(s1, 'bst4', [128, 4, 6], F32)
        mv4 = sbs(s1, 'mv4', [128, 4, 2], F32)
        rs4 = sbs(s1, 'rs4', [128, 4], F32)
        on = sbs(s1, 'on', [128, 512], F32)
        ydb = sbs(s1, 'ydb', [128, 512], BF16)
        P.dma('sp', w_in_sb, k.wb[('od_w_in', o_)].rearrange('(c p) n -> p c n', p=128), reads=[('wb', 'od_w_in', o_)],
              writes=['w_in'], stream='w')
        P.op('dve', lambda e: e.memset(state, 0.0), writes=['state'])

        def proj(bank, c0, c1):
            for kc in range(8):
                P.op('pe', lambda e, kc=kc: e.matmul(k.bank[bank][:, 0:c1 - c0], lhsT=cur['hT'][:, kc, :], rhs=w_in_sb[:, kc, c0:c1],
                                                    start=(kc == 0), stop=(kc == 7)), reads=[cur['hTn'], 'w_in'],
                     writes=['bank%d' % bank])

        def rope(src, dst, nh, hd, half, c_, s_, csn, sname, dname):
            sv = src.rearrange('p (h d) -> p h d', h=nh)
            dv = dst.rearrange('p (h d) -> p h d', h=nh)
            x1, x2 = sv[:, :, 0:half], sv[:, :, half:2 * half]
            cb = c_.unsqueeze(1).broadcast_to([128, nh, half])
            sb_ = s_.unsqueeze(1).broadcast_to([128, nh, half])
            t = [r_[:, 0:nh * half].rearrange('p (h d) -> p h d', h=nh) for r_ in rt]
            P.op('dve', lambda e: e.tensor_tensor(out=t[0], in0=x1, in1=cb, op=ALU.mult), reads=[sname, csn], writes=['rt0'])
            P.op('pool', lambda e: e.tensor_tensor(out=t[1], in0=x2, in1=sb_, op=ALU.mult), reads=[sname, csn], writes=['rt1'])
            P.op('dve', lambda e: e.tensor_tensor(out=t[2], in0=x2, in1=cb, op=ALU.mult), reads=[sname, csn], writes=['rt2'])
            P.op('pool', lambda e: e.tensor_tensor(out=t[3], in0=x1, in1=sb_, op=ALU.mult), reads=[sname, csn], writes=['rt3'])
            if 2 * half < hd:
                P.op('act', lambda e: e.copy(out=dst, in_=src), reads=[sname], writes=[dname])
            P.op('dve', lambda e: e.tensor_tensor(out=dv[:, :, 0:half], in0=t[0], in1=t[1], op=ALU.subtract),
                 reads=['rt0', 'rt1'], writes=[dname])
            P.op('pool', lambda e: e.tensor_tensor(out=dv[:, :, half:2 * half], in0=t[2], in1=t[3], op=ALU.add),
                 reads=['rt2', 'rt3'], writes=[dname])

        def o1_front_a(i):
            P.dma('sp', k.xt[i % 3], xsrc[i * 128:(i + 1) * 128, :], reads=[('x', i)], writes=['xt%d' % (i % 3)], stream='x')
            norm_a(k, k.xt[i % 3], 'xt%d' % (i % 3), 'mix_pre', i % 2)

        def o1_front_t(i):
            trans_t(k, i % 2, hT1s[i % 2], 'hT1_%d' % (i % 2))

        o1_front_a(0)
        o1_front_t(0)
        for i in range(NT):
            cur['hT'] = hT1s[i % 2]
            cur['hTn'] = 'hT1_%d' % (i % 2)
            cs_ = csb[i % 2]
            csn = 'csb%d' % (i % 2)
            P.dma('sp', cs_[:, 0, :], ropeC[:, i, :], reads=['ropeC'], writes=[csn], stream='small')
            P.dma('sp', cs_[:, 1, :], ropeS[:, i, :], reads=['ropeS'], writes=[csn], stream='small')
            cn, sn = cs_[:, 0, 0:8], cs_[:, 1, 0:8]
            cr, sr = cs_[:, 0, 8:72], cs_[:, 1, 8:72]
            proj(1, 0, 512)
            P.op('act', lambda e: e.activation(out=zq, in_=k.bank[1], func=AF.Copy, scale=0.125), reads=['bank1'], writes=['zq'])
            rope(zq, qb, 8, 64, 8, cn, sn, csn, 'zq', 'qb')
            P.dma('sp', qd[i * 128:(i + 1) * 128, :], qb, reads=['qb'], writes=[('qd', i)], stream='xo')
            proj(2, 512, 1024)
            proj(3, 1024, 1304)
            P.op('act', lambda e: e.copy(out=zkv[:, 0:512], in_=k.bank[2]), reads=['bank2'], writes=['zkv'])
            P.op('act', lambda e: e.copy(out=zkv[:, 512:768], in_=k.bank[3][:, 0:256]), reads=['bank3'], writes=['zkv'])
            P.op('act', lambda e: e.activation(out=gsig[:, i, :], in_=k.bank[3][:, 256:280], func=AF.Sigmoid),
                 reads=['bank3'], writes=[('gsig', i)])
            kv4 = zkv.rearrange('p (a b g d) -> p a b g d', a=3, b=2, g=2)[:, :, 0, :, :]
            x1, x2 = kv4[:, :, :, 0:8], kv4[:, :, :, 8:16]
            cb = cn.unsqueeze(1).unsqueeze(1).broadcast_to([128, 3, 2, 8])
            sb_ = sn.unsqueeze(1).unsqueeze(1).broadcast_to([128, 3, 2, 8])
            t = [r_[:, 0:48].rearrange('p (a g d) -> p a g d', a=3, g=2) for r_ in rt]
            P.op('dve', lambda e: e.tensor_tensor(out=t[0], in0=x1, in1=cb, op=ALU.mult), reads=['zkv', csn], writes=['rt0'])
            P.op('dve', lambda e: e.tensor_tensor(out=t[1], in0=x2, in1=sb_, op=ALU.mult), reads=['zkv', csn], writes=['rt1'])
            P.op('dve', lambda e: e.tensor_tensor(out=t[2], in0=x2, in1=cb, op=ALU.mult), reads=['zkv', csn], writes=['rt2'])
            P.op('dve', lambda e: e.tensor_tensor(out=t[3], in0=x1, in1=sb_, op=ALU.mult), reads=['zkv', csn], writes=['rt3'])
            P.op('dve', lambda e: e.tensor_tensor(out=x1, in0=t[0], in1=t[1], op=ALU.subtract), reads=['rt0', 'rt1'], writes=['zkv'])
            P.op('dve', lambda e: e.tensor_tensor(out=x2, in0=t[2], in1=t[3], op=ALU.add), reads=['rt2', 'rt3'], writes=['zkv'])
            P.op('act', lambda e: e.copy(out=kvb, in_=zkv), reads=['zkv'], writes=['kvb'])
            P.op('pool', lambda e: e.tensor_copy(out=vs_aug[:, i, :, 0:64], in_=kvb[:, 384:512].rearrange('p (g d) -> p g d', g=2)),
                 reads=['kvb'], writes=['vs_aug'])
            P.op('pool', lambda e: e.tensor_copy(out=vw_aug[:, i, :, 0:64], in_=kvb[:, 640:768].rearrange('p (g d) -> p g d', g=2)),
                 reads=['kvb'], writes=['vw_aug'])
            tp = k.bankb[0]
            srcs = [0, 64, 128, 192, 512, 576, 256, 320]
            for j, c0 in enumerate(srcs):
                P.op('pe', lambda e, j=j, c0=c0: e.transpose(out=tp[0:64, j * 128:(j + 1) * 128], in_=kvb[:, c0:c0 + 64],
                                                            identity=k.ident), reads=['kvb', 'ident'], writes=['bank0'])
            P.op('act', lambda e: e.copy(out=kcv_t, in_=tp[0:64, 0:512].rearrange('p (j t) -> p j t', j=4)), reads=['bank0'],
                 writes=['kcv_t'])
            P.dma('sp', kcvd[:, :, i * 128:(i + 1) * 128].rearrange('j p t -> p j t'), kcv_t, reads=['kcv_t'], writes=['kcvd'],
                  stream='xo')
            P.op('act', lambda e: e.copy(out=kwT[:, :, i * 128:(i + 1) * 128],
                                         in_=tp[0:64, 512:768].rearrange('p (j t) -> p j t', j=2)), reads=['bank0'], writes=['kwT'])
            P.op('act', lambda e: e.copy(out=KE[0:64, :, i * 128:(i + 1) * 128],
                                         in_=tp[0:64, 768:1024].rearrange('p (j t) -> p j t', j=2)), reads=['bank0'], writes=['KE'])
            proj(4, 1304, 1816)
            proj(5, 1816, 2328)
            proj(6, 2328, 2840)
            proj(7, 2840, 3352)
            if i + 1 < NT:
                o1_front_a(i + 1)
            P.op('act', lambda e: e.copy(out=zrq, in_=k.bank[4]), reads=['bank4'], writes=['zrq'])
            P.op('act', lambda e: e.activation(out=zrk, in_=k.bank[5], func=AF.Copy, scale=128.0 ** -0.5), reads=['bank5'],
                 writes=['zrk'])
            P.op('act', lambda e: e.copy(out=rvb, in_=k.bank[6]), reads=['bank6'], writes=['rvb'])
            P.op('act', lambda e: e.activation(out=gsl, in_=k.bank[7], func=AF.Silu), reads=['bank7'], writes=['gsl'])
            rope(zrq, rqb, 4, 128, 64, cr, sr, csn, 'zrq', 'rqb')
            rope(zrk, rkr, 4, 128, 64, cr, sr, csn, 'zrk', 'rkr')
            P.op('act', lambda e: e.copy(out=rkb, in_=rkr), reads=['rkr'], writes=['rkb'])
            P.op('dve', lambda e: e.tensor_tensor(out=rkz.rearrange('p (h d) -> p h d', h=4),
                                                  in0=rkr.rearrange('p (h d) -> p h d', h=4),
                                                  in1=zeta.unsqueeze(2).broadcast_to([128, 4, 128]), op=ALU.mult),
                 reads=['rkr', 'zeta'], writes=['rkz'])
            for h in range(4):
                P.op('pe', lambda e, h=h: e.transpose(out=tp[:, h * 128:(h + 1) * 128], in_=rqb[:, h * 128:(h + 1) * 128],
                                                      identity=k.ident), reads=['rqb', 'ident'], writes=['bank0'])
            for h in range(4):
                P.op('pe', lambda e, h=h: e.transpose(out=tp[:, (4 + h) * 128:(5 + h) * 128], in_=rkb[:, h * 128:(h + 1) * 128],
                                                      identity=k.ident), reads=['rkb', 'ident'], writes=['bank0'])
            P.op('act', lambda e: e.copy(out=qkT, in_=tp.rearrange('p (c t) -> p c t', c=8)), reads=['bank0'], writes=['qkT'])
            for h in range(4):
                P.op('pe', lambda e, h=h: e.matmul(k.bank[5][:, h * 128:(h + 1) * 128], lhsT=qkT[:, 4 + h, :], rhs=qkT[:, h, :],
                                                  start=True, stop=True), reads=['qkT'], writes=['bank5'])
            P.op('dve', lambda e: e.tensor_tensor(out=STs, in0=k.bank[5], in1=decT, op=ALU.mult), reads=['bank5', 'decT'],
                 writes=['STs'])
            P.op('pool', lambda e: e.tensor_tensor(out=qxiT, in0=qkT[:, 0:4, :].rearrange('p h t -> p (h t)'), in1=xi, op=ALU.mult),
                 reads=['qkT', 'xi'], writes=['qxiT'])
            for h in range(4):
                hs = slice(h * 128, (h + 1) * 128)
                P.op('pe', lambda e, hs=hs: e.matmul(k.bank[7][:, hs], lhsT=STs[:, hs], rhs=rvb[:, hs], start=True, stop=(i == 0)),
                     reads=['STs', 'rvb'], writes=['bank7'])
                if i > 0:
                    P.op('pe', lambda e, hs=hs: e.matmul(k.bank[7][:, hs], lhsT=qxiT[:, hs], rhs=stbf[:, hs], start=False, stop=True),
                         reads=['qxiT', 'stbf'], writes=['bank7'])
            for h in range(4):
                hs = slice(h * 128, (h + 1) * 128)
                P.op('pe', lambda e, hs=hs: e.matmul(k.bank[6][:, hs], lhsT=rkz[:, hs], rhs=rvb[:, hs], start=True, stop=True),
                     reads=['rkz', 'rvb'], writes=['bank6'])
            P.op('dve', lambda e: e.tensor_tensor(out=state.rearrange('p (h d) -> p h d', h=4),
                                                  in0=state.rearrange('p (h d) -> p h d', h=4),
                                                  in1=gch.unsqueeze(2).broadcast_to([128, 4, 128]), op=ALU.mult),
                 reads=['state', 'gch'], writes=['state'])
            P.op('dve', lambda e: e.tensor_tensor(out=state, in0=k.bank[6], in1=state, op=ALU.add), reads=['bank6', 'state'],
                 writes=['state'])
            P.op('act', lambda e: e.copy(out=stbf, in_=state), reads=['state'], writes=['stbf'])
            if i + 1 < NT:
                o1_front_t(i + 1)
            for h in range(4):
                P.op('dve', lambda e, h=h: e.bn_stats(out=bst4[:, h, :], in_=k.bank[7][:, h * 128:(h + 1) * 128]),
                     reads=['bank7'], writes=['bst4'])
            for h in range(4):
                P.op('dve', lambda e, h=h: e.bn_aggr(out=mv4[:, h, :], in_=bst4[:, h, :]), reads=['bst4'], writes=['mv4'])
            P.op('dve', lambda e: e.tensor_scalar(out=rs4, in0=mv4[:, :, 1], scalar1=1e-5, scalar2=None, op0=ALU.add),
                 reads=['mv4'], writes=['rs4'])
            P.op('act', lambda e: e.activation(out=rs4, in_=rs4, func=AF.Sqrt), reads=['rs4'], writes=['rs4'])
            P.op('dve', lambda e: e.reciprocal(out=rs4, in_=rs4), reads=['rs4'], writes=['rs4'])
            for h in range(4):
                hs = slice(h * 128, (h + 1) * 128)
                P.op('dve', lambda e, h=h, hs=hs: e.tensor_scalar(out=on[:, hs], in0=k.bank[7][:, hs], scalar1=mv4[:, h, 0:1],
                                                                  scalar2=rs4[:, h:h + 1], op0=ALU.subtract, op1=ALU.mult),
                     reads=['bank7', 'mv4', 'rs4'], writes=['on'])
            P.op('pool', lambda e: e.tensor_tensor(out=on, in0=on, in1=gng, op=ALU.mult), reads=['on', 'gng'], writes=['on'])
            P.op('pool', lambda e: e.tensor_tensor(out=ydb, in0=on, in1=gsl, op=ALU.mult), reads=['on', 'gsl'], writes=['ydb'])
            P.dma('sp', ydd[i * 128:(i + 1) * 128, :], ydb, reads=['ydb'], writes=[('ydd', i)], stream='xo')

        P.barrier()
        s1.close()
        s1c = ExitStack()
        w1 = sbs(s1c, 'w1', [64, 32, 128], BF16)
        w2 = sbs(s1c, 'w2', [128, 64], BF16)
        posf32 = sbs(s1c, 'posf32', [64, 32], F32)
        posT = sbs(s1c, 'posT', [64, 32], BF16)
        cbias = sbs(s1c, 'cbias', [128, 1], F32)
        ghT = sbs(s1c, 'ghT', [128, 256], BF16)
        csrc = sbs(s1c, 'csrc', [64, T], BF16)
        for kind, (n1, n2, npos) in enumerate([('od_cmp_k_w1', 'od_cmp_k_w2', 'od_cmp_k_pos'),
                                               ('od_cmp_v_w1', 'od_cmp_v_w2', 'od_cmp_v_pos')]):
            P.dma('sp', w1, k.wb[(n1, o_)].rearrange('(l d) j -> d l j', d=64), reads=[('wb', n1, o_)], writes=['w1'], stream='w')
            P.dma('sp', w2, k.wb[(n2, o_)], reads=[('wb', n2, o_)], writes=['w2'], stream='w')
            with nc.allow_non_contiguous_dma(reason='tiny pos-emb transpose'):
                P.dma('sp', posf32, ins[npos][o_].rearrange('l d -> d l'), writes=['posf32'], stream='small')
            P.op('dve', lambda e: e.tensor_copy(out=posT, in_=posf32), reads=['posf32'], writes=['posT'])
            for g in range(2):
                P.dma('sp', csrc, kcvd[2 * kind + g], reads=['kcvd'], writes=['csrc'], stream='w')
                srcv = csrc.rearrange('p (n s) -> p n s', s=16)
                hb_ = k.bank[1]
                for l_ in range(32):
                    P.op('pe', lambda e, l_=l_: e.matmul(hb_[:, 0:255], lhsT=w1[:, l_, :],
                                                        rhs=(srcv[:, 0:255, l_] if l_ < 16 else srcv[:, 1:256, l_ - 16]),
                                                        start=(l_ == 0), stop=(l_ == 31)),
                         reads=['w1', 'csrc'], writes=['bank1'])
                for l_ in range(32):
                    P.op('pe', lambda e, l_=l_: e.matmul(hb_[:, 256:257], lhsT=w1[:, l_, :], rhs=posT[:, l_:l_ + 1],
                                                        start=(l_ == 0), stop=(l_ == 31)), reads=['w1', 'posT'], writes=['bank1'])
                P.op('dve', lambda e: e.tensor_copy(out=cbias, in_=hb_[:, 256:257]), reads=['bank1'], writes=['cbias'])
                P.op('dve', lambda e: e.memset(ghT[:, 255:256], 0.0), writes=['ghT'])
                P.op('act', lambda e: e.activation(out=ghT[:, 0:255], in_=hb_[:, 0:255], func=AF.Gelu_apprx_tanh,
                                                   bias=cbias[:, 0:1]), reads=['bank1', 'cbias'], writes=['ghT'])
                if kind == 0:
                    P.op('pe', lambda e: e.matmul(k.bank[2][0:64, 0:256], lhsT=w2, rhs=ghT, start=True, stop=True),
                         reads=['w2', 'ghT'], writes=['bank2'])
                    P.op('act', lambda e, g=g: e.copy(out=kcmpT[:, g, :], in_=k.bank[2][0:64, 0:256]), reads=['bank2'],
                         writes=['kcmpT'])
                else:
                    for c in range(2):
                        P.op('pe', lambda e, c=c: e.matmul(k.bank[2][:, c * 64:(c + 1) * 64], lhsT=ghT[:, c * 128:(c + 1) * 128],
                                                          rhs=w2, start=True, stop=True), reads=['w2', 'ghT'], writes=['bank2'])
                    P.op('act', lambda e, g=g: e.copy(out=vcmp_aug[:, :, g, 0:64],
                                                      in_=k.bank[2][:, 0:128].rearrange('p (c d) -> p c d', c=2)),
                         reads=['bank2'], writes=['vcmp'])
        P.barrier()
        s1c.close()

    with ExitStack() as s2:
        w_out_sb = sbs(s2, 'w_out', [128, 8, 1024], BF16)
        PT = sbs(s2, 'PT', [128, NT, 512], BF16)
        PW = sbs(s2, 'PW', [128, 5, 512], BF16)
        Pc = sbs(s2, 'Pc', [128, 2, 512], BF16)
        QN = [sbs(s2, 'QN%d' % g, [128, 512], BF16) for g in range(2)]
        qt = [sbs(s2, 'qt%d' % j, [128, 512], BF16) for j in range(2)]
        ydts = [sbs(s2, 'ydt%d' % j, [128, 512], BF16) for j in range(2)]
        negt = sbs(s2, 'negt', [128, 128], BF16)
        expf = sbs(s2, 'expf', [128, 512], F32)
        score = sbs(s2, 'score', [128, 64], F32)
        sc2 = sbs(s2, 'sc2', [128, 64], F32)
        m8a = sbs(s2, 'm8a', [128, 8], F32)
        m8b = sbs(s2, 'm8b', [128, 8], F32)
        thr = sbs(s2, 'thr', [128, 1], F32)
        psl = sbs(s2, 'psl', [128, 64], F32)
        rdenA = [sbs(s2, 'rdenA%d' % g, [128, 4], F32) for g in range(2)]
        rdenB = [sbs(s2, 'rdenB%d' % g, [128, 12], F32) for g in range(2)]
        coefs = [sbs(s2, 'coef%d' % g, [128, 12], F32) for g in range(2)]
        ocw = [sbs(s2, 'ocw%d' % g, [128, 2, 260], F32) for g in range(2)]
        yc = sbs(s2, 'yc', [128, 512], F32)
        ycb = sbs(s2, 'ycb', [128, 512], BF16)
        yT = sbs(s2, 'yT', [128, 8, 128], BF16)
        P.dma('sp', w_out_sb, k.wb[('od_w_out', o_)].rearrange('(c p) n -> p c n', p=128), reads=[('wb', 'od_w_out', o_)],
              writes=['w_out'], stream='w')
        P.op('dve', lambda e: e.memset(negt, 0.0), writes=['negt'])
        tp = k.bankb[0]
        sbank = [0]

        def next_sbank():
            sbank[0] += 1
            return (1, 2, 7)[sbank[0] % 3]

        def den_view(ap260):
            return ap260.rearrange('p (m e) -> p m e', e=65)[:, :, 64]

        def masked_exp(b, nn, dst, mask_ap, dname):
            if mask_ap is None:
                P.op('act', lambda e: e.activation(out=dst[0:nn], in_=k.bank[b][0:nn, :], func=AF.Exp), reads=['bank%d' % b],
                     writes=[dname])
            else:
                P.op('act', lambda e: e.activation(out=expf[0:nn], in_=k.bank[b][0:nn, :], func=AF.Exp), reads=['bank%d' % b],
                     writes=['expf'])
                P.op('pool', lambda e: e.tensor_tensor(out=dst[0:nn].rearrange('p (m q) -> p m q', m=4),
                                                       in0=expf[0:nn].rearrange('p (m q) -> p m q', m=4),
                                                       in1=mask_ap.unsqueeze(1).broadcast_to([nn, 4, 128]), op=ALU.mult),
                     reads=['expf', 'tri', 'ntri'], writes=[dname])

        def o2_loads(i):
            P.dma('sp', k.xt[i % 3], xsrc[i * 128:(i + 1) * 128, :], reads=[('x', i)], writes=['xt%d' % (i % 3)], stream='x')
            P.dma('sp', qt[i % 2], qd[i * 128:(i + 1) * 128, :], reads=[('qd', i)], writes=['qt%d' % (i % 2)], stream='x')
            P.dma('sp', ydts[i % 2], ydd[i * 128:(i + 1) * 128, :], reads=[('ydd', i)], writes=['ydt%d' % (i % 2)], stream='x')

        def stage_a(i, g):
            qti = qt[i % 2]
            qtn = 'qt%d' % (i % 2)
            off = 62 - 2 * i
            Q = QN[g]
            qn = 'QN%d' % g
            rdA = rdenA[g]
            rdAn = 'rdenA%d' % g
            for m in range(4):
                h = 4 * g + m
                P.op('pe', lambda e, m=m, h=h: e.transpose(out=tp[0:64, m * 128:(m + 1) * 128], in_=qti[:, h * 64:(h + 1) * 64],
                                                          identity=k.ident), reads=[qtn, 'ident'], writes=['bank0'])
            P.op('act', lambda e: e.copy(out=Q[0:64, :], in_=tp[0:64, 0:512]), reads=['bank0'], writes=[qn + 'lo'])
            chunks = [(0, 128)] + ([(1, 127)] if 8 * i + 6 >= 128 else [])
            for (c, nn) in chunks:
                b = next_sbank()
                P.op('pe', lambda e, c=c, nn=nn, b=b: e.matmul(k.bank[b][0:nn, :], lhsT=kcmpT[:, g, c * 128:c * 128 + nn],
                                                              rhs=Q[0:64, :], start=True, stop=True),
                     reads=['kcmpT', qn + 'lo'], writes=['bank%d' % b])
                full = (16 * (c * 128 + nn - 1) + 31 <= 128 * i)
                if full:
                    P.op('act', lambda e, c=c, nn=nn, b=b: e.activation(out=Pc[0:nn, c, :], in_=k.bank[b][0:nn, :], func=AF.Exp),
                         reads=['bank%d' % b], writes=['Pc'])
                else:
                    tv = float(128 * i - 31 - 2048 * c)
                    P.op('act', lambda e, nn=nn, b=b: e.activation(out=expf[0:nn], in_=k.bank[b][0:nn, :], func=AF.Exp),
                         reads=['bank%d' % b], writes=['expf'])
                    P.op('dve', lambda e, c=c, nn=nn, tv=tv: e.scalar_tensor_tensor(
                        out=Pc[0:nn, c, :].rearrange('p (m q) -> p m q', m=4),
                        in0=cm[0:nn].unsqueeze(1).broadcast_to([nn, 4, 128]), scalar=tv,
                        in1=expf[0:nn].rearrange('p (m q) -> p m q', m=4), op0=ALU.is_le, op1=ALU.mult),
                         reads=['expf', 'cm'], writes=['Pc'])
            for m in range(4):
                for ci, (c, nn) in enumerate(chunks):
                    P.op('pe', lambda e, m=m, c=c, nn=nn, ci=ci: e.matmul(k.bank[3][:, m * 65:(m + 1) * 65],
                                                                        lhsT=Pc[0:nn, c, m * 128:(m + 1) * 128],
                                                                        rhs=vcmp_aug[0:nn, c, g, :], start=(ci == 0),
                                                                        stop=(ci == len(chunks) - 1)),
                         reads=['Pc', 'vcmp'], writes=['bank3'])
            for m in range(4):
                for ci, (c, nn) in enumerate(chunks):
                    P.op('pe', lambda e, m=m, c=c, nn=nn, ci=ci: e.matmul(k.bank[4][:, m * 64:(m + 1) * 64],
                                                                        lhsT=Pc[0:nn, c, m * 128:(m + 1) * 128],
                                                                        rhs=cts[0:nn, c, :], start=(ci == 0),
                                                                        stop=(ci == len(chunks) - 1)),
                         reads=['Pc', 'cts'], writes=['bank4'])
            jts = list(range(max(0, i - 4), i + 1))
            for sl_, jt in enumerate(jts):
                b = next_sbank()
                P.op('pe', lambda e, jt=jt, b=b: e.matmul(k.bank[b], lhsT=kwT[:, g, jt * 128:(jt + 1) * 128], rhs=Q[0:64, :],
                                                         start=True, stop=True), reads=['kwT', qn + 'lo'], writes=['bank%d' % b])
                mk = tri if jt == i else (ntri if jt == i - 4 else None)
                masked_exp(b, 128, PW[:, sl_, :], mk, ('PW', sl_))
            for m in range(4):
                for sl_, jt in enumerate(jts):
                    P.op('pe', lambda e, m=m, jt=jt, sl_=sl_: e.matmul(k.bank[6][:, m * 65:(m + 1) * 65],
                                                                      lhsT=PW[:, sl_, m * 128:(m + 1) * 128],
                                                                      rhs=vw_aug[:, jt, g, :], start=(sl_ == 0),
                                                                      stop=(sl_ == len(jts) - 1)),
                         reads=[('PW', sl_), 'vw_aug'], writes=['bank6'])
            P.op('dve', lambda e: e.tensor_scalar(out=rdA, in0=den_view(k.bank[3][:, 0:260]), scalar1=1e-30, scalar2=None,
                                                  op0=ALU.max), reads=['bank3'], writes=[rdAn])
            P.op('dve', lambda e: e.reciprocal(out=rdA, in_=rdA), reads=[rdAn], writes=[rdAn])
            P.op('dve', lambda e: e.tensor_scalar(out=psl, in0=k.bank[4][:, 0:64], scalar1=rdA[:, 0:1], scalar2=None,
                                                  op0=ALU.mult), reads=['bank4', rdAn], writes=['psl'])
            for m in range(1, 4):
                P.op('dve', lambda e, m=m: e.scalar_tensor_tensor(out=psl, in0=k.bank[4][:, m * 64:(m + 1) * 64],
                                                                  scalar=rdA[:, m:m + 1], in1=psl, op0=ALU.mult, op1=ALU.add),
                     reads=['bank4', rdAn, 'psl'], writes=['psl'])
            P.op('act', lambda e: e.copy(out=ocw[g][:, 0, :], in_=k.bank[3][:, 0:260]), reads=['bank3'], writes=['ocw%d' % g])
            P.op('act', lambda e: e.copy(out=ocw[g][:, 1, :], in_=k.bank[6][:, 0:260]), reads=['bank6'], writes=['ocw%d' % g])
            P.op('dve', lambda e: e.tensor_tensor(out=score, in0=psl, in1=keep[:, off:off + 64], op=ALU.mult),
                 reads=['psl', 'keep'], writes=['score'])
            P.op('dve', lambda e: e.tensor_tensor(out=score, in0=score, in1=addc[:, off:off + 64], op=ALU.add),
                 reads=['score', 'addc'], writes=['score'])
            P.op('dve', lambda e: e.memset(score[:, 0:1], 1.0e4), reads=['score'], writes=['score'])
            P.op('dve', lambda e: e.max(out=m8a, in_=score), reads=['score'], writes=['m8a'])
            P.op('dve', lambda e: e.match_replace(out=sc2, in_to_replace=m8a, in_values=score, imm_value=-2.0),
                 reads=['score', 'm8a'], writes=['sc2'])
            P.op('dve', lambda e: e.max(out=m8b, in_=sc2), reads=['sc2'], writes=['m8b'])
            P.op('dve', lambda e: e.tensor_scalar(out=thr, in0=m8b[:, 7:8], scalar1=0.0, scalar2=None, op0=ALU.max),
                 reads=['m8b'], writes=['thr'])
            P.op('dve', lambda e: e.tensor_scalar(out=negt[:, 64:128], in0=score, scalar1=thr[:, 0:1], scalar2=-30000.0,
                                                  op0=ALU.is_lt, op1=ALU.mult), reads=['score', 'thr'], writes=['negt'])
            P.op('pe', lambda e: e.transpose(out=tp[:, 512:640], in_=negt, identity=k.ident), reads=['negt', 'ident'],
                 writes=['bank0'])
            P.op('act', lambda e: e.copy(out=Q[64:128, :].rearrange('p (m q) -> p m q', m=4),
                                         in_=tp[64:128, 512:640].unsqueeze(1).broadcast_to([64, 4, 128])),
                 reads=['bank0'], writes=[qn + 'hi'])

        def stage_b(i, g):
            Q = QN[g]
            qn = 'QN%d' % g
            ob = 5
            obn = 'bank%d' % ob
            rdB = rdenB[g]
            rdBn = 'rdenB%d' % g
            coef = coefs[g]
            cfn = 'coef%d' % g
            for jt in range(i + 1):
                b = next_sbank()
                P.op('pe', lambda e, jt=jt, b=b: e.matmul(k.bank[b], lhsT=KE[:, g, jt * 128:(jt + 1) * 128], rhs=Q, start=True,
                                                         stop=True), reads=['KE', qn + 'lo', qn + 'hi'], writes=['bank%d' % b])
                masked_exp(b, 128, PT[:, jt, :], tri if jt == i else None, ('PT', jt))
            for m in range(4):
                for jt in range(i + 1):
                    P.op('pe', lambda e, m=m, jt=jt: e.matmul(k.bank[ob][:, m * 65:(m + 1) * 65],
                                                             lhsT=PT[:, jt, m * 128:(m + 1) * 128], rhs=vs_aug[:, jt, g, :],
                                                             start=(jt == 0), stop=(jt == i)),
                         reads=[('PT', jt), 'vs_aug'], writes=[obn])
            P.op('dve', lambda e: e.tensor_copy(out=rdB[:, 0:4], in_=rdenA[g]), reads=['rdenA%d' % g], writes=[rdBn])
            P.op('dve', lambda e: e.tensor_scalar(out=rdB[:, 4:8], in0=den_view(k.bank[ob][:, 0:260]), scalar1=1e-30, scalar2=None,
                                                  op0=ALU.max), reads=[obn], writes=[rdBn])
            P.op('dve', lambda e: e.tensor_scalar(out=rdB[:, 8:12], in0=den_view(ocw[g][:, 1, :]), scalar1=1e-30, scalar2=None,
                                                  op0=ALU.max), reads=['ocw%d' % g], writes=[rdBn])
            P.op('dve', lambda e: e.reciprocal(out=rdB[:, 4:12], in_=rdB[:, 4:12]), reads=[rdBn], writes=[rdBn])
            P.op('dve', lambda e: e.tensor_tensor(out=coef.rearrange('p (b m) -> p b m', b=3),
                                                  in0=rdB.rearrange('p (b m) -> p b m', b=3),
                                                  in1=gsig[:, i, g * 12:(g + 1) * 12].rearrange('p (m b) -> p b m', b=3),
                                                  op=ALU.mult), reads=[rdBn, ('gsig', i)], writes=[cfn])
            for m in range(4):
                h = 4 * g + m
                ym = yc[:, h * 64:(h + 1) * 64]
                P.op('pool', lambda e, m=m, ym=ym: e.tensor_scalar(out=ym, in0=ocw[g][:, 0, m * 65:m * 65 + 64],
                                                                   scalar1=coef[:, m:m + 1], scalar2=0.0, op0=ALU.mult, op1=ALU.add),
                     reads=['ocw%d' % g, cfn], writes=[('yc', h)])
                P.op('dve', lambda e, m=m, ym=ym: e.scalar_tensor_tensor(out=ym, in0=k.bank[ob][:, m * 65:m * 65 + 64],
                                                                         scalar=coef[:, 4 + m:5 + m], in1=ym, op0=ALU.mult,
                                                                         op1=ALU.add), reads=[obn, cfn, ('yc', h)],
                     writes=[('yc', h)])
                P.op('dve', lambda e, m=m, ym=ym: e.scalar_tensor_tensor(out=ym, in0=ocw[g][:, 1, m * 65:m * 65 + 64],
                                                                         scalar=coef[:, 8 + m:9 + m], in1=ym, op0=ALU.mult,
                                                                         op1=ALU.add), reads=['ocw%d' % g, cfn, ('yc', h)],
                     writes=[('yc', h)])

        def out_proj(i):
            xt = k.xt[i % 3]
            xr = 'xt%d' % (i % 3)
            ydt = ydts[i % 2]
            ydn = 'ydt%d' % (i % 2)
            P.op('act', lambda e: e.copy(out=ycb, in_=yc), reads=[('yc', h) for h in range(8)], writes=['ycb'])
            for c in range(4):
                P.op('pe', lambda e, c=c: e.transpose(out=tp[:, c * 128:(c + 1) * 128], in_=ycb[:, c * 128:(c + 1) * 128],
                                                      identity=k.ident), reads=['ycb', 'ident'], writes=['bank0'])
            for c in range(4):
                P.op('pe', lambda e, c=c: e.transpose(out=tp[:, (4 + c) * 128:(5 + c) * 128], in_=ydt[:, c * 128:(c + 1) * 128],
                                                      identity=k.ident), reads=[ydn, 'ident'], writes=['bank0'])
            P.op('act', lambda e: e.copy(out=yT, in_=tp.rearrange('p (c t) -> p c t', c=8)), reads=['bank0'], writes=['yT'])
            pm = [k.bank[3], k.bank[4]]
            for cb in range(2):
                for kc in range(8):
                    P.op('pe', lambda e, kc=kc, cb=cb: e.matmul(pm[cb], lhsT=yT[:, kc, :], rhs=w_out_sb[:, kc, cb * 512:(cb + 1) * 512],
                                                               start=(kc == 0), stop=(kc == 7)), reads=['yT', 'w_out'],
                         writes=['bank%d' % (3 + cb)])
            post_norm_residual(k, pm, ['bank3', 'bank4'], 'mix_post', xt, xr)
            P.dma('sp', xdst[i * 128:(i + 1) * 128, :], xt, reads=[xr], writes=[('x', i)], stream='xo')

        units = [(i, g) for i in range(NT) for g in range(2)]
        o2_loads(0)
        stage_a(*units[0])
        for n, (i, g) in enumerate(units):
            if n + 1 < len(units):
                ni, ng = units[n + 1]
                if ng == 0:
                    o2_loads(ni)
                stage_a(ni, ng)
            stage_b(i, g)
            if g == 1:
                out_proj(i)

def make_consts():
    c = {}
    Dm = np.zeros((3, 4, 128, 128), np.float32)
    for g, w in enumerate(POOL_WINDOWS):
        for t in range(128):
            lo = max(t + 1 - w, 0)
            for s in range(lo, t + 1):
                Dm[0, g, s, t] += 1.0 / (t + 1 - lo)
            Dm[0, g, t, t] -= 1.0
            for s in range(t + 1 - w, t + 1):
                if s >= 0:
                    Dm[1, g, s, t] += 1.0 / w
                else:
                    Dm[2, g, s + 128, t] += 1.0 / w
            Dm[1, g, t, t] -= 1.0
    c['c_D'] = Dm.astype(ml_dtypes.bfloat16)
    bf = ml_dtypes.bfloat16
    invf = np.concatenate([1.0 / (500000.0 ** (np.arange(0, 16, 2, dtype=np.float32) / 16)),
                           1.0 / (10000.0 ** (np.arange(0, 128, 2, dtype=np.float32) / 128))]).astype(np.float32)
    c['c_invf'] = invf.reshape(1, 72)
    lg = np.log1p(-np.exp2(-5.0 - np.arange(4, dtype=np.float64)))
    idx = np.arange(128, dtype=np.float64)
    rel = idx[None, :] - idx[:, None]
    dec = np.where((rel >= 0)[:, None, :], np.exp(np.maximum(rel, 0)[:, None, :] * lg[None, :, None]), 0.0)
    c['c_decT'] = dec.reshape(128, 512).astype(np.float32)
    xi = np.exp((idx + 1.0)[None, :] * lg[:, None])
    c['c_xi'] = np.broadcast_to(xi.reshape(1, 512), (128, 512)).astype(np.float32).copy()
    c['c_zeta'] = np.exp((127 - idx)[:, None] * lg[None, :]).astype(np.float32)
    c['c_gch'] = np.broadcast_to(np.exp(128 * lg)[None, :], (128, 4)).astype(np.float32).copy()
    p = np.arange(128)
    c['c_tri'] = (p[:, None] <= p[None, :]).astype(np.float32)
    c['c_cm'] = (16.0 * p[:, None] - p[None, :]).astype(np.float32)
    cq = (p >= 64).astype(np.int64)[:, None]
    jj = np.arange(128)[None, :]
    c['c_keep'] = (jj <= 60 + cq).astype(np.float32)
    add = np.zeros((128, 128), np.float32)
    add[(jj == 61 + cq) | (jj == 62 + cq)] = 1.0e4
    add[jj > 62 + cq] = -1.0
    c['c_add'] = add
    n = np.arange(256)
    cs = n * 16
    ss = np.arange(64) * 64
    cts = ((cs[:, None] < ss[None, :] + 64) & (cs[:, None] + 32 > ss[None, :])).astype(np.float32)
    cts[255] = 0
    c['c_cts'] = cts.reshape(2, 128, 64).astype(bf)
    c['c_E'] = (np.arange(4096)[None, :] // 64 == np.arange(64)[:, None]).astype(bf)
    return c


INPUT_NAMES = ["x", "positions", "ln_mix_pre", "ln_mix_post", "ln_ffn_pre", "ln_ffn_post", "ffn_w_gate", "ffn_w_up",
               "ffn_w_down", "ev_w_in", "ev_pool_w", "ev_pool_scale", "ev_sgu_ln_g", "ev_sgu_ln_b", "ev_sgu_w",
               "ev_sgu_b", "ev_w_out", "od_w_in", "od_cmp_k_pos", "od_cmp_k_w1", "od_cmp_k_w2", "od_cmp_v_pos",
               "od_cmp_v_w1", "od_cmp_v_w2", "od_ret_gn_g", "od_w_out"]


def build(shapes, consts, layers=(0, 1, 2, 3), phases=('mix', 'ffn')):
    from contextlib import ExitStack
    nc = bass.Bass("TRN2", target_bir_lowering=False)
    ins = {}
    for n in INPUT_NAMES:
        shp = list(shapes[n])
        if n == 'x':
            shp = [T, D]
        if n == 'positions':
            shp = [1, T]
        ins[n] = nc.dram_tensor(n, shp, I32 if n == 'positions' else F32, kind="ExternalInput").ap()
    for n, v in consts.items():
        ins[n] = nc.dram_tensor(n, list(v.shape), BF16 if v.dtype == ml_dtypes.bfloat16 else F32, kind="ExternalInput").ap()
    y = nc.dram_tensor("y", [T, D], F32, kind="ExternalOutput").ap()
    k = K(nc, layers)
    setup_common(k, ins)
    k.sb2 = k.sb('ssa', [128, 2], F32)
    P = k.P
    xsrc = ins['x']
    for li, l in enumerate(layers):
        load_gains(k, l)
        if li + 1 < len(layers):
            cast_layer(k, layers[li + 1])
        if 'mix' in phases:
            with ExitStack() as es:
                if l % 2 == 0:
                    even_phase(k, l, xsrc, y, es)
                else:
                    odd_phase(k, l, xsrc, y, es)
                P.barrier()
            xsrc = y
        if 'ffn' in phases:
            with ExitStack() as es:
                def sb(name, shape, dt):
                    return es.enter_context(nc.sbuf_tensor('ffs%d_' % l + name, list(shape), dt)).ap()
                k.wd_sb = sb('wd', [128, NFC, 1024], BF16)
                k.xt8 = [sb('xt8_%d' % i, [128, D], F32) for i in range(8)]
                k.wgu = [sb('wgu%d' % i, [128, 2, 8, 512], BF16) for i in range(2)]
                k.hT2 = [sb('hT%d' % i, [128, 8, 512], BF16) for i in range(2)]
                k.actT = sb('actT', [128, NFC, 512], BF16)
                k.sg = [sb('sg%d' % i, [128, 512], F32) for i in range(2)]
                ffn_phase(k, l, xsrc, y)
                P.barrier()
            xsrc = y
    P.finish()
    return nc


_CACHE = {}


def kernel(**inputs):
    consts = make_consts()
    shapes = {n: inputs[n].shape for n in INPUT_NAMES}
    if 'nc' not in _CACHE:
        _CACHE['nc'] = build(shapes, consts)
    nc = _CACHE['nc']
    in_maps = []
    for c in range(4):
        b = c % 4
        m = {n: np.ascontiguousarray(inputs[n]) for n in INPUT_NAMES if n not in ('x', 'positions')}
        m['x'] = np.ascontiguousarray(inputs['x'][b])
        m['positions'] = np.ascontiguousarray(inputs['positions'][b:b + 1]).astype(np.int32)
        m.update(consts)
        in_maps.append(m)
    res = run_bass_kernel_spmd(nc, in_maps, core_ids=list(range(4)))
    out = np.stack([res.results[b]["y"] for b in range(4)], axis=0)
    return out.astype(np.float32)
```

```python
import numpy as np
import ml_dtypes
import concourse.bass as bass
import concourse.mybir as mybir
from concourse.bass_utils import run_bass_kernel_spmd

F32 = mybir.dt.float32
BF16 = mybir.dt.bfloat16
I32 = mybir.dt.int32
AF = mybir.ActivationFunctionType
ALU = mybir.AluOpType
AX = mybir.AxisListType

import os
SAME_ENG_SYNC = os.environ.get("SES", "1") == "1"


class Prog:
    def __init__(self, nc):
        self.nc = nc
        self.E = {'pe': nc.tensor, 'dve': nc.vector, 'act': nc.scalar, 'pool': nc.gpsimd, 'sp': nc.sync}
        self.semh = {k: nc.alloc_semaphore('s_' + k) for k in ['pe', 'dve', 'act', 'pool']}
        self.cnt = {k: 0 for k in self.semh}
        self.seen = {e: {} for e in self.E}
        self.lastw = {}
        self.readers = {}
        self.nwait = 0
        self.dslot = 0
        self.NSLOT = 32
        self.nins = 0

    def _deps(self, reads, writes):
        deps = {}
        for r in reads:
            w = self.lastw.get(r)
            if w and deps.get(w[0], 0) < w[1]:
                deps[w[0]] = w[1]
            if isinstance(r, str) and r.startswith('bank'):
                for k_, v in self.readers.get(r, {}).items():
                    if deps.get(k_, 0) < v:
                        deps[k_] = v
        for w_ in writes:
            w = self.lastw.get(w_)
            if w and deps.get(w[0], 0) < w[1]:
                deps[w[0]] = w[1]
            for k, v in self.readers.get(w_, {}).items():
                if deps.get(k, 0) < v:
                    deps[k] = v
        return deps

    def _wait(self, eng, deps):
        e = self.E[eng]
        seen = self.seen[eng]
        for k, v in deps.items():
            if k == eng and (eng == 'pe' or not SAME_ENG_SYNC):
                continue
            if seen.get(k, 0) >= v:
                continue
            e.wait_ge(self.semh[k], v)
            seen[k] = v
            self.nwait += 1

    def _record(self, tag, reads, writes):
        for w in writes:
            self.lastw[w] = tag
            self.readers[w] = {}
        for r in reads:
            d = self.readers.setdefault(r, {})
            if d.get(tag[0], 0) < tag[1]:
                d[tag[0]] = tag[1]

    def op(self, eng, fn, reads=(), writes=()):
        self._wait(eng, self._deps(reads, writes))
        ins = fn(self.E[eng])
        self.cnt[eng] += 1
        ins.then_inc(self.semh[eng], 1)
        self.nins += 1
        self._record((eng, self.cnt[eng]), reads, writes)

    def dma(self, q, out, in_, reads=(), writes=(), stream='d0', **kw):
        slot = self.dslot % self.NSLOT
        self.dslot += 1
        key = 'd:%d' % slot
        if key not in self.semh:
            self.semh[key] = self.nc.alloc_semaphore('sd_%d' % slot)
            self.cnt[key] = 0
        e = self.E[q]
        if self.cnt[key] > 0 and self.seen[q].get(key, 0) < self.cnt[key]:
            e.wait_ge(self.semh[key], self.cnt[key])
            self.seen[q][key] = self.cnt[key]
        self._wait(q, self._deps(reads, writes))
        e.dma_start(out=out, in_=in_, **kw).then_inc(self.semh[key], 16)
        self.cnt[key] += 16
        self.nins += 1
        self._record((key, self.cnt[key]), reads, writes)

    def barrier(self):
        for eng in self.E:
            deps = {k: v for k, v in self.cnt.items() if v > 0}
            e = self.E[eng]
            for k, v in deps.items():
                if k == eng and eng == 'pe':
                    continue
                if self.seen[eng].get(k, 0) >= v:
                    continue
                e.wait_ge(self.semh[k], v)
                self.seen[eng][k] = v
        self.lastw = {}
        self.readers = {}

    def finish(self, eng='sp'):
        deps = {k: v for k, v in self.cnt.items() if v > 0}
        e = self.E[eng]
        for k, v in deps.items():
            e.wait_ge(self.semh[k], v)


T = 4096
D = 1024
NT = T // 128
FH = 2816
NFC = FH // 128
DEPTH = 4
POOL_WINDOWS = (2, 4, 8, 16)
EPS = 1e-6


def _flat2(ap, c=1024):
    n = len(ap.shape)
    names = ' '.join('d%d' % i for i in range(n))
    f = ap.rearrange('%s -> (%s)' % (names, names)) if n > 1 else ap
    return f.rearrange('(r c) -> r c', c=c)


class K:
    def __init__(self, nc, layers=(0, 1, 2, 3), Tn=T):
        self.nc = nc
        self.P = Prog(nc)
        self.layers = layers
        self.uid = 0

    def sb(self, name, shape, dt):
        return self.nc.alloc_sbuf_tensor(name, list(shape), dt).ap()

    def dram(self, name, shape, dt, kind="Internal"):
        return self.nc.dram_tensor(name, list(shape), dt, kind=kind).ap()


def cast_copy(k, dst, src, res):
    d2 = _flat2(dst)
    s2 = _flat2(src)
    rows = d2.shape[0]
    r0 = 0
    while r0 < rows:
        r1 = min(rows, r0 + 2048)
        k.P.dma('pool', d2[r0:r1, :], s2[r0:r1, :], writes=[res], stream='cast')
        r0 = r1


def cast_layer(k, l):
    ins = k.ins
    order = []
    if l % 2 == 0:
        order += [('ev_w_in', l // 2), ('ev_pool_w', l // 2), ('ev_w_out', l // 2)]
    else:
        order += [('od_w_in', l // 2), ('od_cmp_k_w1', l // 2), ('od_cmp_k_w2', l // 2), ('od_cmp_v_w1', l // 2),
                  ('od_cmp_v_w2', l // 2), ('od_w_out', l // 2)]
    order += [('ffn_w_gate', l), ('ffn_w_up', l), ('ffn_w_down', l)]
    for name, idx in order:
        src = ins[name][idx]
        dst = k.dram('wb_%s_%d' % (name, idx), src.shape, BF16)
        k.wb[(name, idx)] = dst
        cast_copy(k, dst, src, ('wb', name, idx))


def rstd_from_ssq(k, ssq, rstd, tmp, n, eps, tag):
    P = k.P
    P.op('dve', lambda e: e.tensor_scalar(out=tmp, in0=ssq, scalar1=1.0 / n, scalar2=eps, op0=ALU.mult, op1=ALU.add),
         reads=[tag + 'ssq'], writes=[tag + 'tmp'])
    P.op('pool', lambda e: e.tensor_tensor(out=rstd, in0=tmp, in1=k.mhalf[:, 0:1], op=ALU.pow), reads=[tag + 'tmp', 'mhalf'],
         writes=[tag + 'rstd'])


def setup_common(k, ins):
    nc, P = k.nc, k.P
    k.ins = ins
    k.bank = [nc.alloc_psum_tensor('bank%d' % i, [128, 512], F32).ap() for i in range(8)]
    k.bankb = [b.bitcast(BF16) for b in k.bank]
    k.io_f = k.sb('io_f', [128, 128], F32)
    k.iop = k.sb('iop', [128, 1], F32)
    k.ident = k.sb('ident', [128, 128], BF16)
    k.ones_row = k.sb('ones_row', [1, 128], BF16)
    P.op('pool', lambda e: e.iota(k.io_f, pattern=[[1, 128]], base=0, channel_multiplier=0,
                                  allow_small_or_imprecise_dtypes=True), writes=['io_f'])
    P.op('pool', lambda e: e.iota(k.iop, pattern=[[1, 1]], base=0, channel_multiplier=1,
                                  allow_small_or_imprecise_dtypes=True), writes=['iop'])
    P.op('dve', lambda e: e.tensor_scalar(out=k.ident, in0=k.io_f, scalar1=k.iop[:, 0:1], scalar2=None,
                                          op0=ALU.is_equal), reads=['io_f', 'iop'], writes=['ident'])
    P.op('dve', lambda e: e.memset(k.ones_row, 1.0), writes=['ones_row'])
    k.mhalf = k.sb('mhalf', [128, 4], F32)
    P.op('pool', lambda e: e.memset(k.mhalf, -0.5), writes=['mhalf'])
    k.wb = {}
    cast_layer(k, k.layers[0])
    k.xt = [k.sb('xt%d' % s, [128, D], F32) for s in range(3)]
    k.hbs = [k.sb('hb%d' % i, [128, D], BF16) for i in range(2)]
    k.junk = k.sb('junk', [128, D], BF16)
    k.tmpf = k.sb('tmpf', [128, D], F32)
    k.G = {n: k.sb('G_' + n, [128, D], F32) for n in ['mix_pre', 'mix_post', 'ffn_pre', 'ffn_post']}
    k.st = {n: k.sb('st_' + n, [128, 1], F32) for n in ['ssq', 'tmp', 'rstd', 'ssq2', 'tmp2', 'rstd2']}


def load_gains(k, l):
    for n in ['mix_pre', 'mix_post', 'ffn_pre', 'ffn_post']:
        k.P.dma('sp', k.G[n], k.ins['ln_' + n][l:l + 1, :].broadcast_to([128, D]), writes=['G_' + n], stream='small')


def norm_a(k, xt_ap, xres, gname, hbi):
    P = k.P
    st = k.st
    hb = k.hbs[hbi]
    P.op('act', lambda e: e.activation(out=k.junk, in_=xt_ap, func=AF.Square, accum_out=st['ssq']),
         reads=[xres], writes=['junk', 'ssq'])
    rstd_from_ssq(k, st['ssq'], st['rstd'], st['tmp'], D, EPS, '')
    P.op('dve', lambda e: e.scalar_tensor_tensor(out=hb, in0=xt_ap, scalar=st['rstd'][:, 0:1], in1=k.G[gname],
                                                 op0=ALU.mult, op1=ALU.mult),
         reads=[xres, 'rstd', 'G_' + gname], writes=['hb%d' % hbi])


def trans_t(k, hbi, hT_dst, hTres):
    P = k.P
    hb = k.hbs[hbi]
    tp = k.bankb[0]
    for c in range(8):
        P.op('pe', lambda e, c=c: e.transpose(out=tp[:, c * 128:(c + 1) * 128], in_=hb[:, c * 128:(c + 1) * 128],
                                              identity=k.ident), reads=['hb%d' % hbi, 'ident'], writes=['bank0'])
    P.op('act', lambda e: e.copy(out=hT_dst, in_=tp.rearrange('p (c t) -> p c t', c=8)), reads=['bank0'],
         writes=[hTres])


def post_norm_residual(k, pm, pmres, gname, xt_ap, xres):
    P = k.P
    st = k.st
    ssa = k.sb2
    for cb in range(2):
        P.op('act', lambda e, cb=cb: e.activation(out=k.junk[:, cb * 512:(cb + 1) * 512], in_=pm[cb], func=AF.Square,
                                                  accum_out=ssa[:, cb:cb + 1]),
             reads=[pmres[cb]], writes=['junk', 'ssa'])
    P.op('dve', lambda e: e.tensor_tensor(out=st['ssq2'], in0=ssa[:, 0:1], in1=ssa[:, 1:2], op=ALU.add),
         reads=['ssa'], writes=['2ssq'])
    rstd_from_ssq(k, st['ssq2'], st['rstd2'], st['tmp2'], D, EPS, '2')
    for cb in range(2):
        sl = slice(cb * 512, (cb + 1) * 512)
        P.op('dve', lambda e, cb=cb, sl=sl: e.scalar_tensor_tensor(out=k.tmpf[:, sl], in0=pm[cb], scalar=st['rstd2'][:, 0:1],
                                                                   in1=k.G[gname][:, sl], op0=ALU.mult, op1=ALU.mult),
             reads=[pmres[cb], '2rstd', 'G_' + gname], writes=['tmpf'])
    P.op('pool', lambda e: e.tensor_tensor(out=xt_ap, in0=xt_ap, in1=k.tmpf, op=ALU.add), reads=['tmpf', xres],
         writes=[xres])


def ffn_phase(k, l, xsrc, xdst):
    nc, P = k.nc, k.P
    wg, wu, wd = k.wb[('ffn_w_gate', l)], k.wb[('ffn_w_up', l)], k.wb[('ffn_w_down', l)]
    P.dma('sp', k.wd_sb, wd.rearrange('(c p) n -> p c n', p=128), reads=[('wb', 'ffn_w_down', l)], writes=['wd_sb'],
          stream='w')
    groups = [(0, 4), (4, 4), (8, 4), (12, 4), (16, 4), (20, 2)]
    wgv = wg.rearrange('(c p) n -> p c n', p=128)
    wuv = wu.rearrange('(c p) n -> p c n', p=128)
    gi = 0
    NB = T // 512

    def xtile(tb, s):
        j = (tb % 2) * 4 + s
        return k.xt8[j], 'xt8_%d' % j

    def front_a(tb, s):
        t0 = tb * 512 + s * 128
        xt, xr = xtile(tb, s)
        P.dma('sp', xt, xsrc[t0:t0 + 128, :], reads=[('x', t0 // 128)], writes=[xr], stream='x')
        norm_a(k, xt, xr, 'ffn_pre', s % 2)

    def front_t(tb, s):
        trans_t(k, s % 2, k.hT2[tb % 2][:, :, s * 128:(s + 1) * 128], ('hT', tb % 2))

    for s in range(4):
        front_a(0, s)
        front_t(0, s)
    for tb in range(NB):
        hT = k.hT2[tb % 2]
        hTr = ('hT', tb % 2)
        for gidx, (j0, nj) in enumerate(groups):
            pre = gidx < 4 and tb + 1 < NB
            if pre:
                front_a(tb + 1, gidx)
            buf = gi % 2
            gi += 1
            wt = k.wgu[buf]
            P.dma('sp', wt[:, 0, :, 0:nj * 128], wgv[:, :, j0 * 128:(j0 + nj) * 128], reads=[('wb', 'ffn_w_gate', l)],
                  writes=['wgu%d' % buf], stream='w')
            P.dma('sp', wt[:, 1, :, 0:nj * 128], wuv[:, :, j0 * 128:(j0 + nj) * 128], reads=[('wb', 'ffn_w_up', l)],
                  writes=['wgu%d' % buf], stream='w')
            for jj in range(nj):
                j = j0 + jj
                pb = 1 + 2 * (j % 2)
                pg, pu = k.bank[pb], k.bank[pb + 1]
                for kc in range(8):
                    P.op('pe', lambda e, kc=kc, jj=jj: e.matmul(pg, lhsT=wt[:, 0, kc, jj * 128:(jj + 1) * 128], rhs=hT[:, kc, :],
                                                               start=(kc == 0), stop=(kc == 7)),
                         reads=['wgu%d' % buf, hTr], writes=['bank%d' % pb])
                for kc in range(8):
                    P.op('pe', lambda e, kc=kc, jj=jj: e.matmul(pu, lhsT=wt[:, 1, kc, jj * 128:(jj + 1) * 128], rhs=hT[:, kc, :],
                                                               start=(kc == 0), stop=(kc == 7)),
                         reads=['wgu%d' % buf, hTr], writes=['bank%d' % (pb + 1)])
                sg = k.sg[j % 2]
                P.op('act', lambda e: e.activation(out=sg, in_=pg, func=AF.Silu), reads=['bank%d' % pb],
                     writes=['sg%d' % (j % 2)])
                P.op('dve', lambda e, j=j: e.tensor_tensor(out=k.actT[:, j, :], in0=pu, in1=sg, op=ALU.mult),
                     reads=['bank%d' % (pb + 1), 'sg%d' % (j % 2)], writes=[('actT', j)])
            if pre:
                front_t(tb + 1, gidx)
        for s in range(4):
            t0 = tb * 512 + s * 128
            pm = [k.bank[5], k.bank[6]]
            for cb in range(2):
                for kk in range(NFC):
                    P.op('pe', lambda e, kk=kk, cb=cb: e.matmul(pm[cb], lhsT=k.actT[:, kk, s * 128:(s + 1) * 128],
                                                               rhs=k.wd_sb[:, kk, cb * 512:(cb + 1) * 512],
                                                               start=(kk == 0), stop=(kk == NFC - 1)),
                         reads=[('actT', kk), 'wd_sb'], writes=['bank%d' % (5 + cb)])
            xt_, xr_ = xtile(tb, s)
            post_norm_residual(k, pm, ['bank5', 'bank6'], 'ffn_post', xt_, xr_)
            P.dma('sp', xdst[t0:t0 + 128, :], xt_, reads=[xr_], writes=[('x', t0 // 128)], stream='xo')


def even_phase(k, l, xsrc, xdst, es):
    nc, P = k.nc, k.P
    e_ = l // 2
    ins = k.ins

    def sb(name, shape, dt):
        return es.enter_context(nc.sbuf_tensor('evs%d_' % l + name, list(shape), dt)).ap()

    w_in_sb = sb('w_in', [128, 8, 1536], BF16)
    w_out_sb = sb('w_out', [128, 8, 1024], BF16)
    poolw_sb = sb('poolw', [128, 4, 128], BF16)
    Dm_sb = sb('Dm', [128, 3, 4, 128], BF16)
    WmT = sb('WmT', [128, 4, 128], BF16)
    wsf = sb('wsf', [128, 4, 128], F32)
    wsb = sb('wsb', [128, 4, 128], BF16)
    tril = sb('tril', [128, 128], F32)
    Bt = sb('Bt', [128, 512], F32)
    lng = sb('lng', [128, 512], F32)
    lnb = sb('lnb', [128, 512], F32)
    psc = sb('psc', [128, 4], F32)
    a_sb = [sb('a%d' % i, [128, 512], BF16) for i in range(2)]
    uT_sb = sb('uT', [128, 4, 128], BF16)
    vg = sb('vg', [128, 512], F32)
    vn = sb('vn', [128, 512], F32)
    vln = sb('vln', [128, 512], BF16)
    diffT = sb('diffT', [128, 4, 128], BF16)
    yaT = sb('yaT', [128, 4, 128], BF16)
    ybT = sb('ybT', [128, 4, 128], BF16)
    mxb = sb('mxb', [128, 512], F32)
    hT1s = [sb('hT1_%d' % i, [128, 8, 128], BF16) for i in range(2)]
    bst = sb('bst', [128, 6], F32)
    mv = sb('mv', [128, 2], F32)
    lrs = sb('lrs', [128, 1], F32)

    P.dma('sp', w_in_sb, k.wb[('ev_w_in', e_)].rearrange('(c p) n -> p c n', p=128), reads=[('wb', 'ev_w_in', e_)],
          writes=['w_in'], stream='w')
    P.dma('sp', w_out_sb, k.wb[('ev_w_out', e_)].rearrange('(c p) n -> p c n', p=128), reads=[('wb', 'ev_w_out', e_)],
          writes=['w_out'], stream='w')
    P.dma('sp', poolw_sb, k.wb[('ev_pool_w', e_)].rearrange('g c d -> c g d'), reads=[('wb', 'ev_pool_w', e_)],
          writes=['poolw'], stream='w')
    P.dma('sp', Dm_sb, ins['c_D'].rearrange('a g s t -> s a g t'), writes=['Dm'], stream='small')
    P.dma('sp', wsf, ins['ev_sgu_w'][e_].rearrange('g t s -> t g s'), writes=['wsf'], stream='small')
    P.dma('sp', Bt, ins['ev_sgu_b'][e_:e_ + 1].rearrange('o g t -> o (g t)').broadcast_to([128, 512]), writes=['Bt'],
          stream='small')
    P.dma('sp', lng, ins['ev_sgu_ln_g'][e_:e_ + 1, :].broadcast_to([128, 512]), writes=['lng'], stream='small')
    P.dma('sp', lnb, ins['ev_sgu_ln_b'][e_:e_ + 1, :].broadcast_to([128, 512]), writes=['lnb'], stream='small')
    with nc.allow_non_contiguous_dma(reason='tiny per-channel scale'):
        P.dma('sp', psc, ins['ev_pool_scale'][e_].rearrange('(g p) -> p g', p=128), writes=['psc'], stream='small')
    P.op('dve', lambda e: e.tensor_scalar(out=tril, in0=k.io_f, scalar1=k.iop[:, 0:1], scalar2=None, op0=ALU.is_le),
         reads=['io_f', 'iop'], writes=['tril'])
    P.op('dve', lambda e: e.tensor_tensor(out=wsb, in0=wsf, in1=tril.unsqueeze(1).broadcast_to([128, 4, 128]), op=ALU.mult),
         reads=['wsf', 'tril'], writes=['wsb'])
    tp = k.bankb[0]
    for g in range(4):
        P.op('pe', lambda e, g=g: e.transpose(out=tp[:, g * 128:(g + 1) * 128], in_=wsb[:, g, :], identity=k.ident),
             reads=['wsb', 'ident'], writes=['bank0'])
    P.op('act', lambda e: e.copy(out=WmT, in_=tp[:, 0:512].rearrange('p (g t) -> p g t', g=4)), reads=['bank0'],
         writes=['WmT'])

    def front_a(i):
        P.dma('sp', k.xt[i % 3], xsrc[i * 128:(i + 1) * 128, :], reads=[('x', i)], writes=['xt%d' % (i % 3)], stream='x')
        norm_a(k, k.xt[i % 3], 'xt%d' % (i % 3), 'mix_pre', i % 2)

    def front_t(i):
        trans_t(k, i % 2, hT1s[i % 2], 'hT1_%d' % (i % 2))

    front_a(0)
    front_t(0)
    for i in range(NT):
        xt = k.xt[i % 3]
        xr = 'xt%d' % (i % 3)
        hT1 = hT1s[i % 2]
        hT1n = 'hT1_%d' % (i % 2)
        a_cur, a_prev = a_sb[i % 2], a_sb[(i + 1) % 2]
        ar, apr = 'a%d' % (i % 2), 'a%d' % ((i + 1) % 2)
        if i + 1 < NT and i >= 1:
            pass
        pa, pu, pv = k.bank[1], k.bank[2], k.bank[3]
        for kc in range(8):
            P.op('pe', lambda e, kc=kc: e.matmul(pa, lhsT=hT1[:, kc, :], rhs=w_in_sb[:, kc, 0:512], start=(kc == 0),
                                                stop=(kc == 7)), reads=[hT1n, 'w_in'], writes=['bank1'])
        P.op('act', lambda e: e.copy(out=a_cur, in_=pa), reads=['bank1'], writes=[ar])
        for kc in range(8):
            P.op('pe', lambda e, kc=kc: e.matmul(pv, lhsT=hT1[:, kc, :], rhs=w_in_sb[:, kc, 1024:1536], start=(kc == 0),
                                                stop=(kc == 7)), reads=[hT1n, 'w_in'], writes=['bank3'])
        P.op('act', lambda e: e.activation(out=vg, in_=pv, func=AF.Gelu_apprx_tanh), reads=['bank3'], writes=['vg'])
        for c in range(4):
            for kc in range(8):
                P.op('pe', lambda e, kc=kc, c=c: e.matmul(pu[:, c * 128:(c + 1) * 128],
                                                         lhsT=w_in_sb[:, kc, 512 + c * 128:512 + (c + 1) * 128],
                                                         rhs=hT1[:, kc, :], start=(kc == 0), stop=(kc == 7)),
                     reads=[hT1n, 'w_in'], writes=['bank2'])
        P.op('act', lambda e: e.activation(out=uT_sb, in_=pu.rearrange('p (c t) -> p c t', c=4), func=AF.Gelu_apprx_tanh),
             reads=['bank2'], writes=['uT'])
        if i + 1 < NT:
            front_a(i + 1)
        P.op('dve', lambda e: e.bn_stats(out=bst, in_=vg), reads=['vg'], writes=['bst'])
        P.op('dve', lambda e: e.bn_aggr(out=mv, in_=bst), reads=['bst'], writes=['mv'])
        P.op('dve', lambda e: e.tensor_scalar(out=lrs, in0=mv[:, 1:2], scalar1=1e-5, scalar2=None, op0=ALU.add),
             reads=['mv'], writes=['lrs'])
        P.op('pool', lambda e: e.tensor_tensor(out=lrs, in0=lrs, in1=k.mhalf[:, 0:1], op=ALU.pow), reads=['lrs', 'mhalf'], writes=['lrs'])
        P.op('dve', lambda e: e.tensor_scalar(out=vn, in0=vg, scalar1=mv[:, 0:1], scalar2=lrs[:, 0:1], op0=ALU.subtract,
                                              op1=ALU.mult), reads=['vg', 'mv', 'lrs'], writes=['vn'])
        P.op('pool', lambda e: e.tensor_tensor(out=vn, in0=vn, in1=lng, op=ALU.mult), reads=['vn', 'lng'], writes=['vn'])
        P.op('pool', lambda e: e.tensor_tensor(out=vln, in0=vn, in1=lnb, op=ALU.add), reads=['vn', 'lnb'], writes=['vln'])
        pd_ = k.bank[4]
        for g in range(4):
            first = True
            sl = slice(g * 128, (g + 1) * 128)
            P.op('pe', lambda e, g=g, sl=sl: e.matmul(pd_[:, sl], lhsT=a_cur[:, sl], rhs=Dm_sb[:, 0 if i == 0 else 1, g, :],
                                                     start=True, stop=(i == 0)), reads=[ar, 'Dm'], writes=['bank4'])
            if i > 0:
                P.op('pe', lambda e, g=g, sl=sl: e.matmul(pd_[:, sl], lhsT=a_prev[:, sl], rhs=Dm_sb[:, 2, g, :],
                                                         start=False, stop=True), reads=[apr, 'Dm'], writes=['bank4'])
        P.op('dve', lambda e: e.tensor_copy(out=diffT, in_=pd_.rearrange('p (g t) -> p g t', g=4)), reads=['bank4'],
             writes=['diffT'])
        pya = k.bank[5]
        for g in range(4):
            P.op('pe', lambda e, g=g: e.matmul(pya[:, g * 128:(g + 1) * 128], lhsT=poolw_sb[:, g, :], rhs=diffT[:, g, :],
                                              start=True, stop=True), reads=['poolw', 'diffT'], writes=['bank5'])
        P.op('dve', lambda e: e.tensor_tensor(out=yaT, in0=pya.rearrange('p (g t) -> p g t', g=4),
                                              in1=psc.unsqueeze(2).broadcast_to([128, 4, 128]), op=ALU.mult),
             reads=['bank5', 'psc'], writes=['yaT'])
        pmx = k.bank[4]
        for g in range(4):
            P.op('pe', lambda e, g=g: e.matmul(pmx[:, g * 128:(g + 1) * 128], lhsT=vln[:, g * 128:(g + 1) * 128],
                                              rhs=WmT[:, g, :], start=True, stop=True), reads=['vln', 'WmT'],
                 writes=['bank4'])
        P.op('dve', lambda e: e.tensor_tensor(out=mxb, in0=pmx, in1=Bt, op=ALU.add), reads=['bank4', 'Bt'],
             writes=['mxb'])
        P.op('pool', lambda e: e.tensor_tensor(out=ybT, in0=mxb.rearrange('p (g t) -> p g t', g=4), in1=uT_sb, op=ALU.mult),
             reads=['mxb', 'uT'], writes=['ybT'])
        if i + 1 < NT:
            front_t(i + 1)
        pm = [k.bank[6], k.bank[7]]
        for cb in range(2):
            for kc in range(8):
                lh = yaT[:, kc, :] if kc < 4 else ybT[:, kc - 4, :]
                P.op('pe', lambda e, kc=kc, cb=cb, lh=lh: e.matmul(pm[cb], lhsT=lh, rhs=w_out_sb[:, kc, cb * 512:(cb + 1) * 512],
                                                                  start=(kc == 0), stop=(kc == 7)),
                     reads=['yaT', 'ybT', 'w_out'], writes=['bank%d' % (6 + cb)])
        post_norm_residual(k, pm, ['bank6', 'bank7'], 'mix_post', xt, xr)
        P.dma('sp', xdst[i * 128:(i + 1) * 128, :], xt, reads=[xr], writes=[('x', i)], stream='xo')


def odd_phase(k, l, xsrc, xdst, es):
    from contextlib import ExitStack
    nc, P = k.nc, k.P
    o_ = l // 2
    ins = k.ins
    PI = 3.14159265358979

    def sbs(stack, name, shape, dt):
        return stack.enter_context(nc.sbuf_tensor('od%d_' % l + name, list(shape), dt)).ap()

    def sb(name, shape, dt):
        return sbs(es, name, shape, dt)

    qd = k.dram('qd%d' % l, [T, 512], BF16)
    ydd = k.dram('ydd%d' % l, [T, 512], BF16)
    KE = sb('KE', [128, 2, T], BF16)
    kwT = sb('kwT', [64, 2, T], BF16)
    kcvd = k.dram('kcvd%d' % l, [4, 64, T], BF16)
    ropeS = k.dram('ropeS%d' % l, [128, NT, 72], F32)
    ropeC = k.dram('ropeC%d' % l, [128, NT, 72], F32)
    vs_aug = sb('vs_aug', [128, NT, 2, 65], BF16)
    vw_aug = sb('vw_aug', [128, NT, 2, 65], BF16)
    gsig = sb('gsig', [128, NT, 24], F32)
    kcmpT = sb('kcmpT', [64, 2, 256], BF16)
    vcmp_aug = sb('vcmp', [128, 2, 2, 65], BF16)
    tri = sb('tri', [128, 128], F32)
    ntri = sb('ntri', [128, 128], F32)
    cm = sb('cm', [128, 128], F32)
    keep = sb('keep', [128, 128], F32)
    addc = sb('addc', [128, 128], F32)
    cts = sb('cts', [128, 2, 64], BF16)
    decT = sb('decT', [128, 512], F32)
    xi = sb('xi', [128, 512], F32)
    zeta = sb('zeta', [128, 4], F32)
    gch = sb('gch', [128, 4], F32)
    gng = sb('gng', [128, 512], F32)
    for nm, t_, src in [('tri', tri, 'c_tri'), ('cm', cm, 'c_cm'), ('keep', keep, 'c_keep'), ('addc', addc, 'c_add'),
                        ('decT', decT, 'c_decT'), ('xi', xi, 'c_xi'), ('zeta', zeta, 'c_zeta'), ('gch', gch, 'c_gch')]:
        P.dma('sp', t_, ins[src], writes=[nm], stream='small')
    P.dma('sp', cts, ins['c_cts'].rearrange('c n j -> n c j'), writes=['cts'], stream='small')
    P.dma('sp', KE[64:128, 0, :], ins['c_E'], writes=['KE'], stream='small')
    P.dma('sp', KE[64:128, 1, :], ins['c_E'], writes=['KE'], stream='small')
    P.dma('sp', gng, ins['od_ret_gn_g'][o_:o_ + 1, :].broadcast_to([128, 512]), writes=['gng'], stream='small')
    P.op('dve', lambda e: e.tensor_scalar(out=ntri, in0=tri, scalar1=-1.0, scalar2=1.0, op0=ALU.mult, op1=ALU.add),
         reads=['tri'], writes=['ntri'])
    P.op('pool', lambda e: e.memset(vs_aug, 1.0), writes=['vs_aug'])
    P.op('pool', lambda e: e.memset(vw_aug, 1.0), writes=['vw_aug'])
    P.op('pool', lambda e: e.memset(vcmp_aug, 1.0), writes=['vcmp'])
    with ExitStack() as ts:
        posi = sbs(ts, 'posi', [128, NT], I32)
        sinT = sbs(ts, 'sinT', [128, NT, 72], F32)
        cosT = sbs(ts, 'cosT', [128, NT, 72], F32)
        posf = sbs(ts, 'posf', [128, NT], F32)
        invf = sbs(ts, 'invf', [128, 72], F32)
        ang = sbs(ts, 'ang', [128, NT, 72], F32)
        arg = sbs(ts, 'arg', [128, NT, 72], F32)
        kf = sbs(ts, 'kf', [128, NT, 72], F32)
        ki = sbs(ts, 'ki', [128, NT, 72], I32)
        with nc.allow_non_contiguous_dma(reason='positions to token-on-partition layout'):
            P.dma('sp', posi, ins['positions'].rearrange('o (i p) -> p (o i)', p=128), writes=['posi'], stream='small')
        P.dma('sp', invf, ins['c_invf'].broadcast_to([128, 72]), writes=['invf'], stream='small')
        P.op('dve', lambda e: e.tensor_copy(out=posf, in_=posi), reads=['posi'], writes=['posf'])
        P.op('dve', lambda e: e.tensor_tensor(out=ang, in0=posf.unsqueeze(2).broadcast_to([128, NT, 72]),
                                              in1=invf.unsqueeze(1).broadcast_to([128, NT, 72]), op=ALU.mult),
             reads=['posf', 'invf'], writes=['ang'])
        for shift, dst, dn in [(0.0, sinT, 'sinT'), (PI / 2, cosT, 'cosT')]:
            P.op('dve', lambda e, shift=shift: e.tensor_scalar(out=arg, in0=ang, scalar1=shift, scalar2=None, op0=ALU.add),
                 reads=['ang'], writes=['arg'])
            P.op('dve', lambda e: e.tensor_scalar(out=kf, in0=arg, scalar1=1.0 / (2 * PI), scalar2=None, op0=ALU.mult),
                 reads=['arg'], writes=['kf'])
            P.op('dve', lambda e: e.tensor_copy(out=ki, in_=kf), reads=['kf'], writes=['ki'])
            P.op('dve', lambda e: e.tensor_copy(out=kf, in_=ki), reads=['ki'], writes=['kf'])
            P.op('dve', lambda e: e.scalar_tensor_tensor(out=arg, in0=kf, scalar=-6.28125, in1=arg, op0=ALU.mult, op1=ALU.add),
                 reads=['kf', 'arg'], writes=['arg'])
            P.op('dve', lambda e: e.scalar_tensor_tensor(out=arg, in0=kf, scalar=-(2 * PI - 6.28125), in1=arg, op0=ALU.mult,
                                                         op1=ALU.add), reads=['kf', 'arg'], writes=['arg'])
            P.op('dve', lambda e: e.tensor_scalar(out=arg, in0=arg, scalar1=3.1415925, scalar2=-3.1415925, op0=ALU.min,
                                                  op1=ALU.max), reads=['arg'], writes=['arg'])
            P.op('act', lambda e, dst=dst: e.activation(out=dst, in_=arg, func=AF.Sin), reads=['arg'], writes=[dn])
        P.dma('sp', ropeS, sinT, reads=['sinT'], writes=['ropeS'], stream='xo')
        P.dma('sp', ropeC, cosT, reads=['cosT'], writes=['ropeC'], stream='xo')
        P.barrier()

    with ExitStack() as s1:
        w_in_sb = sbs(s1, 'w_in', [128, 8, 3352], BF16)
        hT1s = [sbs(s1, 'hT1_%d' % j, [128, 8, 128], BF16) for j in range(2)]
        cur = {}
        csb = [sbs(s1, 'csb%d' % j, [128, 2, 72], F32) for j in range(2)]
        kcv_t = sbs(s1, 'kcv_t', [64, 4, 128], BF16)
        zqs = [sbs(s1, 'zq%d' % j, [128, 512], F32) for j in range(2)]
        qb = sbs(s1, 'qb', [128, 512], BF16)
        zkvs = [sbs(s1, 'zkv%d' % j, [128, 768], F32) for j in range(2)]
        kvb = sbs(s1, 'kvb', [128, 768], BF16)
        rt = [sbs(s1, 'rt%d' % j, [128, 256], F32) for j in range(4)]
        zrqs = [sbs(s1, 'zrq%d' % j, [128, 512], F32) for j in range(2)]
        zrks = [sbs(s1, 'zrk%d' % j, [128, 512], F32) for j in range(2)]
        rqb = sbs(s1, 'rqb', [128, 512], BF16)
        rkb = sbs(s1, 'rkb', [128, 512], BF16)
        rkr = sbs(s1, 'rkr', [128, 512], F32)
        rkz = sbs(s1, 'rkz', [128, 512], BF16)
        rvbs = [sbs(s1, 'rvb%d' % j, [128, 512], BF16) for j in range(2)]
        gsls = [sbs(s1, 'gsl%d' % j, [128, 512], F32) for j in range(2)]
        qkT = sbs(s1, 'qkT', [128, 8, 128], BF16)
        qxiT = sbs(s1, 'qxiT', [128, 512], BF16)
        STs = sbs(s1, 'STs', [128, 512], BF16)
        state = sbs(s1, 'state', [128, 512], F32)
        stbf = sbs(s1, 'stbf', [128, 512], BF16)
        bst4 = sbs(s1, 'bst4', [128, 4, 6], F32)
        mv4 = sbs(s1, 'mv4', [128, 4, 2], F32)
        rs4 = sbs(s1, 'rs4', [128, 4], F32)
        on = sbs(s1, 'on', [128, 512], F32)
        ydb = sbs(s1, 'ydb', [128, 512], BF16)
        P.dma('sp', w_in_sb, k.wb[('od_w_in', o_)].rearrange('(c p) n -> p c n', p=128), reads=[('wb', 'od_w_in', o_)],
              writes=['w_in'], stream='w')
        P.op('dve', lambda e: e.memset(state, 0.0), writes=['state'])

        def proj(bank, c0, c1):
            for kc in range(8):
                P.op('pe', lambda e, kc=kc: e.matmul(k.bank[bank][:, 0:c1 - c0], lhsT=cur['hT'][:, kc, :], rhs=w_in_sb[:, kc, c0:c1],
                                                    start=(kc == 0), stop=(kc == 7)), reads=[cur['hTn'], 'w_in'],
                     writes=['bank%d' % bank])

        def rope(src, dst, nh, hd, half, c_, s_, csn, sname, dname):
            sv = src.rearrange('p (h d) -> p h d', h=nh)
            dv = dst.rearrange('p (h d) -> p h d', h=nh)
            x1, x2 = sv[:, :, 0:half], sv[:, :, half:2 * half]
            cb = c_.unsqueeze(1).broadcast_to([128, nh, half])
            sb_ = s_.unsqueeze(1).broadcast_to([128, nh, half])
            t = [r_[:, 0:nh * half].rearrange('p (h d) -> p h d', h=nh) for r_ in rt]
            P.op('dve', lambda e: e.tensor_tensor(out=t[0], in0=x1, in1=cb, op=ALU.mult), reads=[sname, csn], writes=['rt0'])
            P.op('pool', lambda e: e.tensor_tensor(out=t[1], in0=x2, in1=sb_, op=ALU.mult), reads=[sname, csn], writes=['rt1'])
            P.op('dve', lambda e: e.tensor_tensor(out=t[2], in0=x2, in1=cb, op=ALU.mult), reads=[sname, csn], writes=['rt2'])
            P.op('pool', lambda e: e.tensor_tensor(out=t[3], in0=x1, in1=sb_, op=ALU.mult), reads=[sname, csn], writes=['rt3'])
            if 2 * half < hd:
                P.op('act', lambda e: e.copy(out=dst, in_=src), reads=[sname], writes=[dname])
            P.op('dve', lambda e: e.tensor_tensor(out=dv[:, :, 0:half], in0=t[0], in1=t[1], op=ALU.subtract),
                 reads=['rt0', 'rt1'], writes=[dname])
            P.op('pool', lambda e: e.tensor_tensor(out=dv[:, :, half:2 * half], in0=t[2], in1=t[3], op=ALU.add),
                 reads=['rt2', 'rt3'], writes=[dname])

        def stage_p(i):
            pz = i % 2
            cur['hT'] = hT1s[pz]
            cur['hTn'] = 'hT1_%d' % pz
            cs_ = csb[pz]
            csn = 'csb%d' % pz
            P.dma('sp', cs_[:, 0, :], ropeC[:, i, :], reads=['ropeC'], writes=[csn], stream='small')
            P.dma('sp', cs_[:, 1, :], ropeS[:, i, :], reads=['ropeS'], writes=[csn], stream='small')
            P.dma('sp', k.xt[i % 3], xsrc[i * 128:(i + 1) * 128, :], reads=[('x', i)], writes=['xt%d' % (i % 3)], stream='x')
            norm_a(k, k.xt[i % 3], 'xt%d' % (i % 3), 'mix_pre', pz)
            trans_t(k, pz, hT1s[pz], 'hT1_%d' % pz)
            proj(1, 0, 512)
            proj(2, 512, 1024)
            proj(3, 1024, 1304)
            P.op('act', lambda e: e.activation(out=zqs[pz], in_=k.bank[1], func=AF.Copy, scale=0.125), reads=['bank1'],
                 writes=['zq%d' % pz])
            P.op('act', lambda e: e.copy(out=zkvs[pz][:, 0:512], in_=k.bank[2]), reads=['bank2'], writes=['zkv%d' % pz])
            P.op('act', lambda e: e.copy(out=zkvs[pz][:, 512:768], in_=k.bank[3][:, 0:256]), reads=['bank3'], writes=['zkv%d' % pz])
            P.op('act', lambda e: e.activation(out=gsig[:, i, :], in_=k.bank[3][:, 256:280], func=AF.Sigmoid),
                 reads=['bank3'], writes=[('gsig', i)])
            proj(4, 1304, 1816)
            proj(5, 1816, 2328)
            proj(6, 2328, 2840)
            proj(7, 2840, 3352)
            P.op('act', lambda e: e.copy(out=zrqs[pz], in_=k.bank[4]), reads=['bank4'], writes=['zrq%d' % pz])
            P.op('act', lambda e: e.activation(out=zrks[pz], in_=k.bank[5], func=AF.Copy, scale=128.0 ** -0.5), reads=['bank5'],
                 writes=['zrk%d' % pz])
            P.op('act', lambda e: e.copy(out=rvbs[pz], in_=k.bank[6]), reads=['bank6'], writes=['rvb%d' % pz])
            P.op('act', lambda e: e.activation(out=gsls[pz], in_=k.bank[7], func=AF.Silu), reads=['bank7'], writes=['gsl%d' % pz])

        def stage_r(i):
            pz = i % 2
            zq, zkv, zrq, zrk, rvb, gsl = zqs[pz], zkvs[pz], zrqs[pz], zrks[pz], rvbs[pz], gsls[pz]
            zqn, zkvn, zrqn, zrkn, rvbn, gsln = ['%s%d' % (n_, pz) for n_ in ('zq', 'zkv', 'zrq', 'zrk', 'rvb', 'gsl')]
            cs_ = csb[pz]
            csn = 'csb%d' % pz
            cn, sn = cs_[:, 0, 0:8], cs_[:, 1, 0:8]
            cr, sr = cs_[:, 0, 8:72], cs_[:, 1, 8:72]
            rope(zq, qb, 8, 64, 8, cn, sn, csn, zqn, 'qb')
            P.dma('sp', qd[i * 128:(i + 1) * 128, :], qb, reads=['qb'], writes=[('qd', i)], stream='xo')
            kv4 = zkv.rearrange('p (a b g d) -> p a b g d', a=3, b=2, g=2)[:, :, 0, :, :]
            x1, x2 = kv4[:, :, :, 0:8], kv4[:, :, :, 8:16]
            cb = cn.unsqueeze(1).unsqueeze(1).broadcast_to([128, 3, 2, 8])
            sb_ = sn.unsqueeze(1).unsqueeze(1).broadcast_to([128, 3, 2, 8])
            t = [r_[:, 0:48].rearrange('p (a g d) -> p a g d', a=3, g=2) for r_ in rt]
            P.op('dve', lambda e: e.tensor_tensor(out=t[0], in0=x1, in1=cb, op=ALU.mult), reads=[zkvn, csn], writes=['rt0'])
            P.op('dve', lambda e: e.tensor_tensor(out=t[1], in0=x2, in1=sb_, op=ALU.mult), reads=[zkvn, csn], writes=['rt1'])
            P.op('dve', lambda e: e.tensor_tensor(out=t[2], in0=x2, in1=cb, op=ALU.mult), reads=[zkvn, csn], writes=['rt2'])
            P.op('dve', lambda e: e.tensor_tensor(out=t[3], in0=x1, in1=sb_, op=ALU.mult), reads=[zkvn, csn], writes=['rt3'])
            P.op('dve', lambda e: e.tensor_tensor(out=x1, in0=t[0], in1=t[1], op=ALU.subtract), reads=['rt0', 'rt1'], writes=[zkvn])
            P.op('dve', lambda e: e.tensor_tensor(out=x2, in0=t[2], in1=t[3], op=ALU.add), reads=['rt2', 'rt3'], writes=[zkvn])
            P.op('act', lambda e: e.copy(out=kvb, in_=zkv), reads=[zkvn], writes=['kvb'])
            P.op('pool', lambda e: e.tensor_copy(out=vs_aug[:, i, :, 0:64], in_=kvb[:, 384:512].rearrange('p (g d) -> p g d', g=2)),
                 reads=['kvb'], writes=['vs_aug'])
            P.op('pool', lambda e: e.tensor_copy(out=vw_aug[:, i, :, 0:64], in_=kvb[:, 640:768].rearrange('p (g d) -> p g d', g=2)),
                 reads=['kvb'], writes=['vw_aug'])
            tp = k.bankb[0]
            srcs = [0, 64, 128, 192, 512, 576, 256, 320]
            for j, c0 in enumerate(srcs):
                P.op('pe', lambda e, j=j, c0=c0: e.transpose(out=tp[0:64, j * 128:(j + 1) * 128], in_=kvb[:, c0:c0 + 64],
                                                            identity=k.ident), reads=['kvb', 'ident'], writes=['bank0'])
            P.op('act', lambda e: e.copy(out=kcv_t, in_=tp[0:64, 0:512].rearrange('p (j t) -> p j t', j=4)), reads=['bank0'],
                 writes=['kcv_t'])
            P.dma('sp', kcvd[:, :, i * 128:(i + 1) * 128].rearrange('j p t -> p j t'), kcv_t, reads=['kcv_t'], writes=['kcvd'],
                  stream='xo')
            P.op('act', lambda e: e.copy(out=kwT[:, :, i * 128:(i + 1) * 128],
                                         in_=tp[0:64, 512:768].rearrange('p (j t) -> p j t', j=2)), reads=['bank0'], writes=['kwT'])
            P.op('act', lambda e: e.copy(out=KE[0:64, :, i * 128:(i + 1) * 128],
                                         in_=tp[0:64, 768:1024].rearrange('p (j t) -> p j t', j=2)), reads=['bank0'], writes=['KE'])
            rope(zrq, rqb, 4, 128, 64, cr, sr, csn, zrqn, 'rqb')
            rope(zrk, rkr, 4, 128, 64, cr, sr, csn, zrkn, 'rkr')
            P.op('act', lambda e: e.copy(out=rkb, in_=rkr), reads=['rkr'], writes=['rkb'])
            P.op('dve', lambda e: e.tensor_tensor(out=rkz.rearrange('p (h d) -> p h d', h=4),
                                                  in0=rkr.rearrange('p (h d) -> p h d', h=4),
                                                  in1=zeta.unsqueeze(2).broadcast_to([128, 4, 128]), op=ALU.mult),
                 reads=['rkr', 'zeta'], writes=['rkz'])
            for h in range(4):
                P.op('pe', lambda e, h=h: e.transpose(out=tp[:, h * 128:(h + 1) * 128], in_=rqb[:, h * 128:(h + 1) * 128],
                                                      identity=k.ident), reads=['rqb', 'ident'], writes=['bank0'])
            for h in range(4):
                P.op('pe', lambda e, h=h: e.transpose(out=tp[:, (4 + h) * 128:(5 + h) * 128], in_=rkb[:, h * 128:(h + 1) * 128],
                                                      identity=k.ident), reads=['rkb', 'ident'], writes=['bank0'])
            P.op('act', lambda e: e.copy(out=qkT, in_=tp.rearrange('p (c t) -> p c t', c=8)), reads=['bank0'], writes=['qkT'])
            for h in range(4):
                P.op('pe', lambda e, h=h: e.matmul(k.bank[5][:, h * 128:(h + 1) * 128], lhsT=qkT[:, 4 + h, :], rhs=qkT[:, h, :],
                                                  start=True, stop=True), reads=['qkT'], writes=['bank5'])
            P.op('dve', lambda e: e.tensor_tensor(out=STs, in0=k.bank[5], in1=decT, op=ALU.mult), reads=['bank5', 'decT'],
                 writes=['STs'])
            P.op('pool', lambda e: e.tensor_tensor(out=qxiT, in0=qkT[:, 0:4, :].rearrange('p h t -> p (h t)'), in1=xi, op=ALU.mult),
                 reads=['qkT', 'xi'], writes=['qxiT'])
            for h in range(4):
                hs = slice(h * 128, (h + 1) * 128)
                P.op('pe', lambda e, hs=hs: e.matmul(k.bank[7][:, hs], lhsT=STs[:, hs], rhs=rvb[:, hs], start=True, stop=(i == 0)),
                     reads=['STs', rvbn], writes=['bank7'])
                if i > 0:
                    P.op('pe', lambda e, hs=hs: e.matmul(k.bank[7][:, hs], lhsT=qxiT[:, hs], rhs=stbf[:, hs], start=False, stop=True),
                         reads=['qxiT', 'stbf'], writes=['bank7'])
            for h in range(4):
                hs = slice(h * 128, (h + 1) * 128)
                P.op('pe', lambda e, hs=hs: e.matmul(k.bank[6][:, hs], lhsT=rkz[:, hs], rhs=rvb[:, hs], start=True, stop=True),
                     reads=['rkz', rvbn], writes=['bank6'])
            P.op('dve', lambda e: e.tensor_tensor(out=state.rearrange('p (h d) -> p h d', h=4),
                                                  in0=state.rearrange('p (h d) -> p h d', h=4),
                                                  in1=gch.unsqueeze(2).broadcast_to([128, 4, 128]), op=ALU.mult),
                 reads=['state', 'gch'], writes=['state'])
            P.op('dve', lambda e: e.tensor_tensor(out=state, in0=k.bank[6], in1=state, op=ALU.add), reads=['bank6', 'state'],
                 writes=['state'])
            P.op('act', lambda e: e.copy(out=stbf, in_=state), reads=['state'], writes=['stbf'])
            for h in range(4):
                P.op('dve', lambda e, h=h: e.bn_stats(out=bst4[:, h, :], in_=k.bank[7][:, h * 128:(h + 1) * 128]),
                     reads=['bank7'], writes=['bst4'])
            for h in range(4):
                P.op('dve', lambda e, h=h: e.bn_aggr(out=mv4[:, h, :], in_=bst4[:, h, :]), reads=['bst4'], writes=['mv4'])
            P.op('dve', lambda e: e.tensor_scalar(out=rs4, in0=mv4[:, :, 1], scalar1=1e-5, scalar2=None, op0=ALU.add),
                 reads=['mv4'], writes=['rs4'])
            P.op('pool', lambda e: e.tensor_tensor(out=rs4, in0=rs4, in1=k.mhalf, op=ALU.pow), reads=['rs4', 'mhalf'], writes=['rs4'])
            for h in range(4):
                hs = slice(h * 128, (h + 1) * 128)
                P.op('dve', lambda e, h=h, hs=hs: e.tensor_scalar(out=on[:, hs], in0=k.bank[7][:, hs], scalar1=mv4[:, h, 0:1],
                                                                  scalar2=rs4[:, h:h + 1], op0=ALU.subtract, op1=ALU.mult),
                     reads=['bank7', 'mv4', 'rs4'], writes=['on'])
            P.op('pool', lambda e: e.tensor_tensor(out=on, in0=on, in1=gng, op=ALU.mult), reads=['on', 'gng'], writes=['on'])
            P.op('pool', lambda e: e.tensor_tensor(out=ydb, in0=on, in1=gsl, op=ALU.mult), reads=['on', gsln], writes=['ydb'])
            P.dma('sp', ydd[i * 128:(i + 1) * 128, :], ydb, reads=['ydb'], writes=[('ydd', i)], stream='xo')

        stage_p(0)
        for i in range(NT):
            if i + 1 < NT:
                stage_p(i + 1)
            stage_r(i)

        P.barrier()
        s1.close()
        s1c = ExitStack()
        w1 = sbs(s1c, 'w1', [64, 32, 128], BF16)
        w2 = sbs(s1c, 'w2', [128, 64], BF16)
        posf32 = sbs(s1c, 'posf32', [64, 32], F32)
        posT = sbs(s1c, 'posT', [64, 32], BF16)
        cbias = sbs(s1c, 'cbias', [128, 1], F32)
        ghT = sbs(s1c, 'ghT', [128, 256], BF16)
        csrc = sbs(s1c, 'csrc', [64, T], BF16)
        for kind, (n1, n2, npos) in enumerate([('od_cmp_k_w1', 'od_cmp_k_w2', 'od_cmp_k_pos'),
                                               ('od_cmp_v_w1', 'od_cmp_v_w2', 'od_cmp_v_pos')]):
            P.dma('sp', w1, k.wb[(n1, o_)].rearrange('(l d) j -> d l j', d=64), reads=[('wb', n1, o_)], writes=['w1'], stream='w')
            P.dma('sp', w2, k.wb[(n2, o_)], reads=[('wb', n2, o_)], writes=['w2'], stream='w')
            with nc.allow_non_contiguous_dma(reason='tiny pos-emb transpose'):
                P.dma('sp', posf32, ins[npos][o_].rearrange('l d -> d l'), writes=['posf32'], stream='small')
            P.op('dve', lambda e: e.tensor_copy(out=posT, in_=posf32), reads=['posf32'], writes=['posT'])
            for g in range(2):
                P.dma('sp', csrc, kcvd[2 * kind + g], reads=['kcvd'], writes=['csrc'], stream='w')
                srcv = csrc.rearrange('p (n s) -> p n s', s=16)
                hb_ = k.bank[1]
                for l_ in range(32):
                    P.op('pe', lambda e, l_=l_: e.matmul(hb_[:, 0:255], lhsT=w1[:, l_, :],
                                                        rhs=(srcv[:, 0:255, l_] if l_ < 16 else srcv[:, 1:256, l_ - 16]),
                                                        start=(l_ == 0), stop=(l_ == 31)),
                         reads=['w1', 'csrc'], writes=['bank1'])
                for l_ in range(32):
                    P.op('pe', lambda e, l_=l_: e.matmul(hb_[:, 256:257], lhsT=w1[:, l_, :], rhs=posT[:, l_:l_ + 1],
                                                        start=(l_ == 0), stop=(l_ == 31)), reads=['w1', 'posT'], writes=['bank1'])
                P.op('dve', lambda e: e.tensor_copy(out=cbias, in_=hb_[:, 256:257]), reads=['bank1'], writes=['cbias'])
                P.op('dve', lambda e: e.memset(ghT[:, 255:256], 0.0), writes=['ghT'])
                P.op('act', lambda e: e.activation(out=ghT[:, 0:255], in_=hb_[:, 0:255], func=AF.Gelu_apprx_tanh,
                                                   bias=cbias[:, 0:1]), reads=['bank1', 'cbias'], writes=['ghT'])
                if kind == 0:
                    P.op('pe', lambda e: e.matmul(k.bank[2][0:64, 0:256], lhsT=w2, rhs=ghT, start=True, stop=True),
                         reads=['w2', 'ghT'], writes=['bank2'])
                    P.op('act', lambda e, g=g: e.copy(out=kcmpT[:, g, :], in_=k.bank[2][0:64, 0:256]), reads=['bank2'],
                         writes=['kcmpT'])
                else:
                    for c in range(2):
                        P.op('pe', lambda e, c=c: e.matmul(k.bank[2][:, c * 64:(c + 1) * 64], lhsT=ghT[:, c * 128:(c + 1) * 128],
                                                          rhs=w2, start=True, stop=True), reads=['w2', 'ghT'], writes=['bank2'])
                    P.op('act', lambda e, g=g: e.copy(out=vcmp_aug[:, :, g, 0:64],
                                                      in_=k.bank[2][:, 0:128].rearrange('p (c d) -> p c d', c=2)),
                         reads=['bank2'], writes=['vcmp'])
        P.barrier()
        s1c.close()

    with ExitStack() as s2:
        w_out_sb = sbs(s2, 'w_out', [128, 8, 1024], BF16)
        PT = sbs(s2, 'PT', [128, NT, 512], BF16)
        PW = sbs(s2, 'PW', [128, 5, 512], BF16)
        Pc = sbs(s2, 'Pc', [128, 2, 512], BF16)
        QN = [sbs(s2, 'QN%d' % g, [128, 512], BF16) for g in range(2)]
        qt = [sbs(s2, 'qt%d' % j, [128, 512], BF16) for j in range(2)]
        ydts = [sbs(s2, 'ydt%d' % j, [128, 512], BF16) for j in range(2)]
        negt = sbs(s2, 'negt', [128, 128], BF16)
        expf = sbs(s2, 'expf', [128, 512], F32)
        score = sbs(s2, 'score', [128, 64], F32)
        sc2 = sbs(s2, 'sc2', [128, 64], F32)
        m8a = sbs(s2, 'm8a', [128, 8], F32)
        m8b = sbs(s2, 'm8b', [128, 8], F32)
        thr = sbs(s2, 'thr', [128, 1], F32)
        psl = sbs(s2, 'psl', [128, 64], F32)
        rdenA = [sbs(s2, 'rdenA%d' % g, [128, 4], F32) for g in range(2)]
        rdenB = [sbs(s2, 'rdenB%d' % g, [128, 12], F32) for g in range(2)]
        coefs = [sbs(s2, 'coef%d' % g, [128, 12], F32) for g in range(2)]
        ocw = [sbs(s2, 'ocw%d' % g, [128, 2, 260], F32) for g in range(2)]
        yc = sbs(s2, 'yc', [128, 512], F32)
        ycb = sbs(s2, 'ycb', [128, 512], BF16)
        yT = sbs(s2, 'yT', [128, 8, 128], BF16)
        P.dma('sp', w_out_sb, k.wb[('od_w_out', o_)].rearrange('(c p) n -> p c n', p=128), reads=[('wb', 'od_w_out', o_)],
              writes=['w_out'], stream='w')
        P.op('dve', lambda e: e.memset(negt, 0.0), writes=['negt'])
        tp = k.bankb[0]
        sbank = [0]

        def next_sbank():
            sbank[0] += 1
            return (1, 2, 7)[sbank[0] % 3]

        def den_view(ap260):
            return ap260.rearrange('p (m e) -> p m e', e=65)[:, :, 64]

        def masked_exp(b, nn, dst, mask_ap, dname):
            if mask_ap is None:
                P.op('act', lambda e: e.activation(out=dst[0:nn], in_=k.bank[b][0:nn, :], func=AF.Exp), reads=['bank%d' % b],
                     writes=[dname])
            else:
                P.op('act', lambda e: e.activation(out=expf[0:nn], in_=k.bank[b][0:nn, :], func=AF.Exp), reads=['bank%d' % b],
                     writes=['expf'])
                P.op('pool', lambda e: e.tensor_tensor(out=dst[0:nn].rearrange('p (m q) -> p m q', m=4),
                                                       in0=expf[0:nn].rearrange('p (m q) -> p m q', m=4),
                                                       in1=mask_ap.unsqueeze(1).broadcast_to([nn, 4, 128]), op=ALU.mult),
                     reads=['expf', 'tri', 'ntri'], writes=[dname])

        def o2_loads(i):
            P.dma('sp', k.xt[i % 3], xsrc[i * 128:(i + 1) * 128, :], reads=[('x', i)], writes=['xt%d' % (i % 3)], stream='x')
            P.dma('sp', qt[i % 2], qd[i * 128:(i + 1) * 128, :], reads=[('qd', i)], writes=['qt%d' % (i % 2)], stream='x')
            P.dma('sp', ydts[i % 2], ydd[i * 128:(i + 1) * 128, :], reads=[('ydd', i)], writes=['ydt%d' % (i % 2)], stream='x')

        def stage_a(i, g):
            qti = qt[i % 2]
            qtn = 'qt%d' % (i % 2)
            off = 62 - 2 * i
            Q = QN[g]
            qn = 'QN%d' % g
            rdA = rdenA[g]
            rdAn = 'rdenA%d' % g
            for m in range(4):
                h = 4 * g + m
                P.op('pe', lambda e, m=m, h=h: e.transpose(out=tp[0:64, m * 128:(m + 1) * 128], in_=qti[:, h * 64:(h + 1) * 64],
                                                          identity=k.ident), reads=[qtn, 'ident'], writes=['bank0'])
            P.op('act', lambda e: e.copy(out=Q[0:64, :], in_=tp[0:64, 0:512]), reads=['bank0'], writes=[qn + 'lo'])
            chunks = [(0, 128)] + ([(1, 127)] if 8 * i + 6 >= 128 else [])
            for (c, nn) in chunks:
                b = next_sbank()
                P.op('pe', lambda e, c=c, nn=nn, b=b: e.matmul(k.bank[b][0:nn, :], lhsT=kcmpT[:, g, c * 128:c * 128 + nn],
                                                              rhs=Q[0:64, :], start=True, stop=True),
                     reads=['kcmpT', qn + 'lo'], writes=['bank%d' % b])
                full = (16 * (c * 128 + nn - 1) + 31 <= 128 * i)
                if full:
                    P.op('act', lambda e, c=c, nn=nn, b=b: e.activation(out=Pc[0:nn, c, :], in_=k.bank[b][0:nn, :], func=AF.Exp),
                         reads=['bank%d' % b], writes=['Pc'])
                else:
                    tv = float(128 * i - 31 - 2048 * c)
                    P.op('act', lambda e, nn=nn, b=b: e.activation(out=expf[0:nn], in_=k.bank[b][0:nn, :], func=AF.Exp),
                         reads=['bank%d' % b], writes=['expf'])
                    P.op('dve', lambda e, c=c, nn=nn, tv=tv: e.scalar_tensor_tensor(
                        out=Pc[0:nn, c, :].rearrange('p (m q) -> p m q', m=4),
                        in0=cm[0:nn].unsqueeze(1).broadcast_to([nn, 4, 128]), scalar=tv,
                        in1=expf[0:nn].rearrange('p (m q) -> p m q', m=4), op0=ALU.is_le, op1=ALU.mult),
                         reads=['expf', 'cm'], writes=['Pc'])
            for m in range(4):
                for ci, (c, nn) in enumerate(chunks):
                    P.op('pe', lambda e, m=m, c=c, nn=nn, ci=ci: e.matmul(k.bank[3][:, m * 65:(m + 1) * 65],
                                                                        lhsT=Pc[0:nn, c, m * 128:(m + 1) * 128],
                                                                        rhs=vcmp_aug[0:nn, c, g, :], start=(ci == 0),
                                                                        stop=(ci == len(chunks) - 1)),
                         reads=['Pc', 'vcmp'], writes=['bank3'])
            for m in range(4):
                for ci, (c, nn) in enumerate(chunks):
                    P.op('pe', lambda e, m=m, c=c, nn=nn, ci=ci: e.matmul(k.bank[4][:, m * 64:(m + 1) * 64],
                                                                        lhsT=Pc[0:nn, c, m * 128:(m + 1) * 128],
                                                                        rhs=cts[0:nn, c, :], start=(ci == 0),
                                                                        stop=(ci == len(chunks) - 1)),
                         reads=['Pc', 'cts'], writes=['bank4'])
            jts = list(range(max(0, i - 4), i + 1))
            for sl_, jt in enumerate(jts):
                b = next_sbank()
                P.op('pe', lambda e, jt=jt, b=b: e.matmul(k.bank[b], lhsT=kwT[:, g, jt * 128:(jt + 1) * 128], rhs=Q[0:64, :],
                                                         start=True, stop=True), reads=['kwT', qn + 'lo'], writes=['bank%d' % b])
                mk = tri if jt == i else (ntri if jt == i - 4 else None)
                masked_exp(b, 128, PW[:, sl_, :], mk, ('PW', sl_))
            for m in range(4):
                for sl_, jt in enumerate(jts):
                    P.op('pe', lambda e, m=m, jt=jt, sl_=sl_: e.matmul(k.bank[6][:, m * 65:(m + 1) * 65],
                                                                      lhsT=PW[:, sl_, m * 128:(m + 1) * 128],
                                                                      rhs=vw_aug[:, jt, g, :], start=(sl_ == 0),
                                                                      stop=(sl_ == len(jts) - 1)),
                         reads=[('PW', sl_), 'vw_aug'], writes=['bank6'])
            P.op('dve', lambda e: e.tensor_scalar(out=rdA, in0=den_view(k.bank[3][:, 0:260]), scalar1=1e-30, scalar2=None,
                                                  op0=ALU.max), reads=['bank3'], writes=[rdAn])
            P.op('dve', lambda e: e.reciprocal(out=rdA, in_=rdA), reads=[rdAn], writes=[rdAn])
            P.op('dve', lambda e: e.tensor_scalar(out=psl, in0=k.bank[4][:, 0:64], scalar1=rdA[:, 0:1], scalar2=None,
                                                  op0=ALU.mult), reads=['bank4', rdAn], writes=['psl'])
            for m in range(1, 4):
                P.op('dve', lambda e, m=m: e.scalar_tensor_tensor(out=psl, in0=k.bank[4][:, m * 64:(m + 1) * 64],
                                                                  scalar=rdA[:, m:m + 1], in1=psl, op0=ALU.mult, op1=ALU.add),
                     reads=['bank4', rdAn, 'psl'], writes=['psl'])
            P.op('act', lambda e: e.copy(out=ocw[g][:, 0, :], in_=k.bank[3][:, 0:260]), reads=['bank3'], writes=['ocw%d' % g])
            P.op('act', lambda e: e.copy(out=ocw[g][:, 1, :], in_=k.bank[6][:, 0:260]), reads=['bank6'], writes=['ocw%d' % g])
            P.op('dve', lambda e: e.tensor_tensor(out=score, in0=psl, in1=keep[:, off:off + 64], op=ALU.mult),
                 reads=['psl', 'keep'], writes=['score'])
            P.op('dve', lambda e: e.tensor_tensor(out=score, in0=score, in1=addc[:, off:off + 64], op=ALU.add),
                 reads=['score', 'addc'], writes=['score'])
            P.op('dve', lambda e: e.memset(score[:, 0:1], 1.0e4), reads=['score'], writes=['score'])
            P.op('dve', lambda e: e.max(out=m8a, in_=score), reads=['score'], writes=['m8a'])
            P.op('dve', lambda e: e.match_replace(out=sc2, in_to_replace=m8a, in_values=score, imm_value=-2.0),
                 reads=['score', 'm8a'], writes=['sc2'])
            P.op('dve', lambda e: e.max(out=m8b, in_=sc2), reads=['sc2'], writes=['m8b'])
            P.op('dve', lambda e: e.tensor_scalar(out=thr, in0=m8b[:, 7:8], scalar1=0.0, scalar2=None, op0=ALU.max),
                 reads=['m8b'], writes=['thr'])
            P.op('dve', lambda e: e.tensor_scalar(out=negt[:, 64:128], in0=score, scalar1=thr[:, 0:1], scalar2=-30000.0,
                                                  op0=ALU.is_lt, op1=ALU.mult), reads=['score', 'thr'], writes=['negt'])
            P.op('pe', lambda e: e.transpose(out=tp[:, 512:640], in_=negt, identity=k.ident), reads=['negt', 'ident'],
                 writes=['bank0'])
            P.op('act', lambda e: e.copy(out=Q[64:128, :].rearrange('p (m q) -> p m q', m=4),
                                         in_=tp[64:128, 512:640].unsqueeze(1).broadcast_to([64, 4, 128])),
                 reads=['bank0'], writes=[qn + 'hi'])

        def stage_b(i, g):
            Q = QN[g]
            qn = 'QN%d' % g
            ob = 5
            obn = 'bank%d' % ob
            rdB = rdenB[g]
            rdBn = 'rdenB%d' % g
            coef = coefs[g]
            cfn = 'coef%d' % g
            for jt in range(i + 1):
                b = next_sbank()
                P.op('pe', lambda e, jt=jt, b=b: e.matmul(k.bank[b], lhsT=KE[:, g, jt * 128:(jt + 1) * 128], rhs=Q, start=True,
                                                         stop=True), reads=['KE', qn + 'lo', qn + 'hi'], writes=['bank%d' % b])
                masked_exp(b, 128, PT[:, jt, :], tri if jt == i else None, ('PT', jt))
            for m in range(4):
                for jt in range(i + 1):
                    P.op('pe', lambda e, m=m, jt=jt: e.matmul(k.bank[ob][:, m * 65:(m + 1) * 65],
                                                             lhsT=PT[:, jt, m * 128:(m + 1) * 128], rhs=vs_aug[:, jt, g, :],
                                                             start=(jt == 0), stop=(jt == i)),
                         reads=[('PT', jt), 'vs_aug'], writes=[obn])
            P.op('dve', lambda e: e.tensor_copy(out=rdB[:, 0:4], in_=rdenA[g]), reads=['rdenA%d' % g], writes=[rdBn])
            P.op('dve', lambda e: e.tensor_scalar(out=rdB[:, 4:8], in0=den_view(k.bank[ob][:, 0:260]), scalar1=1e-30, scalar2=None,
                                                  op0=ALU.max), reads=[obn], writes=[rdBn])
            P.op('dve', lambda e: e.tensor_scalar(out=rdB[:, 8:12], in0=den_view(ocw[g][:, 1, :]), scalar1=1e-30, scalar2=None,
                                                  op0=ALU.max), reads=['ocw%d' % g], writes=[rdBn])
            P.op('dve', lambda e: e.reciprocal(out=rdB[:, 4:12], in_=rdB[:, 4:12]), reads=[rdBn], writes=[rdBn])
            P.op('dve', lambda e: e.tensor_tensor(out=coef.rearrange('p (b m) -> p b m', b=3),
                                                  in0=rdB.rearrange('p (b m) -> p b m', b=3),
                                                  in1=gsig[:, i, g * 12:(g + 1) * 12].rearrange('p (m b) -> p b m', b=3),
                                                  op=ALU.mult), reads=[rdBn, ('gsig', i)], writes=[cfn])
            for m in range(4):
                h = 4 * g + m
                ym = yc[:, h * 64:(h + 1) * 64]
                P.op('pool', lambda e, m=m, ym=ym: e.tensor_scalar(out=ym, in0=ocw[g][:, 0, m * 65:m * 65 + 64],
                                                                   scalar1=coef[:, m:m + 1], scalar2=0.0, op0=ALU.mult, op1=ALU.add),
                     reads=['ocw%d' % g, cfn], writes=[('yc', h)])
                P.op('dve', lambda e, m=m, ym=ym: e.scalar_tensor_tensor(out=ym, in0=k.bank[ob][:, m * 65:m * 65 + 64],
                                                                         scalar=coef[:, 4 + m:5 + m], in1=ym, op0=ALU.mult,
                                                                         op1=ALU.add), reads=[obn, cfn, ('yc', h)],
                     writes=[('yc', h)])
                P.op('dve', lambda e, m=m, ym=ym: e.scalar_tensor_tensor(out=ym, in0=ocw[g][:, 1, m * 65:m * 65 + 64],
                                                                         scalar=coef[:, 8 + m:9 + m], in1=ym, op0=ALU.mult,
                                                                         op1=ALU.add), reads=['ocw%d' % g, cfn, ('yc', h)],
                     writes=[('yc', h)])

        def out_proj(i):
            xt = k.xt[i % 3]
            xr = 'xt%d' % (i % 3)
            ydt = ydts[i % 2]
            ydn = 'ydt%d' % (i % 2)
            P.op('act', lambda e: e.copy(out=ycb, in_=yc), reads=[('yc', h) for h in range(8)], writes=['ycb'])
            for c in range(4):
                P.op('pe', lambda e, c=c: e.transpose(out=tp[:, c * 128:(c + 1) * 128], in_=ycb[:, c * 128:(c + 1) * 128],
                                                      identity=k.ident), reads=['ycb', 'ident'], writes=['bank0'])
            for c in range(4):
                P.op('pe', lambda e, c=c: e.transpose(out=tp[:, (4 + c) * 128:(5 + c) * 128], in_=ydt[:, c * 128:(c + 1) * 128],
                                                      identity=k.ident), reads=[ydn, 'ident'], writes=['bank0'])
            P.op('act', lambda e: e.copy(out=yT, in_=tp.rearrange('p (c t) -> p c t', c=8)), reads=['bank0'], writes=['yT'])
            pm = [k.bank[3], k.bank[4]]
            for cb in range(2):
                for kc in range(8):
                    P.op('pe', lambda e, kc=kc, cb=cb: e.matmul(pm[cb], lhsT=yT[:, kc, :], rhs=w_out_sb[:, kc, cb * 512:(cb + 1) * 512],
                                                               start=(kc == 0), stop=(kc == 7)), reads=['yT', 'w_out'],
                         writes=['bank%d' % (3 + cb)])
            post_norm_residual(k, pm, ['bank3', 'bank4'], 'mix_post', xt, xr)
            P.dma('sp', xdst[i * 128:(i + 1) * 128, :], xt, reads=[xr], writes=[('x', i)], stream='xo')

        units = [(i, g) for i in range(NT) for g in range(2)]
        o2_loads(0)
        stage_a(*units[0])
        for n, (i, g) in enumerate(units):
            if n + 1 < len(units):
                ni, ng = units[n + 1]
                if ng == 0:
                    o2_loads(ni)
                stage_a(ni, ng)
            stage_b(i, g)
            if g == 1:
                out_proj(i)

def make_consts():
    c = {}
    Dm = np.zeros((3, 4, 128, 128), np.float32)
    for g, w in enumerate(POOL_WINDOWS):
        for t in range(128):
            lo = max(t + 1 - w, 0)
            for s in range(lo, t + 1):
                Dm[0, g, s, t] += 1.0 / (t + 1 - lo)
            Dm[0, g, t, t] -= 1.0
            for s in range(t + 1 - w, t + 1):
                if s >= 0:
                    Dm[1, g, s, t] += 1.0 / w
                else:
                    Dm[2, g, s + 128, t] += 1.0 / w
            Dm[1, g, t, t] -= 1.0
    c['c_D'] = Dm.astype(ml_dtypes.bfloat16)
    bf = ml_dtypes.bfloat16
    invf = np.concatenate([1.0 / (500000.0 ** (np.arange(0, 16, 2, dtype=np.float32) / 16)),
                           1.0 / (10000.0 ** (np.arange(0, 128, 2, dtype=np.float32) / 128))]).astype(np.float32)
    c['c_invf'] = invf.reshape(1, 72)
    lg = np.log1p(-np.exp2(-5.0 - np.arange(4, dtype=np.float64)))
    idx = np.arange(128, dtype=np.float64)
    rel = idx[None, :] - idx[:, None]
    dec = np.where((rel >= 0)[:, None, :], np.exp(np.maximum(rel, 0)[:, None, :] * lg[None, :, None]), 0.0)
    c['c_decT'] = dec.reshape(128, 512).astype(np.float32)
    xi = np.exp((idx + 1.0)[None, :] * lg[:, None])
    c['c_xi'] = np.broadcast_to(xi.reshape(1, 512), (128, 512)).astype(np.float32).copy()
    c['c_zeta'] = np.exp((127 - idx)[:, None] * lg[None, :]).astype(np.float32)
    c['c_gch'] = np.broadcast_to(np.exp(128 * lg)[None, :], (128, 4)).astype(np.float32).copy()
    p = np.arange(128)
    c['c_tri'] = (p[:, None] <= p[None, :]).astype(np.float32)
    c['c_cm'] = (16.0 * p[:, None] - p[None, :]).astype(np.float32)
    cq = (p >= 64).astype(np.int64)[:, None]
    jj = np.arange(128)[None, :]
    c['c_keep'] = (jj <= 60 + cq).astype(np.float32)
    add = np.zeros((128, 128), np.float32)
    add[(jj == 61 + cq) | (jj == 62 + cq)] = 1.0e4
    add[jj > 62 + cq] = -1.0
    c['c_add'] = add
    n = np.arange(256)
    cs = n * 16
    ss = np.arange(64) * 64
    cts = ((cs[:, None] < ss[None, :] + 64) & (cs[:, None] + 32 > ss[None, :])).astype(np.float32)
    cts[255] = 0
    c['c_cts'] = cts.reshape(2, 128, 64).astype(bf)
    c['c_E'] = (np.arange(4096)[None, :] // 64 == np.arange(64)[:, None]).astype(bf)
    return c


INPUT_NAMES = ["x", "positions", "ln_mix_pre", "ln_mix_post", "ln_ffn_pre", "ln_ffn_post", "ffn_w_gate", "ffn_w_up",
               "ffn_w_down", "ev_w_in", "ev_pool_w", "ev_pool_scale", "ev_sgu_ln_g", "ev_sgu_ln_b", "ev_sgu_w",
               "ev_sgu_b", "ev_w_out", "od_w_in", "od_cmp_k_pos", "od_cmp_k_w1", "od_cmp_k_w2", "od_cmp_v_pos",
               "od_cmp_v_w1", "od_cmp_v_w2", "od_ret_gn_g", "od_w_out"]


def build(shapes, consts, layers=(0, 1, 2, 3), phases=('mix', 'ffn')):
    from contextlib import ExitStack
    nc = bass.Bass("TRN2", target_bir_lowering=False)
    ins = {}
    for n in INPUT_NAMES:
        shp = list(shapes[n])
        if n == 'x':
            shp = [T, D]
        if n == 'positions':
            shp = [1, T]
        ins[n] = nc.dram_tensor(n, shp, I32 if n == 'positions' else F32, kind="ExternalInput").ap()
    for n, v in consts.items():
        ins[n] = nc.dram_tensor(n, list(v.shape), BF16 if v.dtype == ml_dtypes.bfloat16 else F32, kind="ExternalInput").ap()
    y = nc.dram_tensor("y", [T, D], F32, kind="ExternalOutput").ap()
    k = K(nc, layers)
    setup_common(k, ins)
    k.sb2 = k.sb('ssa', [128, 2], F32)
    P = k.P
    xsrc = ins['x']
    for li, l in enumerate(layers):
        load_gains(k, l)
        if li + 1 < len(layers):
            cast_layer(k, layers[li + 1])
        if 'mix' in phases:
            with ExitStack() as es:
                if l % 2 == 0:
                    even_phase(k, l, xsrc, y, es)
                else:
                    odd_phase(k, l, xsrc, y, es)
                P.barrier()
            xsrc = y
        if 'ffn' in phases:
            with ExitStack() as es:
                def sb(name, shape, dt):
                    return es.enter_context(nc.sbuf_tensor('ffs%d_' % l + name, list(shape), dt)).ap()
                k.wd_sb = sb('wd', [128, NFC, 1024], BF16)
                k.xt8 = [sb('xt8_%d' % i, [128, D], F32) for i in range(8)]
                k.wgu = [sb('wgu%d' % i, [128, 2, 8, 512], BF16) for i in range(2)]
                k.hT2 = [sb('hT%d' % i, [128, 8, 512], BF16) for i in range(2)]
                k.actT = sb('actT', [128, NFC, 512], BF16)
                k.sg = [sb('sg%d' % i, [128, 512], F32) for i in range(2)]
                ffn_phase(k, l, xsrc, y)
                P.barrier()
            xsrc = y
    P.finish()
    return nc


_CACHE = {}


def kernel(**inputs):
    consts = make_consts()
    shapes = {n: inputs[n].shape for n in INPUT_NAMES}
    if 'nc' not in _CACHE:
        _CACHE['nc'] = build(shapes, consts)
    nc = _CACHE['nc']
    in_maps = []
    for c in range(4):
        b = c % 4
        m = {n: np.ascontiguousarray(inputs[n]) for n in INPUT_NAMES if n not in ('x', 'positions')}
        m['x'] = np.ascontiguousarray(inputs['x'][b])
        m['positions'] = np.ascontiguousarray(inputs['positions'][b:b + 1]).astype(np.int32)
        m.update(consts)
        in_maps.append(m)
    res = run_bass_kernel_spmd(nc, in_maps, core_ids=list(range(4)))
    out = np.stack([res.results[b]["y"] for b in range(4)], axis=0)
    return out.astype(np.float32)
```

```python
import numpy as np
import ml_dtypes
import concourse.bass as bass
import concourse.mybir as mybir
from concourse.bass_utils import run_bass_kernel_spmd

F32 = mybir.dt.float32
BF16 = mybir.dt.bfloat16
I32 = mybir.dt.int32
AF = mybir.ActivationFunctionType
ALU = mybir.AluOpType
AX = mybir.AxisListType

import os
SAME_ENG_SYNC = os.environ.get("SES", "1") == "1"


class Prog:
    def __init__(self, nc):
        self.nc = nc
        self.E = {'pe': nc.tensor, 'dve': nc.vector, 'act': nc.scalar, 'pool': nc.gpsimd, 'sp': nc.sync}
        self.semh = {k: nc.alloc_semaphore('s_' + k) for k in ['pe', 'dve', 'act', 'pool']}
        self.cnt = {k: 0 for k in self.semh}
        self.seen = {e: {} for e in self.E}
        self.lastw = {}
        self.readers = {}
        self.nwait = 0
        self.dslot = 0
        self.NSLOT = 32
        self.nins = 0

    def _deps(self, reads, writes):
        deps = {}
        for r in reads:
            w = self.lastw.get(r)
            if w and deps.get(w[0], 0) < w[1]:
                deps[w[0]] = w[1]
            if isinstance(r, str) and r.startswith('bank'):
                for k_, v in self.readers.get(r, {}).items():
                    if deps.get(k_, 0) < v:
                        deps[k_] = v
        for w_ in writes:
            w = self.lastw.get(w_)
            if w and deps.get(w[0], 0) < w[1]:
                deps[w[0]] = w[1]
            for k, v in self.readers.get(w_, {}).items():
                if deps.get(k, 0) < v:
                    deps[k] = v
        return deps

    def _wait(self, eng, deps):
        e = self.E[eng]
        seen = self.seen[eng]
        for k, v in deps.items():
            if k == eng and (eng == 'pe' or not SAME_ENG_SYNC):
                continue
            if seen.get(k, 0) >= v:
                continue
            e.wait_ge(self.semh[k], v)
            seen[k] = v
            self.nwait += 1

    def _record(self, tag, reads, writes):
        for w in writes:
            self.lastw[w] = tag
            self.readers[w] = {}
        for r in reads:
            d = self.readers.setdefault(r, {})
            if d.get(tag[0], 0) < tag[1]:
                d[tag[0]] = tag[1]

    def op(self, eng, fn, reads=(), writes=()):
        self._wait(eng, self._deps(reads, writes))
        ins = fn(self.E[eng])
        self.cnt[eng] += 1
        ins.then_inc(self.semh[eng], 1)
        self.nins += 1
        self._record((eng, self.cnt[eng]), reads, writes)

    def dma(self, q, out, in_, reads=(), writes=(), stream='d0', **kw):
        slot = self.dslot % self.NSLOT
        self.dslot += 1
        key = 'd:%d' % slot
        if key not in self.semh:
            self.semh[key] = self.nc.alloc_semaphore('sd_%d' % slot)
            self.cnt[key] = 0
        e = self.E[q]
        if self.cnt[key] > 0 and self.seen[q].get(key, 0) < self.cnt[key]:
            e.wait_ge(self.semh[key], self.cnt[key])
            self.seen[q][key] = self.cnt[key]
        self._wait(q, self._deps(reads, writes))
        e.dma_start(out=out, in_=in_, **kw).then_inc(self.semh[key], 16)
        self.cnt[key] += 16
        self.nins += 1
        self._record((key, self.cnt[key]), reads, writes)

    def barrier(self):
        for eng in self.E:
            deps = {k: v for k, v in self.cnt.items() if v > 0}
            e = self.E[eng]
            for k, v in deps.items():
                if k == eng and eng == 'pe':
                    continue
                if self.seen[eng].get(k, 0) >= v:
                    continue
                e.wait_ge(self.semh[k], v)
                self.seen[eng][k] = v
        self.lastw = {}
        self.readers = {}

    def finish(self, eng='sp'):
        deps = {k: v for k, v in self.cnt.items() if v > 0}
        e = self.E[eng]
        for k, v in deps.items():
            e.wait_ge(self.semh[k], v)


T = 4096
D = 1024
NT = T // 128
FH = 2816
NFC = FH // 128
DEPTH = 4
POOL_WINDOWS = (2, 4, 8, 16)
EPS = 1e-6


def _flat2(ap, c=1024):
    n = len(ap.shape)
    names = ' '.join('d%d' % i for i in range(n))
    f = ap.rearrange('%s -> (%s)' % (names, names)) if n > 1 else ap
    return f.rearrange('(r c) -> r c', c=c)


class K:
    def __init__(self, nc, layers=(0, 1, 2, 3), Tn=T):
        self.nc = nc
        self.P = Prog(nc)
        self.layers = layers
        self.uid = 0

    def sb(self, name, shape, dt):
        return self.nc.alloc_sbuf_tensor(name, list(shape), dt).ap()

    def dram(self, name, shape, dt, kind="Internal"):
        return self.nc.dram_tensor(name, list(shape), dt, kind=kind).ap()


def cast_copy(k, dst, src, res):
    d2 = _flat2(dst)
    s2 = _flat2(src)
    rows = d2.shape[0]
    r0 = 0
    while r0 < rows:
        r1 = min(rows, r0 + 2048)
        k.P.dma('pool', d2[r0:r1, :], s2[r0:r1, :], writes=[res], stream='cast')
        r0 = r1


def cast_layer(k, l):
    ins = k.ins
    order = []
    if l % 2 == 0:
        order += [('ev_w_in', l // 2), ('ev_pool_w', l // 2), ('ev_w_out', l // 2)]
    else:
        order += [('od_w_in', l // 2), ('od_cmp_k_w1', l // 2), ('od_cmp_k_w2', l // 2), ('od_cmp_v_w1', l // 2),
                  ('od_cmp_v_w2', l // 2), ('od_w_out', l // 2)]
    order += [('ffn_w_gate', l), ('ffn_w_up', l), ('ffn_w_down', l)]
    for name, idx in order:
        src = ins[name][idx]
        dst = k.dram('wb_%s_%d' % (name, idx), src.shape, BF16)
        k.wb[(name, idx)] = dst
        cast_copy(k, dst, src, ('wb', name, idx))


def rstd_from_ssq(k, ssq, rstd, tmp, n, eps, tag):
    P = k.P
    P.op('dve', lambda e: e.tensor_scalar(out=tmp, in0=ssq, scalar1=1.0 / n, scalar2=eps, op0=ALU.mult, op1=ALU.add),
         reads=[tag + 'ssq'], writes=[tag + 'tmp'])
    P.op('pool', lambda e: e.tensor_tensor(out=rstd, in0=tmp, in1=k.mhalf[:, 0:1], op=ALU.pow), reads=[tag + 'tmp', 'mhalf'],
         writes=[tag + 'rstd'])


def setup_common(k, ins):
    nc, P = k.nc, k.P
    k.ins = ins
    k.bank = [nc.alloc_psum_tensor('bank%d' % i, [128, 512], F32).ap() for i in range(8)]
    k.bankb = [b.bitcast(BF16) for b in k.bank]
    k.io_f = k.sb('io_f', [128, 128], F32)
    k.iop = k.sb('iop', [128, 1], F32)
    k.ident = k.sb('ident', [128, 128], BF16)
    k.ones_row = k.sb('ones_row', [1, 128], BF16)
    P.op('pool', lambda e: e.iota(k.io_f, pattern=[[1, 128]], base=0, channel_multiplier=0,
                                  allow_small_or_imprecise_dtypes=True), writes=['io_f'])
    P.op('pool', lambda e: e.iota(k.iop, pattern=[[1, 1]], base=0, channel_multiplier=1,
                                  allow_small_or_imprecise_dtypes=True), writes=['iop'])
    P.op('dve', lambda e: e.tensor_scalar(out=k.ident, in0=k.io_f, scalar1=k.iop[:, 0:1], scalar2=None,
                                          op0=ALU.is_equal), reads=['io_f', 'iop'], writes=['ident'])
    P.op('dve', lambda e: e.memset(k.ones_row, 1.0), writes=['ones_row'])
    k.mhalf = k.sb('mhalf', [128, 4], F32)
    P.op('pool', lambda e: e.memset(k.mhalf, -0.5), writes=['mhalf'])
    k.wb = {}
    cast_layer(k, k.layers[0])
    k.xt = [k.sb('xt%d' % s, [128, D], F32) for s in range(3)]
    k.hbs = [k.sb('hb%d' % i, [128, D], BF16) for i in range(2)]
    k.junk = k.sb('junk', [128, D], BF16)
    k.tmpf = k.sb('tmpf', [128, D], F32)
    k.G = {n: k.sb('G_' + n, [128, D], F32) for n in ['mix_pre', 'mix_post', 'ffn_pre', 'ffn_post']}
    k.st = {n: k.sb('st_' + n, [128, 1], F32) for n in ['ssq', 'tmp', 'rstd', 'ssq2', 'tmp2', 'rstd2']}


def load_gains(k, l):
    for n in ['mix_pre', 'mix_post', 'ffn_pre', 'ffn_post']:
        k.P.dma('sp', k.G[n], k.ins['ln_' + n][l:l + 1, :].broadcast_to([128, D]), writes=['G_' + n], stream='small')


def norm_a(k, xt_ap, xres, gname, hbi):
    P = k.P
    st = k.st
    hb = k.hbs[hbi]
    P.op('act', lambda e: e.activation(out=k.junk, in_=xt_ap, func=AF.Square, accum_out=st['ssq']),
         reads=[xres], writes=['junk', 'ssq'])
    rstd_from_ssq(k, st['ssq'], st['rstd'], st['tmp'], D, EPS, '')
    P.op('dve', lambda e: e.scalar_tensor_tensor(out=hb, in0=xt_ap, scalar=st['rstd'][:, 0:1], in1=k.G[gname],
                                                 op0=ALU.mult, op1=ALU.mult),
         reads=[xres, 'rstd', 'G_' + gname], writes=['hb%d' % hbi])


def trans_t(k, hbi, hT_dst, hTres):
    P = k.P
    hb = k.hbs[hbi]
    tp = k.bankb[0]
    for c in range(8):
        P.op('pe', lambda e, c=c: e.transpose(out=tp[:, c * 128:(c + 1) * 128], in_=hb[:, c * 128:(c + 1) * 128],
                                              identity=k.ident), reads=['hb%d' % hbi, 'ident'], writes=['bank0'])
    P.op('act', lambda e: e.copy(out=hT_dst, in_=tp.rearrange('p (c t) -> p c t', c=8)), reads=['bank0'],
         writes=[hTres])


def post_norm_residual(k, pm, pmres, gname, xt_ap, xres):
    P = k.P
    st = k.st
    ssa = k.sb2
    for cb in range(2):
        P.op('act', lambda e, cb=cb: e.activation(out=k.junk[:, cb * 512:(cb + 1) * 512], in_=pm[cb], func=AF.Square,
                                                  accum_out=ssa[:, cb:cb + 1]),
             reads=[pmres[cb]], writes=['junk', 'ssa'])
    P.op('dve', lambda e: e.tensor_tensor(out=st['ssq2'], in0=ssa[:, 0:1], in1=ssa[:, 1:2], op=ALU.add),
         reads=['ssa'], writes=['2ssq'])
    rstd_from_ssq(k, st['ssq2'], st['rstd2'], st['tmp2'], D, EPS, '2')
    for cb in range(2):
        sl = slice(cb * 512, (cb + 1) * 512)
        P.op('dve', lambda e, cb=cb, sl=sl: e.scalar_tensor_tensor(out=k.tmpf[:, sl], in0=pm[cb], scalar=st['rstd2'][:, 0:1],
                                                                   in1=k.G[gname][:, sl], op0=ALU.mult, op1=ALU.mult),
             reads=[pmres[cb], '2rstd', 'G_' + gname], writes=['tmpf'])
    P.op('pool', lambda e: e.tensor_tensor(out=xt_ap, in0=xt_ap, in1=k.tmpf, op=ALU.add), reads=['tmpf', xres],
         writes=[xres])


def ffn_phase(k, l, xsrc, xdst):
    nc, P = k.nc, k.P
    wg, wu, wd = k.wb[('ffn_w_gate', l)], k.wb[('ffn_w_up', l)], k.wb[('ffn_w_down', l)]
    P.dma('sp', k.wd_sb, wd.rearrange('(c p) n -> p c n', p=128), reads=[('wb', 'ffn_w_down', l)], writes=['wd_sb'],
          stream='w')
    groups = [(0, 4), (4, 4), (8, 4), (12, 4), (16, 4), (20, 2)]
    wgv = wg.rearrange('(c p) n -> p c n', p=128)
    wuv = wu.rearrange('(c p) n -> p c n', p=128)
    gi = 0
    NB = T // 512

    def xtile(tb, s):
        j = (tb % 2) * 4 + s
        return k.xt8[j], 'xt8_%d' % j

    def front_a(tb, s):
        t0 = tb * 512 + s * 128
        xt, xr = xtile(tb, s)
        P.dma('sp', xt, xsrc[t0:t0 + 128, :], reads=[('x', t0 // 128)], writes=[xr], stream='x')
        norm_a(k, xt, xr, 'ffn_pre', s % 2)

    def front_t(tb, s):
        trans_t(k, s % 2, k.hT2[tb % 2][:, :, s * 128:(s + 1) * 128], ('hT', tb % 2))

    for s in range(4):
        front_a(0, s)
        front_t(0, s)
    for tb in range(NB):
        hT = k.hT2[tb % 2]
        hTr = ('hT', tb % 2)
        for gidx, (j0, nj) in enumerate(groups):
            pre = gidx < 4 and tb + 1 < NB
            if pre:
                front_a(tb + 1, gidx)
            buf = gi % 2
            gi += 1
            wt = k.wgu[buf]
            P.dma('sp', wt[:, 0, :, 0:nj * 128], wgv[:, :, j0 * 128:(j0 + nj) * 128], reads=[('wb', 'ffn_w_gate', l)],
                  writes=['wgu%d' % buf], stream='w')
            P.dma('sp', wt[:, 1, :, 0:nj * 128], wuv[:, :, j0 * 128:(j0 + nj) * 128], reads=[('wb', 'ffn_w_up', l)],
                  writes=['wgu%d' % buf], stream='w')
            for jj in range(nj):
                j = j0 + jj
                pb = 1 + 2 * (j % 2)
                pg, pu = k.bank[pb], k.bank[pb + 1]
                for kc in range(8):
                    P.op('pe', lambda e, kc=kc, jj=jj: e.matmul(pg, lhsT=wt[:, 0, kc, jj * 128:(jj + 1) * 128], rhs=hT[:, kc, :],
                                                               start=(kc == 0), stop=(kc == 7)),
                         reads=['wgu%d' % buf, hTr], writes=['bank%d' % pb])
                for kc in range(8):
                    P.op('pe', lambda e, kc=kc, jj=jj: e.matmul(pu, lhsT=wt[:, 1, kc, jj * 128:(jj + 1) * 128], rhs=hT[:, kc, :],
                                                               start=(kc == 0), stop=(kc == 7)),
                         reads=['wgu%d' % buf, hTr], writes=['bank%d' % (pb + 1)])
                sg = k.sg[j % 2]
                P.op('act', lambda e: e.activation(out=sg, in_=pg, func=AF.Silu), reads=['bank%d' % pb],
                     writes=['sg%d' % (j % 2)])
                P.op('dve', lambda e, j=j: e.tensor_tensor(out=k.actT[:, j, :], in0=pu, in1=sg, op=ALU.mult),
                     reads=['bank%d' % (pb + 1), 'sg%d' % (j % 2)], writes=[('actT', j)])
            if pre:
                front_t(tb + 1, gidx)
        for s in range(4):
            t0 = tb * 512 + s * 128
            pm = [k.bank[5], k.bank[6]]
            for cb in range(2):
                for kk in range(NFC):
                    P.op('pe', lambda e, kk=kk, cb=cb: e.matmul(pm[cb], lhsT=k.actT[:, kk, s * 128:(s + 1) * 128],
                                                               rhs=k.wd_sb[:, kk, cb * 512:(cb + 1) * 512],
                                                               start=(kk == 0), stop=(kk == NFC - 1)),
                         reads=[('actT', kk), 'wd_sb'], writes=['bank%d' % (5 + cb)])
            xt_, xr_ = xtile(tb, s)
            post_norm_residual(k, pm, ['bank5', 'bank6'], 'ffn_post', xt_, xr_)
            P.dma('sp', xdst[t0:t0 + 128, :], xt_, reads=[xr_], writes=[('x', t0 // 128)], stream='xo')


def even_phase(k, l, xsrc, xdst, es):
    nc, P = k.nc, k.P
    e_ = l // 2
    ins = k.ins

    def sb(name, shape, dt):
        return es.enter_context(nc.sbuf_tensor('evs%d_' % l + name, list(shape), dt)).ap()

    w_in_sb = sb('w_in', [128, 8, 1536], BF16)
    w_out_sb = sb('w_out', [128, 8, 1024], BF16)
    poolw_sb = sb('poolw', [128, 4, 128], BF16)
    Dm_sb = sb('Dm', [128, 3, 4, 128], BF16)
    WmT = sb('WmT', [128, 4, 128], BF16)
    wsf = sb('wsf', [128, 4, 128], F32)
    wsb = sb('wsb', [128, 4, 128], BF16)
    tril = sb('tril', [128, 128], F32)
    Bt = sb('Bt', [128, 512], F32)
    lng = sb('lng', [128, 512], F32)
    lnb = sb('lnb', [128, 512], F32)
    psc = sb('psc', [128, 4], F32)
    a_sb = [sb('a%d' % i, [128, 512], BF16) for i in range(2)]
    uT_sb = sb('uT', [128, 4, 128], BF16)
    vg = sb('vg', [128, 512], F32)
    vn = sb('vn', [128, 512], F32)
    vln = sb('vln', [128, 512], BF16)
    diffT = sb('diffT', [128, 4, 128], BF16)
    yaT = sb('yaT', [128, 4, 128], BF16)
    ybT = sb('ybT', [128, 4, 128], BF16)
    mxb = sb('mxb', [128, 512], F32)
    hT1s = [sb('hT1_%d' % i, [128, 8, 128], BF16) for i in range(2)]
    bst = sb('bst', [128, 6], F32)
    mv = sb('mv', [128, 2], F32)
    lrs = sb('lrs', [128, 1], F32)

    P.dma('sp', w_in_sb, k.wb[('ev_w_in', e_)].rearrange('(c p) n -> p c n', p=128), reads=[('wb', 'ev_w_in', e_)],
          writes=['w_in'], stream='w')
    P.dma('sp', w_out_sb, k.wb[('ev_w_out', e_)].rearrange('(c p) n -> p c n', p=128), reads=[('wb', 'ev_w_out', e_)],
          writes=['w_out'], stream='w')
    P.dma('sp', poolw_sb, k.wb[('ev_pool_w', e_)].rearrange('g c d -> c g d'), reads=[('wb', 'ev_pool_w', e_)],
          writes=['poolw'], stream='w')
    P.dma('sp', Dm_sb, ins['c_D'].rearrange('a g s t -> s a g t'), writes=['Dm'], stream='small')
    P.dma('sp', wsf, ins['ev_sgu_w'][e_].rearrange('g t s -> t g s'), writes=['wsf'], stream='small')
    P.dma('sp', Bt, ins['ev_sgu_b'][e_:e_ + 1].rearrange('o g t -> o (g t)').broadcast_to([128, 512]), writes=['Bt'],
          stream='small')
    P.dma('sp', lng, ins['ev_sgu_ln_g'][e_:e_ + 1, :].broadcast_to([128, 512]), writes=['lng'], stream='small')
    P.dma('sp', lnb, ins['ev_sgu_ln_b'][e_:e_ + 1, :].broadcast_to([128, 512]), writes=['lnb'], stream='small')
    with nc.allow_non_contiguous_dma(reason='tiny per-channel scale'):
        P.dma('sp', psc, ins['ev_pool_scale'][e_].rearrange('(g p) -> p g', p=128), writes=['psc'], stream='small')
    P.op('dve', lambda e: e.tensor_scalar(out=tril, in0=k.io_f, scalar1=k.iop[:, 0:1], scalar2=None, op0=ALU.is_le),
         reads=['io_f', 'iop'], writes=['tril'])
    P.op('dve', lambda e: e.tensor_tensor(out=wsb, in0=wsf, in1=tril.unsqueeze(1).broadcast_to([128, 4, 128]), op=ALU.mult),
         reads=['wsf', 'tril'], writes=['wsb'])
    tp = k.bankb[0]
    for g in range(4):
        P.op('pe', lambda e, g=g: e.transpose(out=tp[:, g * 128:(g + 1) * 128], in_=wsb[:, g, :], identity=k.ident),
             reads=['wsb', 'ident'], writes=['bank0'])
    P.op('act', lambda e: e.copy(out=WmT, in_=tp[:, 0:512].rearrange('p (g t) -> p g t', g=4)), reads=['bank0'],
         writes=['WmT'])

    def front_a(i):
        P.dma('sp', k.xt[i % 3], xsrc[i * 128:(i + 1) * 128, :], reads=[('x', i)], writes=['xt%d' % (i % 3)], stream='x')
        norm_a(k, k.xt[i % 3], 'xt%d' % (i % 3), 'mix_pre', i % 2)

    def front_t(i):
        trans_t(k, i % 2, hT1s[i % 2], 'hT1_%d' % (i % 2))

    front_a(0)
    front_t(0)
    for i in range(NT):
        xt = k.xt[i % 3]
        xr = 'xt%d' % (i % 3)
        hT1 = hT1s[i % 2]
        hT1n = 'hT1_%d' % (i % 2)
        a_cur, a_prev = a_sb[i % 2], a_sb[(i + 1) % 2]
        ar, apr = 'a%d' % (i % 2), 'a%d' % ((i + 1) % 2)
        if i + 1 < NT and i >= 1:
            pass
        pa, pu, pv = k.bank[1], k.bank[2], k.bank[3]
        for kc in range(8):
            P.op('pe', lambda e, kc=kc: e.matmul(pa, lhsT=hT1[:, kc, :], rhs=w_in_sb[:, kc, 0:512], start=(kc == 0),
                                                stop=(kc == 7)), reads=[hT1n, 'w_in'], writes=['bank1'])
        P.op('act', lambda e: e.copy(out=a_cur, in_=pa), reads=['bank1'], writes=[ar])
        for kc in range(8):
            P.op('pe', lambda e, kc=kc: e.matmul(pv, lhsT=hT1[:, kc, :], rhs=w_in_sb[:, kc, 1024:1536], start=(kc == 0),
                                                stop=(kc == 7)), reads=[hT1n, 'w_in'], writes=['bank3'])
        P.op('act', lambda e: e.activation(out=vg, in_=pv, func=AF.Gelu_apprx_tanh), reads=['bank3'], writes=['vg'])
        for c in range(4):
            for kc in range(8):
                P.op('pe', lambda e, kc=kc, c=c: e.matmul(pu[:, c * 128:(c + 1) * 128],
                                                         lhsT=w_in_sb[:, kc, 512 + c * 128:512 + (c + 1) * 128],
                                                         rhs=hT1[:, kc, :], start=(kc == 0), stop=(kc == 7)),
                     reads=[hT1n, 'w_in'], writes=['bank2'])
        P.op('act', lambda e: e.activation(out=uT_sb, in_=pu.rearrange('p (c t) -> p c t', c=4), func=AF.Gelu_apprx_tanh),
             reads=['bank2'], writes=['uT'])
        if i + 1 < NT:
            front_a(i + 1)
        P.op('dve', lambda e: e.bn_stats(out=bst, in_=vg), reads=['vg'], writes=['bst'])
        P.op('dve', lambda e: e.bn_aggr(out=mv, in_=bst), reads=['bst'], writes=['mv'])
        P.op('dve', lambda e: e.tensor_scalar(out=lrs, in0=mv[:, 1:2], scalar1=1e-5, scalar2=None, op0=ALU.add),
             reads=['mv'], writes=['lrs'])
        P.op('pool', lambda e: e.tensor_tensor(out=lrs, in0=lrs, in1=k.mhalf[:, 0:1], op=ALU.pow), reads=['lrs', 'mhalf'], writes=['lrs'])
        P.op('dve', lambda e: e.tensor_scalar(out=vn, in0=vg, scalar1=mv[:, 0:1], scalar2=lrs[:, 0:1], op0=ALU.subtract,
                                              op1=ALU.mult), reads=['vg', 'mv', 'lrs'], writes=['vn'])
        P.op('pool', lambda e: e.tensor_tensor(out=vn, in0=vn, in1=lng, op=ALU.mult), reads=['vn', 'lng'], writes=['vn'])
        P.op('pool', lambda e: e.tensor_tensor(out=vln, in0=vn, in1=lnb, op=ALU.add), reads=['vn', 'lnb'], writes=['vln'])
        pd_ = k.bank[4]
        for g in range(4):
            first = True
            sl = slice(g * 128, (g + 1) * 128)
            P.op('pe', lambda e, g=g, sl=sl: e.matmul(pd_[:, sl], lhsT=a_cur[:, sl], rhs=Dm_sb[:, 0 if i == 0 else 1, g, :],
                                                     start=True, stop=(i == 0)), reads=[ar, 'Dm'], writes=['bank4'])
            if i > 0:
                P.op('pe', lambda e, g=g, sl=sl: e.matmul(pd_[:, sl], lhsT=a_prev[:, sl], rhs=Dm_sb[:, 2, g, :],
                                                         start=False, stop=True), reads=[apr, 'Dm'], writes=['bank4'])
        P.op('dve', lambda e: e.tensor_copy(out=diffT, in_=pd_.rearrange('p (g t) -> p g t', g=4)), reads=['bank4'],
             writes=['diffT'])
        pya = k.bank[5]
        for g in range(4):
            P.op('pe', lambda e, g=g: e.matmul(pya[:, g * 128:(g + 1) * 128], lhsT=poolw_sb[:, g, :], rhs=diffT[:, g, :],
                                              start=True, stop=True), reads=['poolw', 'diffT'], writes=['bank5'])
        P.op('dve', lambda e: e.tensor_tensor(out=yaT, in0=pya.rearrange('p (g t) -> p g t', g=4),
                                              in1=psc.unsqueeze(2).broadcast_to([128, 4, 128]), op=ALU.mult),
             reads=['bank5', 'psc'], writes=['yaT'])
        pmx = k.bank[4]
        for g in range(4):
            P.op('pe', lambda e, g=g: e.matmul(pmx[:, g * 128:(g + 1) * 128], lhsT=vln[:, g * 128:(g + 1) * 128],
                                              rhs=WmT[:, g, :], start=True, stop=True), reads=['vln', 'WmT'],
                 writes=['bank4'])
        P.op('dve', lambda e: e.tensor_tensor(out=mxb, in0=pmx, in1=Bt, op=ALU.add), reads=['bank4', 'Bt'],
             writes=['mxb'])
        P.op('pool', lambda e: e.tensor_tensor(out=ybT, in0=mxb.rearrange('p (g t) -> p g t', g=4), in1=uT_sb, op=ALU.mult),
             reads=['mxb', 'uT'], writes=['ybT'])
        if i + 1 < NT:
            front_t(i + 1)
        pm = [k.bank[6], k.bank[7]]
        for cb in range(2):
            for kc in range(8):
                lh = yaT[:, kc, :] if kc < 4 else ybT[:, kc - 4, :]
                P.op('pe', lambda e, kc=kc, cb=cb, lh=lh: e.matmul(pm[cb], lhsT=lh, rhs=w_out_sb[:, kc, cb * 512:(cb + 1) * 512],
                                                                  start=(kc == 0), stop=(kc == 7)),
                     reads=['yaT', 'ybT', 'w_out'], writes=['bank%d' % (6 + cb)])
        post_norm_residual(k, pm, ['bank6', 'bank7'], 'mix_post', xt, xr)
        P.dma('sp', xdst[i * 128:(i + 1) * 128, :], xt, reads=[xr], writes=[('x', i)], stream='xo')


def odd_phase(k, l, xsrc, xdst, es):
    from contextlib import ExitStack
    nc, P = k.nc, k.P
    o_ = l // 2
    ins = k.ins
    PI = 3.14159265358979

    def sbs(stack, name, shape, dt):
        return stack.enter_context(nc.sbuf_tensor('od%d_' % l + name, list(shape), dt)).ap()

    def sb(name, shape, dt):
        return sbs(es, name, shape, dt)

    qd = k.dram('qd%d' % l, [T, 512], BF16)
    ydd = k.dram('ydd%d' % l, [T, 512], BF16)
    KE = sb('KE', [128, 2, T], BF16)
    kwT = sb('kwT', [64, 2, T], BF16)
    kcvd = k.dram('kcvd%d' % l, [4, 64, T], BF16)
    ropeS = k.dram('ropeS%d' % l, [128, NT, 72], F32)
    ropeC = k.dram('ropeC%d' % l, [128, NT, 72], F32)
    vs_aug = sb('vs_aug', [128, NT, 2, 65], BF16)
    vw_aug = sb('vw_aug', [128, NT, 2, 65], BF16)
    gsig = sb('gsig', [128, NT, 24], F32)
    kcmpT = sb('kcmpT', [64, 2, 256], BF16)
    vcmp_aug = sb('vcmp', [128, 2, 2, 65], BF16)
    tri = sb('tri', [128, 128], F32)
    ntri = sb('ntri', [128, 128], F32)
    cm = sb('cm', [128, 128], F32)
    keep = sb('keep', [128, 128], F32)
    addc = sb('addc', [128, 128], F32)
    cts = sb('cts', [128, 2, 64], BF16)
    decT = sb('decT', [128, 512], F32)
    xi = sb('xi', [128, 512], F32)
    zeta = sb('zeta', [128, 4], F32)
    gch = sb('gch', [128, 4], F32)
    gng = sb('gng', [128, 512], F32)
    for nm, t_, src in [('tri', tri, 'c_tri'), ('cm', cm, 'c_cm'), ('keep', keep, 'c_keep'), ('addc', addc, 'c_add'),
                        ('decT', decT, 'c_decT'), ('xi', xi, 'c_xi'), ('zeta', zeta, 'c_zeta'), ('gch', gch, 'c_gch')]:
        P.dma('sp', t_, ins[src], writes=[nm], stream='small')
    P.dma('sp', cts, ins['c_cts'].rearrange('c n j -> n c j'), writes=['cts'], stream='small')
    P.dma('sp', KE[64:128, 0, :], ins['c_E'], writes=['KE'], stream='small')
    P.dma('sp', KE[64:128, 1, :], ins['c_E'], writes=['KE'], stream='small')
    P.dma('sp', gng, ins['od_ret_gn_g'][o_:o_ + 1, :].broadcast_to([128, 512]), writes=['gng'], stream='small')
    P.op('dve', lambda e: e.tensor_scalar(out=ntri, in0=tri, scalar1=-1.0, scalar2=1.0, op0=ALU.mult, op1=ALU.add),
         reads=['tri'], writes=['ntri'])
    P.op('pool', lambda e: e.memset(vs_aug, 1.0), writes=['vs_aug'])
    P.op('pool', lambda e: e.memset(vw_aug, 1.0), writes=['vw_aug'])
    P.op('pool', lambda e: e.memset(vcmp_aug, 1.0), writes=['vcmp'])
    with ExitStack() as ts:
        posi = sbs(ts, 'posi', [128, NT], I32)
        sinT = sbs(ts, 'sinT', [128, NT, 72], F32)
        cosT = sbs(ts, 'cosT', [128, NT, 72], F32)
        posf = sbs(ts, 'posf', [128, NT], F32)
        invf = sbs(ts, 'invf', [128, 72], F32)
        ang = sbs(ts, 'ang', [128, NT, 72], F32)
        arg = sbs(ts, 'arg', [128, NT, 72], F32)
        kf = sbs(ts, 'kf', [128, NT, 72], F32)
        ki = sbs(ts, 'ki', [128, NT, 72], I32)
        with nc.allow_non_contiguous_dma(reason='positions to token-on-partition layout'):
            P.dma('sp', posi, ins['positions'].rearrange('o (i p) -> p (o i)', p=128), writes=['posi'], stream='small')
        P.dma('sp', invf, ins['c_invf'].broadcast_to([128, 72]), writes=['invf'], stream='small')
        P.op('dve', lambda e: e.tensor_copy(out=posf, in_=posi), reads=['posi'], writes=['posf'])
        P.op('dve', lambda e: e.tensor_tensor(out=ang, in0=posf.unsqueeze(2).broadcast_to([128, NT, 72]),
                                              in1=invf.unsqueeze(1).broadcast_to([128, NT, 72]), op=ALU.mult),
             reads=['posf', 'invf'], writes=['ang'])
        for shift, dst, dn in [(0.0, sinT, 'sinT'), (PI / 2, cosT, 'cosT')]:
            P.op('dve', lambda e, shift=shift: e.tensor_scalar(out=arg, in0=ang, scalar1=shift, scalar2=None, op0=ALU.add),
                 reads=['ang'], writes=['arg'])
            P.op('dve', lambda e: e.tensor_scalar(out=kf, in0=arg, scalar1=1.0 / (2 * PI), scalar2=None, op0=ALU.mult),
                 reads=['arg'], writes=['kf'])
            P.op('dve', lambda e: e.tensor_copy(out=ki, in_=kf), reads=['kf'], writes=['ki'])
            P.op('dve', lambda e: e.tensor_copy(out=kf, in_=ki), reads=['ki'], writes=['kf'])
            P.op('dve', lambda e: e.scalar_tensor_tensor(out=arg, in0=kf, scalar=-6.28125, in1=arg, op0=ALU.mult, op1=ALU.add),
                 reads=['kf', 'arg'], writes=['arg'])
            P.op('dve', lambda e: e.scalar_tensor_tensor(out=arg, in0=kf, scalar=-(2 * PI - 6.28125), in1=arg, op0=ALU.mult,
                                                         op1=ALU.add), reads=['kf', 'arg'], writes=['arg'])
            P.op('dve', lambda e: e.tensor_scalar(out=arg, in0=arg, scalar1=3.1415925, scalar2=-3.1415925, op0=ALU.min,
                                                  op1=ALU.max), reads=['arg'], writes=['arg'])
            P.op('act', lambda e, dst=dst: e.activation(out=dst, in_=arg, func=AF.Sin), reads=['arg'], writes=[dn])
        P.dma('sp', ropeS, sinT, reads=['sinT'], writes=['ropeS'], stream='xo')
        P.dma('sp', ropeC, cosT, reads=['cosT'], writes=['ropeC'], stream='xo')
        P.barrier()

    with ExitStack() as s1:
        w_in_sb = sbs(s1, 'w_in', [128, 8, 3352], BF16)
        hT1s = [sbs(s1, 'hT1_%d' % j, [128, 8, 128], BF16) for j in range(2)]
        cur = {}
        csb = [sbs(s1, 'csb%d' % j, [128, 2, 72], F32) for j in range(2)]
        kcv_t = sbs(s1, 'kcv_t', [64, 4, 128], BF16)
        zqs = [sbs(s1, 'zq%d' % j, [128, 512], F32) for j in range(2)]
        qb = sbs(s1, 'qb', [128, 512], BF16)
        zkvs = [sbs(s1, 'zkv%d' % j, [128, 768], F32) for j in range(2)]
        kvb = sbs(s1, 'kvb', [128, 768], BF16)
        rt = [sbs(s1, 'rt%d' % j, [128, 256], F32) for j in range(4)]
        zrqs = [sbs(s1, 'zrq%d' % j, [128, 512], F32) for j in range(2)]
        zrks = [sbs(s1, 'zrk%d' % j, [128, 512], F32) for j in range(2)]
        rqb = sbs(s1, 'rqb', [128, 512], BF16)
        rkb = sbs(s1, 'rkb', [128, 512], BF16)
        rkr = sbs(s1, 'rkr', [128, 512], F32)
        rkz = sbs(s1, 'rkz', [128, 512], BF16)
        rvbs = [sbs(s1, 'rvb%d' % j, [128, 512], BF16) for j in range(2)]
        gsls = [sbs(s1, 'gsl%d' % j, [128, 512], F32) for j in range(2)]
        qkT = sbs(s1, 'qkT', [128, 8, 128], BF16)
        qxiT = sbs(s1, 'qxiT', [128, 512], BF16)
        STs = sbs(s1, 'STs', [128, 512], BF16)
        state = sbs(s1, 'state', [128, 512], F32)
        stbf = sbs(s1, 'stbf', [128, 512], BF16)
        bst4 = sbs(s1, 'bst4', [128, 4, 6], F32)
        mv4 = sbs(s1, 'mv4', [128, 4, 2], F32)
        rs4 = sbs(s1, 'rs4', [128, 4], F32)
        on = sbs(s1, 'on', [128, 512], F32)
        ydb = sbs(s1, 'ydb', [128, 512], BF16)
        P.dma('sp', w_in_sb, k.wb[('od_w_in', o_)].rearrange('(c p) n -> p c n', p=128), reads=[('wb', 'od_w_in', o_)],
              writes=['w_in'], stream='w')
        P.op('dve', lambda e: e.memset(state, 0.0), writes=['state'])

        def proj(bank, c0, c1):
            for kc in range(8):
                P.op('pe', lambda e, kc=kc: e.matmul(k.bank[bank][:, 0:c1 - c0], lhsT=cur['hT'][:, kc, :], rhs=w_in_sb[:, kc, c0:c1],
                                                    start=(kc == 0), stop=(kc == 7)), reads=[cur['hTn'], 'w_in'],
                     writes=['bank%d' % bank])

        def rope(src, dst, nh, hd, half, c_, s_, csn, sname, dname):
            sv = src.rearrange('p (h d) -> p h d', h=nh)
            dv = dst.rearrange('p (h d) -> p h d', h=nh)
            x1, x2 = sv[:, :, 0:half], sv[:, :, half:2 * half]
            cb = c_.unsqueeze(1).broadcast_to([128, nh, half])
            sb_ = s_.unsqueeze(1).broadcast_to([128, nh, half])
            t = [r_[:, 0:nh * half].rearrange('p (h d) -> p h d', h=nh) for r_ in rt]
            P.op('dve', lambda e: e.tensor_tensor(out=t[0], in0=x1, in1=cb, op=ALU.mult), reads=[sname, csn], writes=['rt0'])
            P.op('pool', lambda e: e.tensor_tensor(out=t[1], in0=x2, in1=sb_, op=ALU.mult), reads=[sname, csn], writes=['rt1'])
            P.op('dve', lambda e: e.tensor_tensor(out=t[2], in0=x2, in1=cb, op=ALU.mult), reads=[sname, csn], writes=['rt2'])
            P.op('pool', lambda e: e.tensor_tensor(out=t[3], in0=x1, in1=sb_, op=ALU.mult), reads=[sname, csn], writes=['rt3'])
            if 2 * half < hd:
                P.op('act', lambda e: e.copy(out=dst, in_=src), reads=[sname], writes=[dname])
            P.op('dve', lambda e: e.tensor_tensor(out=dv[:, :, 0:half], in0=t[0], in1=t[1], op=ALU.subtract),
                 reads=['rt0', 'rt1'], writes=[dname])
            P.op('pool', lambda e: e.tensor_tensor(out=dv[:, :, half:2 * half], in0=t[2], in1=t[3], op=ALU.add),
                 reads=['rt2', 'rt3'], writes=[dname])

        def stage_p(i):
            pz = i % 2
            cur['hT'] = hT1s[pz]
            cur['hTn'] = 'hT1_%d' % pz
            cs_ = csb[pz]
            csn = 'csb%d' % pz
            P.dma('sp', cs_[:, 0, :], ropeC[:, i, :], reads=['ropeC'], writes=[csn], stream='small')
            P.dma('sp', cs_[:, 1, :], ropeS[:, i, :], reads=['ropeS'], writes=[csn], stream='small')
            P.dma('sp', k.xt[i % 3], xsrc[i * 128:(i + 1) * 128, :], reads=[('x', i)], writes=['xt%d' % (i % 3)], stream='x')
            norm_a(k, k.xt[i % 3], 'xt%d' % (i % 3), 'mix_pre', pz)
            trans_t(k, pz, hT1s[pz], 'hT1_%d' % pz)
            proj(1, 0, 512)
            proj(2, 512, 1024)
            proj(3, 1024, 1304)
            P.op('act', lambda e: e.activation(out=zqs[pz], in_=k.bank[1], func=AF.Copy, scale=0.125), reads=['bank1'],
                 writes=['zq%d' % pz])
            P.op('act', lambda e: e.copy(out=zkvs[pz][:, 0:512], in_=k.bank[2]), reads=['bank2'], writes=['zkv%d' % pz])
            P.op('act', lambda e: e.copy(out=zkvs[pz][:, 512:768], in_=k.bank[3][:, 0:256]), reads=['bank3'], writes=['zkv%d' % pz])
            P.op('act', lambda e: e.activation(out=gsig[:, i, :], in_=k.bank[3][:, 256:280], func=AF.Sigmoid),
                 reads=['bank3'], writes=[('gsig', i)])
            proj(4, 1304, 1816)
            proj(5, 1816, 2328)
            proj(6, 2328, 2840)
            proj(7, 2840, 3352)
            P.op('act', lambda e: e.copy(out=zrqs[pz], in_=k.bank[4]), reads=['bank4'], writes=['zrq%d' % pz])
            P.op('act', lambda e: e.activation(out=zrks[pz], in_=k.bank[5], func=AF.Copy, scale=128.0 ** -0.5), reads=['bank5'],
                 writes=['zrk%d' % pz])
            P.op('act', lambda e: e.copy(out=rvbs[pz], in_=k.bank[6]), reads=['bank6'], writes=['rvb%d' % pz])
            P.op('act', lambda e: e.activation(out=gsls[pz], in_=k.bank[7], func=AF.Silu), reads=['bank7'], writes=['gsl%d' % pz])

        def stage_r(i):
            pz = i % 2
            zq, zkv, zrq, zrk, rvb, gsl = zqs[pz], zkvs[pz], zrqs[pz], zrks[pz], rvbs[pz], gsls[pz]
            zqn, zkvn, zrqn, zrkn, rvbn, gsln = ['%s%d' % (n_, pz) for n_ in ('zq', 'zkv', 'zrq', 'zrk', 'rvb', 'gsl')]
            cs_ = csb[pz]
            csn = 'csb%d' % pz
            cn, sn = cs_[:, 0, 0:8], cs_[:, 1, 0:8]
            cr, sr = cs_[:, 0, 8:72], cs_[:, 1, 8:72]
            rope(zq, qb, 8, 64, 8, cn, sn, csn, zqn, 'qb')
            P.dma('pool', qd[i * 128:(i + 1) * 128, :], qb, reads=['qb'], writes=[('qd', i)], stream='xo')
            kv4 = zkv.rearrange('p (a b g d) -> p a b g d', a=3, b=2, g=2)[:, :, 0, :, :]
            x1, x2 = kv4[:, :, :, 0:8], kv4[:, :, :, 8:16]
            cb = cn.unsqueeze(1).unsqueeze(1).broadcast_to([128, 3, 2, 8])
            sb_ = sn.unsqueeze(1).unsqueeze(1).broadcast_to([128, 3, 2, 8])
            t = [r_[:, 0:48].rearrange('p (a g d) -> p a g d', a=3, g=2) for r_ in rt]
            P.op('dve', lambda e: e.tensor_tensor(out=t[0], in0=x1, in1=cb, op=ALU.mult), reads=[zkvn, csn], writes=['rt0'])
            P.op('dve', lambda e: e.tensor_tensor(out=t[1], in0=x2, in1=sb_, op=ALU.mult), reads=[zkvn, csn], writes=['rt1'])
            P.op('dve', lambda e: e.tensor_tensor(out=t[2], in0=x2, in1=cb, op=ALU.mult), reads=[zkvn, csn], writes=['rt2'])
            P.op('dve', lambda e: e.tensor_tensor(out=t[3], in0=x1, in1=sb_, op=ALU.mult), reads=[zkvn, csn], writes=['rt3'])
            P.op('dve', lambda e: e.tensor_tensor(out=x1, in0=t[0], in1=t[1], op=ALU.subtract), reads=['rt0', 'rt1'], writes=[zkvn])
            P.op('dve', lambda e: e.tensor_tensor(out=x2, in0=t[2], in1=t[3], op=ALU.add), reads=['rt2', 'rt3'], writes=[zkvn])
            P.op('act', lambda e: e.copy(out=kvb, in_=zkv), reads=[zkvn], writes=['kvb'])
            P.op('pool', lambda e: e.tensor_copy(out=vs_aug[:, i, :, 0:64], in_=kvb[:, 384:512].rearrange('p (g d) -> p g d', g=2)),
                 reads=['kvb'], writes=['vs_aug'])
            P.op('pool', lambda e: e.tensor_copy(out=vw_aug[:, i, :, 0:64], in_=kvb[:, 640:768].rearrange('p (g d) -> p g d', g=2)),
                 reads=['kvb'], writes=['vw_aug'])
            tp = k.bankb[0]
            srcs = [0, 64, 128, 192, 512, 576, 256, 320]
            for j, c0 in enumerate(srcs):
                P.op('pe', lambda e, j=j, c0=c0: e.transpose(out=tp[0:64, j * 128:(j + 1) * 128], in_=kvb[:, c0:c0 + 64],
                                                            identity=k.ident), reads=['kvb', 'ident'], writes=['bank0'])
            P.op('act', lambda e: e.copy(out=kcv_t, in_=tp[0:64, 0:512].rearrange('p (j t) -> p j t', j=4)), reads=['bank0'],
                 writes=['kcv_t'])
            P.dma('pool', kcvd[:, :, i * 128:(i + 1) * 128].rearrange('j p t -> p j t'), kcv_t, reads=['kcv_t'], writes=['kcvd'],
                  stream='xo')
            P.op('act', lambda e: e.copy(out=kwT[:, :, i * 128:(i + 1) * 128],
                                         in_=tp[0:64, 512:768].rearrange('p (j t) -> p j t', j=2)), reads=['bank0'], writes=['kwT'])
            P.op('act', lambda e: e.copy(out=KE[0:64, :, i * 128:(i + 1) * 128],
                                         in_=tp[0:64, 768:1024].rearrange('p (j t) -> p j t', j=2)), reads=['bank0'], writes=['KE'])
            rope(zrq, rqb, 4, 128, 64, cr, sr, csn, zrqn, 'rqb')
            rope(zrk, rkr, 4, 128, 64, cr, sr, csn, zrkn, 'rkr')
            P.op('act', lambda e: e.copy(out=rkb, in_=rkr), reads=['rkr'], writes=['rkb'])
            P.op('dve', lambda e: e.tensor_tensor(out=rkz.rearrange('p (h d) -> p h d', h=4),
                                                  in0=rkr.rearrange('p (h d) -> p h d', h=4),
                                                  in1=zeta.unsqueeze(2).broadcast_to([128, 4, 128]), op=ALU.mult),
                 reads=['rkr', 'zeta'], writes=['rkz'])
            for h in range(4):
                P.op('pe', lambda e, h=h: e.transpose(out=tp[:, h * 128:(h + 1) * 128], in_=rqb[:, h * 128:(h + 1) * 128],
                                                      identity=k.ident), reads=['rqb', 'ident'], writes=['bank0'])
            for h in range(4):
                P.op('pe', lambda e, h=h: e.transpose(out=tp[:, (4 + h) * 128:(5 + h) * 128], in_=rkb[:, h * 128:(h + 1) * 128],
                                                      identity=k.ident), reads=['rkb', 'ident'], writes=['bank0'])
            P.op('act', lambda e: e.copy(out=qkT, in_=tp.rearrange('p (c t) -> p c t', c=8)), reads=['bank0'], writes=['qkT'])
            for h in range(4):
                P.op('pe', lambda e, h=h: e.matmul(k.bank[5][:, h * 128:(h + 1) * 128], lhsT=qkT[:, 4 + h, :], rhs=qkT[:, h, :],
                                                  start=True, stop=True), reads=['qkT'], writes=['bank5'])
            P.op('dve', lambda e: e.tensor_tensor(out=STs, in0=k.bank[5], in1=decT, op=ALU.mult), reads=['bank5', 'decT'],
                 writes=['STs'])
            P.op('pool', lambda e: e.tensor_tensor(out=qxiT, in0=qkT[:, 0:4, :].rearrange('p h t -> p (h t)'), in1=xi, op=ALU.mult),
                 reads=['qkT', 'xi'], writes=['qxiT'])
            for h in range(4):
                hs = slice(h * 128, (h + 1) * 128)
                P.op('pe', lambda e, hs=hs: e.matmul(k.bank[7][:, hs], lhsT=STs[:, hs], rhs=rvb[:, hs], start=True, stop=(i == 0)),
                     reads=['STs', rvbn], writes=['bank7'])
                if i > 0:
                    P.op('pe', lambda e, hs=hs: e.matmul(k.bank[7][:, hs], lhsT=qxiT[:, hs], rhs=stbf[:, hs], start=False, stop=True),
                         reads=['qxiT', 'stbf'], writes=['bank7'])
            for h in range(4):
                hs = slice(h * 128, (h + 1) * 128)
                P.op('pe', lambda e, hs=hs: e.matmul(k.bank[6][:, hs], lhsT=rkz[:, hs], rhs=rvb[:, hs], start=True, stop=True),
                     reads=['rkz', rvbn], writes=['bank6'])
            P.op('dve', lambda e: e.tensor_tensor(out=state.rearrange('p (h d) -> p h d', h=4),
                                                  in0=state.rearrange('p (h d) -> p h d', h=4),
                                                  in1=gch.unsqueeze(2).broadcast_to([128, 4, 128]), op=ALU.mult),
                 reads=['state', 'gch'], writes=['state'])
            P.op('dve', lambda e: e.tensor_tensor(out=state, in0=k.bank[6], in1=state, op=ALU.add), reads=['bank6', 'state'],
                 writes=['state'])
            P.op('act', lambda e: e.copy(out=stbf, in_=state), reads=['state'], writes=['stbf'])
            for h in range(4):
                P.op('dve', lambda e, h=h: e.bn_stats(out=bst4[:, h, :], in_=k.bank[7][:, h * 128:(h + 1) * 128]),
                     reads=['bank7'], writes=['bst4'])
            for h in range(4):
                P.op('dve', lambda e, h=h: e.bn_aggr(out=mv4[:, h, :], in_=bst4[:, h, :]), reads=['bst4'], writes=['mv4'])
            P.op('dve', lambda e: e.tensor_scalar(out=rs4, in0=mv4[:, :, 1], scalar1=1e-5, scalar2=None, op0=ALU.add),
                 reads=['mv4'], writes=['rs4'])
            P.op('pool', lambda e: e.tensor_tensor(out=rs4, in0=rs4, in1=k.mhalf, op=ALU.pow), reads=['rs4', 'mhalf'], writes=['rs4'])
            for h in range(4):
                hs = slice(h * 128, (h + 1) * 128)
                P.op('dve', lambda e, h=h, hs=hs: e.tensor_scalar(out=on[:, hs], in0=k.bank[7][:, hs], scalar1=mv4[:, h, 0:1],
                                                                  scalar2=rs4[:, h:h + 1], op0=ALU.subtract, op1=ALU.mult),
                     reads=['bank7', 'mv4', 'rs4'], writes=['on'])
            P.op('pool', lambda e: e.tensor_tensor(out=on, in0=on, in1=gng, op=ALU.mult), reads=['on', 'gng'], writes=['on'])
            P.op('pool', lambda e: e.tensor_tensor(out=ydb, in0=on, in1=gsl, op=ALU.mult), reads=['on', gsln], writes=['ydb'])
            P.dma('pool', ydd[i * 128:(i + 1) * 128, :], ydb, reads=['ydb'], writes=[('ydd', i)], stream='xo')

        stage_p(0)
        for i in range(NT):
            if i + 1 < NT:
                stage_p(i + 1)
            stage_r(i)

        P.barrier()
        s1.close()
        s1c = ExitStack()
        w1 = sbs(s1c, 'w1', [64, 32, 128], BF16)
        w2 = sbs(s1c, 'w2', [128, 64], BF16)
        posf32 = sbs(s1c, 'posf32', [64, 32], F32)
        posT = sbs(s1c, 'posT', [64, 32], BF16)
        cbias = sbs(s1c, 'cbias', [128, 1], F32)
        ghT = sbs(s1c, 'ghT', [128, 256], BF16)
        csrc = sbs(s1c, 'csrc', [64, T], BF16)
        for kind, (n1, n2, npos) in enumerate([('od_cmp_k_w1', 'od_cmp_k_w2', 'od_cmp_k_pos'),
                                               ('od_cmp_v_w1', 'od_cmp_v_w2', 'od_cmp_v_pos')]):
            P.dma('sp', w1, k.wb[(n1, o_)].rearrange('(l d) j -> d l j', d=64), reads=[('wb', n1, o_)], writes=['w1'], stream='w')
            P.dma('sp', w2, k.wb[(n2, o_)], reads=[('wb', n2, o_)], writes=['w2'], stream='w')
            with nc.allow_non_contiguous_dma(reason='tiny pos-emb transpose'):
                P.dma('sp', posf32, ins[npos][o_].rearrange('l d -> d l'), writes=['posf32'], stream='small')
            P.op('dve', lambda e: e.tensor_copy(out=posT, in_=posf32), reads=['posf32'], writes=['posT'])
            for g in range(2):
                P.dma('sp', csrc, kcvd[2 * kind + g], reads=['kcvd'], writes=['csrc'], stream='w')
                srcv = csrc.rearrange('p (n s) -> p n s', s=16)
                hb_ = k.bank[1]
                for l_ in range(32):
                    P.op('pe', lambda e, l_=l_: e.matmul(hb_[:, 0:255], lhsT=w1[:, l_, :],
                                                        rhs=(srcv[:, 0:255, l_] if l_ < 16 else srcv[:, 1:256, l_ - 16]),
                                                        start=(l_ == 0), stop=(l_ == 31)),
                         reads=['w1', 'csrc'], writes=['bank1'])
                for l_ in range(32):
                    P.op('pe', lambda e, l_=l_: e.matmul(hb_[:, 256:257], lhsT=w1[:, l_, :], rhs=posT[:, l_:l_ + 1],
                                                        start=(l_ == 0), stop=(l_ == 31)), reads=['w1', 'posT'], writes=['bank1'])
                P.op('dve', lambda e: e.tensor_copy(out=cbias, in_=hb_[:, 256:257]), reads=['bank1'], writes=['cbias'])
                P.op('dve', lambda e: e.memset(ghT[:, 255:256], 0.0), writes=['ghT'])
                P.op('act', lambda e: e.activation(out=ghT[:, 0:255], in_=hb_[:, 0:255], func=AF.Gelu_apprx_tanh,
                                                   bias=cbias[:, 0:1]), reads=['bank1', 'cbias'], writes=['ghT'])
                if kind == 0:
                    P.op('pe', lambda e: e.matmul(k.bank[2][0:64, 0:256], lhsT=w2, rhs=ghT, start=True, stop=True),
                         reads=['w2', 'ghT'], writes=['bank2'])
                    P.op('act', lambda e, g=g: e.copy(out=kcmpT[:, g, :], in_=k.bank[2][0:64, 0:256]), reads=['bank2'],
                         writes=['kcmpT'])
                else:
                    for c in range(2):
                        P.op('pe', lambda e, c=c: e.matmul(k.bank[2][:, c * 64:(c + 1) * 64], lhsT=ghT[:, c * 128:(c + 1) * 128],
                                                          rhs=w2, start=True, stop=True), reads=['w2', 'ghT'], writes=['bank2'])
                    P.op('act', lambda e, g=g: e.copy(out=vcmp_aug[:, :, g, 0:64],
                                                      in_=k.bank[2][:, 0:128].rearrange('p (c d) -> p c d', c=2)),
                         reads=['bank2'], writes=['vcmp'])
        P.barrier()
        s1c.close()

    with ExitStack() as s2:
        w_out_sb = sbs(s2, 'w_out', [128, 8, 1024], BF16)
        PT = sbs(s2, 'PT', [128, NT, 512], BF16)
        PW = sbs(s2, 'PW', [128, 5, 512], BF16)
        Pc = sbs(s2, 'Pc', [128, 2, 512], BF16)
        QN = [sbs(s2, 'QN%d' % g, [128, 512], BF16) for g in range(2)]
        qt = [sbs(s2, 'qt%d' % j, [128, 512], BF16) for j in range(2)]
        ydts = [sbs(s2, 'ydt%d' % j, [128, 512], BF16) for j in range(2)]
        negt = sbs(s2, 'negt', [128, 128], BF16)
        expf = sbs(s2, 'expf', [128, 512], F32)
        score = sbs(s2, 'score', [128, 64], F32)
        sc2 = sbs(s2, 'sc2', [128, 64], F32)
        m8a = sbs(s2, 'm8a', [128, 8], F32)
        m8b = sbs(s2, 'm8b', [128, 8], F32)
        thr = sbs(s2, 'thr', [128, 1], F32)
        psl = sbs(s2, 'psl', [128, 64], F32)
        rdenA = [sbs(s2, 'rdenA%d' % g, [128, 4], F32) for g in range(2)]
        rdenB = [sbs(s2, 'rdenB%d' % g, [128, 12], F32) for g in range(2)]
        coefs = [sbs(s2, 'coef%d' % g, [128, 12], F32) for g in range(2)]
        ocw = [sbs(s2, 'ocw%d' % g, [128, 2, 260], F32) for g in range(2)]
        yc = sbs(s2, 'yc', [128, 512], F32)
        ycb = sbs(s2, 'ycb', [128, 512], BF16)
        yT = sbs(s2, 'yT', [128, 8, 128], BF16)
        P.dma('sp', w_out_sb, k.wb[('od_w_out', o_)].rearrange('(c p) n -> p c n', p=128), reads=[('wb', 'od_w_out', o_)],
              writes=['w_out'], stream='w')
        P.op('dve', lambda e: e.memset(negt, 0.0), writes=['negt'])
        tp = k.bankb[0]
        sbank = [0]

        def next_sbank():
            sbank[0] += 1
            return (1, 2, 7)[sbank[0] % 3]

        def den_view(ap260):
            return ap260.rearrange('p (m e) -> p m e', e=65)[:, :, 64]

        def masked_exp(b, nn, dst, mask_ap, dname):
            if mask_ap is None:
                P.op('act', lambda e: e.activation(out=dst[0:nn], in_=k.bank[b][0:nn, :], func=AF.Exp), reads=['bank%d' % b],
                     writes=[dname])
            else:
                P.op('act', lambda e: e.activation(out=expf[0:nn], in_=k.bank[b][0:nn, :], func=AF.Exp), reads=['bank%d' % b],
                     writes=['expf'])
                P.op('pool', lambda e: e.tensor_tensor(out=dst[0:nn].rearrange('p (m q) -> p m q', m=4),
                                                       in0=expf[0:nn].rearrange('p (m q) -> p m q', m=4),
                                                       in1=mask_ap.unsqueeze(1).broadcast_to([nn, 4, 128]), op=ALU.mult),
                     reads=['expf', 'tri', 'ntri'], writes=[dname])

        def o2_loads(i):
            P.dma('sp', k.xt[i % 3], xsrc[i * 128:(i + 1) * 128, :], reads=[('x', i)], writes=['xt%d' % (i % 3)], stream='x')
            P.dma('sp', qt[i % 2], qd[i * 128:(i + 1) * 128, :], reads=[('qd', i)], writes=['qt%d' % (i % 2)], stream='x')
            P.dma('sp', ydts[i % 2], ydd[i * 128:(i + 1) * 128, :], reads=[('ydd', i)], writes=['ydt%d' % (i % 2)], stream='x')

        def stage_a(i, g):
            qti = qt[i % 2]
            qtn = 'qt%d' % (i % 2)
            off = 62 - 2 * i
            Q = QN[g]
            qn = 'QN%d' % g
            rdA = rdenA[g]
            rdAn = 'rdenA%d' % g
            for m in range(4):
                h = 4 * g + m
                P.op('pe', lambda e, m=m, h=h: e.transpose(out=tp[0:64, m * 128:(m + 1) * 128], in_=qti[:, h * 64:(h + 1) * 64],
                                                          identity=k.ident), reads=[qtn, 'ident'], writes=['bank0'])
            P.op('act', lambda e: e.copy(out=Q[0:64, :], in_=tp[0:64, 0:512]), reads=['bank0'], writes=[qn + 'lo'])
            chunks = [(0, 128)] + ([(1, 127)] if 8 * i + 6 >= 128 else [])
            for (c, nn) in chunks:
                b = next_sbank()
                P.op('pe', lambda e, c=c, nn=nn, b=b: e.matmul(k.bank[b][0:nn, :], lhsT=kcmpT[:, g, c * 128:c * 128 + nn],
                                                              rhs=Q[0:64, :], start=True, stop=True),
                     reads=['kcmpT', qn + 'lo'], writes=['bank%d' % b])
                full = (16 * (c * 128 + nn - 1) + 31 <= 128 * i)
                if full:
                    P.op('act', lambda e, c=c, nn=nn, b=b: e.activation(out=Pc[0:nn, c, :], in_=k.bank[b][0:nn, :], func=AF.Exp),
                         reads=['bank%d' % b], writes=['Pc'])
                else:
                    tv = float(128 * i - 31 - 2048 * c)
                    P.op('act', lambda e, nn=nn, b=b: e.activation(out=expf[0:nn], in_=k.bank[b][0:nn, :], func=AF.Exp),
                         reads=['bank%d' % b], writes=['expf'])
                    P.op('dve', lambda e, c=c, nn=nn, tv=tv: e.scalar_tensor_tensor(
                        out=Pc[0:nn, c, :].rearrange('p (m q) -> p m q', m=4),
                        in0=cm[0:nn].unsqueeze(1).broadcast_to([nn, 4, 128]), scalar=tv,
                        in1=expf[0:nn].rearrange('p (m q) -> p m q', m=4), op0=ALU.is_le, op1=ALU.mult),
                         reads=['expf', 'cm'], writes=['Pc'])
            for m in range(4):
                for ci, (c, nn) in enumerate(chunks):
                    P.op('pe', lambda e, m=m, c=c, nn=nn, ci=ci: e.matmul(k.bank[3][:, m * 65:(m + 1) * 65],
                                                                        lhsT=Pc[0:nn, c, m * 128:(m + 1) * 128],
                                                                        rhs=vcmp_aug[0:nn, c, g, :], start=(ci == 0),
                                                                        stop=(ci == len(chunks) - 1)),
                         reads=['Pc', 'vcmp'], writes=['bank3'])
            for m in range(4):
                for ci, (c, nn) in enumerate(chunks):
                    P.op('pe', lambda e, m=m, c=c, nn=nn, ci=ci: e.matmul(k.bank[4][:, m * 64:(m + 1) * 64],
                                                                        lhsT=Pc[0:nn, c, m * 128:(m + 1) * 128],
                                                                        rhs=cts[0:nn, c, :], start=(ci == 0),
                                                                        stop=(ci == len(chunks) - 1)),
                         reads=['Pc', 'cts'], writes=['bank4'])
            jts = list(range(max(0, i - 4), i + 1))
            for sl_, jt in enumerate(jts):
                b = next_sbank()
                P.op('pe', lambda e, jt=jt, b=b: e.matmul(k.bank[b], lhsT=kwT[:, g, jt * 128:(jt + 1) * 128], rhs=Q[0:64, :],
                                                         start=True, stop=True), reads=['kwT', qn + 'lo'], writes=['bank%d' % b])
                mk = tri if jt == i else (ntri if jt == i - 4 else None)
                masked_exp(b, 128, PW[:, sl_, :], mk, ('PW', sl_))
            for m in range(4):
                for sl_, jt in enumerate(jts):
                    P.op('pe', lambda e, m=m, jt=jt, sl_=sl_: e.matmul(k.bank[6][:, m * 65:(m + 1) * 65],
                                                                      lhsT=PW[:, sl_, m * 128:(m + 1) * 128],
                                                                      rhs=vw_aug[:, jt, g, :], start=(sl_ == 0),
                                                                      stop=(sl_ == len(jts) - 1)),
                         reads=[('PW', sl_), 'vw_aug'], writes=['bank6'])
            P.op('dve', lambda e: e.tensor_scalar(out=rdA, in0=den_view(k.bank[3][:, 0:260]), scalar1=1e-30, scalar2=None,
                                                  op0=ALU.max), reads=['bank3'], writes=[rdAn])
            P.op('dve', lambda e: e.reciprocal(out=rdA, in_=rdA), reads=[rdAn], writes=[rdAn])
            P.op('dve', lambda e: e.tensor_scalar(out=psl, in0=k.bank[4][:, 0:64], scalar1=rdA[:, 0:1], scalar2=None,
                                                  op0=ALU.mult), reads=['bank4', rdAn], writes=['psl'])
            for m in range(1, 4):
                P.op('dve', lambda e, m=m: e.scalar_tensor_tensor(out=psl, in0=k.bank[4][:, m * 64:(m + 1) * 64],
                                                                  scalar=rdA[:, m:m + 1], in1=psl, op0=ALU.mult, op1=ALU.add),
                     reads=['bank4', rdAn, 'psl'], writes=['psl'])
            P.op('act', lambda e: e.copy(out=ocw[g][:, 0, :], in_=k.bank[3][:, 0:260]), reads=['bank3'], writes=['ocw%d' % g])
            P.op('act', lambda e: e.copy(out=ocw[g][:, 1, :], in_=k.bank[6][:, 0:260]), reads=['bank6'], writes=['ocw%d' % g])
            P.op('dve', lambda e: e.tensor_tensor(out=score, in0=psl, in1=keep[:, off:off + 64], op=ALU.mult),
                 reads=['psl', 'keep'], writes=['score'])
            P.op('dve', lambda e: e.tensor_tensor(out=score, in0=score, in1=addc[:, off:off + 64], op=ALU.add),
                 reads=['score', 'addc'], writes=['score'])
            P.op('dve', lambda e: e.memset(score[:, 0:1], 1.0e4), reads=['score'], writes=['score'])
            P.op('dve', lambda e: e.max(out=m8a, in_=score), reads=['score'], writes=['m8a'])
            P.op('dve', lambda e: e.match_replace(out=sc2, in_to_replace=m8a, in_values=score, imm_value=-2.0),
                 reads=['score', 'm8a'], writes=['sc2'])
            P.op('dve', lambda e: e.max(out=m8b, in_=sc2), reads=['sc2'], writes=['m8b'])
            P.op('dve', lambda e: e.tensor_scalar(out=thr, in0=m8b[:, 7:8], scalar1=0.0, scalar2=None, op0=ALU.max),
                 reads=['m8b'], writes=['thr'])
            P.op('dve', lambda e: e.tensor_scalar(out=negt[:, 64:128], in0=score, scalar1=thr[:, 0:1], scalar2=-30000.0,
                                                  op0=ALU.is_lt, op1=ALU.mult), reads=['score', 'thr'], writes=['negt'])
            P.op('pe', lambda e: e.transpose(out=tp[:, 512:640], in_=negt, identity=k.ident), reads=['negt', 'ident'],
                 writes=['bank0'])
            P.op('act', lambda e: e.copy(out=Q[64:128, :].rearrange('p (m q) -> p m q', m=4),
                                         in_=tp[64:128, 512:640].unsqueeze(1).broadcast_to([64, 4, 128])),
                 reads=['bank0'], writes=[qn + 'hi'])

        def stage_b(i, g):
            Q = QN[g]
            qn = 'QN%d' % g
            ob = 5
            obn = 'bank%d' % ob
            rdB = rdenB[g]
            rdBn = 'rdenB%d' % g
            coef = coefs[g]
            cfn = 'coef%d' % g
            for jt in range(i + 1):
                b = next_sbank()
                P.op('pe', lambda e, jt=jt, b=b: e.matmul(k.bank[b], lhsT=KE[:, g, jt * 128:(jt + 1) * 128], rhs=Q, start=True,
                                                         stop=True), reads=['KE', qn + 'lo', qn + 'hi'], writes=['bank%d' % b])
                masked_exp(b, 128, PT[:, jt, :], tri if jt == i else None, ('PT', jt))
            for m in range(4):
                for jt in range(i + 1):
                    P.op('pe', lambda e, m=m, jt=jt: e.matmul(k.bank[ob][:, m * 65:(m + 1) * 65],
                                                             lhsT=PT[:, jt, m * 128:(m + 1) * 128], rhs=vs_aug[:, jt, g, :],
                                                             start=(jt == 0), stop=(jt == i)),
                         reads=[('PT', jt), 'vs_aug'], writes=[obn])
            P.op('dve', lambda e: e.tensor_copy(out=rdB[:, 0:4], in_=rdenA[g]), reads=['rdenA%d' % g], writes=[rdBn])
            P.op('dve', lambda e: e.tensor_scalar(out=rdB[:, 4:8], in0=den_view(k.bank[ob][:, 0:260]), scalar1=1e-30, scalar2=None,
                                                  op0=ALU.max), reads=[obn], writes=[rdBn])
            P.op('dve', lambda e: e.tensor_scalar(out=rdB[:, 8:12], in0=den_view(ocw[g][:, 1, :]), scalar1=1e-30, scalar2=None,
                                                  op0=ALU.max), reads=['ocw%d' % g], writes=[rdBn])
            P.op('dve', lambda e: e.reciprocal(out=rdB[:, 4:12], in_=rdB[:, 4:12]), reads=[rdBn], writes=[rdBn])
            P.op('dve', lambda e: e.tensor_tensor(out=coef.rearrange('p (b m) -> p b m', b=3),
                                                  in0=rdB.rearrange('p (b m) -> p b m', b=3),
                                                  in1=gsig[:, i, g * 12:(g + 1) * 12].rearrange('p (m b) -> p b m', b=3),
                                                  op=ALU.mult), reads=[rdBn, ('gsig', i)], writes=[cfn])
            for m in range(4):
                h = 4 * g + m
                ym = yc[:, h * 64:(h + 1) * 64]
                P.op('pool', lambda e, m=m, ym=ym: e.tensor_scalar(out=ym, in0=ocw[g][:, 0, m * 65:m * 65 + 64],
                                                                   scalar1=coef[:, m:m + 1], scalar2=0.0, op0=ALU.mult, op1=ALU.add),
                     reads=['ocw%d' % g, cfn], writes=[('yc', h)])
                P.op('dve', lambda e, m=m, ym=ym: e.scalar_tensor_tensor(out=ym, in0=k.bank[ob][:, m * 65:m * 65 + 64],
                                                                         scalar=coef[:, 4 + m:5 + m], in1=ym, op0=ALU.mult,
                                                                         op1=ALU.add), reads=[obn, cfn, ('yc', h)],
                     writes=[('yc', h)])
                P.op('dve', lambda e, m=m, ym=ym: e.scalar_tensor_tensor(out=ym, in0=ocw[g][:, 1, m * 65:m * 65 + 64],
                                                                         scalar=coef[:, 8 + m:9 + m], in1=ym, op0=ALU.mult,
                                                                         op1=ALU.add), reads=['ocw%d' % g, cfn, ('yc', h)],
                     writes=[('yc', h)])

        def out_proj(i):
            xt = k.xt[i % 3]
            xr = 'xt%d' % (i % 3)
            ydt = ydts[i % 2]
            ydn = 'ydt%d' % (i % 2)
            P.op('act', lambda e: e.copy(out=ycb, in_=yc), reads=[('yc', h) for h in range(8)], writes=['ycb'])
            for c in range(4):
                P.op('pe', lambda e, c=c: e.transpose(out=tp[:, c * 128:(c + 1) * 128], in_=ycb[:, c * 128:(c + 1) * 128],
                                                      identity=k.ident), reads=['ycb', 'ident'], writes=['bank0'])
            for c in range(4):
                P.op('pe', lambda e, c=c: e.transpose(out=tp[:, (4 + c) * 128:(5 + c) * 128], in_=ydt[:, c * 128:(c + 1) * 128],
                                                      identity=k.ident), reads=[ydn, 'ident'], writes=['bank0'])
            P.op('act', lambda e: e.copy(out=yT, in_=tp.rearrange('p (c t) -> p c t', c=8)), reads=['bank0'], writes=['yT'])
            pm = [k.bank[3], k.bank[4]]
            for cb in range(2):
                for kc in range(8):
                    P.op('pe', lambda e, kc=kc, cb=cb: e.matmul(pm[cb], lhsT=yT[:, kc, :], rhs=w_out_sb[:, kc, cb * 512:(cb + 1) * 512],
                                                               start=(kc == 0), stop=(kc == 7)), reads=['yT', 'w_out'],
                         writes=['bank%d' % (3 + cb)])
            post_norm_residual(k, pm, ['bank3', 'bank4'], 'mix_post', xt, xr)
            P.dma('sp', xdst[i * 128:(i + 1) * 128, :], xt, reads=[xr], writes=[('x', i)], stream='xo')

        units = [(i, g) for i in range(NT) for g in range(2)]
        o2_loads(0)
        stage_a(*units[0])
        for n, (i, g) in enumerate(units):
            if n + 1 < len(units):
                ni, ng = units[n + 1]
                if ng == 0:
                    o2_loads(ni)
                stage_a(ni, ng)
            stage_b(i, g)
            if g == 1:
                out_proj(i)

def make_consts():
    c = {}
    Dm = np.zeros((3, 4, 128, 128), np.float32)
    for g, w in enumerate(POOL_WINDOWS):
        for t in range(128):
            lo = max(t + 1 - w, 0)
            for s in range(lo, t + 1):
                Dm[0, g, s, t] += 1.0 / (t + 1 - lo)
            Dm[0, g, t, t] -= 1.0
            for s in range(t + 1 - w, t + 1):
                if s >= 0:
                    Dm[1, g, s, t] += 1.0 / w
                else:
                    Dm[2, g, s + 128, t] += 1.0 / w
            Dm[1, g, t, t] -= 1.0
    c['c_D'] = Dm.astype(ml_dtypes.bfloat16)
    bf = ml_dtypes.bfloat16
    invf = np.concatenate([1.0 / (500000.0 ** (np.arange(0, 16, 2, dtype=np.float32) / 16)),
                           1.0 / (10000.0 ** (np.arange(0, 128, 2, dtype=np.float32) / 128))]).astype(np.float32)
    c['c_invf'] = invf.reshape(1, 72)
    lg = np.log1p(-np.exp2(-5.0 - np.arange(4, dtype=np.float64)))
    idx = np.arange(128, dtype=np.float64)
    rel = idx[None, :] - idx[:, None]
    dec = np.where((rel >= 0)[:, None, :], np.exp(np.maximum(rel, 0)[:, None, :] * lg[None, :, None]), 0.0)
    c['c_decT'] = dec.reshape(128, 512).astype(np.float32)
    xi = np.exp((idx + 1.0)[None, :] * lg[:, None])
    c['c_xi'] = np.broadcast_to(xi.reshape(1, 512), (128, 512)).astype(np.float32).copy()
    c['c_zeta'] = np.exp((127 - idx)[:, None] * lg[None, :]).astype(np.float32)
    c['c_gch'] = np.broadcast_to(np.exp(128 * lg)[None, :], (128, 4)).astype(np.float32).copy()
    p = np.arange(128)
    c['c_tri'] = (p[:, None] <= p[None, :]).astype(np.float32)
    c['c_cm'] = (16.0 * p[:, None] - p[None, :]).astype(np.float32)
    cq = (p >= 64).astype(np.int64)[:, None]
    jj = np.arange(128)[None, :]
    c['c_keep'] = (jj <= 60 + cq).astype(np.float32)
    add = np.zeros((128, 128), np.float32)
    add[(jj == 61 + cq) | (jj == 62 + cq)] = 1.0e4
    add[jj > 62 + cq] = -1.0
    c['c_add'] = add
    n = np.arange(256)
    cs = n * 16
    ss = np.arange(64) * 64
    cts = ((cs[:, None] < ss[None, :] + 64) & (cs[:, None] + 32 > ss[None, :])).astype(np.float32)
    cts[255] = 0
    c['c_cts'] = cts.reshape(2, 128, 64).astype(bf)
    c['c_E'] = (np.arange(4096)[None, :] // 64 == np.arange(64)[:, None]).astype(bf)
    return c


INPUT_NAMES = ["x", "positions", "ln_mix_pre", "ln_mix_post", "ln_ffn_pre", "ln_ffn_post", "ffn_w_gate", "ffn_w_up",
               "ffn_w_down", "ev_w_in", "ev_pool_w", "ev_pool_scale", "ev_sgu_ln_g", "ev_sgu_ln_b", "ev_sgu_w",
               "ev_sgu_b", "ev_w_out", "od_w_in", "od_cmp_k_pos", "od_cmp_k_w1", "od_cmp_k_w2", "od_cmp_v_pos",
               "od_cmp_v_w1", "od_cmp_v_w2", "od_ret_gn_g", "od_w_out"]


def build(shapes, consts, layers=(0, 1, 2, 3), phases=('mix', 'ffn')):
    from contextlib import ExitStack
    nc = bass.Bass("TRN2", target_bir_lowering=False)
    ins = {}
    for n in INPUT_NAMES:
        shp = list(shapes[n])
        if n == 'x':
            shp = [T, D]
        if n == 'positions':
            shp = [1, T]
        ins[n] = nc.dram_tensor(n, shp, I32 if n == 'positions' else F32, kind="ExternalInput").ap()
    for n, v in consts.items():
        ins[n] = nc.dram_tensor(n, list(v.shape), BF16 if v.dtype == ml_dtypes.bfloat16 else F32, kind="ExternalInput").ap()
    y = nc.dram_tensor("y", [T, D], F32, kind="ExternalOutput").ap()
    k = K(nc, layers)
    setup_common(k, ins)
    k.sb2 = k.sb('ssa', [128, 2], F32)
    P = k.P
    xsrc = ins['x']
    for li, l in enumerate(layers):
        load_gains(k, l)
        if li + 1 < len(layers):
            cast_layer(k, layers[li + 1])
        if 'mix' in phases:
            with ExitStack() as es:
                if l % 2 == 0:
                    even_phase(k, l, xsrc, y, es)
                else:
                    odd_phase(k, l, xsrc, y, es)
                P.barrier()
            xsrc = y
        if 'ffn' in phases:
            with ExitStack() as es:
                def sb(name, shape, dt):
                    return es.enter_context(nc.sbuf_tensor('ffs%d_' % l + name, list(shape), dt)).ap()
                k.wd_sb = sb('wd', [128, NFC, 1024], BF16)
                k.xt8 = [sb('xt8_%d' % i, [128, D], F32) for i in range(8)]
                k.wgu = [sb('wgu%d' % i, [128, 2, 8, 512], BF16) for i in range(2)]
                k.hT2 = [sb('hT%d' % i, [128, 8, 512], BF16) for i in range(2)]
                k.actT = sb('actT', [128, NFC, 512], BF16)
                k.sg = [sb('sg%d' % i, [128, 512], F32) for i in range(2)]
                ffn_phase(k, l, xsrc, y)
                P.barrier()
            xsrc = y
    P.finish()
    return nc


_CACHE = {}


def kernel(**inputs):
    consts = make_consts()
    shapes = {n: inputs[n].shape for n in INPUT_NAMES}
    if 'nc' not in _CACHE:
        _CACHE['nc'] = build(shapes, consts)
    nc = _CACHE['nc']
    in_maps = []
    for c in range(4):
        b = c % 4
        m = {n: np.ascontiguousarray(inputs[n]) for n in INPUT_NAMES if n not in ('x', 'positions')}
        m['x'] = np.ascontiguousarray(inputs['x'][b])
        m['positions'] = np.ascontiguousarray(inputs['positions'][b:b + 1]).astype(np.int32)
        m.update(consts)
        in_maps.append(m)
    res = run_bass_kernel_spmd(nc, in_maps, core_ids=list(range(4)))
    out = np.stack([res.results[b]["y"] for b in range(4)], axis=0)
    return out.astype(np.float32)
```

```python
import numpy as np
import ml_dtypes
import concourse.bass as bass
import concourse.mybir as mybir
from concourse.bass_utils import run_bass_kernel_spmd

F32 = mybir.dt.float32
BF16 = mybir.dt.bfloat16
I32 = mybir.dt.int32
AF = mybir.ActivationFunctionType
ALU = mybir.AluOpType
AX = mybir.AxisListType

import os
SAME_ENG_SYNC = os.environ.get("SES", "1") == "1"


class Prog:
    def __init__(self, nc):
        self.nc = nc
        self.E = {'pe': nc.tensor, 'dve': nc.vector, 'act': nc.scalar, 'pool': nc.gpsimd, 'sp': nc.sync}
        self.semh = {k: nc.alloc_semaphore('s_' + k) for k in ['pe', 'dve', 'act', 'pool']}
        self.cnt = {k: 0 for k in self.semh}
        self.seen = {e: {} for e in self.E}
        self.lastw = {}
        self.readers = {}
        self.nwait = 0
        self.dslot = 0
        self.NSLOT = 32
        self.nins = 0

    def _deps(self, reads, writes):
        deps = {}
        for r in reads:
            w = self.lastw.get(r)
            if w and deps.get(w[0], 0) < w[1]:
                deps[w[0]] = w[1]
            if isinstance(r, str) and r.startswith('bank'):
                for k_, v in self.readers.get(r, {}).items():
                    if deps.get(k_, 0) < v:
                        deps[k_] = v
        for w_ in writes:
            w = self.lastw.get(w_)
            if w and deps.get(w[0], 0) < w[1]:
                deps[w[0]] = w[1]
            for k, v in self.readers.get(w_, {}).items():
                if deps.get(k, 0) < v:
                    deps[k] = v
        return deps

    def _wait(self, eng, deps):
        e = self.E[eng]
        seen = self.seen[eng]
        for k, v in deps.items():
            if k == eng and (eng == 'pe' or not SAME_ENG_SYNC):
                continue
            if seen.get(k, 0) >= v:
                continue
            e.wait_ge(self.semh[k], v)
            seen[k] = v
            self.nwait += 1

    def _record(self, tag, reads, writes):
        for w in writes:
            self.lastw[w] = tag
            self.readers[w] = {}
        for r in reads:
            d = self.readers.setdefault(r, {})
            if d.get(tag[0], 0) < tag[1]:
                d[tag[0]] = tag[1]

    def op(self, eng, fn, reads=(), writes=()):
        self._wait(eng, self._deps(reads, writes))
        ins = fn(self.E[eng])
        self.cnt[eng] += 1
        ins.then_inc(self.semh[eng], 1)
        self.nins += 1
        self._record((eng, self.cnt[eng]), reads, writes)

    def dma(self, q, out, in_, reads=(), writes=(), stream='d0', **kw):
        slot = self.dslot % self.NSLOT
        self.dslot += 1
        key = 'd:%d' % slot
        if key not in self.semh:
            self.semh[key] = self.nc.alloc_semaphore('sd_%d' % slot)
            self.cnt[key] = 0
        e = self.E[q]
        if self.cnt[key] > 0 and self.seen[q].get(key, 0) < self.cnt[key]:
            e.wait_ge(self.semh[key], self.cnt[key])
            self.seen[q][key] = self.cnt[key]
        self._wait(q, self._deps(reads, writes))
        e.dma_start(out=out, in_=in_, **kw).then_inc(self.semh[key], 16)
        self.cnt[key] += 16
        self.nins += 1
        self._record((key, self.cnt[key]), reads, writes)

    def barrier(self):
        for eng in self.E:
            deps = {k: v for k, v in self.cnt.items() if v > 0}
            e = self.E[eng]
            for k, v in deps.items():
                if k == eng and eng == 'pe':
                    continue
                if self.seen[eng].get(k, 0) >= v:
                    continue
                e.wait_ge(self.semh[k], v)
                self.seen[eng][k] = v
        self.lastw = {}
        self.readers = {}

    def finish(self, eng='sp'):
        deps = {k: v for k, v in self.cnt.items() if v > 0}
        e = self.E[eng]
        for k, v in deps.items():
            e.wait_ge(self.semh[k], v)


T = 4096
D = 1024
NT = T // 128
FH = 2816
NFC = FH // 128
DEPTH = 4
POOL_WINDOWS = (2, 4, 8, 16)
EPS = 1e-6


def _flat2(ap, c=1024):
    n = len(ap.shape)
    names = ' '.join('d%d' % i for i in range(n))
    f = ap.rearrange('%s -> (%s)' % (names, names)) if n > 1 else ap
    return f.rearrange('(r c) -> r c', c=c)


class K:
    def __init__(self, nc, layers=(0, 1, 2, 3), Tn=T):
        self.nc = nc
        self.P = Prog(nc)
        self.layers = layers
        self.uid = 0

    def sb(self, name, shape, dt):
        return self.nc.alloc_sbuf_tensor(name, list(shape), dt).ap()

    def dram(self, name, shape, dt, kind="Internal"):
        return self.nc.dram_tensor(name, list(shape), dt, kind=kind).ap()


def cast_copy(k, dst, src, res):
    d2 = _flat2(dst)
    s2 = _flat2(src)
    rows = d2.shape[0]
    r0 = 0
    while r0 < rows:
        r1 = min(rows, r0 + 2048)
        k.P.dma('pool', d2[r0:r1, :], s2[r0:r1, :], writes=[res], stream='cast')
        r0 = r1


def cast_layer(k, l):
    ins = k.ins
    order = []
    if l % 2 == 0:
        order += [('ev_w_in', l // 2), ('ev_pool_w', l // 2), ('ev_w_out', l // 2)]
    else:
        order += [('od_w_in', l // 2), ('od_cmp_k_w1', l // 2), ('od_cmp_k_w2', l // 2), ('od_cmp_v_w1', l // 2),
                  ('od_cmp_v_w2', l // 2), ('od_w_out', l // 2)]
    order += [('ffn_w_gate', l), ('ffn_w_up', l), ('ffn_w_down', l)]
    for name, idx in order:
        src = ins[name][idx]
        dst = k.dram('wb_%s_%d' % (name, idx), src.shape, BF16)
        k.wb[(name, idx)] = dst
        cast_copy(k, dst, src, ('wb', name, idx))


def rstd_from_ssq(k, ssq, rstd, tmp, n, eps, tag):
    P = k.P
    P.op('dve', lambda e: e.tensor_scalar(out=tmp, in0=ssq, scalar1=1.0 / n, scalar2=eps, op0=ALU.mult, op1=ALU.add),
         reads=[tag + 'ssq'], writes=[tag + 'tmp'])
    P.op('pool', lambda e: e.tensor_tensor(out=rstd, in0=tmp, in1=k.mhalf[:, 0:1], op=ALU.pow), reads=[tag + 'tmp', 'mhalf'],
         writes=[tag + 'rstd'])


def setup_common(k, ins):
    nc, P = k.nc, k.P
    k.ins = ins
    k.bank = [nc.alloc_psum_tensor('bank%d' % i, [128, 512], F32).ap() for i in range(8)]
    k.bankb = [b.bitcast(BF16) for b in k.bank]
    k.io_f = k.sb('io_f', [128, 128], F32)
    k.iop = k.sb('iop', [128, 1], F32)
    k.ident = k.sb('ident', [128, 128], BF16)
    k.ones_row = k.sb('ones_row', [1, 128], BF16)
    P.op('pool', lambda e: e.iota(k.io_f, pattern=[[1, 128]], base=0, channel_multiplier=0,
                                  allow_small_or_imprecise_dtypes=True), writes=['io_f'])
    P.op('pool', lambda e: e.iota(k.iop, pattern=[[1, 1]], base=0, channel_multiplier=1,
                                  allow_small_or_imprecise_dtypes=True), writes=['iop'])
    P.op('dve', lambda e: e.tensor_scalar(out=k.ident, in0=k.io_f, scalar1=k.iop[:, 0:1], scalar2=None,
                                          op0=ALU.is_equal), reads=['io_f', 'iop'], writes=['ident'])
    P.op('dve', lambda e: e.memset(k.ones_row, 1.0), writes=['ones_row'])
    k.mhalf = k.sb('mhalf', [128, 4], F32)
    P.op('pool', lambda e: e.memset(k.mhalf, -0.5), writes=['mhalf'])
    k.wb = {}
    cast_layer(k, k.layers[0])
    k.xt = [k.sb('xt%d' % s, [128, D], F32) for s in range(3)]
    k.hbs = [k.sb('hb%d' % i, [128, D], BF16) for i in range(2)]
    k.junk = k.sb('junk', [128, D], BF16)
    k.tmpf = k.sb('tmpf', [128, D], F32)
    k.G = {n: k.sb('G_' + n, [128, D], F32) for n in ['mix_pre', 'mix_post', 'ffn_pre', 'ffn_post']}
    k.st = {n: k.sb('st_' + n, [128, 1], F32) for n in ['ssq', 'tmp', 'rstd', 'ssq2', 'tmp2', 'rstd2']}


def load_gains(k, l):
    for n in ['mix_pre', 'mix_post', 'ffn_pre', 'ffn_post']:
        k.P.dma('sp', k.G[n], k.ins['ln_' + n][l:l + 1, :].broadcast_to([128, D]), writes=['G_' + n], stream='small')


def norm_a(k, xt_ap, xres, gname, hbi):
    P = k.P
    st = k.st
    hb = k.hbs[hbi]
    P.op('act', lambda e: e.activation(out=k.junk, in_=xt_ap, func=AF.Square, accum_out=st['ssq']),
         reads=[xres], writes=['junk', 'ssq'])
    rstd_from_ssq(k, st['ssq'], st['rstd'], st['tmp'], D, EPS, '')
    P.op('dve', lambda e: e.scalar_tensor_tensor(out=hb, in0=xt_ap, scalar=st['rstd'][:, 0:1], in1=k.G[gname],
                                                 op0=ALU.mult, op1=ALU.mult),
         reads=[xres, 'rstd', 'G_' + gname], writes=['hb%d' % hbi])


def trans_t(k, hbi, hT_dst, hTres):
    P = k.P
    hb = k.hbs[hbi]
    tp = k.bankb[0]
    for c in range(8):
        P.op('pe', lambda e, c=c: e.transpose(out=tp[:, c * 128:(c + 1) * 128], in_=hb[:, c * 128:(c + 1) * 128],
                                              identity=k.ident), reads=['hb%d' % hbi, 'ident'], writes=['bank0'])
    P.op('act', lambda e: e.copy(out=hT_dst, in_=tp.rearrange('p (c t) -> p c t', c=8)), reads=['bank0'],
         writes=[hTres])


def post_norm_residual(k, pm, pmres, gname, xt_ap, xres):
    P = k.P
    st = k.st
    ssa = k.sb2
    for cb in range(2):
        P.op('act', lambda e, cb=cb: e.activation(out=k.junk[:, cb * 512:(cb + 1) * 512], in_=pm[cb], func=AF.Square,
                                                  accum_out=ssa[:, cb:cb + 1]),
             reads=[pmres[cb]], writes=['junk', 'ssa'])
    P.op('dve', lambda e: e.tensor_tensor(out=st['ssq2'], in0=ssa[:, 0:1], in1=ssa[:, 1:2], op=ALU.add),
         reads=['ssa'], writes=['2ssq'])
    rstd_from_ssq(k, st['ssq2'], st['rstd2'], st['tmp2'], D, EPS, '2')
    for cb in range(2):
        sl = slice(cb * 512, (cb + 1) * 512)
        P.op('dve', lambda e, cb=cb, sl=sl: e.scalar_tensor_tensor(out=k.tmpf[:, sl], in0=pm[cb], scalar=st['rstd2'][:, 0:1],
                                                                   in1=k.G[gname][:, sl], op0=ALU.mult, op1=ALU.mult),
             reads=[pmres[cb], '2rstd', 'G_' + gname], writes=['tmpf'])
    P.op('pool', lambda e: e.tensor_tensor(out=xt_ap, in0=xt_ap, in1=k.tmpf, op=ALU.add), reads=['tmpf', xres],
         writes=[xres])


def ffn_phase(k, l, xsrc, xdst):
    nc, P = k.nc, k.P
    wg, wu, wd = k.wb[('ffn_w_gate', l)], k.wb[('ffn_w_up', l)], k.wb[('ffn_w_down', l)]
    P.dma('sp', k.wd_sb, wd.rearrange('(c p) n -> p c n', p=128), reads=[('wb', 'ffn_w_down', l)], writes=['wd_sb'],
          stream='w')
    groups = [(0, 4), (4, 4), (8, 4), (12, 4), (16, 4), (20, 2)]
    wgv = wg.rearrange('(c p) n -> p c n', p=128)
    wuv = wu.rearrange('(c p) n -> p c n', p=128)
    gi = 0
    NB = T // 512

    def xtile(tb, s):
        j = (tb % 2) * 4 + s
        return k.xt8[j], 'xt8_%d' % j

    def front_a(tb, s):
        t0 = tb * 512 + s * 128
        xt, xr = xtile(tb, s)
        P.dma('sp', xt, xsrc[t0:t0 + 128, :], reads=[('x', t0 // 128)], writes=[xr], stream='x')
        norm_a(k, xt, xr, 'ffn_pre', s % 2)

    def front_t(tb, s):
        trans_t(k, s % 2, k.hT2[tb % 2][:, :, s * 128:(s + 1) * 128], ('hT', tb % 2))

    for s in range(4):
        front_a(0, s)
        front_t(0, s)
    for tb in range(NB):
        hT = k.hT2[tb % 2]
        hTr = ('hT', tb % 2)
        for gidx, (j0, nj) in enumerate(groups):
            pre = gidx < 4 and tb + 1 < NB
            if pre:
                front_a(tb + 1, gidx)
            buf = gi % 2
            gi += 1
            wt = k.wgu[buf]
            P.dma('sp', wt[:, 0, :, 0:nj * 128], wgv[:, :, j0 * 128:(j0 + nj) * 128], reads=[('wb', 'ffn_w_gate', l)],
                  writes=['wgu%d' % buf], stream='w')
            P.dma('sp', wt[:, 1, :, 0:nj * 128], wuv[:, :, j0 * 128:(j0 + nj) * 128], reads=[('wb', 'ffn_w_up', l)],
                  writes=['wgu%d' % buf], stream='w')
            for jj in range(nj):
                j = j0 + jj
                pb = 1 + 2 * (j % 2)
                pg, pu = k.bank[pb], k.bank[pb + 1]
                for kc in range(8):
                    P.op('pe', lambda e, kc=kc, jj=jj: e.matmul(pg, lhsT=wt[:, 0, kc, jj * 128:(jj + 1) * 128], rhs=hT[:, kc, :],
                                                               start=(kc == 0), stop=(kc == 7)),
                         reads=['wgu%d' % buf, hTr], writes=['bank%d' % pb])
                for kc in range(8):
                    P.op('pe', lambda e, kc=kc, jj=jj: e.matmul(pu, lhsT=wt[:, 1, kc, jj * 128:(jj + 1) * 128], rhs=hT[:, kc, :],
                                                               start=(kc == 0), stop=(kc == 7)),
                         reads=['wgu%d' % buf, hTr], writes=['bank%d' % (pb + 1)])
                sg = k.sg[j % 2]
                P.op('act', lambda e: e.activation(out=sg, in_=pg, func=AF.Silu), reads=['bank%d' % pb],
                     writes=['sg%d' % (j % 2)])
                P.op('dve', lambda e, j=j: e.tensor_tensor(out=k.actT[:, j, :], in0=pu, in1=sg, op=ALU.mult),
                     reads=['bank%d' % (pb + 1), 'sg%d' % (j % 2)], writes=[('actT', j)])
            if pre:
                front_t(tb + 1, gidx)
        for s in range(4):
            t0 = tb * 512 + s * 128
            pm = [k.bank[5], k.bank[6]]
            for cb in range(2):
                for kk in range(NFC):
                    P.op('pe', lambda e, kk=kk, cb=cb: e.matmul(pm[cb], lhsT=k.actT[:, kk, s * 128:(s + 1) * 128],
                                                               rhs=k.wd_sb[:, kk, cb * 512:(cb + 1) * 512],
                                                               start=(kk == 0), stop=(kk == NFC - 1)),
                         reads=[('actT', kk), 'wd_sb'], writes=['bank%d' % (5 + cb)])
            xt_, xr_ = xtile(tb, s)
            post_norm_residual(k, pm, ['bank5', 'bank6'], 'ffn_post', xt_, xr_)
            P.dma('sp', xdst[t0:t0 + 128, :], xt_, reads=[xr_], writes=[('x', t0 // 128)], stream='xo')


def even_phase(k, l, xsrc, xdst, es):
    nc, P = k.nc, k.P
    e_ = l // 2
    ins = k.ins

    def sb(name, shape, dt):
        return es.enter_context(nc.sbuf_tensor('evs%d_' % l + name, list(shape), dt)).ap()

    w_in_sb = sb('w_in', [128, 8, 1536], BF16)
    w_out_sb = sb('w_out', [128, 8, 1024], BF16)
    poolw_sb = sb('poolw', [128, 4, 128], BF16)
    Dm_sb = sb('Dm', [128, 3, 4, 128], BF16)
    WmT = sb('WmT', [128, 4, 128], BF16)
    wsf = sb('wsf', [128, 4, 128], F32)
    wsb = sb('wsb', [128, 4, 128], BF16)
    tril = sb('tril', [128, 128], F32)
    Bt = sb('Bt', [128, 512], F32)
    lng = sb('lng', [128, 512], F32)
    lnb = sb('lnb', [128, 512], F32)
    psc = sb('psc', [128, 4], F32)
    a_sb = [sb('a%d' % i, [128, 512], BF16) for i in range(2)]
    uT_sb = sb('uT', [128, 4, 128], BF16)
    vg = sb('vg', [128, 512], F32)
    vn = sb('vn', [128, 512], F32)
    vln = sb('vln', [128, 512], BF16)
    diffT = sb('diffT', [128, 4, 128], BF16)
    yaT = sb('yaT', [128, 4, 128], BF16)
    ybT = sb('ybT', [128, 4, 128], BF16)
    mxb = sb('mxb', [128, 512], F32)
    hT1s = [sb('hT1_%d' % i, [128, 8, 128], BF16) for i in range(2)]
    bst = sb('bst', [128, 6], F32)
    mv = sb('mv', [128, 2], F32)
    lrs = sb('lrs', [128, 1], F32)

    P.dma('sp', w_in_sb, k.wb[('ev_w_in', e_)].rearrange('(c p) n -> p c n', p=128), reads=[('wb', 'ev_w_in', e_)],
          writes=['w_in'], stream='w')
    P.dma('sp', w_out_sb, k.wb[('ev_w_out', e_)].rearrange('(c p) n -> p c n', p=128), reads=[('wb', 'ev_w_out', e_)],
          writes=['w_out'], stream='w')
    P.dma('sp', poolw_sb, k.wb[('ev_pool_w', e_)].rearrange('g c d -> c g d'), reads=[('wb', 'ev_pool_w', e_)],
          writes=['poolw'], stream='w')
    P.dma('sp', Dm_sb, ins['c_D'].rearrange('a g s t -> s a g t'), writes=['Dm'], stream='small')
    P.dma('sp', wsf, ins['ev_sgu_w'][e_].rearrange('g t s -> t g s'), writes=['wsf'], stream='small')
    P.dma('sp', Bt, ins['ev_sgu_b'][e_:e_ + 1].rearrange('o g t -> o (g t)').broadcast_to([128, 512]), writes=['Bt'],
          stream='small')
    P.dma('sp', lng, ins['ev_sgu_ln_g'][e_:e_ + 1, :].broadcast_to([128, 512]), writes=['lng'], stream='small')
    P.dma('sp', lnb, ins['ev_sgu_ln_b'][e_:e_ + 1, :].broadcast_to([128, 512]), writes=['lnb'], stream='small')
    with nc.allow_non_contiguous_dma(reason='tiny per-channel scale'):
        P.dma('sp', psc, ins['ev_pool_scale'][e_].rearrange('(g p) -> p g', p=128), writes=['psc'], stream='small')
    P.op('dve', lambda e: e.tensor_scalar(out=tril, in0=k.io_f, scalar1=k.iop[:, 0:1], scalar2=None, op0=ALU.is_le),
         reads=['io_f', 'iop'], writes=['tril'])
    P.op('dve', lambda e: e.tensor_tensor(out=wsb, in0=wsf, in1=tril.unsqueeze(1).broadcast_to([128, 4, 128]), op=ALU.mult),
         reads=['wsf', 'tril'], writes=['wsb'])
    tp = k.bankb[0]
    for g in range(4):
        P.op('pe', lambda e, g=g: e.transpose(out=tp[:, g * 128:(g + 1) * 128], in_=wsb[:, g, :], identity=k.ident),
             reads=['wsb', 'ident'], writes=['bank0'])
    P.op('act', lambda e: e.copy(out=WmT, in_=tp[:, 0:512].rearrange('p (g t) -> p g t', g=4)), reads=['bank0'],
         writes=['WmT'])

    def front_a(i):
        P.dma('sp', k.xt[i % 3], xsrc[i * 128:(i + 1) * 128, :], reads=[('x', i)], writes=['xt%d' % (i % 3)], stream='x')
        norm_a(k, k.xt[i % 3], 'xt%d' % (i % 3), 'mix_pre', i % 2)

    def front_t(i):
        trans_t(k, i % 2, hT1s[i % 2], 'hT1_%d' % (i % 2))

    front_a(0)
    front_t(0)
    for i in range(NT):
        xt = k.xt[i % 3]
        xr = 'xt%d' % (i % 3)
        hT1 = hT1s[i % 2]
        hT1n = 'hT1_%d' % (i % 2)
        a_cur, a_prev = a_sb[i % 2], a_sb[(i + 1) % 2]
        ar, apr = 'a%d' % (i % 2), 'a%d' % ((i + 1) % 2)
        if i + 1 < NT and i >= 1:
            pass
        pa, pu, pv = k.bank[1], k.bank[2], k.bank[3]
        for kc in range(8):
            P.op('pe', lambda e, kc=kc: e.matmul(pa, lhsT=hT1[:, kc, :], rhs=w_in_sb[:, kc, 0:512], start=(kc == 0),
                                                stop=(kc == 7)), reads=[hT1n, 'w_in'], writes=['bank1'])
        P.op('act', lambda e: e.copy(out=a_cur, in_=pa), reads=['bank1'], writes=[ar])
        for kc in range(8):
            P.op('pe', lambda e, kc=kc: e.matmul(pv, lhsT=hT1[:, kc, :], rhs=w_in_sb[:, kc, 1024:1536], start=(kc == 0),
                                                stop=(kc == 7)), reads=[hT1n, 'w_in'], writes=['bank3'])
        P.op('act', lambda e: e.activation(out=vg, in_=pv, func=AF.Gelu_apprx_tanh), reads=['bank3'], writes=['vg'])
        for c in range(4):
            for kc in range(8):
                P.op('pe', lambda e, kc=kc, c=c: e.matmul(pu[:, c * 128:(c + 1) * 128],
                                                         lhsT=w_in_sb[:, kc, 512 + c * 128:512 + (c + 1) * 128],
                                                         rhs=hT1[:, kc, :], start=(kc == 0), stop=(kc == 7)),
                     reads=[hT1n, 'w_in'], writes=['bank2'])
        P.op('act', lambda e: e.activation(out=uT_sb, in_=pu.rearrange('p (c t) -> p c t', c=4), func=AF.Gelu_apprx_tanh),
             reads=['bank2'], writes=['uT'])
        if i + 1 < NT:
            front_a(i + 1)
        P.op('dve', lambda e: e.bn_stats(out=bst, in_=vg), reads=['vg'], writes=['bst'])
        P.op('dve', lambda e: e.bn_aggr(out=mv, in_=bst), reads=['bst'], writes=['mv'])
        P.op('dve', lambda e: e.tensor_scalar(out=lrs, in0=mv[:, 1:2], scalar1=1e-5, scalar2=None, op0=ALU.add),
             reads=['mv'], writes=['lrs'])
        P.op('pool', lambda e: e.tensor_tensor(out=lrs, in0=lrs, in1=k.mhalf[:, 0:1], op=ALU.pow), reads=['lrs', 'mhalf'], writes=['lrs'])
        P.op('dve', lambda e: e.tensor_scalar(out=vn, in0=vg, scalar1=mv[:, 0:1], scalar2=lrs[:, 0:1], op0=ALU.subtract,
                                              op1=ALU.mult), reads=['vg', 'mv', 'lrs'], writes=['vn'])
        P.op('pool', lambda e: e.tensor_tensor(out=vn, in0=vn, in1=lng, op=ALU.mult), reads=['vn', 'lng'], writes=['vn'])
        P.op('pool', lambda e: e.tensor_tensor(out=vln, in0=vn, in1=lnb, op=ALU.add), reads=['vn', 'lnb'], writes=['vln'])
        pd_ = k.bank[4]
        for g in range(4):
            first = True
            sl = slice(g * 128, (g + 1) * 128)
            P.op('pe', lambda e, g=g, sl=sl: e.matmul(pd_[:, sl], lhsT=a_cur[:, sl], rhs=Dm_sb[:, 0 if i == 0 else 1, g, :],
                                                     start=True, stop=(i == 0)), reads=[ar, 'Dm'], writes=['bank4'])
            if i > 0:
                P.op('pe', lambda e, g=g, sl=sl: e.matmul(pd_[:, sl], lhsT=a_prev[:, sl], rhs=Dm_sb[:, 2, g, :],
                                                         start=False, stop=True), reads=[apr, 'Dm'], writes=['bank4'])
        P.op('dve', lambda e: e.tensor_copy(out=diffT, in_=pd_.rearrange('p (g t) -> p g t', g=4)), reads=['bank4'],
             writes=['diffT'])
        pya = k.bank[5]
        for g in range(4):
            P.op('pe', lambda e, g=g: e.matmul(pya[:, g * 128:(g + 1) * 128], lhsT=poolw_sb[:, g, :], rhs=diffT[:, g, :],
                                              start=True, stop=True), reads=['poolw', 'diffT'], writes=['bank5'])
        P.op('dve', lambda e: e.tensor_tensor(out=yaT, in0=pya.rearrange('p (g t) -> p g t', g=4),
                                              in1=psc.unsqueeze(2).broadcast_to([128, 4, 128]), op=ALU.mult),
             reads=['bank5', 'psc'], writes=['yaT'])
        pmx = k.bank[4]
        for g in range(4):
            P.op('pe', lambda e, g=g: e.matmul(pmx[:, g * 128:(g + 1) * 128], lhsT=vln[:, g * 128:(g + 1) * 128],
                                              rhs=WmT[:, g, :], start=True, stop=True), reads=['vln', 'WmT'],
                 writes=['bank4'])
        P.op('dve', lambda e: e.tensor_tensor(out=mxb, in0=pmx, in1=Bt, op=ALU.add), reads=['bank4', 'Bt'],
             writes=['mxb'])
        P.op('pool', lambda e: e.tensor_tensor(out=ybT, in0=mxb.rearrange('p (g t) -> p g t', g=4), in1=uT_sb, op=ALU.mult),
             reads=['mxb', 'uT'], writes=['ybT'])
        if i + 1 < NT:
            front_t(i + 1)
        pm = [k.bank[6], k.bank[7]]
        for cb in range(2):
            for kc in range(8):
                lh = yaT[:, kc, :] if kc < 4 else ybT[:, kc - 4, :]
                P.op('pe', lambda e, kc=kc, cb=cb, lh=lh: e.matmul(pm[cb], lhsT=lh, rhs=w_out_sb[:, kc, cb * 512:(cb + 1) * 512],
                                                                  start=(kc == 0), stop=(kc == 7)),
                     reads=['yaT', 'ybT', 'w_out'], writes=['bank%d' % (6 + cb)])
        post_norm_residual(k, pm, ['bank6', 'bank7'], 'mix_post', xt, xr)
        P.dma('sp', xdst[i * 128:(i + 1) * 128, :], xt, reads=[xr], writes=[('x', i)], stream='xo')


def odd_phase(k, l, xsrc, xdst, es):
    from contextlib import ExitStack
    nc, P = k.nc, k.P
    o_ = l // 2
    ins = k.ins
    PI = 3.14159265358979

    def sbs(stack, name, shape, dt):
        return stack.enter_context(nc.sbuf_tensor('od%d_' % l + name, list(shape), dt)).ap()

    def sb(name, shape, dt):
        return sbs(es, name, shape, dt)

    qd = k.dram('qd%d' % l, [T, 512], BF16)
    ydd = k.dram('ydd%d' % l, [T, 512], BF16)
    KE = sb('KE', [128, 2, T], BF16)
    kwT = sb('kwT', [64, 2, T], BF16)
    kcvd = k.dram('kcvd%d' % l, [4, 64, T], BF16)
    ropeS = k.dram('ropeS%d' % l, [128, NT, 72], F32)
    ropeC = k.dram('ropeC%d' % l, [128, NT, 72], F32)
    vs_aug = sb('vs_aug', [128, NT, 2, 65], BF16)
    vw_aug = sb('vw_aug', [128, NT, 2, 65], BF16)
    gsig = sb('gsig', [128, NT, 24], F32)
    kcmpT = sb('kcmpT', [64, 2, 256], BF16)
    vcmp_aug = sb('vcmp', [128, 2, 2, 65], BF16)
    tri = sb('tri', [128, 128], F32)
    ntri = sb('ntri', [128, 128], F32)
    cm = sb('cm', [128, 128], F32)
    keep = sb('keep', [128, 128], F32)
    addc = sb('addc', [128, 128], F32)
    cts = sb('cts', [128, 2, 64], BF16)
    decT = sb('decT', [128, 512], F32)
    xi = sb('xi', [128, 512], F32)
    zeta = sb('zeta', [128, 4], F32)
    gch = sb('gch', [128, 4], F32)
    gng = sb('gng', [128, 512], F32)
    for nm, t_, src in [('tri', tri, 'c_tri'), ('cm', cm, 'c_cm'), ('keep', keep, 'c_keep'), ('addc', addc, 'c_add'),
                        ('decT', decT, 'c_decT'), ('xi', xi, 'c_xi'), ('zeta', zeta, 'c_zeta'), ('gch', gch, 'c_gch')]:
        P.dma('sp', t_, ins[src], writes=[nm], stream='small')
    P.dma('sp', cts, ins['c_cts'].rearrange('c n j -> n c j'), writes=['cts'], stream='small')
    P.dma('sp', KE[64:128, 0, :], ins['c_E'], writes=['KE'], stream='small')
    P.dma('sp', KE[64:128, 1, :], ins['c_E'], writes=['KE'], stream='small')
    P.dma('sp', gng, ins['od_ret_gn_g'][o_:o_ + 1, :].broadcast_to([128, 512]), writes=['gng'], stream='small')
    P.op('dve', lambda e: e.tensor_scalar(out=ntri, in0=tri, scalar1=-1.0, scalar2=1.0, op0=ALU.mult, op1=ALU.add),
         reads=['tri'], writes=['ntri'])
    P.op('pool', lambda e: e.memset(vs_aug, 1.0), writes=['vs_aug'])
    P.op('pool', lambda e: e.memset(vw_aug, 1.0), writes=['vw_aug'])
    P.op('pool', lambda e: e.memset(vcmp_aug, 1.0), writes=['vcmp'])
    with ExitStack() as ts:
        posi = sbs(ts, 'posi', [128, NT], I32)
        sinT = sbs(ts, 'sinT', [128, NT, 72], F32)
        cosT = sbs(ts, 'cosT', [128, NT, 72], F32)
        posf = sbs(ts, 'posf', [128, NT], F32)
        invf = sbs(ts, 'invf', [128, 72], F32)
        ang = sbs(ts, 'ang', [128, NT, 72], F32)
        arg = sbs(ts, 'arg', [128, NT, 72], F32)
        kf = sbs(ts, 'kf', [128, NT, 72], F32)
        ki = sbs(ts, 'ki', [128, NT, 72], I32)
        with nc.allow_non_contiguous_dma(reason='positions to token-on-partition layout'):
            P.dma('sp', posi, ins['positions'].rearrange('o (i p) -> p (o i)', p=128), writes=['posi'], stream='small')
        P.dma('sp', invf, ins['c_invf'].broadcast_to([128, 72]), writes=['invf'], stream='small')
        P.op('dve', lambda e: e.tensor_copy(out=posf, in_=posi), reads=['posi'], writes=['posf'])
        P.op('dve', lambda e: e.tensor_tensor(out=ang, in0=posf.unsqueeze(2).broadcast_to([128, NT, 72]),
                                              in1=invf.unsqueeze(1).broadcast_to([128, NT, 72]), op=ALU.mult),
             reads=['posf', 'invf'], writes=['ang'])
        for shift, dst, dn in [(0.0, sinT, 'sinT'), (PI / 2, cosT, 'cosT')]:
            P.op('dve', lambda e, shift=shift: e.tensor_scalar(out=arg, in0=ang, scalar1=shift, scalar2=None, op0=ALU.add),
                 reads=['ang'], writes=['arg'])
            P.op('dve', lambda e: e.tensor_scalar(out=kf, in0=arg, scalar1=1.0 / (2 * PI), scalar2=None, op0=ALU.mult),
                 reads=['arg'], writes=['kf'])
            P.op('dve', lambda e: e.tensor_copy(out=ki, in_=kf), reads=['kf'], writes=['ki'])
            P.op('dve', lambda e: e.tensor_copy(out=kf, in_=ki), reads=['ki'], writes=['kf'])
            P.op('dve', lambda e: e.scalar_tensor_tensor(out=arg, in0=kf, scalar=-6.28125, in1=arg, op0=ALU.mult, op1=ALU.add),
                 reads=['kf', 'arg'], writes=['arg'])
            P.op('dve', lambda e: e.scalar_tensor_tensor(out=arg, in0=kf, scalar=-(2 * PI - 6.28125), in1=arg, op0=ALU.mult,
                                                         op1=ALU.add), reads=['kf', 'arg'], writes=['arg'])
            P.op('dve', lambda e: e.tensor_scalar(out=arg, in0=arg, scalar1=3.1415925, scalar2=-3.1415925, op0=ALU.min,
                                                  op1=ALU.max), reads=['arg'], writes=['arg'])
            P.op('act', lambda e, dst=dst: e.activation(out=dst, in_=arg, func=AF.Sin), reads=['arg'], writes=[dn])
        P.dma('sp', ropeS, sinT, reads=['sinT'], writes=['ropeS'], stream='xo')
        P.dma('sp', ropeC, cosT, reads=['cosT'], writes=['ropeC'], stream='xo')
        P.barrier()

    with ExitStack() as s1:
        w_in_sb = sbs(s1, 'w_in', [128, 8, 3352], BF16)
        hT1s = [sbs(s1, 'hT1_%d' % j, [128, 8, 128], BF16) for j in range(2)]
        cur = {}
        csb = [sbs(s1, 'csb%d' % j, [128, 2, 72], F32) for j in range(2)]
        kcv_t = sbs(s1, 'kcv_t', [64, 4, 128], BF16)
        zqs = [sbs(s1, 'zq%d' % j, [128, 512], F32) for j in range(2)]
        qb = sbs(s1, 'qb', [128, 512], BF16)
        zkvs = [sbs(s1, 'zkv%d' % j, [128, 768], F32) for j in range(2)]
        kvb = sbs(s1, 'kvb', [128, 768], BF16)
        rt = [sbs(s1, 'rt%d' % j, [128, 256], F32) for j in range(4)]
        zrqs = [sbs(s1, 'zrq%d' % j, [128, 512], F32) for j in range(2)]
        zrks = [sbs(s1, 'zrk%d' % j, [128, 512], F32) for j in range(2)]
        rqb = sbs(s1, 'rqb', [128, 512], BF16)
        rkb = sbs(s1, 'rkb', [128, 512], BF16)
        rkr = sbs(s1, 'rkr', [128, 512], F32)
        rkz = sbs(s1, 'rkz', [128, 512], BF16)
        rvbs = [sbs(s1, 'rvb%d' % j, [128, 512], BF16) for j in range(2)]
        gsls = [sbs(s1, 'gsl%d' % j, [128, 512], F32) for j in range(2)]
        qkT = sbs(s1, 'qkT', [128, 8, 128], BF16)
        qxiT = sbs(s1, 'qxiT', [128, 512], BF16)
        STs = sbs(s1, 'STs', [128, 512], BF16)
        state = sbs(s1, 'state', [128, 512], F32)
        stbf = sbs(s1, 'stbf', [128, 512], BF16)
        bst4 = sbs(s1, 'bst4', [128, 4, 6], F32)
        mv4 = sbs(s1, 'mv4', [128, 4, 2], F32)
        rs4 = sbs(s1, 'rs4', [128, 4], F32)
        on = sbs(s1, 'on', [128, 512], F32)
        ydb = sbs(s1, 'ydb', [128, 512], BF16)
        P.dma('sp', w_in_sb, k.wb[('od_w_in', o_)].rearrange('(c p) n -> p c n', p=128), reads=[('wb', 'od_w_in', o_)],
              writes=['w_in'], stream='w')
        P.op('dve', lambda e: e.memset(state, 0.0), writes=['state'])

        def proj(bank, c0, c1):
            for kc in range(8):
                P.op('pe', lambda e, kc=kc: e.matmul(k.bank[bank][:, 0:c1 - c0], lhsT=cur['hT'][:, kc, :], rhs=w_in_sb[:, kc, c0:c1],
                                                    start=(kc == 0), stop=(kc == 7)), reads=[cur['hTn'], 'w_in'],
                     writes=['bank%d' % bank])

        def rope(src, dst, nh, hd, half, c_, s_, csn, sname, dname):
            sv = src.rearrange('p (h d) -> p h d', h=nh)
            dv = dst.rearrange('p (h d) -> p h d', h=nh)
            x1, x2 = sv[:, :, 0:half], sv[:, :, half:2 * half]
            cb = c_.unsqueeze(1).broadcast_to([128, nh, half])
            sb_ = s_.unsqueeze(1).broadcast_to([128, nh, half])
            t = [r_[:, 0:nh * half].rearrange('p (h d) -> p h d', h=nh) for r_ in rt]
            P.op('dve', lambda e: e.tensor_tensor(out=t[0], in0=x1, in1=cb, op=ALU.mult), reads=[sname, csn], writes=['rt0'])
            P.op('pool', lambda e: e.tensor_tensor(out=t[1], in0=x2, in1=sb_, op=ALU.mult), reads=[sname, csn], writes=['rt1'])
            P.op('dve', lambda e: e.tensor_tensor(out=t[2], in0=x2, in1=cb, op=ALU.mult), reads=[sname, csn], writes=['rt2'])
            P.op('pool', lambda e: e.tensor_tensor(out=t[3], in0=x1, in1=sb_, op=ALU.mult), reads=[sname, csn], writes=['rt3'])
            if 2 * half < hd:
                P.op('pool', lambda e: e.tensor_copy(out=dst, in_=src), reads=[sname], writes=[dname])
            P.op('dve', lambda e: e.tensor_tensor(out=dv[:, :, 0:half], in0=t[0], in1=t[1], op=ALU.subtract),
                 reads=['rt0', 'rt1'], writes=[dname])
            P.op('pool', lambda e: e.tensor_tensor(out=dv[:, :, half:2 * half], in0=t[2], in1=t[3], op=ALU.add),
                 reads=['rt2', 'rt3'], writes=[dname])

        def stage_p(i):
            pz = i % 2
            cur['hT'] = hT1s[pz]
            cur['hTn'] = 'hT1_%d' % pz
            cs_ = csb[pz]
            csn = 'csb%d' % pz
            P.dma('sp', cs_[:, 0, :], ropeC[:, i, :], reads=['ropeC'], writes=[csn], stream='small')
            P.dma('sp', cs_[:, 1, :], ropeS[:, i, :], reads=['ropeS'], writes=[csn], stream='small')
            P.dma('sp', k.xt[i % 3], xsrc[i * 128:(i + 1) * 128, :], reads=[('x', i)], writes=['xt%d' % (i % 3)], stream='x')
            norm_a(k, k.xt[i % 3], 'xt%d' % (i % 3), 'mix_pre', pz)
            trans_t(k, pz, hT1s[pz], 'hT1_%d' % pz)
            proj(1, 0, 512)
            proj(2, 512, 1024)
            proj(3, 1024, 1304)
            P.op('act', lambda e: e.activation(out=zqs[pz], in_=k.bank[1], func=AF.Copy, scale=0.125), reads=['bank1'],
                 writes=['zq%d' % pz])
            P.op('act', lambda e: e.copy(out=zkvs[pz][:, 0:512], in_=k.bank[2]), reads=['bank2'], writes=['zkv%d' % pz])
            P.op('act', lambda e: e.copy(out=zkvs[pz][:, 512:768], in_=k.bank[3][:, 0:256]), reads=['bank3'], writes=['zkv%d' % pz])
            P.op('act', lambda e: e.activation(out=gsig[:, i, :], in_=k.bank[3][:, 256:280], func=AF.Sigmoid),
                 reads=['bank3'], writes=[('gsig', i)])
            proj(4, 1304, 1816)
            proj(5, 1816, 2328)
            proj(6, 2328, 2840)
            proj(7, 2840, 3352)
            P.op('act', lambda e: e.copy(out=zrqs[pz], in_=k.bank[4]), reads=['bank4'], writes=['zrq%d' % pz])
            P.op('act', lambda e: e.activation(out=zrks[pz], in_=k.bank[5], func=AF.Copy, scale=128.0 ** -0.5), reads=['bank5'],
                 writes=['zrk%d' % pz])
            P.op('act', lambda e: e.copy(out=rvbs[pz], in_=k.bank[6]), reads=['bank6'], writes=['rvb%d' % pz])
            P.op('act', lambda e: e.activation(out=gsls[pz], in_=k.bank[7], func=AF.Silu), reads=['bank7'], writes=['gsl%d' % pz])

        def stage_r(i):
            pz = i % 2
            zq, zkv, zrq, zrk, rvb, gsl = zqs[pz], zkvs[pz], zrqs[pz], zrks[pz], rvbs[pz], gsls[pz]
            zqn, zkvn, zrqn, zrkn, rvbn, gsln = ['%s%d' % (n_, pz) for n_ in ('zq', 'zkv', 'zrq', 'zrk', 'rvb', 'gsl')]
            cs_ = csb[pz]
            csn = 'csb%d' % pz
            cn, sn = cs_[:, 0, 0:8], cs_[:, 1, 0:8]
            cr, sr = cs_[:, 0, 8:72], cs_[:, 1, 8:72]
            rope(zq, qb, 8, 64, 8, cn, sn, csn, zqn, 'qb')
            P.dma('pool', qd[i * 128:(i + 1) * 128, :], qb, reads=['qb'], writes=[('qd', i)], stream='xo')
            kv4 = zkv.rearrange('p (a b g d) -> p a b g d', a=3, b=2, g=2)[:, :, 0, :, :]
            x1, x2 = kv4[:, :, :, 0:8], kv4[:, :, :, 8:16]
            cb = cn.unsqueeze(1).unsqueeze(1).broadcast_to([128, 3, 2, 8])
            sb_ = sn.unsqueeze(1).unsqueeze(1).broadcast_to([128, 3, 2, 8])
            t = [r_[:, 0:48].rearrange('p (a g d) -> p a g d', a=3, g=2) for r_ in rt]
            P.op('dve', lambda e: e.tensor_tensor(out=t[0], in0=x1, in1=cb, op=ALU.mult), reads=[zkvn, csn], writes=['rt0'])
            P.op('dve', lambda e: e.tensor_tensor(out=t[1], in0=x2, in1=sb_, op=ALU.mult), reads=[zkvn, csn], writes=['rt1'])
            P.op('dve', lambda e: e.tensor_tensor(out=t[2], in0=x2, in1=cb, op=ALU.mult), reads=[zkvn, csn], writes=['rt2'])
            P.op('dve', lambda e: e.tensor_tensor(out=t[3], in0=x1, in1=sb_, op=ALU.mult), reads=[zkvn, csn], writes=['rt3'])
            P.op('dve', lambda e: e.tensor_tensor(out=x1, in0=t[0], in1=t[1], op=ALU.subtract), reads=['rt0', 'rt1'], writes=[zkvn])
            P.op('dve', lambda e: e.tensor_tensor(out=x2, in0=t[2], in1=t[3], op=ALU.add), reads=['rt2', 'rt3'], writes=[zkvn])
            P.op('pool', lambda e: e.tensor_copy(out=kvb, in_=zkv), reads=[zkvn], writes=['kvb'])
            P.op('pool', lambda e: e.tensor_copy(out=vs_aug[:, i, :, 0:64], in_=kvb[:, 384:512].rearrange('p (g d) -> p g d', g=2)),
                 reads=['kvb'], writes=['vs_aug'])
            P.op('pool', lambda e: e.tensor_copy(out=vw_aug[:, i, :, 0:64], in_=kvb[:, 640:768].rearrange('p (g d) -> p g d', g=2)),
                 reads=['kvb'], writes=['vw_aug'])
            tp = k.bankb[0]
            srcs = [0, 64, 128, 192, 512, 576, 256, 320]
            for j, c0 in enumerate(srcs):
                P.op('pe', lambda e, j=j, c0=c0: e.transpose(out=tp[0:64, j * 128:(j + 1) * 128], in_=kvb[:, c0:c0 + 64],
                                                            identity=k.ident), reads=['kvb', 'ident'], writes=['bank0'])
            P.op('dve', lambda e: e.tensor_copy(out=kcv_t, in_=tp[0:64, 0:512].rearrange('p (j t) -> p j t', j=4)), reads=['bank0'],
                 writes=['kcv_t'])
            P.dma('pool', kcvd[:, :, i * 128:(i + 1) * 128].rearrange('j p t -> p j t'), kcv_t, reads=['kcv_t'], writes=['kcvd'],
                  stream='xo')
            P.op('dve', lambda e: e.tensor_copy(out=kwT[:, :, i * 128:(i + 1) * 128],
                                         in_=tp[0:64, 512:768].rearrange('p (j t) -> p j t', j=2)), reads=['bank0'], writes=['kwT'])
            P.op('dve', lambda e: e.tensor_copy(out=KE[0:64, :, i * 128:(i + 1) * 128],
                                         in_=tp[0:64, 768:1024].rearrange('p (j t) -> p j t', j=2)), reads=['bank0'], writes=['KE'])
            rope(zrq, rqb, 4, 128, 64, cr, sr, csn, zrqn, 'rqb')
            rope(zrk, rkr, 4, 128, 64, cr, sr, csn, zrkn, 'rkr')
            P.op('pool', lambda e: e.tensor_copy(out=rkb, in_=rkr), reads=['rkr'], writes=['rkb'])
            P.op('dve', lambda e: e.tensor_tensor(out=rkz.rearrange('p (h d) -> p h d', h=4),
                                                  in0=rkr.rearrange('p (h d) -> p h d', h=4),
                                                  in1=zeta.unsqueeze(2).broadcast_to([128, 4, 128]), op=ALU.mult),
                 reads=['rkr', 'zeta'], writes=['rkz'])
            for h in range(4):
                P.op('pe', lambda e, h=h: e.transpose(out=tp[:, h * 128:(h + 1) * 128], in_=rqb[:, h * 128:(h + 1) * 128],
                                                      identity=k.ident), reads=['rqb', 'ident'], writes=['bank0'])
            for h in range(4):
                P.op('pe', lambda e, h=h: e.transpose(out=tp[:, (4 + h) * 128:(5 + h) * 128], in_=rkb[:, h * 128:(h + 1) * 128],
                                                      identity=k.ident), reads=['rkb', 'ident'], writes=['bank0'])
            P.op('dve', lambda e: e.tensor_copy(out=qkT, in_=tp.rearrange('p (c t) -> p c t', c=8)), reads=['bank0'], writes=['qkT'])
            for h in range(4):
                P.op('pe', lambda e, h=h: e.matmul(k.bank[5][:, h * 128:(h + 1) * 128], lhsT=qkT[:, 4 + h, :], rhs=qkT[:, h, :],
                                                  start=True, stop=True), reads=['qkT'], writes=['bank5'])
            P.op('dve', lambda e: e.tensor_tensor(out=STs, in0=k.bank[5], in1=decT, op=ALU.mult), reads=['bank5', 'decT'],
                 writes=['STs'])
            P.op('pool', lambda e: e.tensor_tensor(out=qxiT, in0=qkT[:, 0:4, :].rearrange('p h t -> p (h t)'), in1=xi, op=ALU.mult),
                 reads=['qkT', 'xi'], writes=['qxiT'])
            for h in range(4):
                hs = slice(h * 128, (h + 1) * 128)
                P.op('pe', lambda e, hs=hs: e.matmul(k.bank[7][:, hs], lhsT=STs[:, hs], rhs=rvb[:, hs], start=True, stop=(i == 0)),
                     reads=['STs', rvbn], writes=['bank7'])
                if i > 0:
                    P.op('pe', lambda e, hs=hs: e.matmul(k.bank[7][:, hs], lhsT=qxiT[:, hs], rhs=stbf[:, hs], start=False, stop=True),
                         reads=['qxiT', 'stbf'], writes=['bank7'])
            for h in range(4):
                hs = slice(h * 128, (h + 1) * 128)
                P.op('pe', lambda e, hs=hs: e.matmul(k.bank[6][:, hs], lhsT=rkz[:, hs], rhs=rvb[:, hs], start=True, stop=True),
                     reads=['rkz', rvbn], writes=['bank6'])
            P.op('dve', lambda e: e.tensor_tensor(out=state.rearrange('p (h d) -> p h d', h=4),
                                                  in0=state.rearrange('p (h d) -> p h d', h=4),
                                                  in1=gch.unsqueeze(2).broadcast_to([128, 4, 128]), op=ALU.mult),
                 reads=['state', 'gch'], writes=['state'])
            P.op('dve', lambda e: e.tensor_tensor(out=state, in0=k.bank[6], in1=state, op=ALU.add), reads=['bank6', 'state'],
                 writes=['state'])
            P.op('pool', lambda e: e.tensor_copy(out=stbf, in_=state), reads=['state'], writes=['stbf'])
            for h in range(4):
                P.op('dve', lambda e, h=h: e.bn_stats(out=bst4[:, h, :], in_=k.bank[7][:, h * 128:(h + 1) * 128]),
                     reads=['bank7'], writes=['bst4'])
            for h in range(4):
                P.op('dve', lambda e, h=h: e.bn_aggr(out=mv4[:, h, :], in_=bst4[:, h, :]), reads=['bst4'], writes=['mv4'])
            P.op('dve', lambda e: e.tensor_scalar(out=rs4, in0=mv4[:, :, 1], scalar1=1e-5, scalar2=None, op0=ALU.add),
                 reads=['mv4'], writes=['rs4'])
            P.op('pool', lambda e: e.tensor_tensor(out=rs4, in0=rs4, in1=k.mhalf, op=ALU.pow), reads=['rs4', 'mhalf'], writes=['rs4'])
            for h in range(4):
                hs = slice(h * 128, (h + 1) * 128)
                P.op('dve', lambda e, h=h, hs=hs: e.tensor_scalar(out=on[:, hs], in0=k.bank[7][:, hs], scalar1=mv4[:, h, 0:1],
                                                                  scalar2=rs4[:, h:h + 1], op0=ALU.subtract, op1=ALU.mult),
                     reads=['bank7', 'mv4', 'rs4'], writes=['on'])
            P.op('pool', lambda e: e.tensor_tensor(out=on, in0=on, in1=gng, op=ALU.mult), reads=['on', 'gng'], writes=['on'])
            P.op('pool', lambda e: e.tensor_tensor(out=ydb, in0=on, in1=gsl, op=ALU.mult), reads=['on', gsln], writes=['ydb'])
            P.dma('pool', ydd[i * 128:(i + 1) * 128, :], ydb, reads=['ydb'], writes=[('ydd', i)], stream='xo')

        stage_p(0)
        for i in range(NT):
            if i + 1 < NT:
                stage_p(i + 1)
            stage_r(i)

        P.barrier()
        s1.close()
        s1c = ExitStack()
        w1 = sbs(s1c, 'w1', [64, 32, 128], BF16)
        w2 = sbs(s1c, 'w2', [128, 64], BF16)
        posf32 = sbs(s1c, 'posf32', [64, 32], F32)
        posT = sbs(s1c, 'posT', [64, 32], BF16)
        cbias = sbs(s1c, 'cbias', [128, 1], F32)
        ghT = sbs(s1c, 'ghT', [128, 256], BF16)
        csrc = sbs(s1c, 'csrc', [64, T], BF16)
        for kind, (n1, n2, npos) in enumerate([('od_cmp_k_w1', 'od_cmp_k_w2', 'od_cmp_k_pos'),
                                               ('od_cmp_v_w1', 'od_cmp_v_w2', 'od_cmp_v_pos')]):
            P.dma('sp', w1, k.wb[(n1, o_)].rearrange('(l d) j -> d l j', d=64), reads=[('wb', n1, o_)], writes=['w1'], stream='w')
            P.dma('sp', w2, k.wb[(n2, o_)], reads=[('wb', n2, o_)], writes=['w2'], stream='w')
            with nc.allow_non_contiguous_dma(reason='tiny pos-emb transpose'):
                P.dma('sp', posf32, ins[npos][o_].rearrange('l d -> d l'), writes=['posf32'], stream='small')
            P.op('dve', lambda e: e.tensor_copy(out=posT, in_=posf32), reads=['posf32'], writes=['posT'])
            for g in range(2):
                P.dma('sp', csrc, kcvd[2 * kind + g], reads=['kcvd'], writes=['csrc'], stream='w')
                srcv = csrc.rearrange('p (n s) -> p n s', s=16)
                hb_ = k.bank[1]
                for l_ in range(32):
                    P.op('pe', lambda e, l_=l_: e.matmul(hb_[:, 0:255], lhsT=w1[:, l_, :],
                                                        rhs=(srcv[:, 0:255, l_] if l_ < 16 else srcv[:, 1:256, l_ - 16]),
                                                        start=(l_ == 0), stop=(l_ == 31)),
                         reads=['w1', 'csrc'], writes=['bank1'])
                for l_ in range(32):
                    P.op('pe', lambda e, l_=l_: e.matmul(hb_[:, 256:257], lhsT=w1[:, l_, :], rhs=posT[:, l_:l_ + 1],
                                                        start=(l_ == 0), stop=(l_ == 31)), reads=['w1', 'posT'], writes=['bank1'])
                P.op('dve', lambda e: e.tensor_copy(out=cbias, in_=hb_[:, 256:257]), reads=['bank1'], writes=['cbias'])
                P.op('dve', lambda e: e.memset(ghT[:, 255:256], 0.0), writes=['ghT'])
                P.op('act', lambda e: e.activation(out=ghT[:, 0:255], in_=hb_[:, 0:255], func=AF.Gelu_apprx_tanh,
                                                   bias=cbias[:, 0:1]), reads=['bank1', 'cbias'], writes=['ghT'])
                if kind == 0:
                    P.op('pe', lambda e: e.matmul(k.bank[2][0:64, 0:256], lhsT=w2, rhs=ghT, start=True, stop=True),
                         reads=['w2', 'ghT'], writes=['bank2'])
                    P.op('act', lambda e, g=g: e.copy(out=kcmpT[:, g, :], in_=k.bank[2][0:64, 0:256]), reads=['bank2'],
                         writes=['kcmpT'])
                else:
                    for c in range(2):
                        P.op('pe', lambda e, c=c: e.matmul(k.bank[2][:, c * 64:(c + 1) * 64], lhsT=ghT[:, c * 128:(c + 1) * 128],
                                                          rhs=w2, start=True, stop=True), reads=['w2', 'ghT'], writes=['bank2'])
                    P.op('act', lambda e, g=g: e.copy(out=vcmp_aug[:, :, g, 0:64],
                                                      in_=k.bank[2][:, 0:128].rearrange('p (c d) -> p c d', c=2)),
                         reads=['bank2'], writes=['vcmp'])
        P.barrier()
        s1c.close()

    with ExitStack() as s2:
        w_out_sb = sbs(s2, 'w_out', [128, 8, 1024], BF16)
        PT = sbs(s2, 'PT', [128, NT, 512], BF16)
        PW = sbs(s2, 'PW', [128, 5, 512], BF16)
        Pc = sbs(s2, 'Pc', [128, 2, 512], BF16)
        QN = [sbs(s2, 'QN%d' % g, [128, 512], BF16) for g in range(2)]
        qt = [sbs(s2, 'qt%d' % j, [128, 512], BF16) for j in range(2)]
        ydts = [sbs(s2, 'ydt%d' % j, [128, 512], BF16) for j in range(2)]
        negt = sbs(s2, 'negt', [128, 128], BF16)
        expf = sbs(s2, 'expf', [128, 512], F32)
        score = sbs(s2, 'score', [128, 64], F32)
        sc2 = sbs(s2, 'sc2', [128, 64], F32)
        m8a = sbs(s2, 'm8a', [128, 8], F32)
        m8b = sbs(s2, 'm8b', [128, 8], F32)
        thr = sbs(s2, 'thr', [128, 1], F32)
        psl = sbs(s2, 'psl', [128, 64], F32)
        rdenA = [sbs(s2, 'rdenA%d' % g, [128, 4], F32) for g in range(2)]
        rdenB = [sbs(s2, 'rdenB%d' % g, [128, 12], F32) for g in range(2)]
        coefs = [sbs(s2, 'coef%d' % g, [128, 12], F32) for g in range(2)]
        ocw = [sbs(s2, 'ocw%d' % g, [128, 2, 260], F32) for g in range(2)]
        yc = sbs(s2, 'yc', [128, 512], F32)
        ycb = sbs(s2, 'ycb', [128, 512], BF16)
        yT = sbs(s2, 'yT', [128, 8, 128], BF16)
        P.dma('sp', w_out_sb, k.wb[('od_w_out', o_)].rearrange('(c p) n -> p c n', p=128), reads=[('wb', 'od_w_out', o_)],
              writes=['w_out'], stream='w')
        P.op('dve', lambda e: e.memset(negt, 0.0), writes=['negt'])
        tp = k.bankb[0]
        sbank = [0]

        def next_sbank():
            sbank[0] += 1
            return (1, 2, 7)[sbank[0] % 3]

        def den_view(ap260):
            return ap260.rearrange('p (m e) -> p m e', e=65)[:, :, 64]

        def masked_exp(b, nn, dst, mask_ap, dname):
            if mask_ap is None:
                P.op('act', lambda e: e.activation(out=dst[0:nn], in_=k.bank[b][0:nn, :], func=AF.Exp), reads=['bank%d' % b],
                     writes=[dname])
            else:
                P.op('act', lambda e: e.activation(out=expf[0:nn], in_=k.bank[b][0:nn, :], func=AF.Exp), reads=['bank%d' % b],
                     writes=['expf'])
                P.op('pool', lambda e: e.tensor_tensor(out=dst[0:nn].rearrange('p (m q) -> p m q', m=4),
                                                       in0=expf[0:nn].rearrange('p (m q) -> p m q', m=4),
                                                       in1=mask_ap.unsqueeze(1).broadcast_to([nn, 4, 128]), op=ALU.mult),
                     reads=['expf', 'tri', 'ntri'], writes=[dname])

        def o2_loads(i):
            P.dma('sp', k.xt[i % 3], xsrc[i * 128:(i + 1) * 128, :], reads=[('x', i)], writes=['xt%d' % (i % 3)], stream='x')
            P.dma('sp', qt[i % 2], qd[i * 128:(i + 1) * 128, :], reads=[('qd', i)], writes=['qt%d' % (i % 2)], stream='x')
            P.dma('sp', ydts[i % 2], ydd[i * 128:(i + 1) * 128, :], reads=[('ydd', i)], writes=['ydt%d' % (i % 2)], stream='x')

        def stage_a(i, g):
            qti = qt[i % 2]
            qtn = 'qt%d' % (i % 2)
            off = 62 - 2 * i
            Q = QN[g]
            qn = 'QN%d' % g
            rdA = rdenA[g]
            rdAn = 'rdenA%d' % g
            for m in range(4):
                h = 4 * g + m
                P.op('pe', lambda e, m=m, h=h: e.transpose(out=tp[0:64, m * 128:(m + 1) * 128], in_=qti[:, h * 64:(h + 1) * 64],
                                                          identity=k.ident), reads=[qtn, 'ident'], writes=['bank0'])
            P.op('act', lambda e: e.copy(out=Q[0:64, :], in_=tp[0:64, 0:512]), reads=['bank0'], writes=[qn + 'lo'])
            chunks = [(0, 128)] + ([(1, 127)] if 8 * i + 6 >= 128 else [])
            for (c, nn) in chunks:
                b = next_sbank()
                P.op('pe', lambda e, c=c, nn=nn, b=b: e.matmul(k.bank[b][0:nn, :], lhsT=kcmpT[:, g, c * 128:c * 128 + nn],
                                                              rhs=Q[0:64, :], start=True, stop=True),
                     reads=['kcmpT', qn + 'lo'], writes=['bank%d' % b])
                full = (16 * (c * 128 + nn - 1) + 31 <= 128 * i)
                if full:
                    P.op('act', lambda e, c=c, nn=nn, b=b: e.activation(out=Pc[0:nn, c, :], in_=k.bank[b][0:nn, :], func=AF.Exp),
                         reads=['bank%d' % b], writes=['Pc'])
                else:
                    tv = float(128 * i - 31 - 2048 * c)
                    P.op('act', lambda e, nn=nn, b=b: e.activation(out=expf[0:nn], in_=k.bank[b][0:nn, :], func=AF.Exp),
                         reads=['bank%d' % b], writes=['expf'])
                    P.op('dve', lambda e, c=c, nn=nn, tv=tv: e.scalar_tensor_tensor(
                        out=Pc[0:nn, c, :].rearrange('p (m q) -> p m q', m=4),
                        in0=cm[0:nn].unsqueeze(1).broadcast_to([nn, 4, 128]), scalar=tv,
                        in1=expf[0:nn].rearrange('p (m q) -> p m q', m=4), op0=ALU.is_le, op1=ALU.mult),
                         reads=['expf', 'cm'], writes=['Pc'])
            for m in range(4):
                for ci, (c, nn) in enumerate(chunks):
                    P.op('pe', lambda e, m=m, c=c, nn=nn, ci=ci: e.matmul(k.bank[3][:, m * 65:(m + 1) * 65],
                                                                        lhsT=Pc[0:nn, c, m * 128:(m + 1) * 128],
                                                                        rhs=vcmp_aug[0:nn, c, g, :], start=(ci == 0),
                                                                        stop=(ci == len(chunks) - 1)),
                         reads=['Pc', 'vcmp'], writes=['bank3'])
            for m in range(4):
                for ci, (c, nn) in enumerate(chunks):
                    P.op('pe', lambda e, m=m, c=c, nn=nn, ci=ci: e.matmul(k.bank[4][:, m * 64:(m + 1) * 64],
                                                                        lhsT=Pc[0:nn, c, m * 128:(m + 1) * 128],
                                                                        rhs=cts[0:nn, c, :], start=(ci == 0),
                                                                        stop=(ci == len(chunks) - 1)),
                         reads=['Pc', 'cts'], writes=['bank4'])
            jts = list(range(max(0, i - 4), i + 1))
            for sl_, jt in enumerate(jts):
                b = next_sbank()
                P.op('pe', lambda e, jt=jt, b=b: e.matmul(k.bank[b], lhsT=kwT[:, g, jt * 128:(jt + 1) * 128], rhs=Q[0:64, :],
                                                         start=True, stop=True), reads=['kwT', qn + 'lo'], writes=['bank%d' % b])
                mk = tri if jt == i else (ntri if jt == i - 4 else None)
                masked_exp(b, 128, PW[:, sl_, :], mk, ('PW', sl_))
            for m in range(4):
                for sl_, jt in enumerate(jts):
                    P.op('pe', lambda e, m=m, jt=jt, sl_=sl_: e.matmul(k.bank[6][:, m * 65:(m + 1) * 65],
                                                                      lhsT=PW[:, sl_, m * 128:(m + 1) * 128],
                                                                      rhs=vw_aug[:, jt, g, :], start=(sl_ == 0),
                                                                      stop=(sl_ == len(jts) - 1)),
                         reads=[('PW', sl_), 'vw_aug'], writes=['bank6'])
            P.op('dve', lambda e: e.tensor_scalar(out=rdA, in0=den_view(k.bank[3][:, 0:260]), scalar1=1e-30, scalar2=None,
                                                  op0=ALU.max), reads=['bank3'], writes=[rdAn])
            P.op('dve', lambda e: e.reciprocal(out=rdA, in_=rdA), reads=[rdAn], writes=[rdAn])
            P.op('dve', lambda e: e.tensor_scalar(out=psl, in0=k.bank[4][:, 0:64], scalar1=rdA[:, 0:1], scalar2=None,
                                                  op0=ALU.mult), reads=['bank4', rdAn], writes=['psl'])
            for m in range(1, 4):
                P.op('dve', lambda e, m=m: e.scalar_tensor_tensor(out=psl, in0=k.bank[4][:, m * 64:(m + 1) * 64],
                                                                  scalar=rdA[:, m:m + 1], in1=psl, op0=ALU.mult, op1=ALU.add),
                     reads=['bank4', rdAn, 'psl'], writes=['psl'])
            P.op('act', lambda e: e.copy(out=ocw[g][:, 0, :], in_=k.bank[3][:, 0:260]), reads=['bank3'], writes=['ocw%d' % g])
            P.op('act', lambda e: e.copy(out=ocw[g][:, 1, :], in_=k.bank[6][:, 0:260]), reads=['bank6'], writes=['ocw%d' % g])
            P.op('dve', lambda e: e.tensor_tensor(out=score, in0=psl, in1=keep[:, off:off + 64], op=ALU.mult),
                 reads=['psl', 'keep'], writes=['score'])
            P.op('dve', lambda e: e.tensor_tensor(out=score, in0=score, in1=addc[:, off:off + 64], op=ALU.add),
                 reads=['score', 'addc'], writes=['score'])
            P.op('dve', lambda e: e.memset(score[:, 0:1], 1.0e4), reads=['score'], writes=['score'])
            P.op('dve', lambda e: e.max(out=m8a, in_=score), reads=['score'], writes=['m8a'])
            P.op('dve', lambda e: e.match_replace(out=sc2, in_to_replace=m8a, in_values=score, imm_value=-2.0),
                 reads=['score', 'm8a'], writes=['sc2'])
            P.op('dve', lambda e: e.max(out=m8b, in_=sc2), reads=['sc2'], writes=['m8b'])
            P.op('dve', lambda e: e.tensor_scalar(out=thr, in0=m8b[:, 7:8], scalar1=0.0, scalar2=None, op0=ALU.max),
                 reads=['m8b'], writes=['thr'])
            P.op('dve', lambda e: e.tensor_scalar(out=negt[:, 64:128], in0=score, scalar1=thr[:, 0:1], scalar2=-30000.0,
                                                  op0=ALU.is_lt, op1=ALU.mult), reads=['score', 'thr'], writes=['negt'])
            P.op('pe', lambda e: e.transpose(out=tp[:, 512:640], in_=negt, identity=k.ident), reads=['negt', 'ident'],
                 writes=['bank0'])
            P.op('act', lambda e: e.copy(out=Q[64:128, :].rearrange('p (m q) -> p m q', m=4),
                                         in_=tp[64:128, 512:640].unsqueeze(1).broadcast_to([64, 4, 128])),
                 reads=['bank0'], writes=[qn + 'hi'])

        def stage_b(i, g):
            Q = QN[g]
            qn = 'QN%d' % g
            ob = 5
            obn = 'bank%d' % ob
            rdB = rdenB[g]
            rdBn = 'rdenB%d' % g
            coef = coefs[g]
            cfn = 'coef%d' % g
            for jt in range(i + 1):
                b = next_sbank()
                P.op('pe', lambda e, jt=jt, b=b: e.matmul(k.bank[b], lhsT=KE[:, g, jt * 128:(jt + 1) * 128], rhs=Q, start=True,
                                                         stop=True), reads=['KE', qn + 'lo', qn + 'hi'], writes=['bank%d' % b])
                masked_exp(b, 128, PT[:, jt, :], tri if jt == i else None, ('PT', jt))
            for m in range(4):
                for jt in range(i + 1):
                    P.op('pe', lambda e, m=m, jt=jt: e.matmul(k.bank[ob][:, m * 65:(m + 1) * 65],
                                                             lhsT=PT[:, jt, m * 128:(m + 1) * 128], rhs=vs_aug[:, jt, g, :],
                                                             start=(jt == 0), stop=(jt == i)),
                         reads=[('PT', jt), 'vs_aug'], writes=[obn])
            P.op('dve', lambda e: e.tensor_copy(out=rdB[:, 0:4], in_=rdenA[g]), reads=['rdenA%d' % g], writes=[rdBn])
            P.op('dve', lambda e: e.tensor_scalar(out=rdB[:, 4:8], in0=den_view(k.bank[ob][:, 0:260]), scalar1=1e-30, scalar2=None,
                                                  op0=ALU.max), reads=[obn], writes=[rdBn])
            P.op('dve', lambda e: e.tensor_scalar(out=rdB[:, 8:12], in0=den_view(ocw[g][:, 1, :]), scalar1=1e-30, scalar2=None,
                                                  op0=ALU.max), reads=['ocw%d' % g], writes=[rdBn])
            P.op('dve', lambda e: e.reciprocal(out=rdB[:, 4:12], in_=rdB[:, 4:12]), reads=[rdBn], writes=[rdBn])
            P.op('dve', lambda e: e.tensor_tensor(out=coef.rearrange('p (b m) -> p b m', b=3),
                                                  in0=rdB.rearrange('p (b m) -> p b m', b=3),
                                                  in1=gsig[:, i, g * 12:(g + 1) * 12].rearrange('p (m b) -> p b m', b=3),
                                                  op=ALU.mult), reads=[rdBn, ('gsig', i)], writes=[cfn])
            for m in range(4):
                h = 4 * g + m
                ym = yc[:, h * 64:(h + 1) * 64]
                P.op('pool', lambda e, m=m, ym=ym: e.tensor_scalar(out=ym, in0=ocw[g][:, 0, m * 65:m * 65 + 64],
                                                                   scalar1=coef[:, m:m + 1], scalar2=0.0, op0=ALU.mult, op1=ALU.add),
                     reads=['ocw%d' % g, cfn], writes=[('yc', h)])
                P.op('dve', lambda e, m=m, ym=ym: e.scalar_tensor_tensor(out=ym, in0=k.bank[ob][:, m * 65:m * 65 + 64],
                                                                         scalar=coef[:, 4 + m:5 + m], in1=ym, op0=ALU.mult,
                                                                         op1=ALU.add), reads=[obn, cfn, ('yc', h)],
                     writes=[('yc', h)])
                P.op('dve', lambda e, m=m, ym=ym: e.scalar_tensor_tensor(out=ym, in0=ocw[g][:, 1, m * 65:m * 65 + 64],
                                                                         scalar=coef[:, 8 + m:9 + m], in1=ym, op0=ALU.mult,
                                                                         op1=ALU.add), reads=['ocw%d' % g, cfn, ('yc', h)],
                     writes=[('yc', h)])

        def out_proj(i):
            xt = k.xt[i % 3]
            xr = 'xt%d' % (i % 3)
            ydt = ydts[i % 2]
            ydn = 'ydt%d' % (i % 2)
            P.op('act', lambda e: e.copy(out=ycb, in_=yc), reads=[('yc', h) for h in range(8)], writes=['ycb'])
            for c in range(4):
                P.op('pe', lambda e, c=c: e.transpose(out=tp[:, c * 128:(c + 1) * 128], in_=ycb[:, c * 128:(c + 1) * 128],
                                                      identity=k.ident), reads=['ycb', 'ident'], writes=['bank0'])
            for c in range(4):
                P.op('pe', lambda e, c=c: e.transpose(out=tp[:, (4 + c) * 128:(5 + c) * 128], in_=ydt[:, c * 128:(c + 1) * 128],
                                                      identity=k.ident), reads=[ydn, 'ident'], writes=['bank0'])
            P.op('act', lambda e: e.copy(out=yT, in_=tp.rearrange('p (c t) -> p c t', c=8)), reads=['bank0'], writes=['yT'])
            pm = [k.bank[3], k.bank[4]]
            for cb in range(2):
                for kc in range(8):
                    P.op('pe', lambda e, kc=kc, cb=cb: e.matmul(pm[cb], lhsT=yT[:, kc, :], rhs=w_out_sb[:, kc, cb * 512:(cb + 1) * 512],
                                                               start=(kc == 0), stop=(kc == 7)), reads=['yT', 'w_out'],
                         writes=['bank%d' % (3 + cb)])
            post_norm_residual(k, pm, ['bank3', 'bank4'], 'mix_post', xt, xr)
            P.dma('sp', xdst[i * 128:(i + 1) * 128, :], xt, reads=[xr], writes=[('x', i)], stream='xo')

        units = [(i, g) for i in range(NT) for g in range(2)]
        o2_loads(0)
        stage_a(*units[0])
        for n, (i, g) in enumerate(units):
            if n + 1 < len(units):
                ni, ng = units[n + 1]
                if ng == 0:
                    o2_loads(ni)
                stage_a(ni, ng)
            stage_b(i, g)
            if g == 1:
                out_proj(i)

def make_consts():
    c = {}
    Dm = np.zeros((3, 4, 128, 128), np.float32)
    for g, w in enumerate(POOL_WINDOWS):
        for t in range(128):
            lo = max(t + 1 - w, 0)
            for s in range(lo, t + 1):
                Dm[0, g, s, t] += 1.0 / (t + 1 - lo)
            Dm[0, g, t, t] -= 1.0
            for s in range(t + 1 - w, t + 1):
                if s >= 0:
                    Dm[1, g, s, t] += 1.0 / w
                else:
                    Dm[2, g, s + 128, t] += 1.0 / w
            Dm[1, g, t, t] -= 1.0
    c['c_D'] = Dm.astype(ml_dtypes.bfloat16)
    bf = ml_dtypes.bfloat16
    invf = np.concatenate([1.0 / (500000.0 ** (np.arange(0, 16, 2, dtype=np.float32) / 16)),
                           1.0 / (10000.0 ** (np.arange(0, 128, 2, dtype=np.float32) / 128))]).astype(np.float32)
    c['c_invf'] = invf.reshape(1, 72)
    lg = np.log1p(-np.exp2(-5.0 - np.arange(4, dtype=np.float64)))
    idx = np.arange(128, dtype=np.float64)
    rel = idx[None, :] - idx[:, None]
    dec = np.where((rel >= 0)[:, None, :], np.exp(np.maximum(rel, 0)[:, None, :] * lg[None, :, None]), 0.0)
    c['c_decT'] = dec.reshape(128, 512).astype(np.float32)
    xi = np.exp((idx + 1.0)[None, :] * lg[:, None])
    c['c_xi'] = np.broadcast_to(xi.reshape(1, 512), (128, 512)).astype(np.float32).copy()
    c['c_zeta'] = np.exp((127 - idx)[:, None] * lg[None, :]).astype(np.float32)
    c['c_gch'] = np.broadcast_to(np.exp(128 * lg)[None, :], (128, 4)).astype(np.float32).copy()
    p = np.arange(128)
    c['c_tri'] = (p[:, None] <= p[None, :]).astype(np.float32)
    c['c_cm'] = (16.0 * p[:, None] - p[None, :]).astype(np.float32)
    cq = (p >= 64).astype(np.int64)[:, None]
    jj = np.arange(128)[None, :]
    c['c_keep'] = (jj <= 60 + cq).astype(np.float32)
    add = np.zeros((128, 128), np.float32)
    add[(jj == 61 + cq) | (jj == 62 + cq)] = 1.0e4
    add[jj > 62 + cq] = -1.0
    c['c_add'] = add
    n = np.arange(256)
    cs = n * 16
    ss = np.arange(64) * 64
    cts = ((cs[:, None] < ss[None, :] + 64) & (cs[:, None] + 32 > ss[None, :])).astype(np.float32)
    cts[255] = 0
    c['c_cts'] = cts.reshape(2, 128, 64).astype(bf)
    c['c_E'] = (np.arange(4096)[None, :] // 64 == np.arange(64)[:, None]).astype(bf)
    return c


INPUT_NAMES = ["x", "positions", "ln_mix_pre", "ln_mix_post", "ln_ffn_pre", "ln_ffn_post", "ffn_w_gate", "ffn_w_up",
               "ffn_w_down", "ev_w_in", "ev_pool_w", "ev_pool_scale", "ev_sgu_ln_g", "ev_sgu_ln_b", "ev_sgu_w",
               "ev_sgu_b", "ev_w_out", "od_w_in", "od_cmp_k_pos", "od_cmp_k_w1", "od_cmp_k_w2", "od_cmp_v_pos",
               "od_cmp_v_w1", "od_cmp_v_w2", "od_ret_gn_g", "od_w_out"]


def build(shapes, consts, layers=(0, 1, 2, 3), phases=('mix', 'ffn')):
    from contextlib import ExitStack
    nc = bass.Bass("TRN2", target_bir_lowering=False)
    ins = {}
    for n in INPUT_NAMES:
        shp = list(shapes[n])
        if n == 'x':
            shp = [T, D]
        if n == 'positions':
            shp = [1, T]
        ins[n] = nc.dram_tensor(n, shp, I32 if n == 'positions' else F32, kind="ExternalInput").ap()
    for n, v in consts.items():
        ins[n] = nc.dram_tensor(n, list(v.shape), BF16 if v.dtype == ml_dtypes.bfloat16 else F32, kind="ExternalInput").ap()
    y = nc.dram_tensor("y", [T, D], F32, kind="ExternalOutput").ap()
    k = K(nc, layers)
    setup_common(k, ins)
    k.sb2 = k.sb('ssa', [128, 2], F32)
    P = k.P
    xsrc = ins['x']
    for li, l in enumerate(layers):
        load_gains(k, l)
        if 'mix' in phases:
            with ExitStack() as es:
                if l % 2 == 0:
                    even_phase(k, l, xsrc, y, es)
                else:
                    odd_phase(k, l, xsrc, y, es)
                P.barrier()
            xsrc = y
        if li + 1 < len(layers):
            cast_layer(k, layers[li + 1])
        if 'ffn' in phases:
            with ExitStack() as es:
                def sb(name, shape, dt):
                    return es.enter_context(nc.sbuf_tensor('ffs%d_' % l + name, list(shape), dt)).ap()
                k.wd_sb = sb('wd', [128, NFC, 1024], BF16)
                k.xt8 = [sb('xt8_%d' % i, [128, D], F32) for i in range(8)]
                k.wgu = [sb('wgu%d' % i, [128, 2, 8, 512], BF16) for i in range(2)]
                k.hT2 = [sb('hT%d' % i, [128, 8, 512], BF16) for i in range(2)]
                k.actT = sb('actT', [128, NFC, 512], BF16)
                k.sg = [sb('sg%d' % i, [128, 512], F32) for i in range(2)]
                ffn_phase(k, l, xsrc, y)
                P.barrier()
            xsrc = y
    P.finish()
    return nc


_CACHE = {}


def kernel(**inputs):
    consts = make_consts()
    shapes = {n: inputs[n].shape for n in INPUT_NAMES}
    if 'nc' not in _CACHE:
        _CACHE['nc'] = build(shapes, consts)
    nc = _CACHE['nc']
    in_maps = []
    for c in range(4):
        b = c % 4
        m = {n: np.ascontiguousarray(inputs[n]) for n in INPUT_NAMES if n not in ('x', 'positions')}
        m['x'] = np.ascontiguousarray(inputs['x'][b])
        m['positions'] = np.ascontiguousarray(inputs['positions'][b:b + 1]).astype(np.int32)
        m.update(consts)
        in_maps.append(m)
    res = run_bass_kernel_spmd(nc, in_maps, core_ids=list(range(4)))
    out = np.stack([res.results[b]["y"] for b in range(4)], axis=0)
    return out.astype(np.float32)
```

```python
import numpy as np
import ml_dtypes
import concourse.bass as bass
import concourse.mybir as mybir
from concourse.bass_utils import run_bass_kernel_spmd

F32 = mybir.dt.float32
BF16 = mybir.dt.bfloat16
I32 = mybir.dt.int32
AF = mybir.ActivationFunctionType
ALU = mybir.AluOpType
AX = mybir.AxisListType

import os
SAME_ENG_SYNC = os.environ.get("SES", "1") == "1"


class Prog:
    def __init__(self, nc):
        self.nc = nc
        self.E = {'pe': nc.tensor, 'dve': nc.vector, 'act': nc.scalar, 'pool': nc.gpsimd, 'sp': nc.sync}
        self.semh = {k: nc.alloc_semaphore('s_' + k) for k in ['pe', 'dve', 'act', 'pool']}
        self.cnt = {k: 0 for k in self.semh}
        self.seen = {e: {} for e in self.E}
        self.lastw = {}
        self.readers = {}
        self.nwait = 0
        self.dslot = 0
        self.NSLOT = 32
        self.nins = 0

    def _deps(self, reads, writes):
        deps = {}
        for r in reads:
            w = self.lastw.get(r)
            if w and deps.get(w[0], 0) < w[1]:
                deps[w[0]] = w[1]
            if isinstance(r, str) and r.startswith('bank'):
                for k_, v in self.readers.get(r, {}).items():
                    if deps.get(k_, 0) < v:
                        deps[k_] = v
        for w_ in writes:
            w = self.lastw.get(w_)
            if w and deps.get(w[0], 0) < w[1]:
                deps[w[0]] = w[1]
            for k, v in self.readers.get(w_, {}).items():
                if deps.get(k, 0) < v:
                    deps[k] = v
        return deps

    def _wait(self, eng, deps):
        e = self.E[eng]
        seen = self.seen[eng]
        for k, v in deps.items():
            if k == eng and (eng == 'pe' or not SAME_ENG_SYNC):
                continue
            if seen.get(k, 0) >= v:
                continue
            e.wait_ge(self.semh[k], v)
            seen[k] = v
            self.nwait += 1

    def _record(self, tag, reads, writes):
        for w in writes:
            self.lastw[w] = tag
            self.readers[w] = {}
        for r in reads:
            d = self.readers.setdefault(r, {})
            if d.get(tag[0], 0) < tag[1]:
                d[tag[0]] = tag[1]

    def op(self, eng, fn, reads=(), writes=()):
        self._wait(eng, self._deps(reads, writes))
        ins = fn(self.E[eng])
        self.cnt[eng] += 1
        ins.then_inc(self.semh[eng], 1)
        self.nins += 1
        self._record((eng, self.cnt[eng]), reads, writes)

    def dma(self, q, out, in_, reads=(), writes=(), stream='d0', **kw):
        slot = self.dslot % self.NSLOT
        self.dslot += 1
        key = 'd:%d' % slot
        if key not in self.semh:
            self.semh[key] = self.nc.alloc_semaphore('sd_%d' % slot)
            self.cnt[key] = 0
        e = self.E[q]
        if self.cnt[key] > 0 and self.seen[q].get(key, 0) < self.cnt[key]:
            e.wait_ge(self.semh[key], self.cnt[key])
            self.seen[q][key] = self.cnt[key]
        self._wait(q, self._deps(reads, writes))
        e.dma_start(out=out, in_=in_, **kw).then_inc(self.semh[key], 16)
        self.cnt[key] += 16
        self.nins += 1
        self._record((key, self.cnt[key]), reads, writes)

    def barrier(self):
        for eng in self.E:
            deps = {k: v for k, v in self.cnt.items() if v > 0}
            e = self.E[eng]
            for k, v in deps.items():
                if k == eng and eng == 'pe':
                    continue
                if self.seen[eng].get(k, 0) >= v:
                    continue
                e.wait_ge(self.semh[k], v)
                self.seen[eng][k] = v
        self.lastw = {}
        self.readers = {}

    def finish(self, eng='sp'):
        deps = {k: v for k, v in self.cnt.items() if v > 0}
        e = self.E[eng]
        for k, v in deps.items():
            e.wait_ge(self.semh[k], v)


T = 4096
D = 1024
NT = T // 128
FH = 2816
NFC = FH // 128
DEPTH = 4
POOL_WINDOWS = (2, 4, 8, 16)
EPS = 1e-6


def _flat2(ap, c=1024):
    n = len(ap.shape)
    names = ' '.join('d%d' % i for i in range(n))
    f = ap.rearrange('%s -> (%s)' % (names, names)) if n > 1 else ap
    return f.rearrange('(r c) -> r c', c=c)


class K:
    def __init__(self, nc, layers=(0, 1, 2, 3), Tn=T):
        self.nc = nc
        self.P = Prog(nc)
        self.layers = layers
        self.uid = 0

    def sb(self, name, shape, dt):
        return self.nc.alloc_sbuf_tensor(name, list(shape), dt).ap()

    def dram(self, name, shape, dt, kind="Internal"):
        return self.nc.dram_tensor(name, list(shape), dt, kind=kind).ap()


def cast_copy(k, dst, src, res):
    d2 = _flat2(dst)
    s2 = _flat2(src)
    rows = d2.shape[0]
    r0 = 0
    while r0 < rows:
        r1 = min(rows, r0 + 2048)
        k.P.dma('pool', d2[r0:r1, :], s2[r0:r1, :], writes=[res], stream='cast')
        r0 = r1


def cast_layer(k, l):
    ins = k.ins
    order = []
    if l % 2 == 0:
        order += [('ev_w_in', l // 2), ('ev_pool_w', l // 2), ('ev_w_out', l // 2)]
    else:
        order += [('od_w_in', l // 2), ('od_cmp_k_w1', l // 2), ('od_cmp_k_w2', l // 2), ('od_cmp_v_w1', l // 2),
                  ('od_cmp_v_w2', l // 2), ('od_w_out', l // 2)]
    order += [('ffn_w_gate', l), ('ffn_w_up', l), ('ffn_w_down', l)]
    for name, idx in order:
        src = ins[name][idx]
        dst = k.dram('wb_%s_%d' % (name, idx), src.shape, BF16)
        k.wb[(name, idx)] = dst
        cast_copy(k, dst, src, ('wb', name, idx))


def rstd_from_ssq(k, ssq, rstd, tmp, n, eps, tag):
    P = k.P
    P.op('dve', lambda e: e.tensor_scalar(out=tmp, in0=ssq, scalar1=1.0 / n, scalar2=eps, op0=ALU.mult, op1=ALU.add),
         reads=[tag + 'ssq'], writes=[tag + 'tmp'])
    P.op('pool', lambda e: e.tensor_tensor(out=rstd, in0=tmp, in1=k.mhalf[:, 0:1], op=ALU.pow), reads=[tag + 'tmp', 'mhalf'],
         writes=[tag + 'rstd'])


def setup_common(k, ins):
    nc, P = k.nc, k.P
    k.ins = ins
    k.bank = [nc.alloc_psum_tensor('bank%d' % i, [128, 512], F32).ap() for i in range(8)]
    k.bankb = [b.bitcast(BF16) for b in k.bank]
    k.io_f = k.sb('io_f', [128, 128], F32)
    k.iop = k.sb('iop', [128, 1], F32)
    k.ident = k.sb('ident', [128, 128], BF16)
    k.ones_row = k.sb('ones_row', [1, 128], BF16)
    P.op('pool', lambda e: e.iota(k.io_f, pattern=[[1, 128]], base=0, channel_multiplier=0,
                                  allow_small_or_imprecise_dtypes=True), writes=['io_f'])
    P.op('pool', lambda e: e.iota(k.iop, pattern=[[1, 1]], base=0, channel_multiplier=1,
                                  allow_small_or_imprecise_dtypes=True), writes=['iop'])
    P.op('dve', lambda e: e.tensor_scalar(out=k.ident, in0=k.io_f, scalar1=k.iop[:, 0:1], scalar2=None,
                                          op0=ALU.is_equal), reads=['io_f', 'iop'], writes=['ident'])
    P.op('dve', lambda e: e.memset(k.ones_row, 1.0), writes=['ones_row'])
    k.mhalf = k.sb('mhalf', [128, 4], F32)
    P.op('pool', lambda e: e.memset(k.mhalf, -0.5), writes=['mhalf'])
    k.wb = {}
    cast_layer(k, k.layers[0])
    k.xt = [k.sb('xt%d' % s, [128, D], F32) for s in range(3)]
    k.hbs = [k.sb('hb%d' % i, [128, D], BF16) for i in range(2)]
    k.junk = k.sb('junk', [128, D], BF16)
    k.tmpf = k.sb('tmpf', [128, D], F32)
    k.G = {n: k.sb('G_' + n, [128, D], F32) for n in ['mix_pre', 'mix_post', 'ffn_pre', 'ffn_post']}
    k.st = {n: k.sb('st_' + n, [128, 1], F32) for n in ['ssq', 'tmp', 'rstd', 'ssq2', 'tmp2', 'rstd2']}


def load_gains(k, l):
    for n in ['mix_pre', 'mix_post', 'ffn_pre', 'ffn_post']:
        k.P.dma('sp', k.G[n], k.ins['ln_' + n][l:l + 1, :].broadcast_to([128, D]), writes=['G_' + n], stream='small')


def norm_a(k, xt_ap, xres, gname, hbi):
    P = k.P
    st = k.st
    hb = k.hbs[hbi]
    P.op('act', lambda e: e.activation(out=k.junk, in_=xt_ap, func=AF.Square, accum_out=st['ssq']),
         reads=[xres], writes=['junk', 'ssq'])
    rstd_from_ssq(k, st['ssq'], st['rstd'], st['tmp'], D, EPS, '')
    P.op('dve', lambda e: e.scalar_tensor_tensor(out=hb, in0=xt_ap, scalar=st['rstd'][:, 0:1], in1=k.G[gname],
                                                 op0=ALU.mult, op1=ALU.mult),
         reads=[xres, 'rstd', 'G_' + gname], writes=['hb%d' % hbi])


def trans_t(k, hbi, hT_dst, hTres):
    P = k.P
    hb = k.hbs[hbi]
    tp = k.bankb[0]
    for c in range(8):
        P.op('pe', lambda e, c=c: e.transpose(out=tp[:, c * 128:(c + 1) * 128], in_=hb[:, c * 128:(c + 1) * 128],
                                              identity=k.ident), reads=['hb%d' % hbi, 'ident'], writes=['bank0'])
    P.op('act', lambda e: e.copy(out=hT_dst, in_=tp.rearrange('p (c t) -> p c t', c=8)), reads=['bank0'],
         writes=[hTres])


def post_norm_residual(k, pm, pmres, gname, xt_ap, xres):
    P = k.P
    st = k.st
    ssa = k.sb2
    for cb in range(2):
        P.op('act', lambda e, cb=cb: e.activation(out=k.junk[:, cb * 512:(cb + 1) * 512], in_=pm[cb], func=AF.Square,
                                                  accum_out=ssa[:, cb:cb + 1]),
             reads=[pmres[cb]], writes=['junk', 'ssa'])
    P.op('dve', lambda e: e.tensor_tensor(out=st['ssq2'], in0=ssa[:, 0:1], in1=ssa[:, 1:2], op=ALU.add),
         reads=['ssa'], writes=['2ssq'])
    rstd_from_ssq(k, st['ssq2'], st['rstd2'], st['tmp2'], D, EPS, '2')
    for cb in range(2):
        sl = slice(cb * 512, (cb + 1) * 512)
        P.op('dve', lambda e, cb=cb, sl=sl: e.scalar_tensor_tensor(out=k.tmpf[:, sl], in0=pm[cb], scalar=st['rstd2'][:, 0:1],
                                                                   in1=k.G[gname][:, sl], op0=ALU.mult, op1=ALU.mult),
             reads=[pmres[cb], '2rstd', 'G_' + gname], writes=['tmpf'])
    P.op('pool', lambda e: e.tensor_tensor(out=xt_ap, in0=xt_ap, in1=k.tmpf, op=ALU.add), reads=['tmpf', xres],
         writes=[xres])


def ffn_phase(k, l, xsrc, xdst):
    nc, P = k.nc, k.P
    wg, wu, wd = k.wb[('ffn_w_gate', l)], k.wb[('ffn_w_up', l)], k.wb[('ffn_w_down', l)]
    P.dma('sp', k.wd_sb, wd.rearrange('(c p) n -> p c n', p=128), reads=[('wb', 'ffn_w_down', l)], writes=['wd_sb'],
          stream='w')
    groups = [(0, 4), (4, 4), (8, 4), (12, 4), (16, 4), (20, 2)]
    wgv = wg.rearrange('(c p) n -> p c n', p=128)
    wuv = wu.rearrange('(c p) n -> p c n', p=128)
    gi = 0
    NB = T // 512

    def xtile(tb, s):
        j = (tb % 2) * 4 + s
        return k.xt8[j], 'xt8_%d' % j

    def front_a(tb, s):
        t0 = tb * 512 + s * 128
        xt, xr = xtile(tb, s)
        P.dma('sp', xt, xsrc[t0:t0 + 128, :], reads=[('x', t0 // 128)], writes=[xr], stream='x')
        norm_a(k, xt, xr, 'ffn_pre', s % 2)

    def front_t(tb, s):
        trans_t(k, s % 2, k.hT2[tb % 2][:, :, s * 128:(s + 1) * 128], ('hT', tb % 2))

    for s in range(4):
        front_a(0, s)
        front_t(0, s)
    for tb in range(NB):
        hT = k.hT2[tb % 2]
        hTr = ('hT', tb % 2)
        for gidx, (j0, nj) in enumerate(groups):
            pre = gidx < 4 and tb + 1 < NB
            if pre:
                front_a(tb + 1, gidx)
            buf = gi % 2
            gi += 1
            wt = k.wgu[buf]
            P.dma('sp', wt[:, 0, :, 0:nj * 128], wgv[:, :, j0 * 128:(j0 + nj) * 128], reads=[('wb', 'ffn_w_gate', l)],
                  writes=['wgu%d' % buf], stream='w')
            P.dma('sp', wt[:, 1, :, 0:nj * 128], wuv[:, :, j0 * 128:(j0 + nj) * 128], reads=[('wb', 'ffn_w_up', l)],
                  writes=['wgu%d' % buf], stream='w')
            for jj in range(nj):
                j = j0 + jj
                pb = 1 + 2 * (j % 2)
                pg, pu = k.bank[pb], k.bank[pb + 1]
                for kc in range(8):
                    P.op('pe', lambda e, kc=kc, jj=jj: e.matmul(pg, lhsT=wt[:, 0, kc, jj * 128:(jj + 1) * 128], rhs=hT[:, kc, :],
                                                               start=(kc == 0), stop=(kc == 7)),
                         reads=['wgu%d' % buf, hTr], writes=['bank%d' % pb])
                for kc in range(8):
                    P.op('pe', lambda e, kc=kc, jj=jj: e.matmul(pu, lhsT=wt[:, 1, kc, jj * 128:(jj + 1) * 128], rhs=hT[:, kc, :],
                                                               start=(kc == 0), stop=(kc == 7)),
                         reads=['wgu%d' % buf, hTr], writes=['bank%d' % (pb + 1)])
                sg = k.sg[j % 2]
                P.op('act', lambda e: e.activation(out=sg, in_=pg, func=AF.Silu), reads=['bank%d' % pb],
                     writes=['sg%d' % (j % 2)])
                P.op('dve', lambda e, j=j: e.tensor_tensor(out=k.actT[:, j, :], in0=pu, in1=sg, op=ALU.mult),
                     reads=['bank%d' % (pb + 1), 'sg%d' % (j % 2)], writes=[('actT', j)])
            if pre:
                front_t(tb + 1, gidx)
        for s in range(4):
            t0 = tb * 512 + s * 128
            pm = [k.bank[5], k.bank[6]]
            for cb in range(2):
                for kk in range(NFC):
                    P.op('pe', lambda e, kk=kk, cb=cb: e.matmul(pm[cb], lhsT=k.actT[:, kk, s * 128:(s + 1) * 128],
                                                               rhs=k.wd_sb[:, kk, cb * 512:(cb + 1) * 512],
                                                               start=(kk == 0), stop=(kk == NFC - 1)),
                         reads=[('actT', kk), 'wd_sb'], writes=['bank%d' % (5 + cb)])
            xt_, xr_ = xtile(tb, s)
            post_norm_residual(k, pm, ['bank5', 'bank6'], 'ffn_post', xt_, xr_)
            P.dma('sp', xdst[t0:t0 + 128, :], xt_, reads=[xr_], writes=[('x', t0 // 128)], stream='xo')


def even_phase(k, l, xsrc, xdst, es):
    nc, P = k.nc, k.P
    e_ = l // 2
    ins = k.ins

    def sb(name, shape, dt):
        return es.enter_context(nc.sbuf_tensor('evs%d_' % l + name, list(shape), dt)).ap()

    w_in_sb = sb('w_in', [128, 8, 1536], BF16)
    w_out_sb = sb('w_out', [128, 8, 1024], BF16)
    poolw_sb = sb('poolw', [128, 4, 128], BF16)
    Dm_sb = sb('Dm', [128, 3, 4, 128], BF16)
    WmT = sb('WmT', [128, 4, 128], BF16)
    wsf = sb('wsf', [128, 4, 128], F32)
    wsb = sb('wsb', [128, 4, 128], BF16)
    tril = sb('tril', [128, 128], F32)
    Bt = sb('Bt', [128, 512], F32)
    lng = sb('lng', [128, 512], F32)
    lnb = sb('lnb', [128, 512], F32)
    psc = sb('psc', [128, 4], F32)
    a_sb = [sb('a%d' % i, [128, 512], BF16) for i in range(2)]
    uT_sb = sb('uT', [128, 4, 128], BF16)
    vg = sb('vg', [128, 512], F32)
    vn = sb('vn', [128, 512], F32)
    vln = sb('vln', [128, 512], BF16)
    diffT = sb('diffT', [128, 4, 128], BF16)
    yaT = sb('yaT', [128, 4, 128], BF16)
    ybT = sb('ybT', [128, 4, 128], BF16)
    mxb = sb('mxb', [128, 512], F32)
    hT1s = [sb('hT1_%d' % i, [128, 8, 128], BF16) for i in range(2)]
    bst = sb('bst', [128, 6], F32)
    mv = sb('mv', [128, 2], F32)
    lrs = sb('lrs', [128, 1], F32)

    P.dma('sp', w_in_sb, k.wb[('ev_w_in', e_)].rearrange('(c p) n -> p c n', p=128), reads=[('wb', 'ev_w_in', e_)],
          writes=['w_in'], stream='w')
    P.dma('sp', w_out_sb, k.wb[('ev_w_out', e_)].rearrange('(c p) n -> p c n', p=128), reads=[('wb', 'ev_w_out', e_)],
          writes=['w_out'], stream='w')
    P.dma('sp', poolw_sb, k.wb[('ev_pool_w', e_)].rearrange('g c d -> c g d'), reads=[('wb', 'ev_pool_w', e_)],
          writes=['poolw'], stream='w')
    P.dma('sp', Dm_sb, ins['c_D'].rearrange('a g s t -> s a g t'), writes=['Dm'], stream='small')
    P.dma('sp', wsf, ins['ev_sgu_w'][e_].rearrange('g t s -> t g s'), writes=['wsf'], stream='small')
    P.dma('sp', Bt, ins['ev_sgu_b'][e_:e_ + 1].rearrange('o g t -> o (g t)').broadcast_to([128, 512]), writes=['Bt'],
          stream='small')
    P.dma('sp', lng, ins['ev_sgu_ln_g'][e_:e_ + 1, :].broadcast_to([128, 512]), writes=['lng'], stream='small')
    P.dma('sp', lnb, ins['ev_sgu_ln_b'][e_:e_ + 1, :].broadcast_to([128, 512]), writes=['lnb'], stream='small')
    with nc.allow_non_contiguous_dma(reason='tiny per-channel scale'):
        P.dma('sp', psc, ins['ev_pool_scale'][e_].rearrange('(g p) -> p g', p=128), writes=['psc'], stream='small')
    P.op('dve', lambda e: e.tensor_scalar(out=tril, in0=k.io_f, scalar1=k.iop[:, 0:1], scalar2=None, op0=ALU.is_le),
         reads=['io_f', 'iop'], writes=['tril'])
    P.op('dve', lambda e: e.tensor_tensor(out=wsb, in0=wsf, in1=tril.unsqueeze(1).broadcast_to([128, 4, 128]), op=ALU.mult),
         reads=['wsf', 'tril'], writes=['wsb'])
    tp = k.bankb[0]
    for g in range(4):
        P.op('pe', lambda e, g=g: e.transpose(out=tp[:, g * 128:(g + 1) * 128], in_=wsb[:, g, :], identity=k.ident),
             reads=['wsb', 'ident'], writes=['bank0'])
    P.op('act', lambda e: e.copy(out=WmT, in_=tp[:, 0:512].rearrange('p (g t) -> p g t', g=4)), reads=['bank0'],
         writes=['WmT'])

    def front_a(i):
        P.dma('sp', k.xt[i % 3], xsrc[i * 128:(i + 1) * 128, :], reads=[('x', i)], writes=['xt%d' % (i % 3)], stream='x')
        norm_a(k, k.xt[i % 3], 'xt%d' % (i % 3), 'mix_pre', i % 2)

    def front_t(i):
        trans_t(k, i % 2, hT1s[i % 2], 'hT1_%d' % (i % 2))

    front_a(0)
    front_t(0)
    for i in range(NT):
        xt = k.xt[i % 3]
        xr = 'xt%d' % (i % 3)
        hT1 = hT1s[i % 2]
        hT1n = 'hT1_%d' % (i % 2)
        a_cur, a_prev = a_sb[i % 2], a_sb[(i + 1) % 2]
        ar, apr = 'a%d' % (i % 2), 'a%d' % ((i + 1) % 2)
        if i + 1 < NT and i >= 1:
            pass
        pa, pu, pv = k.bank[1], k.bank[2], k.bank[3]
        for kc in range(8):
            P.op('pe', lambda e, kc=kc: e.matmul(pa, lhsT=hT1[:, kc, :], rhs=w_in_sb[:, kc, 0:512], start=(kc == 0),
                                                stop=(kc == 7)), reads=[hT1n, 'w_in'], writes=['bank1'])
        P.op('act', lambda e: e.copy(out=a_cur, in_=pa), reads=['bank1'], writes=[ar])
        for kc in range(8):
            P.op('pe', lambda e, kc=kc: e.matmul(pv, lhsT=hT1[:, kc, :], rhs=w_in_sb[:, kc, 1024:1536], start=(kc == 0),
                                                stop=(kc == 7)), reads=[hT1n, 'w_in'], writes=['bank3'])
        P.op('act', lambda e: e.activation(out=vg, in_=pv, func=AF.Gelu_apprx_tanh), reads=['bank3'], writes=['vg'])
        for c in range(4):
            for kc in range(8):
                P.op('pe', lambda e, kc=kc, c=c: e.matmul(pu[:, c * 128:(c + 1) * 128],
                                                         lhsT=w_in_sb[:, kc, 512 + c * 128:512 + (c + 1) * 128],
                                                         rhs=hT1[:, kc, :], start=(kc == 0), stop=(kc == 7)),
                     reads=[hT1n, 'w_in'], writes=['bank2'])
        P.op('act', lambda e: e.activation(out=uT_sb, in_=pu.rearrange('p (c t) -> p c t', c=4), func=AF.Gelu_apprx_tanh),
             reads=['bank2'], writes=['uT'])
        if i + 1 < NT:
            front_a(i + 1)
        P.op('dve', lambda e: e.bn_stats(out=bst, in_=vg), reads=['vg'], writes=['bst'])
        P.op('dve', lambda e: e.bn_aggr(out=mv, in_=bst), reads=['bst'], writes=['mv'])
        P.op('dve', lambda e: e.tensor_scalar(out=lrs, in0=mv[:, 1:2], scalar1=1e-5, scalar2=None, op0=ALU.add),
             reads=['mv'], writes=['lrs'])
        P.op('pool', lambda e: e.tensor_tensor(out=lrs, in0=lrs, in1=k.mhalf[:, 0:1], op=ALU.pow), reads=['lrs', 'mhalf'], writes=['lrs'])
        P.op('dve', lambda e: e.tensor_scalar(out=vn, in0=vg, scalar1=mv[:, 0:1], scalar2=lrs[:, 0:1], op0=ALU.subtract,
                                              op1=ALU.mult), reads=['vg', 'mv', 'lrs'], writes=['vn'])
        P.op('pool', lambda e: e.tensor_tensor(out=vn, in0=vn, in1=lng, op=ALU.mult), reads=['vn', 'lng'], writes=['vn'])
        P.op('pool', lambda e: e.tensor_tensor(out=vln, in0=vn, in1=lnb, op=ALU.add), reads=['vn', 'lnb'], writes=['vln'])
        pd_ = k.bank[4]
        for g in range(4):
            first = True
            sl = slice(g * 128, (g + 1) * 128)
            P.op('pe', lambda e, g=g, sl=sl: e.matmul(pd_[:, sl], lhsT=a_cur[:, sl], rhs=Dm_sb[:, 0 if i == 0 else 1, g, :],
                                                     start=True, stop=(i == 0)), reads=[ar, 'Dm'], writes=['bank4'])
            if i > 0:
                P.op('pe', lambda e, g=g, sl=sl: e.matmul(pd_[:, sl], lhsT=a_prev[:, sl], rhs=Dm_sb[:, 2, g, :],
                                                         start=False, stop=True), reads=[apr, 'Dm'], writes=['bank4'])
        P.op('dve', lambda e: e.tensor_copy(out=diffT, in_=pd_.rearrange('p (g t) -> p g t', g=4)), reads=['bank4'],
             writes=['diffT'])
        pya = k.bank[5]
        for g in range(4):
            P.op('pe', lambda e, g=g: e.matmul(pya[:, g * 128:(g + 1) * 128], lhsT=poolw_sb[:, g, :], rhs=diffT[:, g, :],
                                              start=True, stop=True), reads=['poolw', 'diffT'], writes=['bank5'])
        P.op('dve', lambda e: e.tensor_tensor(out=yaT, in0=pya.rearrange('p (g t) -> p g t', g=4),
                                              in1=psc.unsqueeze(2).broadcast_to([128, 4, 128]), op=ALU.mult),
             reads=['bank5', 'psc'], writes=['yaT'])
        pmx = k.bank[4]
        for g in range(4):
            P.op('pe', lambda e, g=g: e.matmul(pmx[:, g * 128:(g + 1) * 128], lhsT=vln[:, g * 128:(g + 1) * 128],
                                              rhs=WmT[:, g, :], start=True, stop=True), reads=['vln', 'WmT'],
                 writes=['bank4'])
        P.op('dve', lambda e: e.tensor_tensor(out=mxb, in0=pmx, in1=Bt, op=ALU.add), reads=['bank4', 'Bt'],
             writes=['mxb'])
        P.op('pool', lambda e: e.tensor_tensor(out=ybT, in0=mxb.rearrange('p (g t) -> p g t', g=4), in1=uT_sb, op=ALU.mult),
             reads=['mxb', 'uT'], writes=['ybT'])
        if i + 1 < NT:
            front_t(i + 1)
        pm = [k.bank[6], k.bank[7]]
        for cb in range(2):
            for kc in range(8):
                lh = yaT[:, kc, :] if kc < 4 else ybT[:, kc - 4, :]
                P.op('pe', lambda e, kc=kc, cb=cb, lh=lh: e.matmul(pm[cb], lhsT=lh, rhs=w_out_sb[:, kc, cb * 512:(cb + 1) * 512],
                                                                  start=(kc == 0), stop=(kc == 7)),
                     reads=['yaT', 'ybT', 'w_out'], writes=['bank%d' % (6 + cb)])
        post_norm_residual(k, pm, ['bank6', 'bank7'], 'mix_post', xt, xr)
        P.dma('sp', xdst[i * 128:(i + 1) * 128, :], xt, reads=[xr], writes=[('x', i)], stream='xo')


def odd_phase(k, l, xsrc, xdst, es):
    from contextlib import ExitStack
    nc, P = k.nc, k.P
    o_ = l // 2
    ins = k.ins
    PI = 3.14159265358979

    def sbs(stack, name, shape, dt):
        return stack.enter_context(nc.sbuf_tensor('od%d_' % l + name, list(shape), dt)).ap()

    def sb(name, shape, dt):
        return sbs(es, name, shape, dt)

    qd = k.dram('qd%d' % l, [T, 512], BF16)
    ydd = k.dram('ydd%d' % l, [T, 512], BF16)
    KE = sb('KE', [128, 2, T], BF16)
    kwT = sb('kwT', [64, 2, T], BF16)
    kcvd = k.dram('kcvd%d' % l, [4, 64, T], BF16)
    ropeS = k.dram('ropeS%d' % l, [128, NT, 72], F32)
    ropeC = k.dram('ropeC%d' % l, [128, NT, 72], F32)
    vs_aug = sb('vs_aug', [128, NT, 2, 65], BF16)
    vw_aug = sb('vw_aug', [128, NT, 2, 65], BF16)
    gsig = sb('gsig', [128, NT, 24], F32)
    kcmpT = sb('kcmpT', [64, 2, 256], BF16)
    vcmp_aug = sb('vcmp', [128, 2, 2, 65], BF16)
    tri = sb('tri', [128, 128], F32)
    ntri = sb('ntri', [128, 128], F32)
    cm = sb('cm', [128, 128], F32)
    keep = sb('keep', [128, 128], F32)
    addc = sb('addc', [128, 128], F32)
    cts = sb('cts', [128, 2, 64], BF16)
    decT = sb('decT', [128, 512], F32)
    xi = sb('xi', [128, 512], F32)
    zeta = sb('zeta', [128, 4], F32)
    gch = sb('gch', [128, 4], F32)
    gng = sb('gng', [128, 512], F32)
    for nm, t_, src in [('tri', tri, 'c_tri'), ('cm', cm, 'c_cm'), ('keep', keep, 'c_keep'), ('addc', addc, 'c_add'),
                        ('decT', decT, 'c_decT'), ('xi', xi, 'c_xi'), ('zeta', zeta, 'c_zeta'), ('gch', gch, 'c_gch')]:
        P.dma('sp', t_, ins[src], writes=[nm], stream='small')
    P.dma('sp', cts, ins['c_cts'].rearrange('c n j -> n c j'), writes=['cts'], stream='small')
    P.dma('sp', KE[64:128, 0, :], ins['c_E'], writes=['KE'], stream='small')
    P.dma('sp', KE[64:128, 1, :], ins['c_E'], writes=['KE'], stream='small')
    P.dma('sp', gng, ins['od_ret_gn_g'][o_:o_ + 1, :].broadcast_to([128, 512]), writes=['gng'], stream='small')
    P.op('dve', lambda e: e.tensor_scalar(out=ntri, in0=tri, scalar1=-1.0, scalar2=1.0, op0=ALU.mult, op1=ALU.add),
         reads=['tri'], writes=['ntri'])
    P.op('pool', lambda e: e.memset(vs_aug, 1.0), writes=['vs_aug'])
    P.op('pool', lambda e: e.memset(vw_aug, 1.0), writes=['vw_aug'])
    P.op('pool', lambda e: e.memset(vcmp_aug, 1.0), writes=['vcmp'])
    with ExitStack() as ts:
        posi = sbs(ts, 'posi', [128, NT], I32)
        sinT = sbs(ts, 'sinT', [128, NT, 72], F32)
        cosT = sbs(ts, 'cosT', [128, NT, 72], F32)
        posf = sbs(ts, 'posf', [128, NT], F32)
        invf = sbs(ts, 'invf', [128, 72], F32)
        ang = sbs(ts, 'ang', [128, NT, 72], F32)
        arg = sbs(ts, 'arg', [128, NT, 72], F32)
        kf = sbs(ts, 'kf', [128, NT, 72], F32)
        ki = sbs(ts, 'ki', [128, NT, 72], I32)
        with nc.allow_non_contiguous_dma(reason='positions to token-on-partition layout'):
            P.dma('sp', posi, ins['positions'].rearrange('o (i p) -> p (o i)', p=128), writes=['posi'], stream='small')
        P.dma('sp', invf, ins['c_invf'].broadcast_to([128, 72]), writes=['invf'], stream='small')
        P.op('dve', lambda e: e.tensor_copy(out=posf, in_=posi), reads=['posi'], writes=['posf'])
        P.op('dve', lambda e: e.tensor_tensor(out=ang, in0=posf.unsqueeze(2).broadcast_to([128, NT, 72]),
                                              in1=invf.unsqueeze(1).broadcast_to([128, NT, 72]), op=ALU.mult),
             reads=['posf', 'invf'], writes=['ang'])
        for shift, dst, dn in [(0.0, sinT, 'sinT'), (PI / 2, cosT, 'cosT')]:
            P.op('dve', lambda e, shift=shift: e.tensor_scalar(out=arg, in0=ang, scalar1=shift, scalar2=None, op0=ALU.add),
                 reads=['ang'], writes=['arg'])
            P.op('dve', lambda e: e.tensor_scalar(out=kf, in0=arg, scalar1=1.0 / (2 * PI), scalar2=None, op0=ALU.mult),
                 reads=['arg'], writes=['kf'])
            P.op('dve', lambda e: e.tensor_copy(out=ki, in_=kf), reads=['kf'], writes=['ki'])
            P.op('dve', lambda e: e.tensor_copy(out=kf, in_=ki), reads=['ki'], writes=['kf'])
            P.op('dve', lambda e: e.scalar_tensor_tensor(out=arg, in0=kf, scalar=-6.28125, in1=arg, op0=ALU.mult, op1=ALU.add),
                 reads=['kf', 'arg'], writes=['arg'])
            P.op('dve', lambda e: e.scalar_tensor_tensor(out=arg, in0=kf, scalar=-(2 * PI - 6.28125), in1=arg, op0=ALU.mult,
                                                         op1=ALU.add), reads=['kf', 'arg'], writes=['arg'])
            P.op('dve', lambda e: e.tensor_scalar(out=arg, in0=arg, scalar1=3.1415925, scalar2=-3.1415925, op0=ALU.min,
                                                  op1=ALU.max), reads=['arg'], writes=['arg'])
            P.op('act', lambda e, dst=dst: e.activation(out=dst, in_=arg, func=AF.Sin), reads=['arg'], writes=[dn])
        P.dma('sp', ropeS, sinT, reads=['sinT'], writes=['ropeS'], stream='xo')
        P.dma('sp', ropeC, cosT, reads=['cosT'], writes=['ropeC'], stream='xo')
        P.barrier()

    with ExitStack() as s1:
        w_in_sb = sbs(s1, 'w_in', [128, 8, 3352], BF16)
        hT1s = [sbs(s1, 'hT1_%d' % j, [128, 8, 128], BF16) for j in range(2)]
        cur = {}
        csb = [sbs(s1, 'csb%d' % j, [128, 2, 72], F32) for j in range(2)]
        kcv_t = sbs(s1, 'kcv_t', [64, 4, 128], BF16)
        zqs = [sbs(s1, 'zq%d' % j, [128, 512], F32) for j in range(2)]
        qb = sbs(s1, 'qb', [128, 512], BF16)
        zkvs = [sbs(s1, 'zkv%d' % j, [128, 768], F32) for j in range(2)]
        kvb = sbs(s1, 'kvb', [128, 768], BF16)
        rt = [sbs(s1, 'rt%d' % j, [128, 256], F32) for j in range(4)]
        zrqs = [sbs(s1, 'zrq%d' % j, [128, 512], F32) for j in range(2)]
        zrks = [sbs(s1, 'zrk%d' % j, [128, 512], F32) for j in range(2)]
        rqb = sbs(s1, 'rqb', [128, 512], BF16)
        rkb = sbs(s1, 'rkb', [128, 512], BF16)
        rkr = sbs(s1, 'rkr', [128, 512], F32)
        rkz = sbs(s1, 'rkz', [128, 512], BF16)
        rvbs = [sbs(s1, 'rvb%d' % j, [128, 512], BF16) for j in range(2)]
        gsls = [sbs(s1, 'gsl%d' % j, [128, 512], F32) for j in range(2)]
        qkT = sbs(s1, 'qkT', [128, 8, 128], BF16)
        qxiT = sbs(s1, 'qxiT', [128, 512], BF16)
        STs = sbs(s1, 'STs', [128, 512], BF16)
        state = sbs(s1, 'state', [128, 512], F32)
        stbf = sbs(s1, 'stbf', [128, 512], BF16)
        bst4 = sbs(s1, 'bst4', [128, 4, 6], F32)
        mv4 = sbs(s1, 'mv4', [128, 4, 2], F32)
        rs4 = sbs(s1, 'rs4', [128, 4], F32)
        on = sbs(s1, 'on', [128, 512], F32)
        ydb = sbs(s1, 'ydb', [128, 512], BF16)
        P.dma('sp', w_in_sb, k.wb[('od_w_in', o_)].rearrange('(c p) n -> p c n', p=128), reads=[('wb', 'od_w_in', o_)],
              writes=['w_in'], stream='w')
        P.op('dve', lambda e: e.memset(state, 0.0), writes=['state'])

        def proj(bank, c0, c1):
            for kc in range(8):
                P.op('pe', lambda e, kc=kc: e.matmul(k.bank[bank][:, 0:c1 - c0], lhsT=cur['hT'][:, kc, :], rhs=w_in_sb[:, kc, c0:c1],
                                                    start=(kc == 0), stop=(kc == 7)), reads=[cur['hTn'], 'w_in'],
                     writes=['bank%d' % bank])

        def rope(src, dst, nh, hd, half, c_, s_, csn, sname, dname):
            sv = src.rearrange('p (h d) -> p h d', h=nh)
            dv = dst.rearrange('p (h d) -> p h d', h=nh)
            x1, x2 = sv[:, :, 0:half], sv[:, :, half:2 * half]
            cb = c_.unsqueeze(1).broadcast_to([128, nh, half])
            sb_ = s_.unsqueeze(1).broadcast_to([128, nh, half])
            t = [r_[:, 0:nh * half].rearrange('p (h d) -> p h d', h=nh) for r_ in rt]
            P.op('dve', lambda e: e.tensor_tensor(out=t[0], in0=x1, in1=cb, op=ALU.mult), reads=[sname, csn], writes=['rt0'])
            P.op('pool', lambda e: e.tensor_tensor(out=t[1], in0=x2, in1=sb_, op=ALU.mult), reads=[sname, csn], writes=['rt1'])
            P.op('dve', lambda e: e.tensor_tensor(out=t[2], in0=x2, in1=cb, op=ALU.mult), reads=[sname, csn], writes=['rt2'])
            P.op('pool', lambda e: e.tensor_tensor(out=t[3], in0=x1, in1=sb_, op=ALU.mult), reads=[sname, csn], writes=['rt3'])
            if 2 * half < hd:
                P.op('pool', lambda e: e.tensor_copy(out=dst, in_=src), reads=[sname], writes=[dname])
            P.op('dve', lambda e: e.tensor_tensor(out=dv[:, :, 0:half], in0=t[0], in1=t[1], op=ALU.subtract),
                 reads=['rt0', 'rt1'], writes=[dname])
            P.op('pool', lambda e: e.tensor_tensor(out=dv[:, :, half:2 * half], in0=t[2], in1=t[3], op=ALU.add),
                 reads=['rt2', 'rt3'], writes=[dname])

        def stage_p(i):
            pz = i % 2
            cur['hT'] = hT1s[pz]
            cur['hTn'] = 'hT1_%d' % pz
            cs_ = csb[pz]
            csn = 'csb%d' % pz
            P.dma('sp', cs_[:, 0, :], ropeC[:, i, :], reads=['ropeC'], writes=[csn], stream='small')
            P.dma('sp', cs_[:, 1, :], ropeS[:, i, :], reads=['ropeS'], writes=[csn], stream='small')
            P.dma('sp', k.xt[i % 3], xsrc[i * 128:(i + 1) * 128, :], reads=[('x', i)], writes=['xt%d' % (i % 3)], stream='x')
            norm_a(k, k.xt[i % 3], 'xt%d' % (i % 3), 'mix_pre', pz)
            trans_t(k, pz, hT1s[pz], 'hT1_%d' % pz)
            proj(1, 0, 512)
            proj(2, 512, 1024)
            proj(3, 1024, 1304)
            P.op('act', lambda e: e.activation(out=zqs[pz], in_=k.bank[1], func=AF.Copy, scale=0.125), reads=['bank1'],
                 writes=['zq%d' % pz])
            P.op('act', lambda e: e.copy(out=zkvs[pz][:, 0:512], in_=k.bank[2]), reads=['bank2'], writes=['zkv%d' % pz])
            P.op('act', lambda e: e.copy(out=zkvs[pz][:, 512:768], in_=k.bank[3][:, 0:256]), reads=['bank3'], writes=['zkv%d' % pz])
            P.op('act', lambda e: e.activation(out=gsig[:, i, :], in_=k.bank[3][:, 256:280], func=AF.Sigmoid),
                 reads=['bank3'], writes=[('gsig', i)])
            proj(4, 1304, 1816)
            proj(5, 1816, 2328)
            proj(6, 2328, 2840)
            proj(7, 2840, 3352)
            P.op('act', lambda e: e.copy(out=zrqs[pz], in_=k.bank[4]), reads=['bank4'], writes=['zrq%d' % pz])
            P.op('act', lambda e: e.activation(out=zrks[pz], in_=k.bank[5], func=AF.Copy, scale=128.0 ** -0.5), reads=['bank5'],
                 writes=['zrk%d' % pz])
            P.op('act', lambda e: e.copy(out=rvbs[pz], in_=k.bank[6]), reads=['bank6'], writes=['rvb%d' % pz])
            P.op('act', lambda e: e.activation(out=gsls[pz], in_=k.bank[7], func=AF.Silu), reads=['bank7'], writes=['gsl%d' % pz])

        def stage_r(i):
            pz = i % 2
            zq, zkv, zrq, zrk, rvb, gsl = zqs[pz], zkvs[pz], zrqs[pz], zrks[pz], rvbs[pz], gsls[pz]
            zqn, zkvn, zrqn, zrkn, rvbn, gsln = ['%s%d' % (n_, pz) for n_ in ('zq', 'zkv', 'zrq', 'zrk', 'rvb', 'gsl')]
            cs_ = csb[pz]
            csn = 'csb%d' % pz
            cn, sn = cs_[:, 0, 0:8], cs_[:, 1, 0:8]
            cr, sr = cs_[:, 0, 8:72], cs_[:, 1, 8:72]
            rope(zq, qb, 8, 64, 8, cn, sn, csn, zqn, 'qb')
            P.dma('pool', qd[i * 128:(i + 1) * 128, :], qb, reads=['qb'], writes=[('qd', i)], stream='xo')
            kv4 = zkv.rearrange('p (a b g d) -> p a b g d', a=3, b=2, g=2)[:, :, 0, :, :]
            x1, x2 = kv4[:, :, :, 0:8], kv4[:, :, :, 8:16]
            cb = cn.unsqueeze(1).unsqueeze(1).broadcast_to([128, 3, 2, 8])
            sb_ = sn.unsqueeze(1).unsqueeze(1).broadcast_to([128, 3, 2, 8])
            t = [r_[:, 0:48].rearrange('p (a g d) -> p a g d', a=3, g=2) for r_ in rt]
            P.op('dve', lambda e: e.tensor_tensor(out=t[0], in0=x1, in1=cb, op=ALU.mult), reads=[zkvn, csn], writes=['rt0'])
            P.op('dve', lambda e: e.tensor_tensor(out=t[1], in0=x2, in1=sb_, op=ALU.mult), reads=[zkvn, csn], writes=['rt1'])
            P.op('dve', lambda e: e.tensor_tensor(out=t[2], in0=x2, in1=cb, op=ALU.mult), reads=[zkvn, csn], writes=['rt2'])
            P.op('dve', lambda e: e.tensor_tensor(out=t[3], in0=x1, in1=sb_, op=ALU.mult), reads=[zkvn, csn], writes=['rt3'])
            P.op('dve', lambda e: e.tensor_tensor(out=x1, in0=t[0], in1=t[1], op=ALU.subtract), reads=['rt0', 'rt1'], writes=[zkvn])
            P.op('dve', lambda e: e.tensor_tensor(out=x2, in0=t[2], in1=t[3], op=ALU.add), reads=['rt2', 'rt3'], writes=[zkvn])
            P.op('pool', lambda e: e.tensor_copy(out=kvb, in_=zkv), reads=[zkvn], writes=['kvb'])
            P.op('pool', lambda e: e.tensor_copy(out=vs_aug[:, i, :, 0:64], in_=kvb[:, 384:512].rearrange('p (g d) -> p g d', g=2)),
                 reads=['kvb'], writes=['vs_aug'])
            P.op('pool', lambda e: e.tensor_copy(out=vw_aug[:, i, :, 0:64], in_=kvb[:, 640:768].rearrange('p (g d) -> p g d', g=2)),
                 reads=['kvb'], writes=['vw_aug'])
            rope(zrq, rqb, 4, 128, 64, cr, sr, csn, zrqn, 'rqb')
            rope(zrk, rkr, 4, 128, 64, cr, sr, csn, zrkn, 'rkr')
            P.op('pool', lambda e: e.tensor_copy(out=rkb, in_=rkr), reads=['rkr'], writes=['rkb'])
            P.op('dve', lambda e: e.tensor_tensor(out=rkz.rearrange('p (h d) -> p h d', h=4),
                                                  in0=rkr.rearrange('p (h d) -> p h d', h=4),
                                                  in1=zeta.unsqueeze(2).broadcast_to([128, 4, 128]), op=ALU.mult),
                 reads=['rkr', 'zeta'], writes=['rkz'])
            tp = k.bankb[0]
            srcs = [0, 64, 128, 192, 512, 576, 256, 320]
            for j, c0 in enumerate(srcs):
                P.op('pe', lambda e, j=j, c0=c0: e.transpose(out=tp[0:64, j * 128:(j + 1) * 128], in_=kvb[:, c0:c0 + 64],
                                                            identity=k.ident), reads=['kvb', 'ident'], writes=['bank0'])
            P.op('dve', lambda e: e.tensor_copy(out=kcv_t, in_=tp[0:64, 0:512].rearrange('p (j t) -> p j t', j=4)), reads=['bank0'],
                 writes=['kcv_t'])
            P.dma('pool', kcvd[:, :, i * 128:(i + 1) * 128].rearrange('j p t -> p j t'), kcv_t, reads=['kcv_t'], writes=['kcvd'],
                  stream='xo')
            P.op('dve', lambda e: e.tensor_copy(out=kwT[:, :, i * 128:(i + 1) * 128],
                                         in_=tp[0:64, 512:768].rearrange('p (j t) -> p j t', j=2)), reads=['bank0'], writes=['kwT'])
            P.op('dve', lambda e: e.tensor_copy(out=KE[0:64, :, i * 128:(i + 1) * 128],
                                         in_=tp[0:64, 768:1024].rearrange('p (j t) -> p j t', j=2)), reads=['bank0'], writes=['KE'])
            for h in range(4):
                P.op('pe', lambda e, h=h: e.transpose(out=tp[:, h * 128:(h + 1) * 128], in_=rqb[:, h * 128:(h + 1) * 128],
                                                      identity=k.ident), reads=['rqb', 'ident'], writes=['bank0'])
            for h in range(4):
                P.op('pe', lambda e, h=h: e.transpose(out=tp[:, (4 + h) * 128:(5 + h) * 128], in_=rkb[:, h * 128:(h + 1) * 128],
                                                      identity=k.ident), reads=['rkb', 'ident'], writes=['bank0'])
            P.op('dve', lambda e: e.tensor_copy(out=qkT, in_=tp.rearrange('p (c t) -> p c t', c=8)), reads=['bank0'], writes=['qkT'])
            for h in range(4):
                P.op('pe', lambda e, h=h: e.matmul(k.bank[5][:, h * 128:(h + 1) * 128], lhsT=qkT[:, 4 + h, :], rhs=qkT[:, h, :],
                                                  start=True, stop=True), reads=['qkT'], writes=['bank5'])
            P.op('dve', lambda e: e.tensor_tensor(out=STs, in0=k.bank[5], in1=decT, op=ALU.mult), reads=['bank5', 'decT'],
                 writes=['STs'])
            P.op('pool', lambda e: e.tensor_tensor(out=qxiT, in0=qkT[:, 0:4, :].rearrange('p h t -> p (h t)'), in1=xi, op=ALU.mult),
                 reads=['qkT', 'xi'], writes=['qxiT'])
            for h in range(4):
                hs = slice(h * 128, (h + 1) * 128)
                P.op('pe', lambda e, hs=hs: e.matmul(k.bank[7][:, hs], lhsT=STs[:, hs], rhs=rvb[:, hs], start=True, stop=(i == 0)),
                     reads=['STs', rvbn], writes=['bank7'])
                if i > 0:
                    P.op('pe', lambda e, hs=hs: e.matmul(k.bank[7][:, hs], lhsT=qxiT[:, hs], rhs=stbf[:, hs], start=False, stop=True),
                         reads=['qxiT', 'stbf'], writes=['bank7'])
            for h in range(4):
                hs = slice(h * 128, (h + 1) * 128)
                P.op('pe', lambda e, hs=hs: e.matmul(k.bank[6][:, hs], lhsT=rkz[:, hs], rhs=rvb[:, hs], start=True, stop=True),
                     reads=['rkz', rvbn], writes=['bank6'])
            P.op('dve', lambda e: e.tensor_tensor(out=state.rearrange('p (h d) -> p h d', h=4),
                                                  in0=state.rearrange('p (h d) -> p h d', h=4),
                                                  in1=gch.unsqueeze(2).broadcast_to([128, 4, 128]), op=ALU.mult),
                 reads=['state', 'gch'], writes=['state'])
            P.op('dve', lambda e: e.tensor_tensor(out=state, in0=k.bank[6], in1=state, op=ALU.add), reads=['bank6', 'state'],
                 writes=['state'])
            P.op('pool', lambda e: e.tensor_copy(out=stbf, in_=state), reads=['state'], writes=['stbf'])
            for h in range(4):
                P.op('dve', lambda e, h=h: e.bn_stats(out=bst4[:, h, :], in_=k.bank[7][:, h * 128:(h + 1) * 128]),
                     reads=['bank7'], writes=['bst4'])
            for h in range(4):
                P.op('dve', lambda e, h=h: e.bn_aggr(out=mv4[:, h, :], in_=bst4[:, h, :]), reads=['bst4'], writes=['mv4'])
            P.op('dve', lambda e: e.tensor_scalar(out=rs4, in0=mv4[:, :, 1], scalar1=1e-5, scalar2=None, op0=ALU.add),
                 reads=['mv4'], writes=['rs4'])
            P.op('pool', lambda e: e.tensor_tensor(out=rs4, in0=rs4, in1=k.mhalf, op=ALU.pow), reads=['rs4', 'mhalf'], writes=['rs4'])
            for h in range(4):
                hs = slice(h * 128, (h + 1) * 128)
                P.op('dve', lambda e, h=h, hs=hs: e.tensor_scalar(out=on[:, hs], in0=k.bank[7][:, hs], scalar1=mv4[:, h, 0:1],
                                                                  scalar2=rs4[:, h:h + 1], op0=ALU.subtract, op1=ALU.mult),
                     reads=['bank7', 'mv4', 'rs4'], writes=['on'])
            P.op('pool', lambda e: e.tensor_tensor(out=on, in0=on, in1=gng, op=ALU.mult), reads=['on', 'gng'], writes=['on'])
            P.op('pool', lambda e: e.tensor_tensor(out=ydb, in0=on, in1=gsl, op=ALU.mult), reads=['on', gsln], writes=['ydb'])
            P.dma('pool', ydd[i * 128:(i + 1) * 128, :], ydb, reads=['ydb'], writes=[('ydd', i)], stream='xo')

        stage_p(0)
        for i in range(NT):
            if i + 1 < NT:
                stage_p(i + 1)
            stage_r(i)

        P.barrier()
        s1.close()
        s1c = ExitStack()
        w1 = sbs(s1c, 'w1', [64, 32, 128], BF16)
        w2 = sbs(s1c, 'w2', [128, 64], BF16)
        posf32 = sbs(s1c, 'posf32', [64, 32], F32)
        posT = sbs(s1c, 'posT', [64, 32], BF16)
        cbias = sbs(s1c, 'cbias', [128, 1], F32)
        ghT = sbs(s1c, 'ghT', [128, 256], BF16)
        csrc = sbs(s1c, 'csrc', [64, T], BF16)
        for kind, (n1, n2, npos) in enumerate([('od_cmp_k_w1', 'od_cmp_k_w2', 'od_cmp_k_pos'),
                                               ('od_cmp_v_w1', 'od_cmp_v_w2', 'od_cmp_v_pos')]):
            P.dma('sp', w1, k.wb[(n1, o_)].rearrange('(l d) j -> d l j', d=64), reads=[('wb', n1, o_)], writes=['w1'], stream='w')
            P.dma('sp', w2, k.wb[(n2, o_)], reads=[('wb', n2, o_)], writes=['w2'], stream='w')
            with nc.allow_non_contiguous_dma(reason='tiny pos-emb transpose'):
                P.dma('sp', posf32, ins[npos][o_].rearrange('l d -> d l'), writes=['posf32'], stream='small')
            P.op('dve', lambda e: e.tensor_copy(out=posT, in_=posf32), reads=['posf32'], writes=['posT'])
            for g in range(2):
                P.dma('sp', csrc, kcvd[2 * kind + g], reads=['kcvd'], writes=['csrc'], stream='w')
                srcv = csrc.rearrange('p (n s) -> p n s', s=16)
                hb_ = k.bank[1]
                for l_ in range(32):
                    P.op('pe', lambda e, l_=l_: e.matmul(hb_[:, 0:255], lhsT=w1[:, l_, :],
                                                        rhs=(srcv[:, 0:255, l_] if l_ < 16 else srcv[:, 1:256, l_ - 16]),
                                                        start=(l_ == 0), stop=(l_ == 31)),
                         reads=['w1', 'csrc'], writes=['bank1'])
                for l_ in range(32):
                    P.op('pe', lambda e, l_=l_: e.matmul(hb_[:, 256:257], lhsT=w1[:, l_, :], rhs=posT[:, l_:l_ + 1],
                                                        start=(l_ == 0), stop=(l_ == 31)), reads=['w1', 'posT'], writes=['bank1'])
                P.op('dve', lambda e: e.tensor_copy(out=cbias, in_=hb_[:, 256:257]), reads=['bank1'], writes=['cbias'])
                P.op('dve', lambda e: e.memset(ghT[:, 255:256], 0.0), writes=['ghT'])
                P.op('act', lambda e: e.activation(out=ghT[:, 0:255], in_=hb_[:, 0:255], func=AF.Gelu_apprx_tanh,
                                                   bias=cbias[:, 0:1]), reads=['bank1', 'cbias'], writes=['ghT'])
                if kind == 0:
                    P.op('pe', lambda e: e.matmul(k.bank[2][0:64, 0:256], lhsT=w2, rhs=ghT, start=True, stop=True),
                         reads=['w2', 'ghT'], writes=['bank2'])
                    P.op('act', lambda e, g=g: e.copy(out=kcmpT[:, g, :], in_=k.bank[2][0:64, 0:256]), reads=['bank2'],
                         writes=['kcmpT'])
                else:
                    for c in range(2):
                        P.op('pe', lambda e, c=c: e.matmul(k.bank[2][:, c * 64:(c + 1) * 64], lhsT=ghT[:, c * 128:(c + 1) * 128],
                                                          rhs=w2, start=True, stop=True), reads=['w2', 'ghT'], writes=['bank2'])
                    P.op('act', lambda e, g=g: e.copy(out=vcmp_aug[:, :, g, 0:64],
                                                      in_=k.bank[2][:, 0:128].rearrange('p (c d) -> p c d', c=2)),
                         reads=['bank2'], writes=['vcmp'])
        P.barrier()
        s1c.close()

    with ExitStack() as s2:
        w_out_sb = sbs(s2, 'w_out', [128, 8, 1024], BF16)
        PT = sbs(s2, 'PT', [128, NT, 512], BF16)
        PW = sbs(s2, 'PW', [128, 5, 512], BF16)
        Pc = sbs(s2, 'Pc', [128, 2, 512], BF16)
        QN = [sbs(s2, 'QN%d' % g, [128, 512], BF16) for g in range(2)]
        qt = [sbs(s2, 'qt%d' % j, [128, 512], BF16) for j in range(2)]
        ydts = [sbs(s2, 'ydt%d' % j, [128, 512], BF16) for j in range(2)]
        negt = sbs(s2, 'negt', [128, 128], BF16)
        expf = sbs(s2, 'expf', [128, 512], F32)
        score = sbs(s2, 'score', [128, 64], F32)
        sc2 = sbs(s2, 'sc2', [128, 64], F32)
        m8a = sbs(s2, 'm8a', [128, 8], F32)
        m8b = sbs(s2, 'm8b', [128, 8], F32)
        thr = sbs(s2, 'thr', [128, 1], F32)
        psl = sbs(s2, 'psl', [128, 64], F32)
        rdenA = [sbs(s2, 'rdenA%d' % g, [128, 4], F32) for g in range(2)]
        rdenB = [sbs(s2, 'rdenB%d' % g, [128, 12], F32) for g in range(2)]
        coefs = [sbs(s2, 'coef%d' % g, [128, 12], F32) for g in range(2)]
        ocw = [sbs(s2, 'ocw%d' % g, [128, 2, 260], F32) for g in range(2)]
        yc = sbs(s2, 'yc', [128, 512], F32)
        ycb = sbs(s2, 'ycb', [128, 512], BF16)
        yT = sbs(s2, 'yT', [128, 8, 128], BF16)
        P.dma('sp', w_out_sb, k.wb[('od_w_out', o_)].rearrange('(c p) n -> p c n', p=128), reads=[('wb', 'od_w_out', o_)],
              writes=['w_out'], stream='w')
        P.op('dve', lambda e: e.memset(negt, 0.0), writes=['negt'])
        tp = k.bankb[0]
        sbank = [0]

        def next_sbank():
            sbank[0] += 1
            return (1, 2, 7)[sbank[0] % 3]

        def den_view(ap260):
            return ap260.rearrange('p (m e) -> p m e', e=65)[:, :, 64]

        def masked_exp(b, nn, dst, mask_ap, dname):
            if mask_ap is None:
                P.op('act', lambda e: e.activation(out=dst[0:nn], in_=k.bank[b][0:nn, :], func=AF.Exp), reads=['bank%d' % b],
                     writes=[dname])
            else:
                P.op('act', lambda e: e.activation(out=expf[0:nn], in_=k.bank[b][0:nn, :], func=AF.Exp), reads=['bank%d' % b],
                     writes=['expf'])
                P.op('pool', lambda e: e.tensor_tensor(out=dst[0:nn].rearrange('p (m q) -> p m q', m=4),
                                                       in0=expf[0:nn].rearrange('p (m q) -> p m q', m=4),
                                                       in1=mask_ap.unsqueeze(1).broadcast_to([nn, 4, 128]), op=ALU.mult),
                     reads=['expf', 'tri', 'ntri'], writes=[dname])

        def o2_loads(i):
            P.dma('sp', k.xt[i % 3], xsrc[i * 128:(i + 1) * 128, :], reads=[('x', i)], writes=['xt%d' % (i % 3)], stream='x')
            P.dma('sp', qt[i % 2], qd[i * 128:(i + 1) * 128, :], reads=[('qd', i)], writes=['qt%d' % (i % 2)], stream='x')
            P.dma('sp', ydts[i % 2], ydd[i * 128:(i + 1) * 128, :], reads=[('ydd', i)], writes=['ydt%d' % (i % 2)], stream='x')

        def stage_a(i, g):
            qti = qt[i % 2]
            qtn = 'qt%d' % (i % 2)
            off = 62 - 2 * i
            Q = QN[g]
            qn = 'QN%d' % g
            rdA = rdenA[g]
            rdAn = 'rdenA%d' % g
            for m in range(4):
                h = 4 * g + m
                P.op('pe', lambda e, m=m, h=h: e.transpose(out=tp[0:64, m * 128:(m + 1) * 128], in_=qti[:, h * 64:(h + 1) * 64],
                                                          identity=k.ident), reads=[qtn, 'ident'], writes=['bank0'])
            P.op('act', lambda e: e.copy(out=Q[0:64, :], in_=tp[0:64, 0:512]), reads=['bank0'], writes=[qn + 'lo'])
            chunks = [(0, 128)] + ([(1, 127)] if 8 * i + 6 >= 128 else [])
            for (c, nn) in chunks:
                b = next_sbank()
                P.op('pe', lambda e, c=c, nn=nn, b=b: e.matmul(k.bank[b][0:nn, :], lhsT=kcmpT[:, g, c * 128:c * 128 + nn],
                                                              rhs=Q[0:64, :], start=True, stop=True),
                     reads=['kcmpT', qn + 'lo'], writes=['bank%d' % b])
                full = (16 * (c * 128 + nn - 1) + 31 <= 128 * i)
                if full:
                    P.op('act', lambda e, c=c, nn=nn, b=b: e.activation(out=Pc[0:nn, c, :], in_=k.bank[b][0:nn, :], func=AF.Exp),
                         reads=['bank%d' % b], writes=['Pc'])
                else:
                    tv = float(128 * i - 31 - 2048 * c)
                    P.op('act', lambda e, nn=nn, b=b: e.activation(out=expf[0:nn], in_=k.bank[b][0:nn, :], func=AF.Exp),
                         reads=['bank%d' % b], writes=['expf'])
                    P.op('dve', lambda e, c=c, nn=nn, tv=tv: e.scalar_tensor_tensor(
                        out=Pc[0:nn, c, :].rearrange('p (m q) -> p m q', m=4),
                        in0=cm[0:nn].unsqueeze(1).broadcast_to([nn, 4, 128]), scalar=tv,
                        in1=expf[0:nn].rearrange('p (m q) -> p m q', m=4), op0=ALU.is_le, op1=ALU.mult),
                         reads=['expf', 'cm'], writes=['Pc'])
            for m in range(4):
                for ci, (c, nn) in enumerate(chunks):
                    P.op('pe', lambda e, m=m, c=c, nn=nn, ci=ci: e.matmul(k.bank[3][:, m * 65:(m + 1) * 65],
                                                                        lhsT=Pc[0:nn, c, m * 128:(m + 1) * 128],
                                                                        rhs=vcmp_aug[0:nn, c, g, :], start=(ci == 0),
                                                                        stop=(ci == len(chunks) - 1)),
                         reads=['Pc', 'vcmp'], writes=['bank3'])
            for m in range(4):
                for ci, (c, nn) in enumerate(chunks):
                    P.op('pe', lambda e, m=m, c=c, nn=nn, ci=ci: e.matmul(k.bank[4][:, m * 64:(m + 1) * 64],
                                                                        lhsT=Pc[0:nn, c, m * 128:(m + 1) * 128],
                                                                        rhs=cts[0:nn, c, :], start=(ci == 0),
                                                                        stop=(ci == len(chunks) - 1)),
                         reads=['Pc', 'cts'], writes=['bank4'])
            jts = list(range(max(0, i - 4), i + 1))
            for sl_, jt in enumerate(jts):
                b = next_sbank()
                P.op('pe', lambda e, jt=jt, b=b: e.matmul(k.bank[b], lhsT=kwT[:, g, jt * 128:(jt + 1) * 128], rhs=Q[0:64, :],
                                                         start=True, stop=True), reads=['kwT', qn + 'lo'], writes=['bank%d' % b])
                mk = tri if jt == i else (ntri if jt == i - 4 else None)
                masked_exp(b, 128, PW[:, sl_, :], mk, ('PW', sl_))
            for m in range(4):
                for sl_, jt in enumerate(jts):
                    P.op('pe', lambda e, m=m, jt=jt, sl_=sl_: e.matmul(k.bank[6][:, m * 65:(m + 1) * 65],
                                                                      lhsT=PW[:, sl_, m * 128:(m + 1) * 128],
                                                                      rhs=vw_aug[:, jt, g, :], start=(sl_ == 0),
                                                                      stop=(sl_ == len(jts) - 1)),
                         reads=[('PW', sl_), 'vw_aug'], writes=['bank6'])
            P.op('dve', lambda e: e.tensor_scalar(out=rdA, in0=den_view(k.bank[3][:, 0:260]), scalar1=1e-30, scalar2=None,
                                                  op0=ALU.max), reads=['bank3'], writes=[rdAn])
            P.op('dve', lambda e: e.reciprocal(out=rdA, in_=rdA), reads=[rdAn], writes=[rdAn])
            P.op('dve', lambda e: e.tensor_scalar(out=psl, in0=k.bank[4][:, 0:64], scalar1=rdA[:, 0:1], scalar2=None,
                                                  op0=ALU.mult), reads=['bank4', rdAn], writes=['psl'])
            for m in range(1, 4):
                P.op('dve', lambda e, m=m: e.scalar_tensor_tensor(out=psl, in0=k.bank[4][:, m * 64:(m + 1) * 64],
                                                                  scalar=rdA[:, m:m + 1], in1=psl, op0=ALU.mult, op1=ALU.add),
                     reads=['bank4', rdAn, 'psl'], writes=['psl'])
            P.op('act', lambda e: e.copy(out=ocw[g][:, 0, :], in_=k.bank[3][:, 0:260]), reads=['bank3'], writes=['ocw%d' % g])
            P.op('act', lambda e: e.copy(out=ocw[g][:, 1, :], in_=k.bank[6][:, 0:260]), reads=['bank6'], writes=['ocw%d' % g])
            P.op('dve', lambda e: e.tensor_tensor(out=score, in0=psl, in1=keep[:, off:off + 64], op=ALU.mult),
                 reads=['psl', 'keep'], writes=['score'])
            P.op('dve', lambda e: e.tensor_tensor(out=score, in0=score, in1=addc[:, off:off + 64], op=ALU.add),
                 reads=['score', 'addc'], writes=['score'])
            P.op('dve', lambda e: e.memset(score[:, 0:1], 1.0e4), reads=['score'], writes=['score'])
            P.op('dve', lambda e: e.max(out=m8a, in_=score), reads=['score'], writes=['m8a'])
            P.op('dve', lambda e: e.match_replace(out=sc2, in_to_replace=m8a, in_values=score, imm_value=-2.0),
                 reads=['score', 'm8a'], writes=['sc2'])
            P.op('dve', lambda e: e.max(out=m8b, in_=sc2), reads=['sc2'], writes=['m8b'])
            P.op('dve', lambda e: e.tensor_scalar(out=thr, in0=m8b[:, 7:8], scalar1=0.0, scalar2=None, op0=ALU.max),
                 reads=['m8b'], writes=['thr'])
            P.op('dve', lambda e: e.tensor_scalar(out=negt[:, 64:128], in0=score, scalar1=thr[:, 0:1], scalar2=-30000.0,
                                                  op0=ALU.is_lt, op1=ALU.mult), reads=['score', 'thr'], writes=['negt'])
            P.op('pe', lambda e: e.transpose(out=tp[:, 512:640], in_=negt, identity=k.ident), reads=['negt', 'ident'],
                 writes=['bank0'])
            P.op('act', lambda e: e.copy(out=Q[64:128, :].rearrange('p (m q) -> p m q', m=4),
                                         in_=tp[64:128, 512:640].unsqueeze(1).broadcast_to([64, 4, 128])),
                 reads=['bank0'], writes=[qn + 'hi'])

        def stage_b(i, g):
            Q = QN[g]
            qn = 'QN%d' % g
            ob = 5
            obn = 'bank%d' % ob
            rdB = rdenB[g]
            rdBn = 'rdenB%d' % g
            coef = coefs[g]
            cfn = 'coef%d' % g
            for jt in range(i + 1):
                b = next_sbank()
                P.op('pe', lambda e, jt=jt, b=b: e.matmul(k.bank[b], lhsT=KE[:, g, jt * 128:(jt + 1) * 128], rhs=Q, start=True,
                                                         stop=True), reads=['KE', qn + 'lo', qn + 'hi'], writes=['bank%d' % b])
                masked_exp(b, 128, PT[:, jt, :], tri if jt == i else None, ('PT', jt))
            for m in range(4):
                for jt in range(i + 1):
                    P.op('pe', lambda e, m=m, jt=jt: e.matmul(k.bank[ob][:, m * 65:(m + 1) * 65],
                                                             lhsT=PT[:, jt, m * 128:(m + 1) * 128], rhs=vs_aug[:, jt, g, :],
                                                             start=(jt == 0), stop=(jt == i)),
                         reads=[('PT', jt), 'vs_aug'], writes=[obn])
            P.op('dve', lambda e: e.tensor_copy(out=rdB[:, 0:4], in_=rdenA[g]), reads=['rdenA%d' % g], writes=[rdBn])
            P.op('dve', lambda e: e.tensor_scalar(out=rdB[:, 4:8], in0=den_view(k.bank[ob][:, 0:260]), scalar1=1e-30, scalar2=None,
                                                  op0=ALU.max), reads=[obn], writes=[rdBn])
            P.op('dve', lambda e: e.tensor_scalar(out=rdB[:, 8:12], in0=den_view(ocw[g][:, 1, :]), scalar1=1e-30, scalar2=None,
                                                  op0=ALU.max), reads=['ocw%d' % g], writes=[rdBn])
            P.op('dve', lambda e: e.reciprocal(out=rdB[:, 4:12], in_=rdB[:, 4:12]), reads=[rdBn], writes=[rdBn])
            P.op('dve', lambda e: e.tensor_tensor(out=coef.rearrange('p (b m) -> p b m', b=3),
                                                  in0=rdB.rearrange('p (b m) -> p b m', b=3),
                                                  in1=gsig[:, i, g * 12:(g + 1) * 12].rearrange('p (m b) -> p b m', b=3),
                                                  op=ALU.mult), reads=[rdBn, ('gsig', i)], writes=[cfn])
            for m in range(4):
                h = 4 * g + m
                ym = yc[:, h * 64:(h + 1) * 64]
                P.op('pool', lambda e, m=m, ym=ym: e.tensor_scalar(out=ym, in0=ocw[g][:, 0, m * 65:m * 65 + 64],
                                                                   scalar1=coef[:, m:m + 1], scalar2=0.0, op0=ALU.mult, op1=ALU.add),
                     reads=['ocw%d' % g, cfn], writes=[('yc', h)])
                P.op('dve', lambda e, m=m, ym=ym: e.scalar_tensor_tensor(out=ym, in0=k.bank[ob][:, m * 65:m * 65 + 64],
                                                                         scalar=coef[:, 4 + m:5 + m], in1=ym, op0=ALU.mult,
                                                                         op1=ALU.add), reads=[obn, cfn, ('yc', h)],
                     writes=[('yc', h)])
                P.op('dve', lambda e, m=m, ym=ym: e.scalar_tensor_tensor(out=ym, in0=ocw[g][:, 1, m * 65:m * 65 + 64],
                                                                         scalar=coef[:, 8 + m:9 + m], in1=ym, op0=ALU.mult,
                                                                         op1=ALU.add), reads=['ocw%d' % g, cfn, ('yc', h)],
                     writes=[('yc', h)])

        def out_proj(i):
            xt = k.xt[i % 3]
            xr = 'xt%d' % (i % 3)
            ydt = ydts[i % 2]
            ydn = 'ydt%d' % (i % 2)
            P.op('act', lambda e: e.copy(out=ycb, in_=yc), reads=[('yc', h) for h in range(8)], writes=['ycb'])
            for c in range(4):
                P.op('pe', lambda e, c=c: e.transpose(out=tp[:, c * 128:(c + 1) * 128], in_=ycb[:, c * 128:(c + 1) * 128],
                                                      identity=k.ident), reads=['ycb', 'ident'], writes=['bank0'])
            for c in range(4):
                P.op('pe', lambda e, c=c: e.transpose(out=tp[:, (4 + c) * 128:(5 + c) * 128], in_=ydt[:, c * 128:(c + 1) * 128],
                                                      identity=k.ident), reads=[ydn, 'ident'], writes=['bank0'])
            P.op('act', lambda e: e.copy(out=yT, in_=tp.rearrange('p (c t) -> p c t', c=8)), reads=['bank0'], writes=['yT'])
            pm = [k.bank[3], k.bank[4]]
            for cb in range(2):
                for kc in range(8):
                    P.op('pe', lambda e, kc=kc, cb=cb: e.matmul(pm[cb], lhsT=yT[:, kc, :], rhs=w_out_sb[:, kc, cb * 512:(cb + 1) * 512],
                                                               start=(kc == 0), stop=(kc == 7)), reads=['yT', 'w_out'],
                         writes=['bank%d' % (3 + cb)])
            post_norm_residual(k, pm, ['bank3', 'bank4'], 'mix_post', xt, xr)
            P.dma('sp', xdst[i * 128:(i + 1) * 128, :], xt, reads=[xr], writes=[('x', i)], stream='xo')

        units = [(i, g) for i in range(NT) for g in range(2)]
        o2_loads(0)
        stage_a(*units[0])
        for n, (i, g) in enumerate(units):
            if n + 1 < len(units):
                ni, ng = units[n + 1]
                if ng == 0:
                    o2_loads(ni)
                stage_a(ni, ng)
            stage_b(i, g)
            if g == 1:
                out_proj(i)

def make_consts():
    c = {}
    Dm = np.zeros((3, 4, 128, 128), np.float32)
    for g, w in enumerate(POOL_WINDOWS):
        for t in range(128):
            lo = max(t + 1 - w, 0)
            for s in range(lo, t + 1):
                Dm[0, g, s, t] += 1.0 / (t + 1 - lo)
            Dm[0, g, t, t] -= 1.0
            for s in range(t + 1 - w, t + 1):
                if s >= 0:
                    Dm[1, g, s, t] += 1.0 / w
                else:
                    Dm[2, g, s + 128, t] += 1.0 / w
            Dm[1, g, t, t] -= 1.0
    c['c_D'] = Dm.astype(ml_dtypes.bfloat16)
    bf = ml_dtypes.bfloat16
    invf = np.concatenate([1.0 / (500000.0 ** (np.arange(0, 16, 2, dtype=np.float32) / 16)),
                           1.0 / (10000.0 ** (np.arange(0, 128, 2, dtype=np.float32) / 128))]).astype(np.float32)
    c['c_invf'] = invf.reshape(1, 72)
    lg = np.log1p(-np.exp2(-5.0 - np.arange(4, dtype=np.float64)))
    idx = np.arange(128, dtype=np.float64)
    rel = idx[None, :] - idx[:, None]
    dec = np.where((rel >= 0)[:, None, :], np.exp(np.maximum(rel, 0)[:, None, :] * lg[None, :, None]), 0.0)
    c['c_decT'] = dec.reshape(128, 512).astype(np.float32)
    xi = np.exp((idx + 1.0)[None, :] * lg[:, None])
    c['c_xi'] = np.broadcast_to(xi.reshape(1, 512), (128, 512)).astype(np.float32).copy()
    c['c_zeta'] = np.exp((127 - idx)[:, None] * lg[None, :]).astype(np.float32)
    c['c_gch'] = np.broadcast_to(np.exp(128 * lg)[None, :], (128, 4)).astype(np.float32).copy()
    p = np.arange(128)
    c['c_tri'] = (p[:, None] <= p[None, :]).astype(np.float32)
    c['c_cm'] = (16.0 * p[:, None] - p[None, :]).astype(np.float32)
    cq = (p >= 64).astype(np.int64)[:, None]
    jj = np.arange(128)[None, :]
    c['c_keep'] = (jj <= 60 + cq).astype(np.float32)
    add = np.zeros((128, 128), np.float32)
    add[(jj == 61 + cq) | (jj == 62 + cq)] = 1.0e4
    add[jj > 62 + cq] = -1.0
    c['c_add'] = add
    n = np.arange(256)
    cs = n * 16
    ss = np.arange(64) * 64
    cts = ((cs[:, None] < ss[None, :] + 64) & (cs[:, None] + 32 > ss[None, :])).astype(np.float32)
    cts[255] = 0
    c['c_cts'] = cts.reshape(2, 128, 64).astype(bf)
    c['c_E'] = (np.arange(4096)[None, :] // 64 == np.arange(64)[:, None]).astype(bf)
    return c


INPUT_NAMES = ["x", "positions", "ln_mix_pre", "ln_mix_post", "ln_ffn_pre", "ln_ffn_post", "ffn_w_gate", "ffn_w_up",
               "ffn_w_down", "ev_w_in", "ev_pool_w", "ev_pool_scale", "ev_sgu_ln_g", "ev_sgu_ln_b", "ev_sgu_w",
               "ev_sgu_b", "ev_w_out", "od_w_in", "od_cmp_k_pos", "od_cmp_k_w1", "od_cmp_k_w2", "od_cmp_v_pos",
               "od_cmp_v_w1", "od_cmp_v_w2", "od_ret_gn_g", "od_w_out"]


def build(shapes, consts, layers=(0, 1, 2, 3), phases=('mix', 'ffn')):
    from contextlib import ExitStack
    nc = bass.Bass("TRN2", target_bir_lowering=False)
    ins = {}
    for n in INPUT_NAMES:
        shp = list(shapes[n])
        if n == 'x':
            shp = [T, D]
        if n == 'positions':
            shp = [1, T]
        ins[n] = nc.dram_tensor(n, shp, I32 if n == 'positions' else F32, kind="ExternalInput").ap()
    for n, v in consts.items():
        ins[n] = nc.dram_tensor(n, list(v.shape), BF16 if v.dtype == ml_dtypes.bfloat16 else F32, kind="ExternalInput").ap()
    y = nc.dram_tensor("y", [T, D], F32, kind="ExternalOutput").ap()
    k = K(nc, layers)
    setup_common(k, ins)
    k.sb2 = k.sb('ssa', [128, 2], F32)
    P = k.P
    xsrc = ins['x']
    for li, l in enumerate(layers):
        load_gains(k, l)
        if 'mix' in phases:
            with ExitStack() as es:
                if l % 2 == 0:
                    even_phase(k, l, xsrc, y, es)
                else:
                    odd_phase(k, l, xsrc, y, es)
                P.barrier()
            xsrc = y
        if li + 1 < len(layers):
            cast_layer(k, layers[li + 1])
        if 'ffn' in phases:
            with ExitStack() as es:
                def sb(name, shape, dt):
                    return es.enter_context(nc.sbuf_tensor('ffs%d_' % l + name, list(shape), dt)).ap()
                k.wd_sb = sb('wd', [128, NFC, 1024], BF16)
                k.xt8 = [sb('xt8_%d' % i, [128, D], F32) for i in range(8)]
                k.wgu = [sb('wgu%d' % i, [128, 2, 8, 512], BF16) for i in range(2)]
                k.hT2 = [sb('hT%d' % i, [128, 8, 512], BF16) for i in range(2)]
                k.actT = sb('actT', [128, NFC, 512], BF16)
                k.sg = [sb('sg%d' % i, [128, 512], F32) for i in range(2)]
                ffn_phase(k, l, xsrc, y)
                P.barrier()
            xsrc = y
    P.finish()
    return nc


_CACHE = {}


def kernel(**inputs):
    consts = make_consts()
    shapes = {n: inputs[n].shape for n in INPUT_NAMES}
    if 'nc' not in _CACHE:
        _CACHE['nc'] = build(shapes, consts)
    nc = _CACHE['nc']
    in_maps = []
    for c in range(4):
        b = c % 4
        m = {n: np.ascontiguousarray(inputs[n]) for n in INPUT_NAMES if n not in ('x', 'positions')}
        m['x'] = np.ascontiguousarray(inputs['x'][b])
        m['positions'] = np.ascontiguousarray(inputs['positions'][b:b + 1]).astype(np.int32)
        m.update(consts)
        in_maps.append(m)
    res = run_bass_kernel_spmd(nc, in_maps, core_ids=list(range(4)))
    out = np.stack([res.results[b]["y"] for b in range(4)], axis=0)
    return out.astype(np.float32)
```
